# Optimizing a Trainium2 kernel written in Bass

```python
import math
import jax
import jax.numpy as jnp
from jax import lax
import numpy as np

D_MODEL = 1024
BATCH = 8
SEQ = 2048
DEPTH = 4

MEM_LEN = 256
N_BRANCHES = 4
MIX_WIDTH = D_MODEL // N_BRANCHES
HEAD_DIM = 64
MIX_HEADS = MIX_WIDTH // HEAD_DIM

NSA_CMP_LEN = 32
NSA_CMP_STRIDE = 16
NSA_SLC_LEN = 64
NSA_TOP_N = 8
NSA_WINDOW = 512
Q_BLOCK = 128
FORCE_BONUS = 1e4
N_BUCKETS = 32
MAX_DISTANCE = 128

RET_CHUNK = 128
ROPE_BASE = 10000.0
RET_NORM_EPS = 1e-5

RWKV_DECAY_LORA = 32
RWKV_AAA_LORA = 32
RWKV_GATE_LORA = 64
RWKV_GN_EPS = 64e-5
RWKV_SIZES = (MIX_WIDTH, MIX_WIDTH, MIX_WIDTH, RWKV_DECAY_LORA, RWKV_AAA_LORA, RWKV_GATE_LORA)
RWKV_COLS = sum(RWKV_SIZES)

CONV_WIDTH = 3

XA_HEADS = 4
XA_HEAD_DIM = D_MODEL // XA_HEADS
D_FF = 4 * D_MODEL

IN_SIZES = (MIX_WIDTH, 6 * HEAD_DIM, 3 * MIX_HEADS, 4 * MIX_WIDTH, RWKV_COLS, 3 * MIX_WIDTH, N_BRANCHES * D_MODEL)
IN_COLS = sum(IN_SIZES)

RMS_EPS = 1e-6
NEG_INF = -1e30

kernel_name = 'hybrid_nsa_retention_rwkv7_shortconv_block'


def split_cols(z, sizes):
    return jnp.split(z, np.cumsum(sizes)[:-1].tolist(), axis=-1)


def rms_norm(x, g):
    xf = x.astype(jnp.float32)
    y = xf * lax.rsqrt(jnp.mean(xf * xf, axis=-1, keepdims=True) + RMS_EPS)
    return (y * g.astype(jnp.float32)).astype(x.dtype)


def head_norm(u, eps):
    mu = jnp.mean(u, axis=-1, keepdims=True)
    var = jnp.mean(jnp.square(u - mu), axis=-1, keepdims=True)
    return (u - mu) * lax.rsqrt(var + eps)


def t5_bucket(dist):
    n = jnp.maximum(dist, 0)
    max_exact = N_BUCKETS // 2
    nf = jnp.maximum(n, 1).astype(jnp.float32)
    large = max_exact + (jnp.log(nf / max_exact) / math.log(MAX_DISTANCE / max_exact)
                         * (N_BUCKETS - max_exact)).astype(jnp.int32)
    large = jnp.minimum(large, N_BUCKETS - 1)
    return jnp.where(n < max_exact, n, large)


def masked_softmax(s, valid):
    return jax.nn.softmax(jnp.where(valid, s, NEG_INF), axis=-1)


def nsa_mixer(q, kv, gate_logits, cmp_w, cmp_pe, rel_bias):
    B, S, H, Dh = q.shape
    dt = q.dtype
    f32 = jnp.float32
    scale = Dh ** -0.5
    k_c, v_c, k_s, v_s, k_w, v_w = jnp.split(kv, 6, axis=-1)

    n_cmp = (S - NSA_CMP_LEN) // NSA_CMP_STRIDE + 1
    cmp_start = jnp.arange(n_cmp) * NSA_CMP_STRIDE
    cmp_idx = cmp_start[:, None] + jnp.arange(NSA_CMP_LEN)[None, :]
    k_cmp = jnp.einsum('bnld,lde->bne', k_c[:, cmp_idx] + cmp_pe, cmp_w[0])
    v_cmp = jnp.einsum('bnld,lde->bne', v_c[:, cmp_idx] + cmp_pe, cmp_w[1])
    cmp_end = cmp_start + NSA_CMP_LEN - 1

    n_slc = S // NSA_SLC_LEN
    top_n = min(NSA_TOP_N, n_slc)
    slc_start = jnp.arange(n_slc) * NSA_SLC_LEN
    overlap = jnp.clip(jnp.minimum(cmp_start[:, None] + NSA_CMP_LEN, slc_start[None, :] + NSA_SLC_LEN)
                       - jnp.maximum(cmp_start[:, None], slc_start[None, :]), 0).astype(f32) / NSA_CMP_LEN
    k_blk = k_s.reshape(B, n_slc, NSA_SLC_LEN, Dh)
    v_blk = v_s.reshape(B, n_slc, NSA_SLC_LEN, Dh)
    b_idx = jnp.arange(B)[:, None, None]

    k_wp = jnp.pad(k_w, ((0, 0), (NSA_WINDOW, 0), (0, 0)))
    v_wp = jnp.pad(v_w, ((0, 0), (NSA_WINDOW, 0), (0, 0)))

    gates = jax.nn.sigmoid(gate_logits.astype(f32)).reshape(B, S, H, 3)
    n_qb = S // Q_BLOCK
    q_b = q.reshape(B, n_qb, Q_BLOCK, H, Dh).swapaxes(0, 1)
    g_b = gates.reshape(B, n_qb, Q_BLOCK, H, 3).swapaxes(0, 1)
    bias_f = rel_bias.astype(f32)

    def block_fn(args):
        qb, gb, bi = args
        t = bi * Q_BLOCK + jnp.arange(Q_BLOCK)

        d_c = t[:, None] - cmp_end[None, :]
        s_c = (jnp.einsum('bqhd,bnd->bhqn', qb, k_cmp).astype(f32) * scale
               + bias_f[t5_bucket(d_c)].transpose(2, 0, 1))
        p_c = masked_softmax(s_c, d_c >= 0) * (t >= NSA_CMP_LEN - 1)[:, None].astype(f32)
        o_c = jnp.einsum('bhqn,bnd->bqhd', p_c.astype(dt), v_cmp)

        imp = jnp.einsum('bhqn,nm->bqm', p_c, overlap)
        blk = jnp.arange(n_slc)
        cur = t // NSA_SLC_LEN
        forced = (blk[None, :] == 0) | (blk[None, :] == cur[:, None]) | (blk[None, :] == cur[:, None] - 1)
        imp = jnp.where(forced, imp + FORCE_BONUS, imp)
        imp = jnp.where(blk[None, :] <= cur[:, None], imp, NEG_INF)
        _, sel = lax.top_k(imp, top_n)

        k_g = k_blk[b_idx, sel].reshape(B, Q_BLOCK, top_n * NSA_SLC_LEN, Dh)
        v_g = v_blk[b_idx, sel].reshape(B, Q_BLOCK, top_n * NSA_SLC_LEN, Dh)
        pos_s = (sel[..., None] * NSA_SLC_LEN + jnp.arange(NSA_SLC_LEN)).reshape(B, Q_BLOCK, -1)
        d_s = t[None, :, None] - pos_s
        s_s = (jnp.einsum('bqhd,bqkd->bhqk', qb, k_g).astype(f32) * scale
               + bias_f[t5_bucket(d_s)].transpose(0, 3, 1, 2))
        p_s = masked_softmax(s_s, (d_s >= 0)[:, None])
        o_s = jnp.einsum('bhqk,bqkd->bqhd', p_s.astype(dt), v_g)

        k_wb = lax.dynamic_slice_in_dim(k_wp, bi * Q_BLOCK, NSA_WINDOW + Q_BLOCK, axis=1)
        v_wb = lax.dynamic_slice_in_dim(v_wp, bi * Q_BLOCK, NSA_WINDOW + Q_BLOCK, axis=1)
        pos_w = bi * Q_BLOCK - NSA_WINDOW + jnp.arange(NSA_WINDOW + Q_BLOCK)
        d_w = t[:, None] - pos_w[None, :]
        valid_w = (d_w >= 0) & (d_w < NSA_WINDOW) & (pos_w[None, :] >= 0)
        s_w = (jnp.einsum('bqhd,bkd->bhqk', qb, k_wb).astype(f32) * scale
               + bias_f[t5_bucket(d_w)].transpose(2, 0, 1))
        p_w = masked_softmax(s_w, valid_w)
        o_w = jnp.einsum('bhqk,bkd->bqhd', p_w.astype(dt), v_wb)

        out = gb[..., 0:1] * o_c + gb[..., 1:2] * o_s + gb[..., 2:3] * o_w
        return out.astype(dt)

    out = lax.map(block_fn, (q_b, g_b, jnp.arange(n_qb)))
    return out.swapaxes(0, 1).reshape(B, S, H * Dh)


def rotary(u, pos):
    half = u.shape[-1] // 2
    inv_freq = ROPE_BASE ** (-jnp.arange(half, dtype=jnp.float32) / half)
    ang = pos.astype(jnp.float32)[:, None] * inv_freq[None, :]
    cos = jnp.cos(ang)[None, :, None, :]
    sin = jnp.sin(ang)[None, :, None, :]
    u1, u2 = u[..., :half], u[..., half:]
    return jnp.concatenate([u1 * cos - u2 * sin, u2 * cos + u1 * sin], axis=-1)


def retention_mixer(q, k, v, g, norm_g):
    B, S, C = q.shape
    H = C // HEAD_DIM
    f32 = jnp.float32
    pos = jnp.arange(S)
    qh = rotary(q.reshape(B, S, H, HEAD_DIM).astype(f32), pos) * HEAD_DIM ** -0.5
    kh = rotary(k.reshape(B, S, H, HEAD_DIM).astype(f32), pos)
    vh = v.reshape(B, S, H, HEAD_DIM).astype(f32)
    L = min(RET_CHUNK, S)
    nc = S // L
    lg = jnp.log(1.0 - 2.0 ** (-5.0 - jnp.arange(H, dtype=f32)))
    n = jnp.arange(L, dtype=f32)
    diff = n[:, None] - n[None, :]
    inner_decay = jnp.where(diff >= 0, jnp.exp(jnp.maximum(diff, 0.0)[None] * lg[:, None, None]), 0.0)
    q_decay = jnp.exp((n + 1.0)[None, :] * lg[:, None])
    k_decay = jnp.exp((L - 1.0 - n)[None, :] * lg[:, None])
    chunk_decay = jnp.exp(L * lg)

    def to_chunks(u):
        return u.reshape(B, nc, L, H, HEAD_DIM).transpose(1, 0, 3, 2, 4)

    def step(state, inp):
        qc, kc, vc = inp
        att = jnp.einsum('bhld,bhmd->bhlm', qc, kc) * inner_decay
        o = (jnp.einsum('bhlm,bhmd->bhld', att, vc)
             + jnp.einsum('bhld,bhde->bhle', qc, state) * q_decay[None, :, :, None])
        state = (state * chunk_decay[None, :, None, None]
                 + jnp.einsum('bhmd,bhme->bhde', kc * k_decay[None, :, :, None], vc))
        return state, o

    state0 = jnp.zeros((B, H, HEAD_DIM, HEAD_DIM), f32)
    _, o = lax.scan(step, state0, (to_chunks(qh), to_chunks(kh), to_chunks(vh)))
    o = o.transpose(1, 0, 3, 2, 4).reshape(B, S, H, HEAD_DIM)
    o = head_norm(o, RET_NORM_EPS) * norm_g.astype(f32).reshape(H, HEAD_DIM)
    return (jax.nn.silu(g.astype(f32)) * o.reshape(B, S, C)).astype(q.dtype)


def rwkv7_mixer(z, mu, w0, w2, a0, a2, g2, k_k, k_a, r_k, ln_g, ln_b):
    B, S, _ = z.shape
    C = MIX_WIDTH
    H = C // HEAD_DIM
    N = HEAD_DIM
    dt = z.dtype
    f32 = jnp.float32
    zf = z.astype(f32)
    zf = zf + (jnp.pad(zf, ((0, 0), (1, 0), (0, 0)))[:, :-1] - zf) * mu
    r, k, v, wl, al, gl = split_cols(zf, RWKV_SIZES)
    w_log = -jax.nn.softplus(-(w0 + jnp.tanh(wl) @ w2)) - 0.5
    decay = jnp.exp(-jnp.exp(w_log))
    a = jax.nn.sigmoid(a0 + al @ a2)
    g = jax.nn.sigmoid(gl) @ g2
    kk = (k * k_k).reshape(B, S, H, N)
    kk = kk / jnp.maximum(jnp.sqrt(jnp.sum(kk * kk, axis=-1, keepdims=True)), 1e-12)
    k = k * (1.0 + (a - 1.0) * k_a)

    def heads(u):
        return u.reshape(B, S, H, N)

    rh, wh, kh, vh, ah = heads(r), heads(decay), heads(k), heads(v), heads(a)

    def seq_first(u):
        return u.transpose(1, 0, 2, 3)

    def step(state, inp):
        r_t, w_t, k_t, v_t, kk_t, a_t = inp
        sa = jnp.einsum('bhvk,bhk->bhv', state, -kk_t)
        state = (state * w_t[:, :, None, :]
                 + sa[..., None] * (kk_t * a_t)[:, :, None, :]
                 + v_t[..., None] * k_t[:, :, None, :])
        return state, jnp.einsum('bhvk,bhk->bhv', state, r_t)

    state0 = jnp.zeros((B, H, N, N), f32)
    xs = (seq_first(rh), seq_first(wh), seq_first(kh), seq_first(vh), seq_first(kk), seq_first(ah))
    _, y = lax.scan(step, state0, xs)
    y = y.transpose(1, 0, 2, 3)
    y = head_norm(y, RWKV_GN_EPS) * ln_g.astype(f32).reshape(H, N) + ln_b.astype(f32).reshape(H, N)
    y = y + jnp.sum(rh * kh * r_k.astype(f32).reshape(H, N), axis=-1, keepdims=True) * vh
    return (y.reshape(B, S, C) * g).astype(dt)


def short_conv_mixer(z, conv_w):
    b_g, c_g, xt = jnp.split(z, 3, axis=-1)
    u = c_g * xt
    C = u.shape[-1]
    y = lax.conv_general_dilated(u, conv_w[:, None, :].astype(u.dtype), window_strides=(1,),
                                 padding=[(CONV_WIDTH - 1, 0)],
                                 dimension_numbers=('NWC', 'WIO', 'NWC'),
                                 feature_group_count=C)
    return b_g * y


def cross_attention(h, m, wq, wkv, wo):
    B, S, D = h.shape
    M = m.shape[1]
    q = (h @ wq).reshape(B, S, XA_HEADS, XA_HEAD_DIM)
    k, v = jnp.split(m @ wkv, 2, axis=-1)
    k = k.reshape(B, M, XA_HEADS, XA_HEAD_DIM)
    v = v.reshape(B, M, XA_HEADS, XA_HEAD_DIM)
    s = jnp.einsum('bshd,bmhd->bhsm', q, k).astype(jnp.float32) * XA_HEAD_DIM ** -0.5
    p = jax.nn.softmax(s, axis=-1).astype(h.dtype)
    o = jnp.einsum('bhsm,bmhd->bshd', p, v).reshape(B, S, D)
    return o @ wo


def setup_inputs(seed: int = 0) -> dict:
    key = jax.random.key(seed)
    ks = iter(jax.random.split(key, 48))
    f32 = jnp.float32

    def nrm(shape, scale):
        return scale * jax.random.normal(next(ks), shape, f32)

    def gain(shape):
        return 1.0 + 0.02 * jax.random.normal(next(ks), shape, f32)

    def unif(shape, lo, hi):
        return jax.random.uniform(next(ks), shape, f32, lo, hi)

    return {
        'x': nrm((BATCH, SEQ, D_MODEL), 1.0),
        'mem': nrm((BATCH, MEM_LEN, D_MODEL), 1.0),
        'ln_mix_pre': gain((DEPTH, D_MODEL)),
        'w_in': nrm((DEPTH, D_MODEL, IN_COLS), D_MODEL ** -0.5),
        'nsa_cmp_w': nrm((DEPTH, 2, NSA_CMP_LEN, HEAD_DIM, HEAD_DIM), (NSA_CMP_LEN * HEAD_DIM) ** -0.5),
        'nsa_cmp_pe': nrm((DEPTH, NSA_CMP_LEN, HEAD_DIM), 0.02),
        'rel_bias': nrm((N_BUCKETS, MIX_HEADS), 0.5),
        'ret_norm_g': gain((DEPTH, MIX_WIDTH)),
        'rwkv_mu': unif((DEPTH, RWKV_COLS), 0.0, 1.0),
        'rwkv_w0': unif((DEPTH, MIX_WIDTH), -6.0, 1.0),
        'rwkv_w2': nrm((DEPTH, RWKV_DECAY_LORA, MIX_WIDTH), 0.1),
        'rwkv_a0': nrm((DEPTH, MIX_WIDTH), 0.1),
        'rwkv_a2': nrm((DEPTH, RWKV_AAA_LORA, MIX_WIDTH), 0.1),
        'rwkv_g2': nrm((DEPTH, RWKV_GATE_LORA, MIX_WIDTH), RWKV_GATE_LORA ** -0.5),
        'rwkv_k_k': 0.85 + 0.02 * jax.random.normal(next(ks), (DEPTH, MIX_WIDTH), f32),
        'rwkv_k_a': gain((DEPTH, MIX_WIDTH)),
        'rwkv_r_k': nrm((DEPTH, MIX_WIDTH), 0.1),
        'rwkv_ln_g': gain((DEPTH, MIX_WIDTH)),
        'rwkv_ln_b': nrm((DEPTH, MIX_WIDTH), 0.02),
        'conv_w': nrm((DEPTH, CONV_WIDTH, MIX_WIDTH), CONV_WIDTH ** -0.5),
        'w_branch': nrm((DEPTH, N_BRANCHES, MIX_WIDTH, D_MODEL), MIX_WIDTH ** -0.5),
        'w_mix_out': nrm((DEPTH, D_MODEL, D_MODEL), D_MODEL ** -0.5),
        'ln_mix_post': gain((DEPTH, D_MODEL)),
        'ln_xa_pre': gain((DEPTH, D_MODEL)),
        'ln_mem': gain((DEPTH, D_MODEL)),
        'xa_wq': nrm((DEPTH, D_MODEL, D_MODEL), D_MODEL ** -0.5),
        'xa_wkv': nrm((DEPTH, D_MODEL, 2 * D_MODEL), D_MODEL ** -0.5),
        'xa_wo': nrm((DEPTH, D_MODEL, D_MODEL), D_MODEL ** -0.5),
        'ln_xa_post': gain((DEPTH, D_MODEL)),
        'ln_mlp_pre': gain((DEPTH, D_MODEL)),
        'mlp_w1': nrm((DEPTH, D_MODEL, D_FF), D_MODEL ** -0.5),
        'mlp_w2': nrm((DEPTH, D_FF, D_MODEL), D_FF ** -0.5),
        'ln_mlp_post': gain((DEPTH, D_MODEL)),
    }


def reference(x, mem, ln_mix_pre, w_in, nsa_cmp_w, nsa_cmp_pe, rel_bias, ret_norm_g,
              rwkv_mu, rwkv_w0, rwkv_w2, rwkv_a0, rwkv_a2, rwkv_g2, rwkv_k_k, rwkv_k_a, rwkv_r_k,
              rwkv_ln_g, rwkv_ln_b, conv_w, w_branch, w_mix_out, ln_mix_post,
              ln_xa_pre, ln_mem, xa_wq, xa_wkv, xa_wo, ln_xa_post,
              ln_mlp_pre, mlp_w1, mlp_w2, ln_mlp_post):
    B, S, _ = x.shape
    for l in range(DEPTH):
        h = rms_norm(x, ln_mix_pre[l])
        z = h @ w_in[l]
        z_q, z_kv, z_g, z_ret, z_rwkv, z_conv, z_gate = split_cols(z, IN_SIZES)
        o_nsa = nsa_mixer(z_q.reshape(B, S, MIX_HEADS, HEAD_DIM), z_kv, z_g,
                          nsa_cmp_w[l], nsa_cmp_pe[l], rel_bias)
        r_q, r_k, r_v, r_g = jnp.split(z_ret, 4, axis=-1)
        o_ret = retention_mixer(r_q, r_k, r_v, r_g, ret_norm_g[l])
        o_rwkv = rwkv7_mixer(z_rwkv, rwkv_mu[l], rwkv_w0[l], rwkv_w2[l], rwkv_a0[l], rwkv_a2[l],
                             rwkv_g2[l], rwkv_k_k[l], rwkv_k_a[l], rwkv_r_k[l],
                             rwkv_ln_g[l], rwkv_ln_b[l])
        o_conv = short_conv_mixer(z_conv, conv_w[l])
        gates = jax.nn.sigmoid(z_gate).reshape(B, S, N_BRANCHES, D_MODEL)
        branches = (o_nsa, o_ret, o_rwkv, o_conv)
        merged = gates[:, :, 0] * (branches[0] @ w_branch[l, 0])
        for m in range(1, N_BRANCHES):
            merged = merged + gates[:, :, m] * (branches[m] @ w_branch[l, m])
        x = x + rms_norm(merged @ w_mix_out[l], ln_mix_post[l])

        h = rms_norm(x, ln_xa_pre[l])
        mn = rms_norm(mem, ln_mem[l])
        x = x + rms_norm(cross_attention(h, mn, xa_wq[l], xa_wkv[l], xa_wo[l]), ln_xa_post[l])

        h = rms_norm(x, ln_mlp_pre[l])
        x = x + rms_norm(jnp.square(jax.nn.relu(h @ mlp_w1[l])) @ mlp_w2[l], ln_mlp_post[l])
    return x
```

```python
import numpy as np
import concourse.bass as bass
import concourse.mybir as mybir
from concourse.bass_utils import run_bass_kernel_spmd

F32 = mybir.dt.float32
AF = mybir.ActivationFunctionType
ALU = mybir.AluOpType
AX = mybir.AxisListType

D = 1024
S = 2048
DEPTH = 4
MEM = 256
NCH = D // 128
HD = 64
DFF = 4096


class Op:
    __slots__ = ("eng", "emit", "deps", "idx", "flag", "val", "dma", "slot", "dval", "prev_slot_op")

    def __init__(self, eng, emit, dma=False):
        self.eng = eng
        self.emit = emit
        self.deps = []
        self.idx = -1
        self.flag = False
        self.val = 0
        self.dma = dma
        self.slot = -1
        self.dval = 0
        self.prev_slot_op = None


class TState:
    __slots__ = ("w", "r")

    def __init__(self):
        self.w = None
        self.r = {}

    def add_reader(self, o):
        k = o.slot if o.dma else o.eng
        p = self.r.get(k)
        if p is None or (o.dval > p.dval if o.dma else o.idx > p.idx):
            self.r[k] = o

    def all_ops(self):
        o = list(self.r.values())
        if self.w is not None:
            o.append(self.w)
        return o


class Tile:
    def __init__(self, name, ap, start=0, size=0):
        self.name = name
        self.ap = ap
        self.st = {}
        self.start = start
        self.size = size

    def __getitem__(self, k):
        return self.ap[k]


ND = 16


class Prog:
    COMPUTE = ("pe", "act", "dve", "pool")
    QUEUES = ("sp", "gq")

    def __init__(self, nc, arena_cols):
        self.nc = nc
        self.ops = {e: [] for e in ("pe", "act", "dve", "pool", "sp")}
        self.ndma = {"sp": 0, "gq": 0}
        self.slot_last = {"sp": [None] * ND, "gq": [None] * ND}
        self.arena = nc.alloc_sbuf_tensor("arena", [128, arena_cols], F32)
        self.free = [[0, arena_cols, []]]
        self.psum = [Tile(f"ps{i}", nc.alloc_psum_tensor(f"ps{i}", [128, 512], F32).ap()) for i in range(8)]
        self.ps_rr = 0
        self.held = set()

    def alloc(self, name, cols):
        for i, (st, sz, pend) in enumerate(self.free):
            if sz >= cols:
                t = Tile(name, self.arena[:, st:st + cols], st, cols)
                if pend:
                    s = TState()
                    for o in pend:
                        s.add_reader(o)
                    t.st[None] = s
                if sz == cols:
                    self.free.pop(i)
                else:
                    self.free[i] = [st + cols, sz - cols, pend]
                return t
        raise RuntimeError(f"SBUF arena full allocating {name} ({cols} cols); free={[(a, b) for a, b, _ in self.free]}")

    def release(self, *tiles):
        for t in tiles:
            tmp = TState()
            for s in t.st.values():
                for o in s.all_ops():
                    tmp.add_reader(o)
            self.free.append([t.start, t.size, list(tmp.r.values())])
        self.free.sort(key=lambda x: x[0])
        m = []
        for blk in self.free:
            if m and m[-1][0] + m[-1][1] == blk[0]:
                m[-1][1] += blk[1]
                tmp = TState()
                for o in m[-1][2] + blk[2]:
                    tmp.add_reader(o)
                m[-1][2] = list(tmp.r.values())
            else:
                m.append(blk)
        self.free = m

    def dram(self, name, shape, kind="Internal"):
        h = self.nc.dram_tensor(name, list(shape), F32, kind=kind)
        return Tile(name, h.ap())

    def ps(self, hold=False):
        while True:
            t = self.psum[self.ps_rr % 8]
            self.ps_rr += 1
            if t.name not in self.held:
                break
        if hold:
            self.held.add(t.name)
        return t

    def ps_free(self, *ts):
        for t in ts:
            self.held.discard(t.name)

    @staticmethod
    def _norm(x):
        if isinstance(x, tuple):
            return (x[0], None) if x[0].name.startswith("ps") else x
        return (x, None)

    def _track(self, op, reads, writes):
        pr = [x for x in reads if self._norm(x)[0].name.startswith("ps")]
        if pr:
            reads = [x for x in reads if not self._norm(x)[0].name.startswith("ps")]
            writes = list(writes) + [x for x in pr if all(self._norm(x)[0] is not self._norm(w)[0] for w in writes)]
        deps = []
        for x in reads:
            t, k = self._norm(x)
            for kk, s in t.st.items():
                if k is None or kk is None or kk == k:
                    if s.w is not None:
                        deps.append(s.w)
        for x in writes:
            t, k = self._norm(x)
            for kk, s in t.st.items():
                if k is None or kk is None or kk == k:
                    deps.extend(s.all_ops())
        for x in reads:
            t, k = self._norm(x)
            s = t.st.get(k)
            if s is None:
                s = t.st[k] = TState()
            s.add_reader(op)
        for x in writes:
            t, k = self._norm(x)
            if k is None:
                t.st.clear()
            s = t.st[k] = TState()
            s.w = op
        seen = set()
        for d in deps:
            if d is op or id(d) in seen:
                continue
            if op.eng == "pe" and d.eng == "pe" and not d.dma:
                continue
            seen.add(id(d))
            d.flag = True
            op.deps.append(d)

    def op(self, eng, emit, reads=(), writes=()):
        o = Op(eng, emit)
        o.idx = len(self.ops[eng])
        self._track(o, reads, writes)
        self.ops[eng].append(o)
        return o

    def dma(self, out_ap, in_ap, reads=(), writes=(), q="sp"):
        eng = "sp" if q == "sp" else "pool"
        o = Op(eng, None, dma=True)
        o.emit = lambda e: e.dma_start(out=out_ap, in_=in_ap)
        n = self.ndma[q]
        self.ndma[q] = n + 1
        o.slot = (q, n % ND)
        o.dval = 16 * (n // ND + 1)
        o.prev_slot_op = self.slot_last[q][n % ND]
        self.slot_last[q][n % ND] = o
        o.idx = len(self.ops[eng])
        self._track(o, reads, writes)
        self.ops[eng].append(o)
        return o

    def finalize(self, final_wait_ops):
        nc = self.nc
        esem = {e: nc.alloc_semaphore(f"sem_{e}") for e in self.COMPUTE}
        dsem = {q: [nc.alloc_semaphore(f"dsem_{q}{i}") for i in range(ND)] for q in self.QUEUES}
        for e in self.COMPUTE:
            c = 0
            for o in self.ops[e]:
                if o.dma:
                    continue
                if o.flag:
                    c += 1
                    o.val = c

        def token(d):
            if d.dma:
                return dsem[d.slot[0]][d.slot[1]], d.dval
            return esem[d.eng], d.val

        ops = self.ops

        def run(ename, eng):
            waited = {}
            for o in ops[ename]:
                deps = list(o.deps)
                if o.dma and o.prev_slot_op is not None:
                    deps.append(o.prev_slot_op)
                need = {}
                for d in deps:
                    sem, v = token(d)
                    if waited.get(sem.num, 0) >= v:
                        continue
                    if need.get(sem.num, (None, 0))[1] < v:
                        need[sem.num] = (sem, v)
                for num, (sem, v) in need.items():
                    eng.wait_ge(sem, v)
                    waited[num] = v
                ins = o.emit(eng)
                if o.dma:
                    ins.then_inc(dsem[o.slot[0]][o.slot[1]], 16)
                elif o.flag:
                    ins.then_inc(esem[o.eng], 1)
            if ename == "sp":
                for d in final_wait_ops:
                    sem, v = token(d)
                    if waited.get(sem.num, 0) < v:
                        eng.wait_ge(sem, v)
                        waited[sem.num] = v

        with nc.Block() as block:
            @block.tensor
            def _(e):
                run("pe", e)

            @block.scalar
            def _(e):
                run("act", e)

            @block.vector
            def _(e):
                run("dve", e)

            @block.gpsimd
            def _(e):
                run("pool", e)

            @block.sync
            def _(e):
                run("sp", e)


class Builder:
    def __init__(self, depth=DEPTH, phases=("mix", "xa", "mlp"), debug_outs=(), branches=("nsa", "ret", "rwkv", "conv")):
        self.depth = depth
        self.branches = branches
        self.phases = phases
        nc = self.nc = bass.Bass("TRN2", target_bir_lowering=False)
        P = self.P = Prog(nc, 51200)
        self.inp = {}
        self.debug_outs = debug_outs

    def din(self, name, shape):
        t = self.P.dram(name, shape, kind="ExternalInput")
        self.inp[name] = t
        return t

    def setup(self):
        P = self.P
        self.x_in = self.din("x", [S, D])
        self.out = self.P.dram("out", [S, D], kind="ExternalOutput")
        self.ident_d = self.din("ident", [128, 128])
        self.gains_d = self.din("gains", [128, self.depth * 7 * NCH])
        self.w1 = self.din("mlp_w1", [self.depth, D, DFF])
        self.w2 = self.din("mlp_w2", [self.depth, DFF, D])

        self.xT = P.alloc("xT", NCH * S)
        self.xT3 = self.xT.ap.rearrange("p (c t) -> p c t", c=NCH)
        self.ident = P.alloc("ident", 128)
        self.ones = P.alloc("ones", 128)
        self.gains = P.alloc("gains", self.depth * 7 * NCH)
        P.dma(self.ident.ap, self.ident_d.ap, reads=[self.ident_d], writes=[self.ident])
        P.dma(self.gains.ap, self.gains_d.ap, reads=[self.gains_d], writes=[self.gains])
        P.op("dve", lambda e: e.memset(self.ones.ap, 1.0), writes=[self.ones])

    def gain(self, l, which, c):
        i = (l * 7 + which) * NCH + c
        return self.gains.ap[:, i:i + 1]

    def load_x(self):
        P = self.P
        for tt in range(S // 128):
            xin = P.alloc("xin", D)
            P.dma(xin.ap, self.x_in.ap[tt * 128:(tt + 1) * 128, :], reads=[self.x_in], writes=[xin])
            for half in range(2):
                ps = P.ps()
                for j in range(4):
                    c = half * 4 + j
                    P.op("pe", lambda e, ps=ps, j=j, c=c, xin=xin: e.transpose(
                        ps.ap[:, j * 128:(j + 1) * 128], xin.ap[:, c * 128:(c + 1) * 128], self.ident.ap),
                        reads=[xin, self.ident], writes=[(ps, j)])
                dst = self.xT3[:, half * 4:half * 4 + 4, tt * 128:(tt + 1) * 128]
                P.op("act", lambda e, ps=ps, dst=dst: e.copy(dst, ps.ap.rearrange("p (c t) -> p c t", c=4)),
                     reads=[ps], writes=[(self.xT, tt)])
            P.release(xin)

    def store_x(self):
        P = self.P
        fin = []
        for tt in range(S // 128):
            xo = P.alloc("xo", D)
            for half in range(2):
                ps = P.ps()
                for j in range(4):
                    c = half * 4 + j
                    P.op("pe", lambda e, ps=ps, j=j, c=c, tt=tt: e.transpose(
                        ps.ap[:, j * 128:(j + 1) * 128], self.xT3[:, c, tt * 128:(tt + 1) * 128], self.ident.ap),
                        reads=[self.xT, self.ident], writes=[(ps, j)])
                P.op("act", lambda e, ps=ps, xo=xo, half=half: e.copy(xo.ap[:, half * 512:(half + 1) * 512], ps.ap),
                     reads=[ps], writes=[(xo, half)])
            fin.append(P.dma(self.out.ap[tt * 128:(tt + 1) * 128, :], xo.ap, reads=[xo], writes=[(self.out, tt)]))
            P.release(xo)
        return fin

    def rstd_of(self, src3, src_tile, n, rstd, eps=1e-6):
        P = self.P
        ps = P.ps()
        for c in range(NCH):
            sq = P.alloc("sq", n)
            P.op("act", lambda e, sq=sq, c=c: e.activation(sq.ap, src3[:, c, :], AF.Square),
                 reads=[src_tile], writes=[sq])
            P.op("pe", lambda e, sq=sq, c=c, ps=ps: e.matmul(ps.ap[:, 0:n], self.ones.ap, sq.ap,
                                                         start=(c == 0), stop=(c == NCH - 1)),
                 reads=[sq, self.ones], writes=[ps])
            P.release(sq)
        P.op("act", lambda e, ps=ps: e.activation(rstd.ap[:, 0:n], ps.ap[:, 0:n], AF.Sqrt, bias=self.epsb(eps), scale=1.0 / D),
             reads=[ps, self.epst], writes=[rstd])
        P.op("dve", lambda e: e.reciprocal(rstd.ap[:, 0:n], rstd.ap[:, 0:n]), reads=[rstd], writes=[rstd])

    def epsb(self, eps):
        return self.epst.ap[:, 0:1]

    def setup_eps(self):
        P = self.P
        self.epst = P.alloc("eps", 4)
        P.op("dve", lambda e: e.memset(self.epst.ap[:, 0:1], 1e-6), writes=[self.epst])
        P.op("dve", lambda e: e.memset(self.epst.ap[:, 1:2], 1e-5), writes=[self.epst])
        P.op("dve", lambda e: e.memset(self.epst.ap[:, 2:3], 64e-5), writes=[self.epst])

    def pre_norm_block(self, l, which, t0, n, hblk):
        h3 = hblk.ap.rearrange("p (c t) -> p c t", c=NCH)
        self.norm_to(l, which, self.xT3[:, :, t0:t0 + n], self.xT, n, h3, hblk)

    def norm_to(self, l, which, src3, src_tile, n, dst3, dst_tile):
        P = self.P
        rstd = P.alloc("rstd", n)
        self.rstd_of(src3, src_tile, n, rstd)
        for c in range(NCH):
            P.op("dve", lambda e, c=c: e.scalar_tensor_tensor(
                out=dst3[:, c, :], in0=src3[:, c, :], scalar=self.gain(l, which, c), in1=rstd.ap[:, 0:n],
                op0=ALU.mult, op1=ALU.mult), reads=[src_tile, rstd, self.gains], writes=[dst_tile])
        P.release(rstd)

    def post_norm_residual(self, l, which, t0, n, yblk):
        P = self.P
        rstd = P.alloc("rstd", n)
        y3 = yblk.ap.rearrange("p (c t) -> p c t", c=NCH)
        self.rstd_of(y3, yblk, n, rstd)
        for c in range(NCH):
            P.op("dve", lambda e, c=c: e.tensor_tensor(out=y3[:, c, :], in0=y3[:, c, :], in1=rstd.ap[:, 0:n], op=ALU.mult),
                 reads=[yblk, rstd], writes=[(yblk, c)])
            dst = self.xT3[:, c, t0:t0 + n]
            P.op("dve", lambda e, c=c, dst=dst: e.scalar_tensor_tensor(
                out=dst, in0=y3[:, c, :], scalar=self.gain(l, which, c), in1=dst, op0=ALU.mult, op1=ALU.add),
                reads=[(yblk, c), self.xT, self.gains], writes=[self.xT])
        P.release(rstd)

    def mlp(self, l):
        P = self.P
        TB = 256
        w1 = self.w1.ap[l]
        w2 = self.w2.ap[l]
        for tb in range(S // TB):
            self.mlp_blk(l, tb, TB, w1, w2)

    def mlp_blk(self, l, tb, TB, w1, w2):
        P = self.P
        if True:
            t0 = tb * TB
            hblk = P.alloc("hblk", NCH * TB)
            self.pre_norm_block(l, 4, t0, TB, hblk)
            h3 = hblk.ap.rearrange("p (c t) -> p c t", c=NCH)
            ablk = P.alloc("ablk", (DFF // 128) * TB)
            a3 = ablk.ap.rearrange("p (c t) -> p c t", c=DFF // 128)
            for fg in range(DFF // 512):
                wt = P.alloc("w1t", NCH * 512)
                wt3 = wt.ap.rearrange("p (k n) -> p k n", k=NCH)
                P.dma(wt3, w1[:, fg * 512:(fg + 1) * 512].rearrange("(k p) n -> p k n", p=128), reads=[self.w1], writes=[wt])
                for j in range(4):
                    f = fg * 4 + j
                    ps = P.ps()
                    for k in range(NCH):
                        P.op("pe", lambda e, ps=ps, k=k, j=j, wt3=wt3: e.matmul(
                            ps.ap[:, 0:TB], wt3[:, k, j * 128:(j + 1) * 128], h3[:, k, :], start=(k == 0), stop=(k == NCH - 1)),
                            reads=[wt, hblk], writes=[ps])
                    r = P.alloc("relu", TB)
                    P.op("act", lambda e, ps=ps, r=r: e.activation(r.ap, ps.ap[:, 0:TB], AF.Relu), reads=[ps], writes=[r])
                    P.op("dve", lambda e, r=r, f=f: e.tensor_tensor(out=a3[:, f, :], in0=r.ap, in1=r.ap, op=ALU.mult),
                         reads=[r], writes=[(ablk, f)])
                    P.release(r)
                P.release(wt)
            yblk = P.alloc("yblk", NCH * TB)
            y3 = yblk.ap.rearrange("p (c t) -> p c t", c=NCH)
            KG = 8
            pss = [P.ps(hold=True) for _ in range(4)]
            for kg in range(DFF // 128 // KG):
                wt = P.alloc("w2t", KG * D)
                wt3 = wt.ap.rearrange("p (k n) -> p k n", k=KG)
                P.dma(wt3, w2[kg * KG * 128:(kg + 1) * KG * 128, :].rearrange("(k p) n -> p k n", p=128), reads=[self.w2], writes=[wt])
                for f in range(NCH):
                    ps = pss[f // 2]
                    o = (f % 2) * TB
                    for k in range(KG):
                        kk = kg * KG + k
                        first = (kk == 0 and f % 2 == 0)
                        P.op("pe", lambda e, ps=ps, o=o, k=k, kk=kk, f=f, wt3=wt3, first=first: e.matmul(
                            ps.ap[:, o:o + TB], wt3[:, k, f * 128:(f + 1) * 128], a3[:, kk, :], start=first,
                            stop=(kk == DFF // 128 - 1), skip_group_check=True),
                            reads=[wt, ablk], writes=[(ps, f % 2)])
                P.release(wt)
            for f in range(NCH):
                ps = pss[f // 2]
                o = (f % 2) * TB
                P.op("act", lambda e, ps=ps, o=o, f=f: e.copy(y3[:, f, :], ps.ap[:, o:o + TB]), reads=[(ps, f % 2)], writes=[(yblk, f)])
            P.ps_free(*pss)
            P.release(ablk, hblk)
            self.post_norm_residual(l, 5, t0, TB, yblk)
            P.release(yblk)


    ZT_ROWS = 3456
    ZQ, ZKC, ZVC, ZKS, ZKW, ZRQ, ZRQS, ZRK, ZRKS, ZRG, ZRW, ZCV = 0, 256, 320, 384, 448, 512, 768, 1024, 1280, 1536, 1792, 2688
    ZN_COLS = 396
    NVS, NVW, NG, NRV = 0, 64, 128, 140

    def setup_mixer(self):
        L = self.depth
        self.wt_d = self.din("w_T", [L, D, self.ZT_ROWS])
        self.wn_d = self.din("w_N", [L, D, self.ZN_COLS])
        self.wgate_d = self.din("w_gate", [L, D, 4 * D])
        self.wbr_d = self.din("w_branch", [L, 4, 256, D])
        self.wmo_d = self.din("w_mix_out", [L, D, D])
        self.convw_d = self.din("conv_wT", [L, 128, 6])
        self.rot_d = self.din("rot_tab", [64, 2, S])
        self.rdec_d = self.din("ret_dec", [128, 4 * 2 * 512])
        self.retg_d = self.din("ret_gT", [L, 64, 4])
        self.ZT = self.P.dram("ZT", [self.ZT_ROWS, S])
        self.ZN = self.P.dram("ZN", [S, self.ZN_COLS])
        self.OBR = self.P.dram("OBR", [D, S])

    def project(self, l, hT):
        P = self.P
        h3 = hT.ap.rearrange("p (c t) -> p c t", c=NCH)
        wn = P.alloc("wn", NCH * self.ZN_COLS)
        wn3 = wn.ap.rearrange("p (k n) -> p k n", k=NCH)
        P.dma(wn3, self.wn_d.ap[l].rearrange("(k p) n -> p k n", p=128), reads=[self.wn_d], writes=[wn])
        for tt in range(S // 128):
            ps = P.ps()
            for k in range(NCH):
                P.op("pe", lambda e, ps=ps, k=k, tt=tt: e.matmul(ps.ap[:, 0:self.ZN_COLS], h3[:, k, tt * 128:(tt + 1) * 128], wn3[:, k, :],
                                                             start=(k == 0), stop=(k == NCH - 1)), reads=[hT, wn], writes=[ps])
            stg = P.alloc("stgn", self.ZN_COLS)
            P.op("act", lambda e, ps=ps, stg=stg: e.copy(stg.ap, ps.ap[:, 0:self.ZN_COLS]), reads=[ps], writes=[stg])
            P.dma(self.ZN.ap[tt * 128:(tt + 1) * 128, :], stg.ap, reads=[stg], writes=[(self.ZN, tt)], q="gq")
            P.release(stg)
        P.release(wn)
        for ch in range(self.ZT_ROWS // 128):
            wt = P.alloc("wt", NCH * 128)
            wt3 = wt.ap.rearrange("p (k n) -> p k n", k=NCH)
            P.dma(wt3, self.wt_d.ap[l][:, ch * 128:(ch + 1) * 128].rearrange("(k p) n -> p k n", p=128), reads=[self.wt_d], writes=[wt])
            for tb in range(S // 512):
                ps = P.ps()
                for k in range(NCH):
                    P.op("pe", lambda e, ps=ps, k=k, tb=tb, wt3=wt3: e.matmul(ps.ap, wt3[:, k, :], h3[:, k, tb * 512:(tb + 1) * 512],
                                                                       start=(k == 0), stop=(k == NCH - 1)), reads=[hT, wt], writes=[ps])
                stg = P.alloc("stgt", 512)
                eng = "act" if tb % 2 == 0 else "dve"
                if eng == "act":
                    P.op("act", lambda e, ps=ps, stg=stg: e.copy(stg.ap, ps.ap), reads=[ps], writes=[stg])
                else:
                    P.op("dve", lambda e, ps=ps, stg=stg: e.tensor_copy(stg.ap, ps.ap), reads=[ps], writes=[stg])
                P.dma(self.ZT.ap[ch * 128:(ch + 1) * 128, tb * 512:(tb + 1) * 512], stg.ap, reads=[stg], writes=[(self.ZT, ch)], q="gq")
                P.release(stg)
            P.release(wt)

    def conv_branch(self, l):
        P = self.P
        cw = P.alloc("convw", 6)
        P.dma(cw.ap, self.convw_d.ap[l], reads=[self.convw_d], writes=[cw])
        for c in range(2):
            self.conv_chunk(cw, c)
        P.release(cw)

    def conv_chunk(self, cw, c):
        P = self.P
        if True:
            bg = P.alloc("cv_b", S)
            cg = P.alloc("cv_c", S)
            xt = P.alloc("cv_x", S)
            for j, t in enumerate((bg, cg, xt)):
                r0 = self.ZCV + j * 256 + c * 128
                P.dma(t.ap, self.ZT.ap[r0:r0 + 128, :], reads=[(self.ZT, r0 // 128)], writes=[t])
            w = lambda j: cw.ap[:, c * 3 + j:c * 3 + j + 1]
            P.op("dve", lambda e: e.tensor_tensor(out=cg.ap, in0=cg.ap, in1=xt.ap, op=ALU.mult), reads=[cg, xt], writes=[cg])
            P.op("dve", lambda e, w=w: e.tensor_scalar(out=xt.ap, in0=cg.ap, scalar1=w(2), scalar2=None, op0=ALU.mult), reads=[cg, cw], writes=[xt])
            P.op("dve", lambda e, w=w: e.scalar_tensor_tensor(out=xt.ap[:, 1:S], in0=cg.ap[:, 0:S - 1], scalar=w(1), in1=xt.ap[:, 1:S],
                                                              op0=ALU.mult, op1=ALU.add), reads=[cg, cw, xt], writes=[xt])
            P.op("dve", lambda e, w=w: e.scalar_tensor_tensor(out=xt.ap[:, 2:S], in0=cg.ap[:, 0:S - 2], scalar=w(0), in1=xt.ap[:, 2:S],
                                                              op0=ALU.mult, op1=ALU.add), reads=[cg, cw, xt], writes=[xt])
            P.op("dve", lambda e: e.tensor_tensor(out=bg.ap, in0=bg.ap, in1=xt.ap, op=ALU.mult), reads=[bg, xt], writes=[bg])
            P.dma(self.OBR.ap[768 + c * 128:768 + (c + 1) * 128, :], bg.ap, reads=[bg], writes=[(self.OBR, 6 + c)], q="gq")
            P.release(bg, cg, xt)

    def retention_branch(self, l):
        P = self.P
        rot = P.alloc("rot", 2 * S)
        rot3 = rot.ap.rearrange("p (a t) -> p a t", a=2)
        P.dma(rot3[0:64], self.rot_d.ap, reads=[self.rot_d], writes=[rot])
        dec = P.alloc("rdec", 4 * 2 * 512)
        dec4 = dec.ap.rearrange("p (h a q) -> p h a q", h=4, a=2)
        P.dma(dec.ap, self.rdec_d.ap, reads=[self.rdec_d], writes=[dec])
        rg = P.alloc("retg", 4)
        P.dma(rg.ap[0:64], self.retg_d.ap[l], reads=[self.retg_d], writes=[rg])
        for h in range(4):
            self.ret_head(l, h, rot, rot3, dec, dec4, rg)
        P.release(rot, dec, rg)

    def ret_head(self, l, h, rot, rot3, dec, dec4, rg):
        P = self.P
        if True:
            lg = float(np.log(1.0 - 2.0 ** (-5.0 - h)))
            qk = []
            for base, bsw in ((self.ZRQ, self.ZRQS), (self.ZRK, self.ZRKS)):
                u = P.alloc("ru", S)
                us = P.alloc("rus", S)
                P.dma(u.ap[0:64], self.ZT.ap[base + h * 64:base + (h + 1) * 64, :], reads=[(self.ZT, (base + h * 64) // 128)], writes=[u])
                P.dma(us.ap[0:64], self.ZT.ap[bsw + h * 64:bsw + (h + 1) * 64, :], reads=[(self.ZT, (bsw + h * 64) // 128)], writes=[us])
                P.op("dve", lambda e, u=u: e.tensor_tensor(out=u.ap[0:64], in0=u.ap[0:64], in1=rot3[0:64, 0, :], op=ALU.mult), reads=[u, rot], writes=[u])
                P.op("dve", lambda e, us=us: e.tensor_tensor(out=us.ap[0:64], in0=us.ap[0:64], in1=rot3[0:64, 1, :], op=ALU.mult), reads=[us, rot], writes=[us])
                P.op("dve", lambda e, u=u, us=us: e.tensor_tensor(out=u.ap[0:64], in0=u.ap[0:64], in1=us.ap[0:64], op=ALU.add), reads=[u, us], writes=[u])
                P.release(us)
                qk.append(u)
            qT, kT = qk
            vh = P.alloc("rv", 16 * 64)
            vh3 = vh.ap.rearrange("p (t d) -> p t d", t=16)
            P.dma(vh3, self.ZN.ap[:, self.NRV + h * 64:self.NRV + (h + 1) * 64].rearrange("(t p) d -> p t d", p=128), reads=[self.ZN], writes=[vh])
            for Q in range(4):
                pso = P.ps(hold=True)
                first = True
                nkb = 4 * Q + 4
                for kb in range(nkb):
                    j = kb - 4 * Q
                    c0 = 128 * j if j > 0 else 0
                    nq = 512 - c0
                    pss = P.ps()
                    P.op("pe", lambda e, pss=pss, kb=kb, Q=Q, c0=c0, nq=nq: e.matmul(
                        pss.ap[:, 0:nq], kT.ap[0:64, kb * 128:(kb + 1) * 128], qT.ap[0:64, Q * 512 + c0:(Q + 1) * 512], start=True, stop=True),
                        reads=[kT, qT], writes=[pss])
                    pT = P.alloc("rp", 512)
                    if j < 0:
                        sc = float(np.exp(lg * 128.0 * (4 * Q - kb)))
                        P.op("dve", lambda e, pss=pss, pT=pT, sc=sc, h=h: e.scalar_tensor_tensor(
                            out=pT.ap, in0=pss.ap, scalar=sc, in1=dec4[:, h, 0, :], op0=ALU.mult, op1=ALU.mult), reads=[pss, dec], writes=[pT])
                    else:
                        P.op("dve", lambda e, pss=pss, pT=pT, nq=nq, h=h: e.tensor_tensor(
                            out=pT.ap[:, 0:nq], in0=pss.ap[:, 0:nq], in1=dec4[:, h, 1, 0:nq], op=ALU.mult), reads=[pss, dec], writes=[pT])
                    P.op("pe", lambda e, pso=pso, pT=pT, kb=kb, c0=c0, nq=nq, first=first, last=(kb == nkb - 1): e.matmul(
                        pso.ap[0:64, c0:512], vh3[:, kb, :], pT.ap[:, 0:nq], start=first, stop=last, skip_group_check=True),
                        reads=[vh, pT], writes=[pso])
                    first = False
                    P.release(pT)
                self.ret_epilogue(l, h, Q, pso, rg)
                P.ps_free(pso)
            P.release(qT, kT, vh)

    def ret_epilogue(self, l, h, Q, pso, rg):
        P = self.P
        n = 512
        o = P.alloc("ro", n)
        sq = P.alloc("rsq", n)
        P.op("act", lambda e: e.copy(o.ap[0:64], pso.ap[0:64, :]), reads=[pso], writes=[o])
        P.op("act", lambda e: e.activation(sq.ap[0:64], pso.ap[0:64, :], AF.Square), reads=[pso], writes=[sq])
        p1 = P.ps()
        p2 = P.ps()
        P.op("pe", lambda e: e.matmul(p1.ap[0:64, :], self.ones.ap[0:64, 0:64], o.ap[0:64], start=True, stop=True), reads=[o, self.ones], writes=[p1])
        P.op("pe", lambda e: e.matmul(p2.ap[0:64, :], self.ones.ap[0:64, 0:64], sq.ap[0:64], start=True, stop=True), reads=[sq, self.ones], writes=[p2])
        mean = P.alloc("rmean", n)
        P.op("dve", lambda e: e.tensor_scalar(out=mean.ap[0:64], in0=p1.ap[0:64, :], scalar1=1.0 / 64, scalar2=None, op0=ALU.mult), reads=[p1], writes=[mean])
        P.op("dve", lambda e: e.tensor_tensor(out=o.ap[0:64], in0=o.ap[0:64], in1=mean.ap[0:64], op=ALU.subtract), reads=[o, mean], writes=[o])
        P.op("dve", lambda e: e.tensor_tensor(out=mean.ap[0:64], in0=mean.ap[0:64], in1=mean.ap[0:64], op=ALU.mult), reads=[mean], writes=[mean])
        P.op("dve", lambda e: e.scalar_tensor_tensor(out=sq.ap[0:64], in0=p2.ap[0:64, :], scalar=1.0 / 64, in1=mean.ap[0:64],
                                                     op0=ALU.mult, op1=ALU.subtract), reads=[p2, mean], writes=[sq])
        P.op("act", lambda e: e.activation(sq.ap[0:64], sq.ap[0:64], AF.Sqrt, bias=self.epst.ap[0:64, 1:2], scale=1.0), reads=[sq, self.epst], writes=[sq])
        P.op("dve", lambda e: e.reciprocal(sq.ap[0:64], sq.ap[0:64]), reads=[sq], writes=[sq])
        P.op("dve", lambda e: e.tensor_tensor(out=o.ap[0:64], in0=o.ap[0:64], in1=sq.ap[0:64], op=ALU.mult), reads=[o, sq], writes=[o])
        g = P.alloc("rgate", n)
        r0 = self.ZRG + h * 64
        P.dma(g.ap[0:64], self.ZT.ap[r0:r0 + 64, Q * n:(Q + 1) * n], reads=[(self.ZT, r0 // 128)], writes=[g])
        P.op("act", lambda e: e.activation(g.ap[0:64], g.ap[0:64], AF.Silu), reads=[g], writes=[g])
        P.op("dve", lambda e: e.scalar_tensor_tensor(out=o.ap[0:64], in0=o.ap[0:64], scalar=rg.ap[0:64, h:h + 1], in1=g.ap[0:64],
                                                     op0=ALU.mult, op1=ALU.mult), reads=[o, rg, g], writes=[o])
        P.dma(self.OBR.ap[256 + h * 64:256 + (h + 1) * 64, Q * n:(Q + 1) * n], o.ap[0:64], reads=[o], writes=[(self.OBR, 2 + h // 2)], q="gq")
        P.release(o, sq, mean, g)

    def merge(self, l, hT):
        P = self.P
        TB = 256
        for tb in range(S // TB):
            self.merge_blk(l, hT, tb, TB)

    def merge_blk(self, l, hT, tb, TB):
        P = self.P
        h3 = hT.ap.rearrange("p (c t) -> p c t", c=NCH)
        if True:
            t0 = tb * TB
            obr = P.alloc("obr", NCH * TB)
            obr3 = obr.ap.rearrange("p (c t) -> p c t", c=NCH)
            P.dma(obr3, self.OBR.ap[:, t0:t0 + TB].rearrange("(c p) t -> p c t", p=128), reads=[self.OBR], writes=[obr])
            mrg = P.alloc("mrg", NCH * TB)
            m3 = mrg.ap.rearrange("p (c t) -> p c t", c=NCH)
            for f in range(NCH):
                for m in range(4):
                    wg = P.alloc("wg", NCH * 128)
                    wg3 = wg.ap.rearrange("p (k n) -> p k n", k=NCH)
                    c0 = m * D + f * 128
                    P.dma(wg3, self.wgate_d.ap[l][:, c0:c0 + 128].rearrange("(k p) n -> p k n", p=128), reads=[self.wgate_d], writes=[wg])
                    wb = P.alloc("wb", 2 * 128)
                    wb3 = wb.ap.rearrange("p (k n) -> p k n", k=2)
                    P.dma(wb3, self.wbr_d.ap[l, m][:, f * 128:(f + 1) * 128].rearrange("(k p) n -> p k n", p=128), reads=[self.wbr_d], writes=[wb])
                    ps1 = P.ps()
                    for k in range(NCH):
                        P.op("pe", lambda e, ps1=ps1, k=k, wg3=wg3: e.matmul(ps1.ap[:, 0:TB], wg3[:, k, :], h3[:, k, t0:t0 + TB],
                                                                          start=(k == 0), stop=(k == NCH - 1)), reads=[wg, hT], writes=[ps1])
                    ps2 = P.ps()
                    for k in range(2):
                        P.op("pe", lambda e, ps2=ps2, k=k, m=m, wb3=wb3: e.matmul(ps2.ap[:, 0:TB], wb3[:, k, :], obr3[:, 2 * m + k, :],
                                                                               start=(k == 0), stop=(k == 1)), reads=[wb, obr], writes=[ps2])
                    g = P.alloc("mg", TB)
                    P.op("act", lambda e, ps1=ps1, g=g: e.activation(g.ap, ps1.ap[:, 0:TB], AF.Sigmoid), reads=[ps1], writes=[g])
                    if m == 0:
                        P.op("dve", lambda e, g=g, ps2=ps2, f=f: e.tensor_tensor(out=m3[:, f, :], in0=g.ap, in1=ps2.ap[:, 0:TB], op=ALU.mult),
                             reads=[g, ps2], writes=[(mrg, f)])
                    else:
                        P.op("dve", lambda e, g=g, ps2=ps2: e.tensor_tensor(out=g.ap, in0=g.ap, in1=ps2.ap[:, 0:TB], op=ALU.mult),
                             reads=[g, ps2], writes=[g])
                        P.op("dve", lambda e, g=g, f=f: e.tensor_tensor(out=m3[:, f, :], in0=m3[:, f, :], in1=g.ap, op=ALU.add),
                             reads=[g, (mrg, f)], writes=[(mrg, f)])
                    P.release(g, wg, wb)
            P.release(obr)
            yblk = P.alloc("yblk", NCH * TB)
            y3 = yblk.ap.rearrange("p (c t) -> p c t", c=NCH)
            for f in range(NCH):
                wo = P.alloc("wmo", NCH * 128)
                wo3 = wo.ap.rearrange("p (k n) -> p k n", k=NCH)
                P.dma(wo3, self.wmo_d.ap[l][:, f * 128:(f + 1) * 128].rearrange("(k p) n -> p k n", p=128), reads=[self.wmo_d], writes=[wo])
                ps = P.ps()
                for k in range(NCH):
                    P.op("pe", lambda e, ps=ps, k=k, wo3=wo3: e.matmul(ps.ap[:, 0:TB], wo3[:, k, :], m3[:, k, :], start=(k == 0), stop=(k == NCH - 1)),
                         reads=[wo, mrg], writes=[ps])
                P.op("act", lambda e, ps=ps, f=f: e.copy(y3[:, f, :], ps.ap[:, 0:TB]), reads=[ps], writes=[(yblk, f)])
                P.release(wo)
            P.release(mrg)
            self.post_norm_residual(l, 1, t0, TB, yblk)
            P.release(yblk)

    def zero_obr(self, r0, r1):
        P = self.P
        z = P.alloc("zero", S)
        P.op("dve", lambda e: e.memset(z.ap, 0.0), writes=[z])
        for r in range(r0, r1, 128):
            P.dma(self.OBR.ap[r:r + 128, :], z.ap, reads=[z], writes=[(self.OBR, r // 128)], q="gq")
        P.release(z)

    def mixer(self, l):
        P = self.P
        hT = P.alloc("hT", NCH * S)
        h3 = hT.ap.rearrange("p (c t) -> p c t", c=NCH)
        for tb in range(4):
            self.norm_to(l, 0, self.xT3[:, :, tb * 512:(tb + 1) * 512], self.xT, 512, h3[:, :, tb * 512:(tb + 1) * 512], hT)
        self.project(l, hT)
        P.release(hT)
        if "nsa" in self.branches:
            self.nsa_branch(l)
        else:
            self.zero_obr(0, 256)
        if "ret" in self.branches:
            self.retention_branch(l)
        else:
            self.zero_obr(256, 512)
        if "rwkv" in self.branches:
            self.rwkv_branch(l)
        else:
            self.zero_obr(512, 768)
        if "conv" in self.branches:
            self.conv_branch(l)
        else:
            self.zero_obr(768, 1024)
        if "nomerge" not in self.phases:
            hT = P.alloc("hT", NCH * S)
            h3 = hT.ap.rearrange("p (c t) -> p c t", c=NCH)
            for tb in range(4):
                self.norm_to(l, 0, self.xT3[:, :, tb * 512:(tb + 1) * 512], self.xT, 512, h3[:, :, tb * 512:(tb + 1) * 512], hT)
            self.merge(l, hT)
            P.release(hT)


    def setup_nsa(self):
        L = self.depth
        self.cmpw_d = self.din("nsa_cmp_w", [L, 2, 32, 64, 64])
        self.peT_d = self.din("nsa_peT", [L, 64, 32])
        self.biasc_d = self.din("nsa_biasc", [128, 4, S])
        self.ntab_d = self.din("nsa_tab", [128, 4 * 512])
        self.e2_d = self.din("nsa_e2", [32, S])
        self.ovl_d = self.din("nsa_ovl", [128, 32])
        self.addt_d = self.din("nsa_addtab", [S, 32])

    def nsa_branch(self, l):
        P = self.P
        W = P.alloc("cmpw", 2 * 32 * 64)
        W3 = W.ap.rearrange("p (a e) -> p a e", a=64)
        P.dma(W3[0:64], self.cmpw_d.ap[l].rearrange("a l d e -> d (a l) e"), reads=[self.cmpw_d], writes=[W])
        peT = P.alloc("peT", 32)
        P.dma(peT.ap[0:64], self.peT_d.ap[l], reads=[self.peT_d], writes=[peT])
        kc = P.alloc("kcT", S)
        vc = P.alloc("vcT", S)
        P.dma(kc.ap[0:64], self.ZT.ap[self.ZKC:self.ZKC + 64, :], reads=[(self.ZT, 2)], writes=[kc])
        P.dma(vc.ap[0:64], self.ZT.ap[self.ZVC:self.ZVC + 64, :], reads=[(self.ZT, 2)], writes=[vc])
        kcmp = P.alloc("kcmpT", 128)
        vaug = P.alloc("vcmp_aug", 97)
        P.op("dve", lambda e: e.memset(kcmp.ap, 0.0), writes=[kcmp])
        P.op("dve", lambda e: e.memset(vaug.ap, 0.0), writes=[vaug])
        P.op("dve", lambda e: e.memset(vaug.ap[0:127, 64:65], 1.0), reads=[vaug], writes=[vaug])
        P.dma(vaug.ap[:, 65:97], self.ovl_d.ap, reads=[vaug, self.ovl_d], writes=[vaug])
        kc3 = kc.ap.rearrange("p (n s) -> p n s", s=16)
        vc3 = vc.ap.rearrange("p (n s) -> p n s", s=16)
        psk = P.ps()
        psb = P.ps()
        for li in range(32):
            rhs = kc3[0:64, li // 16:li // 16 + 127, li % 16]
            P.op("pe", lambda e, li=li, rhs=rhs: e.matmul(psk.ap[0:64, 0:127], W3[0:64, li, :], rhs, start=(li == 0), stop=(li == 31)),
                 reads=[W, kc], writes=[psk])
        for li in range(32):
            P.op("pe", lambda e, li=li: e.matmul(psb.ap[0:64, 0:1], W3[0:64, li, :], peT.ap[0:64, li:li + 1], start=(li == 0), stop=(li == 31)),
                 reads=[W, peT], writes=[psb])
        bk = P.alloc("bk", 1)
        P.op("act", lambda e: e.copy(bk.ap[0:64], psb.ap[0:64, 0:1]), reads=[psb], writes=[bk])
        P.op("dve", lambda e: e.tensor_scalar(out=kcmp.ap[0:64, 0:127], in0=psk.ap[0:64, 0:127], scalar1=bk.ap[0:64, 0:1], scalar2=None, op0=ALU.add),
             reads=[psk, bk, kcmp], writes=[kcmp])
        psv = P.ps()
        psbv = P.ps()
        for li in range(32):
            P.op("pe", lambda e, li=li: e.matmul(psbv.ap[0:1, 0:64], peT.ap[0:64, li:li + 1], W3[0:64, 32 + li, :], start=(li == 0), stop=(li == 31)),
                 reads=[W, peT], writes=[psbv])
        bv = P.alloc("bv", 64)
        P.op("act", lambda e: e.copy(bv.ap[0:1], psbv.ap[0:1, 0:64]), reads=[psbv], writes=[bv])
        for li in range(32):
            lhs = vc3[0:64, li // 16:li // 16 + 127, li % 16]
            P.op("pe", lambda e, li=li, lhs=lhs: e.matmul(psv.ap[0:127, 0:64], lhs, W3[0:64, 32 + li, :], start=(li == 0), stop=False),
                 reads=[W, vc], writes=[psv])
        P.op("pe", lambda e: e.matmul(psv.ap[0:127, 0:64], self.ones.ap[0:1, 0:127], bv.ap[0:1, 0:64], start=False, stop=True),
             reads=[bv, self.ones], writes=[psv])
        P.op("act", lambda e: e.copy(vaug.ap[0:127, 0:64], psv.ap[0:127, 0:64]), reads=[psv, vaug], writes=[vaug])
        P.release(W, peT, kc, vc, bk, bv)
        ks = P.alloc("ksT", S)
        kw = P.alloc("kwT", S)
        P.dma(ks.ap[0:64], self.ZT.ap[self.ZKS:self.ZKS + 64, :], reads=[(self.ZT, 3)], writes=[ks])
        P.dma(kw.ap[0:64], self.ZT.ap[self.ZKW:self.ZKW + 64, :], reads=[(self.ZT, 3)], writes=[kw])
        e2 = P.alloc("e2", S)
        P.dma(e2.ap[0:32], self.e2_d.ap, reads=[self.e2_d], writes=[e2])
        tab = P.alloc("ntab", 4 * 512)
        P.dma(tab.ap, self.ntab_d.ap, reads=[self.ntab_d], writes=[tab])
        vaugs = []
        for c0 in (self.NVS, self.NVW):
            va = P.alloc("vaug", 16 * 65)
            va3 = va.ap.rearrange("p (t d) -> p t d", t=16)
            P.op("dve", lambda e, va=va: e.memset(va.ap, 1.0), writes=[va])
            P.dma(va3[:, :, 0:64], self.ZN.ap[:, c0:c0 + 64].rearrange("(t p) d -> p t d", p=128), reads=[self.ZN, va], writes=[va])
            vaugs.append((va, va3))
        gl = P.alloc("ngl", 16 * 12)
        gl3 = gl.ap.rearrange("p (t g) -> p t g", t=16)
        P.dma(gl3, self.ZN.ap[:, self.NG:self.NG + 12].rearrange("(t p) g -> p t g", p=128), reads=[self.ZN], writes=[gl])
        P.op("act", lambda e: e.activation(gl.ap, gl.ap, AF.Sigmoid), reads=[gl], writes=[gl])
        for qb in range(S // 128):
            self.nsa_qblock(l, qb, kcmp, vaug, ks, kw, e2, tab, vaugs, gl3, gl)
        P.release(kcmp, vaug, ks, kw, e2, tab, vaugs[0][0], vaugs[1][0], gl)

    def nsa_scores(self, ps_s, tabsl, tab, pso, vaug_ap, vaug_tile, first, width):
        P = self.P
        tmp = P.alloc("ntmp", 512)
        src_tiles = [ps_s, tab]
        P.op("dve", lambda e: e.scalar_tensor_tensor(out=tmp.ap, in0=ps_s.ap, scalar=0.125, in1=tabsl, op0=ALU.mult, op1=ALU.add),
             reads=src_tiles, writes=[tmp])
        P.op("act", lambda e: e.activation(tmp.ap, tmp.ap, AF.Exp), reads=[tmp], writes=[tmp])
        for h in range(4):
            P.op("pe", lambda e, h=h: e.matmul(pso.ap[:, h * width:(h + 1) * width], tmp.ap[:, h * 128:(h + 1) * 128], vaug_ap,
                                              start=(first and h == 0), stop=True, skip_group_check=True),
                 reads=[tmp, vaug_tile], writes=[pso])
        P.release(tmp)

    def nsa_qblock(self, l, qb, kcmp, vaug, ks, kw, e2, tab, vaugs, gl3, gl):
        P = self.P
        q0 = qb * 128
        q4 = P.alloc("q4", 512)
        P.dma(q4.ap[0:64].rearrange("p (h t) -> p h t", h=4), self.ZT.ap[0:256, q0:q0 + 128].rearrange("(h d) t -> d h t", d=64),
              reads=[(self.ZT, 0), (self.ZT, 1)], writes=[q4])
        bc = P.alloc("bc", 512)
        P.dma(bc.ap.rearrange("p (h t) -> p h t", h=4), self.biasc_d.ap[:, :, q0:q0 + 128], reads=[self.biasc_d], writes=[bc])
        ps_c = P.ps()
        P.op("pe", lambda e: e.matmul(ps_c.ap, kcmp.ap[0:64, :], q4.ap[0:64, :], start=True, stop=True), reads=[kcmp, q4], writes=[ps_c])
        ps_oc = P.ps(hold=True)
        self.nsa_scores(ps_c, bc.ap, bc, ps_oc, vaug.ap, vaug, True, 97)
        P.release(bc)
        oc3 = ps_oc.ap[:, 0:388].rearrange("p (h w) -> p h w", h=4)
        rdc = P.alloc("rdc", 4)
        P.op("dve", lambda e: e.tensor_scalar(out=rdc.ap, in0=oc3[:, :, 64], scalar1=1e-30, scalar2=None, op0=ALU.max), reads=[ps_oc], writes=[rdc])
        P.op("dve", lambda e: e.reciprocal(rdc.ap, rdc.ap), reads=[rdc], writes=[rdc])
        imp = P.alloc("imp", 32)
        P.dma(imp.ap, self.addt_d.ap[q0:q0 + 128, :], reads=[self.addt_d], writes=[imp])
        for h in range(4):
            P.op("dve", lambda e, h=h: e.scalar_tensor_tensor(out=imp.ap, in0=oc3[:, h, 65:97], scalar=rdc.ap[:, h:h + 1], in1=imp.ap,
                                                              op0=ALU.mult, op1=ALU.add), reads=[ps_oc, rdc, imp], writes=[imp])
        top8 = P.alloc("top8", 8)
        P.op("dve", lambda e: e.max(out=top8.ap, in_=imp.ap), reads=[imp], writes=[top8])
        P.op("dve", lambda e: e.tensor_scalar(out=imp.ap, in0=imp.ap, scalar1=top8.ap[:, 7:8], scalar2=1.0, op0=ALU.is_ge, op1=ALU.subtract),
             reads=[imp, top8], writes=[imp])
        ps_t = P.ps()
        P.op("pe", lambda e: e.transpose(ps_t.ap[0:32, 0:128], imp.ap, self.ident.ap), reads=[imp, self.ident], writes=[ps_t])
        ns4 = P.alloc("ns4", 512)
        P.op("dve", lambda e: e.tensor_copy(ns4.ap[0:32].rearrange("p (h t) -> p h t", h=4),
                                            ps_t.ap[0:32, 0:128].rearrange("p (o t) -> p o t", o=1).broadcast_to([32, 4, 128])),
             reads=[ps_t], writes=[ns4])
        P.release(imp, top8)
        ps_os = P.ps(hold=True)
        for kb in range(qb + 1):
            dlt = qb - kb
            ti = min(dlt, 2)
            ps_s = P.ps()
            P.op("pe", lambda e, kb=kb, ps_s=ps_s: e.matmul(ps_s.ap, ks.ap[0:64, kb * 128:(kb + 1) * 128], q4.ap[0:64, :], start=True, stop=False),
                 reads=[ks, q4], writes=[ps_s])
            P.op("pe", lambda e, kb=kb, ps_s=ps_s: e.matmul(ps_s.ap, e2.ap[0:32, kb * 128:(kb + 1) * 128], ns4.ap[0:32, :], start=False, stop=True),
                 reads=[e2, ns4], writes=[ps_s])
            self.nsa_scores(ps_s, tab.ap[:, ti * 512:(ti + 1) * 512], tab, ps_os, vaugs[0][1][:, kb, :], vaugs[0][0], kb == 0, 65)
        ps_ow = P.ps(hold=True)
        kb0 = max(0, qb - 4)
        for kb in range(kb0, qb + 1):
            dlt = qb - kb
            ti = (0, 1, 2, 2, 3)[dlt]
            ps_s = P.ps()
            P.op("pe", lambda e, kb=kb, ps_s=ps_s: e.matmul(ps_s.ap, kw.ap[0:64, kb * 128:(kb + 1) * 128], q4.ap[0:64, :], start=True, stop=True),
                 reads=[kw, q4], writes=[ps_s])
            self.nsa_scores(ps_s, tab.ap[:, ti * 512:(ti + 1) * 512], tab, ps_ow, vaugs[1][1][:, kb, :], vaugs[1][0], kb == kb0, 65)
        P.release(q4, ns4)
        acc = P.alloc("nacc", 256)
        acc3 = acc.ap.rearrange("p (h d) -> p h d", h=4)
        g3 = gl3[:, qb, :].rearrange("p (h b) -> p h b", b=3)
        for b, (pso, w) in enumerate(((ps_oc, 97), (ps_os, 65), (ps_ow, 65))):
            o3 = pso.ap[:, 0:4 * w].rearrange("p (h w) -> p h w", h=4)
            scl = P.alloc("nscl", 4)
            P.op("dve", lambda e, o3=o3, scl=scl: e.tensor_scalar(out=scl.ap, in0=o3[:, :, 64], scalar1=1e-30, scalar2=None, op0=ALU.max),
                 reads=[pso], writes=[scl])
            P.op("dve", lambda e, scl=scl: e.reciprocal(scl.ap, scl.ap), reads=[scl], writes=[scl])
            P.op("dve", lambda e, scl=scl, b=b: e.tensor_tensor(out=scl.ap, in0=scl.ap, in1=g3[:, :, b], op=ALU.mult), reads=[scl, gl], writes=[scl])
            sb = scl.ap.rearrange("p (h o) -> p h o", o=1).broadcast_to([128, 4, 64])
            if b == 0:
                P.op("dve", lambda e, o3=o3, sb=sb: e.tensor_tensor(out=acc3, in0=o3[:, :, 0:64], in1=sb, op=ALU.mult), reads=[pso, scl], writes=[acc])
            else:
                t2 = P.alloc("nt2", 256)
                t23 = t2.ap.rearrange("p (h d) -> p h d", h=4)
                P.op("dve", lambda e, o3=o3, sb=sb, t23=t23: e.tensor_tensor(out=t23, in0=o3[:, :, 0:64], in1=sb, op=ALU.mult), reads=[pso, scl], writes=[t2])
                P.op("dve", lambda e, t2=t2: e.tensor_tensor(out=acc.ap, in0=acc.ap, in1=t2.ap, op=ALU.add), reads=[acc, t2], writes=[acc])
                P.release(t2)
            P.release(scl)
        P.ps_free(ps_oc, ps_os, ps_ow)
        P.release(rdc)
        ps_o = P.ps()
        for c in range(2):
            P.op("pe", lambda e, c=c: e.transpose(ps_o.ap[:, c * 128:(c + 1) * 128], acc.ap[:, c * 128:(c + 1) * 128], self.ident.ap),
                 reads=[acc, self.ident], writes=[ps_o])
        stg = P.alloc("nstg", 256)
        P.op("act", lambda e: e.copy(stg.ap, ps_o.ap[:, 0:256]), reads=[ps_o], writes=[stg])
        P.dma(self.OBR.ap[0:256, q0:q0 + 128].rearrange("(c p) t -> p c t", p=128), stg.ap.rearrange("p (c t) -> p c t", c=2),
              reads=[stg], writes=[(self.OBR, 0), (self.OBR, 1)], q="gq")
        P.release(acc, stg)


    RC = 64

    def setup_rwkv(self):
        L = self.depth
        self.rwpar_d = self.din("rw_par", [L, 64, 43])
        self.rww2_d = self.din("rwkv_w2", [L, 32, 256])
        self.rwa2_d = self.din("rwkv_a2", [L, 32, 256])
        self.rwg2_d = self.din("rwkv_g2", [L, 64, 256])
        self.rwmask_d = self.din("rw_mask5", [64, 320])

    def rw_shift(self, z, n, mu_ap, par):
        P = self.P
        d = P.alloc("rwd", S)
        P.op("dve", lambda e: e.tensor_tensor(out=d.ap[0:n, 1:S], in0=z.ap[0:n, 0:S - 1], in1=z.ap[0:n, 1:S], op=ALU.subtract), reads=[z], writes=[d])
        P.op("dve", lambda e: e.tensor_scalar(out=d.ap[0:n, 0:1], in0=z.ap[0:n, 0:1], scalar1=-1.0, scalar2=None, op0=ALU.mult), reads=[z, d], writes=[d])
        P.op("dve", lambda e: e.scalar_tensor_tensor(out=z.ap[0:n], in0=d.ap[0:n], scalar=mu_ap, in1=z.ap[0:n], op0=ALU.mult, op1=ALU.add),
             reads=[d, z, par], writes=[z])
        P.release(d)

    def rwkv_branch(self, l):
        P = self.P
        par = P.alloc("rwpar", 43)
        P.dma(par.ap[0:64], self.rwpar_d.ap[l], reads=[self.rwpar_d], writes=[par])
        omk = P.alloc("rwomk", 4)
        P.op("dve", lambda e: e.tensor_scalar(out=omk.ap[0:64], in0=par.ap[0:64, 15 + 3 * 4:15 + 4 * 4], scalar1=-1.0, scalar2=1.0, op0=ALU.mult, op1=ALU.add),
             reads=[par], writes=[omk])
        lw = P.alloc("rwlw", 3 * 256)
        P.dma(lw.ap[0:32, 0:256], self.rww2_d.ap[l], reads=[self.rww2_d], writes=[(lw, 0)])
        P.dma(lw.ap[0:32, 256:512], self.rwa2_d.ap[l], reads=[self.rwa2_d], writes=[(lw, 1)])
        P.dma(lw.ap[0:64, 512:768], self.rwg2_d.ap[l], reads=[self.rwg2_d], writes=[(lw, 2)])
        m5 = P.alloc("rwm5", 320)
        P.dma(m5.ap[0:64], self.rwmask_d.ap, reads=[self.rwmask_d], writes=[m5])
        smask = P.alloc("rwsm", S)
        P.op("dve", lambda e: e.memset(smask.ap, 1.0), writes=[smask])
        P.op("dve", lambda e: e.memset(smask.ap.rearrange("p (n c) -> p n c", c=self.RC)[:, :, 0:1], 0.0), reads=[smask], writes=[smask])
        base = self.ZRW + 768
        twl = P.alloc("rwtwl", S)
        tal = P.alloc("rwtal", S)
        tgl = P.alloc("rwtgl", S)
        for t, r0, n, mc, fn in ((twl, base, 32, 12, AF.Tanh), (tal, base + 32, 32, 13, None), (tgl, base + 64, 64, 14, AF.Sigmoid)):
            self.rw_lora_in(t, r0, n, mc, fn, par)
        import os
        for h in range(4 if int(os.environ.get("RWDBG", "9")) >= 9 else 1):
            self.rwkv_head(l, h, par, omk, lw, m5, smask, twl, tal, tgl)
        P.release(par, omk, lw, m5, smask, twl, tal, tgl)

    def rw_lora_in(self, t, r0, n, mc, fn, par):
        P = self.P
        P.dma(t.ap[0:n], self.ZT.ap[r0:r0 + n, :], reads=[(self.ZT, r0 // 128)], writes=[t])
        self.rw_shift(t, n, par.ap[0:n, mc:mc + 1], par)
        if fn is not None:
            P.op("act", lambda e: e.activation(t.ap[0:n], t.ap[0:n], fn), reads=[t], writes=[t])

    def rwkv_head(self, l, h, par, omk, lw, m5, smask, twl, tal, tgl):
        P = self.P
        C = self.RC
        NCK = S // C
        pc = lambda which: par.ap[0:64, 15 + which * 4 + h:15 + which * 4 + h + 1]
        hc = slice(h * 64, (h + 1) * 64)
        r = P.alloc("rw_r", S)
        k = P.alloc("rw_k", S)
        v = P.alloc("rw_v", S)
        for j, t in enumerate((r, k, v)):
            r0 = self.ZRW + j * 256 + h * 64
            P.dma(t.ap[0:64], self.ZT.ap[r0:r0 + 64, :], reads=[(self.ZT, r0 // 128)], writes=[t])
            self.rw_shift(t, 64, par.ap[0:64, j * 4 + h:j * 4 + h + 1], par)
        a = P.alloc("rw_a", S)
        logw = P.alloc("rw_lw", S)
        kkn = P.alloc("rw_kkn", S)
        P.op("dve", lambda e: e.tensor_scalar(out=kkn.ap[0:64], in0=k.ap[0:64], scalar1=pc(2), scalar2=None, op0=ALU.mult), reads=[k, par], writes=[kkn])
        for tb in range(4):
            self.rw_prep_blk(h, tb, par, pc, lw, twl, tal, a, logw, kkn)
        kt = P.alloc("rw_kt", S)
        P.op("dve", lambda e: e.tensor_scalar(out=kt.ap[0:64], in0=a.ap[0:64], scalar1=pc(3), scalar2=omk.ap[0:64, h:h + 1], op0=ALU.mult, op1=ALU.add),
             reads=[a, par, omk], writes=[kt])
        P.op("dve", lambda e: e.tensor_tensor(out=kt.ap[0:64], in0=kt.ap[0:64], in1=k.ap[0:64], op=ALU.mult), reads=[kt, k], writes=[kt])
        P.release(k)
        P.op("dve", lambda e: e.tensor_tensor(out=a.ap[0:64], in0=a.ap[0:64], in1=kkn.ap[0:64], op=ALU.mult), reads=[a, kkn], writes=[a])
        bb = a
        import os
        dbg = int(os.environ.get("RWDBG", "9"))
        bonv = P.alloc("rw_bon", S)
        P.op("dve", lambda e: e.scalar_tensor_tensor(out=bonv.ap[0:64], in0=r.ap[0:64], scalar=pc(4), in1=kt.ap[0:64], op0=ALU.mult, op1=ALU.mult),
             reads=[r, kt, par], writes=[bonv])
        for tb in range(4):
            ps = P.ps()
            P.op("pe", lambda e, ps=ps, tb=tb: e.matmul(ps.ap[0:64, :], self.ones.ap[0:64, 0:64], bonv.ap[0:64, tb * 512:(tb + 1) * 512], start=True, stop=True),
                 reads=[bonv, self.ones], writes=[ps])
            P.op("dve", lambda e, ps=ps, tb=tb: e.tensor_tensor(out=bonv.ap[0:64, tb * 512:(tb + 1) * 512], in0=ps.ap[0:64, :], in1=v.ap[0:64, tb * 512:(tb + 1) * 512], op=ALU.mult),
                 reads=[ps, v, bonv], writes=[bonv])
        if dbg <= 1:
            return
        cum = P.alloc("rw_cum", S)
        P.op("dve", lambda e: e.tensor_tensor_scan(out=cum.ap[0:64], data0=smask.ap[0:64], data1=logw.ap[0:64], initial=0.0, op0=ALU.mult, op1=ALU.add),
             reads=[smask, logw], writes=[cum])
        eg = P.alloc("rw_eg", S)
        P.op("act", lambda e: e.activation(eg.ap[0:64], cum.ap[0:64], AF.Exp), reads=[cum], writes=[eg])
        gC = P.alloc("rw_gC", NCK)
        P.op("dve", lambda e: e.tensor_copy(gC.ap[0:64], eg.ap[0:64].rearrange("p (n c) -> p n c", c=C)[:, :, C - 1]), reads=[eg], writes=[gC])
        P.op("dve", lambda e: e.tensor_tensor(out=r.ap[0:64], in0=r.ap[0:64], in1=eg.ap[0:64], op=ALU.mult), reads=[r, eg], writes=[r])
        P.release(eg)
        RH = r
        P.op("dve", lambda e: e.tensor_tensor(out=logw.ap[0:64], in0=cum.ap[0:64], in1=logw.ap[0:64], op=ALU.subtract), reads=[cum, logw], writes=[logw])
        P.op("act", lambda e: e.activation(logw.ap[0:64], logw.ap[0:64], AF.Exp), reads=[logw], writes=[logw])
        P.op("dve", lambda e: e.tensor_tensor(out=kkn.ap[0:64], in0=kkn.ap[0:64], in1=logw.ap[0:64], op=ALU.mult), reads=[kkn, logw], writes=[kkn])
        P.release(logw)
        KH = kkn
        P.op("act", lambda e: e.activation(cum.ap[0:64], cum.ap[0:64], AF.Exp, scale=-1.0), reads=[cum], writes=[cum])
        P.op("dve", lambda e: e.tensor_tensor(out=kt.ap[0:64], in0=kt.ap[0:64], in1=cum.ap[0:64], op=ALU.mult), reads=[kt, cum], writes=[kt])
        P.op("dve", lambda e: e.tensor_tensor(out=bb.ap[0:64], in0=bb.ap[0:64], in1=cum.ap[0:64], op=ALU.mult), reads=[bb, cum], writes=[bb])
        P.release(cum)
        KG, BG = kt, bb
        if dbg <= 2:
            return
        tms = []
        for src in (v, KG, BG):
            tm = P.alloc("rw_tm", NCK * 64)
            tm3 = tm.ap.rearrange("p (n c) -> p n c", c=64)
            for g8 in range(NCK // 8):
                ps = P.ps()
                for j in range(8):
                    n = g8 * 8 + j
                    P.op("pe", lambda e, ps=ps, j=j, n=n, src=src: e.transpose(ps.ap[0:64, j * 64:(j + 1) * 64], src.ap[0:64, n * C:(n + 1) * C], self.ident.ap[0:64, 0:64]),
                         reads=[src, self.ident], writes=[ps])
                P.op("act", lambda e, ps=ps, g8=g8, tm=tm: e.copy(tm.ap[0:64, g8 * 512:(g8 + 1) * 512], ps.ap[0:64, :]), reads=[ps], writes=[(tm, g8)])
            tms.append((tm, tm3))
        P.release(v)
        (Vt, Vt3), (KGt, KGt3), (BGt, BGt3) = tms
        if dbg <= 3:
            return
        yT = P.alloc("rw_y", S)
        ST = P.alloc("rw_ST", 64)
        P.op("dve", lambda e: e.memset(ST.ap[0:64], 0.0), writes=[ST])
        G = 4
        for g0 in range(0, NCK, G):
            As, TTs = self.rw_group_prep(g0, G, KH, RH, KG, BG, m5)
            for gi in range(G):
                if dbg > 4:
                    self.rw_chunk(g0 + gi, As[gi], TTs[gi], KH, RH, Vt3, Vt, KGt3, KGt, BGt3, BGt, ST, gC, yT)
            P.release(*As)
            P.release(*TTs)
        P.release(RH, KH, KG, BG, Vt, KGt, BGt, ST, gC)
        self.rw_epilogue(l, h, yT, bonv, pc, par, lw, tgl)
        P.release(yT, bonv)

    def rw_prep_blk(self, h, tb, par, pc, lw, twl, tal, a, logw, kkn):
        P = self.P
        ts = slice(tb * 512, (tb + 1) * 512)
        ps = P.ps()
        P.op("pe", lambda e: e.matmul(ps.ap[0:64, :], lw.ap[0:32, h * 64:(h + 1) * 64], twl.ap[0:32, ts], start=True, stop=True), reads=[lw, twl], writes=[ps])
        P.op("act", lambda e: e.activation(logw.ap[0:64, ts], ps.ap[0:64, :], AF.Sigmoid, bias=pc(0), scale=1.0), reads=[ps, par], writes=[logw])
        P.op("dve", lambda e: e.tensor_scalar(out=logw.ap[0:64, ts], in0=logw.ap[0:64, ts], scalar1=-0.6065306597126334, scalar2=None, op0=ALU.mult),
             reads=[logw], writes=[logw])
        ps2 = P.ps()
        P.op("pe", lambda e: e.matmul(ps2.ap[0:64, :], lw.ap[0:32, 256 + h * 64:256 + (h + 1) * 64], tal.ap[0:32, ts], start=True, stop=True), reads=[lw, tal], writes=[ps2])
        P.op("act", lambda e: e.activation(a.ap[0:64, ts], ps2.ap[0:64, :], AF.Sigmoid, bias=pc(1), scale=1.0), reads=[ps2, par], writes=[a])
        sq = P.alloc("rw_sq", 512)
        P.op("act", lambda e: e.activation(sq.ap[0:64], kkn.ap[0:64, ts], AF.Square), reads=[kkn], writes=[sq])
        ps3 = P.ps()
        P.op("pe", lambda e: e.matmul(ps3.ap[0:64, :], self.ones.ap[0:64, 0:64], sq.ap[0:64], start=True, stop=True), reads=[sq, self.ones], writes=[ps3])
        P.op("act", lambda e: e.activation(sq.ap[0:64], ps3.ap[0:64, :], AF.Sqrt), reads=[ps3], writes=[sq])
        P.op("dve", lambda e: e.tensor_scalar(out=sq.ap[0:64], in0=sq.ap[0:64], scalar1=1e-12, scalar2=None, op0=ALU.max), reads=[sq], writes=[sq])
        P.op("dve", lambda e: e.reciprocal(sq.ap[0:64], sq.ap[0:64]), reads=[sq], writes=[sq])
        P.op("dve", lambda e: e.tensor_tensor(out=kkn.ap[0:64, ts], in0=kkn.ap[0:64, ts], in1=sq.ap[0:64], op=ALU.mult), reads=[kkn, sq], writes=[kkn])
        P.release(sq)

    def rw_group_prep(self, g0, G, KH, RH, KG, BG, m5):
        P = self.P
        C = self.RC
        As, Ms, Ps = [], [], []
        for gi in range(G):
            cs = slice((g0 + gi) * C, (g0 + gi + 1) * C)
            ps = P.ps()
            for j, (lh, rh) in enumerate(((KG, KH), (KG, RH), (BG, KH), (BG, RH), (KH, BG))):
                P.op("pe", lambda e, ps=ps, j=j, lh=lh, rh=rh, cs=cs: e.matmul(ps.ap[0:64, j * 64:(j + 1) * 64], lh.ap[0:64, cs], rh.ap[0:64, cs], start=True, stop=True),
                     reads=[lh, rh], writes=[ps])
            A = P.alloc("rw_A", 320)
            P.op("dve", lambda e, ps=ps, A=A: e.tensor_tensor(out=A.ap[0:64], in0=ps.ap[0:64, 0:320], in1=m5.ap[0:64], op=ALU.mult), reads=[ps, m5], writes=[A])
            As.append(A)
            Pm = P.alloc("rw_P", 64)
            P.op("dve", lambda e, A=A, Pm=Pm: e.tensor_tensor(out=Pm.ap[0:64], in0=self.ident.ap[0:64, 0:64], in1=A.ap[0:64, 128:192], op=ALU.subtract),
                 reads=[A, self.ident], writes=[Pm])
            Ps.append(Pm)
            Ms.append((A.ap[0:64, 128:192], A.ap[0:64, 256:320], A))
        import os
        nsteps = int(os.environ.get("RWSTEPS", "6"))
        for step in range(6):
            if step >= nsteps:
                for gi in range(G):
                    if Ms[gi][2] is not As[gi]:
                        P.release(Ms[gi][2])
                break
            pss = []
            for gi in range(G):
                M, MT, Mt = Ms[gi]
                ps = P.ps()
                if step < 5:
                    P.op("pe", lambda e, ps=ps, M=M, MT=MT: e.matmul(ps.ap[0:64, 0:64], MT, M, start=True, stop=True), reads=[Mt], writes=[ps])
                    P.op("pe", lambda e, ps=ps, M=M, MT=MT: e.matmul(ps.ap[0:64, 64:128], M, MT, start=True, stop=True), reads=[Mt], writes=[ps])
                if step > 0:
                    P.op("pe", lambda e, ps=ps, MT=MT, Pm=Ps[gi]: e.matmul(ps.ap[0:64, 128:192], MT, Pm.ap[0:64], start=True, stop=True), reads=[Mt, Ps[gi]], writes=[ps])
                pss.append(ps)
            for gi in range(G):
                ps = pss[gi]
                if step > 0:
                    P.op("dve", lambda e, ps=ps, Pm=Ps[gi]: e.tensor_tensor(out=Pm.ap[0:64], in0=Pm.ap[0:64], in1=ps.ap[0:64, 128:192], op=ALU.add),
                         reads=[ps, Ps[gi]], writes=[Ps[gi]])
                if step < 5:
                    Mn = P.alloc("rw_M", 128)
                    P.op("act", lambda e, ps=ps, Mn=Mn: e.copy(Mn.ap[0:64], ps.ap[0:64, 0:128]), reads=[ps], writes=[Mn])
                    old = Ms[gi][2]
                    Ms[gi] = (Mn.ap[0:64, 0:64], Mn.ap[0:64, 64:128], Mn)
                    if old is not As[gi]:
                        P.release(old)
                elif Ms[gi][2] is not As[gi]:
                    P.release(Ms[gi][2])
        return As, Ps

    def rw_chunk(self, n, A, TT, KH, RH, Vt3, Vt, KGt3, KGt, BGt3, BGt, ST, gC, yT):
        P = self.P
        C = self.RC
        cs = slice(n * C, (n + 1) * C)
        psx = P.ps()
        P.op("pe", lambda e: e.matmul(psx.ap[0:64, 0:64], KH.ap[0:64, cs], ST.ap[0:64], start=True, stop=False), reads=[KH, ST], writes=[psx])
        P.op("pe", lambda e: e.matmul(psx.ap[0:64, 0:64], A.ap[0:64, 0:64], Vt3[0:64, n, :], start=False, stop=True), reads=[A, Vt], writes=[psx])
        nx = P.alloc("rw_nx", 64)
        P.op("act", lambda e: e.mul(nx.ap[0:64], psx.ap[0:64, 0:64], -1.0), reads=[psx], writes=[nx])
        psu = P.ps()
        P.op("pe", lambda e: e.matmul(psu.ap[0:64, 0:64], TT.ap[0:64], nx.ap[0:64], start=True, stop=True), reads=[TT, nx], writes=[psu])
        U = P.alloc("rw_U", 64)
        P.op("act", lambda e: e.copy(U.ap[0:64], psu.ap[0:64, 0:64]), reads=[psu], writes=[U])
        psy = P.ps()
        P.op("pe", lambda e: e.matmul(psy.ap[0:64, 0:64], ST.ap[0:64], RH.ap[0:64, cs], start=True, stop=False), reads=[ST, RH], writes=[psy])
        P.op("pe", lambda e: e.matmul(psy.ap[0:64, 0:64], Vt3[0:64, n, :], A.ap[0:64, 64:128], start=False, stop=False), reads=[Vt, A], writes=[psy])
        P.op("pe", lambda e: e.matmul(psy.ap[0:64, 0:64], U.ap[0:64], A.ap[0:64, 192:256], start=False, stop=True), reads=[U, A], writes=[psy])
        P.op("act", lambda e: e.copy(yT.ap[0:64, cs], psy.ap[0:64, 0:64]), reads=[psy], writes=[(yT, n)])
        pss = P.ps()
        P.op("pe", lambda e: e.matmul(pss.ap[0:64, 0:64], KGt3[0:64, n, :], Vt3[0:64, n, :], start=True, stop=False), reads=[KGt, Vt], writes=[pss])
        P.op("pe", lambda e: e.matmul(pss.ap[0:64, 0:64], BGt3[0:64, n, :], U.ap[0:64], start=False, stop=True), reads=[BGt, U], writes=[pss])
        P.op("dve", lambda e: e.tensor_tensor(out=ST.ap[0:64], in0=ST.ap[0:64], in1=pss.ap[0:64, 0:64], op=ALU.add), reads=[ST, pss], writes=[ST])
        P.op("dve", lambda e: e.tensor_scalar(out=ST.ap[0:64], in0=ST.ap[0:64], scalar1=gC.ap[0:64, n:n + 1], scalar2=None, op0=ALU.mult),
             reads=[ST, gC], writes=[ST])
        P.release(nx, U)

    def rw_epilogue(self, l, h, yT, bonv, pc, par, lw, tgl):
        P = self.P
        n = 512
        for Q in range(S // n):
            self.rw_epi_blk(h, Q, n, yT, bonv, pc, par, lw, tgl)

    def rw_epi_blk(self, h, Q, n, yT, bonv, pc, par, lw, tgl):
        P = self.P
        ts = slice(Q * n, (Q + 1) * n)
        o = P.alloc("ro", n)
        sq = P.alloc("rsq", n)
        P.op("act", lambda e: e.activation(sq.ap[0:64], yT.ap[0:64, ts], AF.Square), reads=[yT], writes=[sq])
        p1 = P.ps()
        p2 = P.ps()
        P.op("pe", lambda e: e.matmul(p1.ap[0:64, :], self.ones.ap[0:64, 0:64], yT.ap[0:64, ts], start=True, stop=True), reads=[yT, self.ones], writes=[p1])
        P.op("pe", lambda e: e.matmul(p2.ap[0:64, :], self.ones.ap[0:64, 0:64], sq.ap[0:64], start=True, stop=True), reads=[sq, self.ones], writes=[p2])
        mean = P.alloc("rmean", n)
        P.op("dve", lambda e: e.tensor_scalar(out=mean.ap[0:64], in0=p1.ap[0:64, :], scalar1=1.0 / 64, scalar2=None, op0=ALU.mult), reads=[p1], writes=[mean])
        P.op("dve", lambda e: e.tensor_tensor(out=o.ap[0:64], in0=yT.ap[0:64, ts], in1=mean.ap[0:64], op=ALU.subtract), reads=[yT, mean], writes=[o])
        P.op("dve", lambda e: e.tensor_tensor(out=mean.ap[0:64], in0=mean.ap[0:64], in1=mean.ap[0:64], op=ALU.mult), reads=[mean], writes=[mean])
        P.op("dve", lambda e: e.scalar_tensor_tensor(out=sq.ap[0:64], in0=p2.ap[0:64, :], scalar=1.0 / 64, in1=mean.ap[0:64],
                                                     op0=ALU.mult, op1=ALU.subtract), reads=[p2, mean], writes=[sq])
        P.op("act", lambda e: e.activation(sq.ap[0:64], sq.ap[0:64], AF.Sqrt, bias=self.epst.ap[0:64, 2:3], scale=1.0), reads=[sq, self.epst], writes=[sq])
        P.op("dve", lambda e: e.reciprocal(sq.ap[0:64], sq.ap[0:64]), reads=[sq], writes=[sq])
        P.op("dve", lambda e: e.tensor_tensor(out=o.ap[0:64], in0=o.ap[0:64], in1=sq.ap[0:64], op=ALU.mult), reads=[o, sq], writes=[o])
        P.op("dve", lambda e: e.tensor_scalar(out=o.ap[0:64], in0=o.ap[0:64], scalar1=pc(5), scalar2=pc(6), op0=ALU.mult, op1=ALU.add), reads=[o, par], writes=[o])
        P.op("dve", lambda e: e.tensor_tensor(out=o.ap[0:64], in0=o.ap[0:64], in1=bonv.ap[0:64, ts], op=ALU.add), reads=[o, bonv], writes=[o])
        pg = P.ps()
        P.op("pe", lambda e: e.matmul(pg.ap[0:64, :], lw.ap[0:64, 512 + h * 64:512 + (h + 1) * 64], tgl.ap[0:64, ts], start=True, stop=True), reads=[lw, tgl], writes=[pg])
        P.op("dve", lambda e: e.tensor_tensor(out=o.ap[0:64], in0=o.ap[0:64], in1=pg.ap[0:64, :], op=ALU.mult), reads=[o, pg], writes=[o])
        P.dma(self.OBR.ap[512 + h * 64:512 + (h + 1) * 64, ts], o.ap[0:64], reads=[o], writes=[(self.OBR, 4 + h // 2)], q="gq")
        P.release(o, sq, mean)

    def setup_xa(self):
        P = self.P
        L = self.depth
        self.mem_d = self.din("mem", [MEM, D])
        self.wq_d = self.din("xa_wq", [L, D, D])
        self.wkv_d = self.din("xa_wkv", [L, D, 2 * D])
        self.wo_d = self.din("xa_wo", [L, D, D])
        self.memT = P.alloc("memT", NCH * MEM)
        memT3 = self.memT.ap.rearrange("p (c t) -> p c t", c=NCH)
        for tt in range(MEM // 128):
            self.xa_load_mem(tt, memT3)

    def xa_load_mem(self, tt, memT3):
        P = self.P
        xin = P.alloc("xin", D)
        P.dma(xin.ap, self.mem_d.ap[tt * 128:(tt + 1) * 128, :], reads=[self.mem_d], writes=[xin])
        for half in range(2):
            ps = P.ps()
            for j in range(4):
                c = half * 4 + j
                P.op("pe", lambda e, ps=ps, j=j, c=c: e.transpose(ps.ap[:, j * 128:(j + 1) * 128], xin.ap[:, c * 128:(c + 1) * 128], self.ident.ap),
                     reads=[xin, self.ident], writes=[ps])
            dst = memT3[:, half * 4:half * 4 + 4, tt * 128:(tt + 1) * 128]
            P.op("act", lambda e, ps=ps, dst=dst: e.copy(dst, ps.ap.rearrange("p (c t) -> p c t", c=4)), reads=[ps], writes=[self.memT])
        P.release(xin)

    def xattn(self, l):
        P = self.P
        mnT = P.alloc("mnT", NCH * MEM)
        mn3 = mnT.ap.rearrange("p (c t) -> p c t", c=NCH)
        self.norm_to(l, 6, self.memT.ap.rearrange("p (c t) -> p c t", c=NCH), self.memT, MEM, mn3, mnT)
        kT = P.alloc("xkT", NCH * MEM)
        kT3 = kT.ap.rearrange("p (c t) -> p c t", c=NCH)
        vN = P.alloc("xv", 2 * D)
        vN3 = vN.ap.rearrange("p (m n) -> p m n", m=2)
        for f in range(NCH):
            self.xa_k(l, f, mn3, mnT, kT3, kT)
        for half in range(2):
            self.xa_v(l, half, mn3, mnT, vN3, vN)
        P.release(mnT)
        TB = 256
        for tb in range(S // TB):
            self.xa_blk(l, tb, TB, kT3, kT, vN3, vN)
        P.release(kT, vN)

    def xa_k(self, l, f, mn3, mnT, kT3, kT):
        P = self.P
        w = P.alloc("xw", NCH * 128)
        w3 = w.ap.rearrange("p (k n) -> p k n", k=NCH)
        P.dma(w3, self.wkv_d.ap[l][:, f * 128:(f + 1) * 128].rearrange("(k p) n -> p k n", p=128), reads=[self.wkv_d], writes=[w])
        ps = P.ps()
        for k in range(NCH):
            P.op("pe", lambda e, k=k: e.matmul(ps.ap[:, 0:MEM], w3[:, k, :], mn3[:, k, :], start=(k == 0), stop=(k == NCH - 1)),
                 reads=[w, mnT], writes=[ps])
        P.op("act", lambda e: e.copy(kT3[:, f, :], ps.ap[:, 0:MEM]), reads=[ps], writes=[(kT, f)])
        P.release(w)

    def xa_v(self, l, half, mn3, mnT, vN3, vN):
        P = self.P
        w = P.alloc("xwv", NCH * 512)
        w3 = w.ap.rearrange("p (k n) -> p k n", k=NCH)
        c0 = D + half * 512
        P.dma(w3, self.wkv_d.ap[l][:, c0:c0 + 512].rearrange("(k p) n -> p k n", p=128), reads=[self.wkv_d], writes=[w])
        for mc in range(2):
            ps = P.ps()
            for k in range(NCH):
                P.op("pe", lambda e, k=k, ps=ps, mc=mc: e.matmul(ps.ap, mn3[:, k, mc * 128:(mc + 1) * 128], w3[:, k, :], start=(k == 0), stop=(k == NCH - 1)),
                     reads=[w, mnT], writes=[ps])
            P.op("act", lambda e, ps=ps, mc=mc: e.copy(vN3[:, mc, half * 512:(half + 1) * 512], ps.ap), reads=[ps], writes=[(vN, (mc, half))])
        P.release(w)

    def lin8(self, w_ap, src3, src_tile, dst3, dst_tile, TB):
        P = self.P
        for f in range(NCH):
            self.lin8_f(w_ap, f, src3, src_tile, dst3, dst_tile, TB)

    def lin8_f(self, w_ap, f, src3, src_tile, dst3, dst_tile, TB):
        P = self.P
        w = P.alloc("xw", NCH * 128)
        w3 = w.ap.rearrange("p (k n) -> p k n", k=NCH)
        P.dma(w3, w_ap[:, f * 128:(f + 1) * 128].rearrange("(k p) n -> p k n", p=128), reads=[], writes=[w])
        ps = P.ps()
        for k in range(NCH):
            P.op("pe", lambda e, k=k: e.matmul(ps.ap[:, 0:TB], w3[:, k, :], src3[:, k, :], start=(k == 0), stop=(k == NCH - 1)),
                 reads=[w, src_tile], writes=[ps])
        P.op("act", lambda e: e.copy(dst3[:, f, :], ps.ap[:, 0:TB]), reads=[ps], writes=[(dst_tile, f)])
        P.release(w)

    def xa_blk(self, l, tb, TB, kT3, kT, vN3, vN):
        P = self.P
        t0 = tb * TB
        hblk = P.alloc("hblk", NCH * TB)
        self.pre_norm_block(l, 2, t0, TB, hblk)
        h3 = hblk.ap.rearrange("p (c t) -> p c t", c=NCH)
        qT = P.alloc("xq", NCH * TB)
        q3 = qT.ap.rearrange("p (c t) -> p c t", c=NCH)
        self.lin8(self.wq_d.ap[l], h3, hblk, q3, qT, TB)
        P.release(hblk)
        at = P.alloc("xat", NCH * TB)
        at3 = at.ap.rearrange("p (c t) -> p c t", c=NCH)
        for h in range(4):
            self.xa_head(h, TB, kT3, kT, vN3, vN, q3, qT, at3, at)
        P.release(qT)
        yblk = P.alloc("yblk", NCH * TB)
        y3 = yblk.ap.rearrange("p (c t) -> p c t", c=NCH)
        self.lin8(self.wo_d.ap[l], at3, at, y3, yblk, TB)
        P.release(at)
        self.post_norm_residual(l, 3, t0, TB, yblk)
        P.release(yblk)

    def xa_head(self, h, TB, kT3, kT, vN3, vN, q3, qT, at3, at):
        P = self.P
        pT = P.alloc("xp", 2 * TB)
        p3 = pT.ap.rearrange("p (m t) -> p m t", m=2)
        for mc in range(2):
            ps = P.ps()
            for c in range(2):
                P.op("pe", lambda e, ps=ps, c=c, mc=mc: e.matmul(ps.ap[:, 0:TB], kT3[:, 2 * h + c, mc * 128:(mc + 1) * 128], q3[:, 2 * h + c, :],
                                                             start=(c == 0), stop=(c == 1)), reads=[kT, qT], writes=[ps])
            P.op("act", lambda e, ps=ps, mc=mc: e.activation(p3[:, mc, :], ps.ap[:, 0:TB], AF.Exp, scale=1.0 / 16.0), reads=[ps], writes=[(pT, mc)])
        psd = P.ps()
        for mc in range(2):
            P.op("pe", lambda e, mc=mc: e.matmul(psd.ap[:, 0:TB], self.ones.ap, p3[:, mc, :], start=(mc == 0), stop=(mc == 1)),
                 reads=[pT, self.ones], writes=[psd])
        rden = P.alloc("xrd", TB)
        P.op("dve", lambda e: e.reciprocal(rden.ap, psd.ap[:, 0:TB]), reads=[psd], writes=[rden])
        for dc in range(2):
            pso = P.ps()
            for mc in range(2):
                P.op("pe", lambda e, pso=pso, mc=mc, dc=dc: e.matmul(pso.ap[:, 0:TB], vN3[:, mc, h * 256 + dc * 128:h * 256 + (dc + 1) * 128], p3[:, mc, :],
                                                                 start=(mc == 0), stop=(mc == 1)), reads=[vN, pT], writes=[pso])
            P.op("dve", lambda e, pso=pso, dc=dc: e.tensor_tensor(out=at3[:, 2 * h + dc, :], in0=pso.ap[:, 0:TB], in1=rden.ap, op=ALU.mult),
                 reads=[pso, rden], writes=[(at, 2 * h + dc)])
        P.release(pT, rden)

    def build(self):
        self.setup()
        self.setup_eps()
        if "mix" in self.phases:
            self.setup_mixer()
            if "nsa" in self.branches:
                self.setup_nsa()
            if "rwkv" in self.branches:
                self.setup_rwkv()
        self.load_x()
        if "xa" in self.phases:
            self.setup_xa()
        for l in range(self.depth):
            if "mix" in self.phases:
                self.mixer(l)
            if "xa" in self.phases:
                self.xattn(l)
            if "mlp" in self.phases:
                self.mlp(l)
        fin = self.store_x()
        self.P.finalize(fin)
        return self.nc


def host_inputs(inputs, depth=DEPTH):
    f = np.float32
    g = np.stack([np.asarray(inputs[k], f)[:depth] for k in
                  ("ln_mix_pre", "ln_mix_post", "ln_xa_pre", "ln_xa_post", "ln_mlp_pre", "ln_mlp_post", "ln_mem")], axis=1)
    gains = np.ascontiguousarray(g.reshape(depth, 7, NCH, 128).transpose(3, 0, 1, 2).reshape(128, depth * 7 * NCH))
    common = {
        "ident": np.eye(128, dtype=f),
        "gains": gains,
        "mlp_w1": np.ascontiguousarray(np.asarray(inputs["mlp_w1"], f)[:depth]),
        "mlp_w2": np.ascontiguousarray(np.asarray(inputs["mlp_w2"], f)[:depth]),
    }
    w_in = np.asarray(inputs["w_in"], f)[:depth]
    q = w_in[:, :, 0:256]
    kv = w_in[:, :, 256:640]
    gl = w_in[:, :, 640:652]
    ret = w_in[:, :, 652:1676]
    rw = w_in[:, :, 1676:2572]
    cv = w_in[:, :, 2572:3340]

    def swp(w):
        w4 = w.reshape(w.shape[0], w.shape[1], 4, 2, 32)
        return w4[:, :, :, ::-1, :].reshape(w.shape)
    rq, rk, rv, rgt = ret[..., 0:256], ret[..., 256:512], ret[..., 512:768], ret[..., 768:1024]
    common["w_T"] = np.ascontiguousarray(np.concatenate(
        [q, kv[..., 0:64], kv[..., 64:128], kv[..., 128:192], kv[..., 256:320], rq, swp(rq), rk, swp(rk), rgt, rw, cv], axis=-1))
    common["w_N"] = np.ascontiguousarray(np.concatenate([kv[..., 192:256], kv[..., 320:384], gl, rv], axis=-1))
    common["w_gate"] = np.ascontiguousarray(w_in[:, :, 3340:7436])
    common["w_branch"] = np.ascontiguousarray(np.asarray(inputs["w_branch"], f)[:depth])
    common["w_mix_out"] = np.ascontiguousarray(np.asarray(inputs["w_mix_out"], f)[:depth])
    cw = np.asarray(inputs["conv_w"], f)[:depth]
    common["conv_wT"] = np.ascontiguousarray(cw.reshape(depth, 3, 2, 128).transpose(0, 3, 2, 1).reshape(depth, 128, 6))
    common["ret_gT"] = np.ascontiguousarray(np.asarray(inputs["ret_norm_g"], f)[:depth].reshape(depth, 4, 64).transpose(0, 2, 1))
    half = 32
    inv_freq = (10000.0 ** (-np.arange(half, dtype=np.float32) / half)).astype(f)
    ang = np.arange(S, dtype=f)[:, None] * inv_freq[None, :]
    cosT = np.concatenate([np.cos(ang), np.cos(ang)], axis=1).T
    sinT = np.concatenate([-np.sin(ang), np.sin(ang)], axis=1).T
    common["rot_tab"] = np.ascontiguousarray(np.stack([cosT, sinT], axis=1).astype(f))
    kk = np.arange(128)[:, None]
    qq = np.arange(512)[None, :]
    dec = np.zeros((128, 4, 2, 512), np.float64)
    for h in range(4):
        lg = np.log(1.0 - 2.0 ** (-5.0 - h))
        full = np.exp(lg * (qq - kk)) * 0.125
        dec[:, h, 0] = full
        dec[:, h, 1] = np.where(qq >= kk, full, 0.0)
    common["ret_dec"] = np.ascontiguousarray(dec.reshape(128, -1).astype(f))
    import math
    common["nsa_cmp_w"] = np.ascontiguousarray(np.asarray(inputs["nsa_cmp_w"], f)[:depth])
    common["nsa_peT"] = np.ascontiguousarray(np.asarray(inputs["nsa_cmp_pe"], f)[:depth].transpose(0, 2, 1))
    rb = np.asarray(inputs["rel_bias"], f)

    def bucket(dist):
        n = np.maximum(dist, 0)
        nf = np.maximum(n, 1).astype(np.float32)
        large = 16 + (np.log(nf / np.float32(16)) / np.float32(math.log(128 / 16)) * np.float32(16)).astype(np.int32)
        large = np.minimum(large, 31)
        return np.where(n < 16, n, large)
    NEGB = np.float32(-30000.0)
    tpos = np.arange(S)
    nidx = np.arange(128)
    d_c = tpos[None, :] - (16 * nidx[:, None] + 31)
    bc = rb[bucket(d_c)]
    bc = np.where(((d_c >= 0) & (nidx[:, None] < 127))[:, :, None], bc, NEGB).transpose(0, 2, 1)
    common["nsa_biasc"] = np.ascontiguousarray(bc.astype(f))
    kk = np.arange(128)[:, None]
    qq = np.arange(128)[None, :]
    tabs = np.zeros((128, 4, 4, 128), f)
    d0 = qq - kk
    tabs[:, 0] = np.where((d0 >= 0)[:, None, :], rb[bucket(d0)].transpose(0, 2, 1), NEGB)
    tabs[:, 1] = rb[bucket(d0 + 128)].transpose(0, 2, 1)
    tabs[:, 2] = rb[31][None, :, None]
    tabs[:, 3] = np.where((d0 < 0)[:, None, :], rb[31][None, :, None], NEGB)
    common["nsa_tab"] = np.ascontiguousarray(tabs.reshape(128, -1))
    keys = np.arange(S)
    common["nsa_e2"] = np.ascontiguousarray(((keys[None, :] // 64) == np.arange(32)[:, None]).astype(f) * f(240000.0))
    cs = np.arange(127) * 16
    ss = np.arange(32) * 64
    ov = np.clip(np.minimum(cs[:, None] + 32, ss[None, :] + 64) - np.maximum(cs[:, None], ss[None, :]), 0, None).astype(f) / f(32)
    common["nsa_ovl"] = np.ascontiguousarray(np.concatenate([ov, np.zeros((1, 32), f)], axis=0))
    cur = tpos // 64
    blk = np.arange(32)
    forced = (blk[None, :] == 0) | (blk[None, :] == cur[:, None]) | (blk[None, :] == cur[:, None] - 1)
    addt = np.where(blk[None, :] <= cur[:, None], np.where(forced, f(1e4), f(0.0)), f(-1e30)).astype(f)
    common["nsa_addtab"] = np.ascontiguousarray(addt)
    mu = np.asarray(inputs["rwkv_mu"], f)[:depth]
    par = np.zeros((depth, 64, 43), f)
    par[:, :, 0:12] = mu[:, 0:768].reshape(depth, 3, 4, 64).transpose(0, 3, 1, 2).reshape(depth, 64, 12)
    par[:, 0:32, 12] = mu[:, 768:800]
    par[:, 0:32, 13] = mu[:, 800:832]
    par[:, 0:64, 14] = mu[:, 832:896]
    for wi, nm in enumerate(("rwkv_w0", "rwkv_a0", "rwkv_k_k", "rwkv_k_a", "rwkv_r_k", "rwkv_ln_g", "rwkv_ln_b")):
        par[:, :, 15 + wi * 4:15 + (wi + 1) * 4] = np.asarray(inputs[nm], f)[:depth].reshape(depth, 4, 64).transpose(0, 2, 1)
    common["rw_par"] = par
    for nm in ("rwkv_w2", "rwkv_a2", "rwkv_g2"):
        common[nm] = np.ascontiguousarray(np.asarray(inputs[nm], f)[:depth])
    ii = np.arange(64)[:, None]
    tt2 = np.arange(64)[None, :]
    mu_ = (ii < tt2).astype(f)
    mui = (ii <= tt2).astype(f)
    ml = (ii > tt2).astype(f)
    common["rw_mask5"] = np.ascontiguousarray(np.concatenate([mu_, mui, mu_, mui, ml], axis=1))
    for k in ("xa_wq", "xa_wkv", "xa_wo"):
        common[k] = np.ascontiguousarray(np.asarray(inputs[k], f)[:depth])
    maps = []
    for b in range(8):
        m = dict(common)
        m["mem"] = np.ascontiguousarray(np.asarray(inputs["mem"], f)[b])
        m["x"] = np.ascontiguousarray(np.asarray(inputs["x"], f)[b])
        maps.append(m)
    return maps


def kernel(**inputs):
    bld = Builder()
    nc = bld.build()
    maps = host_inputs(inputs)
    maps = [{k: v for k, v in m.items() if k in bld.inp} for m in maps]
    res = run_bass_kernel_spmd(nc, maps, core_ids=list(range(8)))
    return np.stack([np.asarray(r["out"], np.float32) for r in res.results], axis=0)
```

```python
import numpy as np
import concourse.bass as bass
import concourse.mybir as mybir
from concourse.bass_utils import run_bass_kernel_spmd

F32 = mybir.dt.float32
AF = mybir.ActivationFunctionType
ALU = mybir.AluOpType
AX = mybir.AxisListType

D = 1024
S = 2048
DEPTH = 4
MEM = 256
NCH = D // 128
HD = 64
DFF = 4096


class Op:
    __slots__ = ("eng", "emit", "deps", "idx", "flag", "val", "dma", "slot", "dval", "prev_slot_op")

    def __init__(self, eng, emit, dma=False):
        self.eng = eng
        self.emit = emit
        self.deps = []
        self.idx = -1
        self.flag = False
        self.val = 0
        self.dma = dma
        self.slot = -1
        self.dval = 0
        self.prev_slot_op = None


class TState:
    __slots__ = ("w", "r")

    def __init__(self):
        self.w = None
        self.r = {}

    def add_reader(self, o):
        k = o.slot if o.dma else o.eng
        p = self.r.get(k)
        if p is None or (o.dval > p.dval if o.dma else o.idx > p.idx):
            self.r[k] = o

    def all_ops(self):
        o = list(self.r.values())
        if self.w is not None:
            o.append(self.w)
        return o


class Tile:
    def __init__(self, name, ap, start=0, size=0):
        self.name = name
        self.ap = ap
        self.st = {}
        self.start = start
        self.size = size

    def __getitem__(self, k):
        return self.ap[k]


ND = 16


class Prog:
    SMALL = 1100
    COMPUTE = ("pe", "act", "dve", "pool")
    QUEUES = ("sp", "gq")

    def __init__(self, nc, arena_cols):
        self.nc = nc
        self.ops = {e: [] for e in ("pe", "act", "dve", "pool", "sp")}
        self.ndma = {"sp": 0, "gq": 0}
        self.slot_last = {"sp": [None] * ND, "gq": [None] * ND}
        self.arena = nc.alloc_sbuf_tensor("arena", [128, arena_cols], F32)
        self.free = [[0, arena_cols, []]]
        self.ncols = arena_cols
        self.cursor = arena_cols
        self.psum = [Tile(f"ps{i}", nc.alloc_psum_tensor(f"ps{i}", [128, 512], F32).ap()) for i in range(8)]
        self.ps_rr = 0
        self.held = set()

    def _take(self, i, name, cols, from_end):
        st, sz, pend = self.free[i]
        a = st + sz - cols if from_end else st
        t = Tile(name, self.arena[:, a:a + cols], a, cols)
        if pend:
            s = TState()
            for o in pend:
                s.add_reader(o)
            t.st[None] = s
        rest = []
        if a > st:
            rest.append([st, a - st, pend])
        if a + cols < st + sz:
            rest.append([a + cols, st + sz - a - cols, pend])
        self.free[i:i + 1] = rest
        return t

    def alloc(self, name, cols):
        if cols <= self.SMALL:
            for attempt in range(2):
                for i in range(len(self.free) - 1, -1, -1):
                    st, sz, _ = self.free[i]
                    if sz >= cols and st + cols <= self.cursor:
                        end = min(st + sz, self.cursor)
                        if end - st >= cols:
                            if end < st + sz:
                                pend = self.free[i][2]
                                self.free[i:i + 1] = [[st, end - st, pend], [end, st + sz - end, pend]]
                            t = self._take(i, name, cols, True)
                            self.cursor = t.start
                            return t
                self.cursor = self.ncols
        for i, (st, sz, pend) in enumerate(self.free):
            if sz >= cols:
                return self._take(i, name, cols, False)
        raise RuntimeError(f"SBUF arena full allocating {name} ({cols} cols); free={[(a, b) for a, b, _ in self.free]}")

    def release(self, *tiles):
        for t in tiles:
            tmp = TState()
            for s in t.st.values():
                for o in s.all_ops():
                    tmp.add_reader(o)
            self.free.append([t.start, t.size, list(tmp.r.values())])
        self.free.sort(key=lambda x: x[0])
        m = []
        for blk in self.free:
            if m and m[-1][0] + m[-1][1] == blk[0]:
                m[-1][1] += blk[1]
                tmp = TState()
                for o in m[-1][2] + blk[2]:
                    tmp.add_reader(o)
                m[-1][2] = list(tmp.r.values())
            else:
                m.append(blk)
        self.free = m

    def dram(self, name, shape, kind="Internal"):
        h = self.nc.dram_tensor(name, list(shape), F32, kind=kind)
        return Tile(name, h.ap())

    def ps(self, hold=False):
        while True:
            t = self.psum[self.ps_rr % 8]
            self.ps_rr += 1
            if t.name not in self.held:
                break
        if hold:
            self.held.add(t.name)
        return t

    def ps_free(self, *ts):
        for t in ts:
            self.held.discard(t.name)

    @staticmethod
    def _norm(x):
        if isinstance(x, tuple):
            return (x[0], None) if x[0].name.startswith("ps") else x
        return (x, None)

    def _track(self, op, reads, writes):
        pr = [x for x in reads if self._norm(x)[0].name.startswith("ps")]
        if pr:
            reads = [x for x in reads if not self._norm(x)[0].name.startswith("ps")]
            writes = list(writes) + [x for x in pr if all(self._norm(x)[0] is not self._norm(w)[0] for w in writes)]
        deps = []
        for x in reads:
            t, k = self._norm(x)
            for kk, s in t.st.items():
                if k is None or kk is None or kk == k:
                    if s.w is not None:
                        deps.append(s.w)
        for x in writes:
            t, k = self._norm(x)
            for kk, s in t.st.items():
                if k is None or kk is None or kk == k:
                    deps.extend(s.all_ops())
        for x in reads:
            t, k = self._norm(x)
            s = t.st.get(k)
            if s is None:
                s = t.st[k] = TState()
            s.add_reader(op)
        for x in writes:
            t, k = self._norm(x)
            if k is None:
                t.st.clear()
            s = t.st[k] = TState()
            s.w = op
        seen = set()
        for d in deps:
            if d is op or id(d) in seen:
                continue
            if op.eng == "pe" and d.eng == "pe" and not d.dma:
                continue
            seen.add(id(d))
            d.flag = True
            op.deps.append(d)

    def op(self, eng, emit, reads=(), writes=()):
        o = Op(eng, emit)
        o.idx = len(self.ops[eng])
        self._track(o, reads, writes)
        self.ops[eng].append(o)
        return o

    def dma(self, out_ap, in_ap, reads=(), writes=(), q="sp"):
        eng = "sp" if q == "sp" else "pool"
        o = Op(eng, None, dma=True)
        o.emit = lambda e: e.dma_start(out=out_ap, in_=in_ap)
        n = self.ndma[q]
        self.ndma[q] = n + 1
        o.slot = (q, n % ND)
        o.dval = 16 * (n // ND + 1)
        o.prev_slot_op = self.slot_last[q][n % ND]
        self.slot_last[q][n % ND] = o
        o.idx = len(self.ops[eng])
        self._track(o, reads, writes)
        self.ops[eng].append(o)
        return o

    def finalize(self, final_wait_ops):
        nc = self.nc
        esem = {e: nc.alloc_semaphore(f"sem_{e}") for e in self.COMPUTE}
        dsem = {q: [nc.alloc_semaphore(f"dsem_{q}{i}") for i in range(ND)] for q in self.QUEUES}
        for e in self.COMPUTE:
            c = 0
            for o in self.ops[e]:
                if o.dma:
                    continue
                if o.flag:
                    c += 1
                    o.val = c

        def token(d):
            if d.dma:
                return dsem[d.slot[0]][d.slot[1]], d.dval
            return esem[d.eng], d.val

        ops = self.ops

        def run(ename, eng):
            waited = {}
            for o in ops[ename]:
                deps = list(o.deps)
                if o.dma and o.prev_slot_op is not None:
                    deps.append(o.prev_slot_op)
                need = {}
                for d in deps:
                    sem, v = token(d)
                    if waited.get(sem.num, 0) >= v:
                        continue
                    if need.get(sem.num, (None, 0))[1] < v:
                        need[sem.num] = (sem, v)
                for num, (sem, v) in need.items():
                    eng.wait_ge(sem, v)
                    waited[num] = v
                ins = o.emit(eng)
                if o.dma:
                    ins.then_inc(dsem[o.slot[0]][o.slot[1]], 16)
                elif o.flag:
                    ins.then_inc(esem[o.eng], 1)
            if ename == "sp":
                for d in final_wait_ops:
                    sem, v = token(d)
                    if waited.get(sem.num, 0) < v:
                        eng.wait_ge(sem, v)
                        waited[sem.num] = v

        with nc.Block() as block:
            @block.tensor
            def _(e):
                run("pe", e)

            @block.scalar
            def _(e):
                run("act", e)

            @block.vector
            def _(e):
                run("dve", e)

            @block.gpsimd
            def _(e):
                run("pool", e)

            @block.sync
            def _(e):
                run("sp", e)


class Builder:
    def __init__(self, depth=DEPTH, phases=("mix", "xa", "mlp"), debug_outs=(), branches=("nsa", "ret", "rwkv", "conv")):
        self.depth = depth
        self.branches = branches
        self.phases = phases
        nc = self.nc = bass.Bass("TRN2", target_bir_lowering=False)
        P = self.P = Prog(nc, 51200)
        self.inp = {}
        self.debug_outs = debug_outs

    def din(self, name, shape):
        t = self.P.dram(name, shape, kind="ExternalInput")
        self.inp[name] = t
        return t

    def setup(self):
        P = self.P
        self.x_in = self.din("x", [S, D])
        self.out = self.P.dram("out", [S, D], kind="ExternalOutput")
        self.ident_d = self.din("ident", [128, 128])
        self.gains_d = self.din("gains", [128, self.depth * 7 * NCH])
        self.w1 = self.din("mlp_w1", [self.depth, D, DFF])
        self.w2 = self.din("mlp_w2", [self.depth, DFF, D])

        self.xT = P.alloc("xT", NCH * S)
        self.xT3 = self.xT.ap.rearrange("p (c t) -> p c t", c=NCH)
        self.ident = P.alloc("ident", 128)
        self.ones = P.alloc("ones", 128)
        self.gains = P.alloc("gains", self.depth * 7 * NCH)
        P.dma(self.ident.ap, self.ident_d.ap, reads=[self.ident_d], writes=[self.ident])
        P.dma(self.gains.ap, self.gains_d.ap, reads=[self.gains_d], writes=[self.gains])
        P.op("dve", lambda e: e.memset(self.ones.ap, 1.0), writes=[self.ones])

    def gain(self, l, which, c):
        i = (l * 7 + which) * NCH + c
        return self.gains.ap[:, i:i + 1]

    def load_x(self):
        P = self.P
        for tt in range(S // 128):
            xin = P.alloc("xin", D)
            P.dma(xin.ap, self.x_in.ap[tt * 128:(tt + 1) * 128, :], reads=[self.x_in], writes=[xin])
            for half in range(2):
                ps = P.ps()
                for j in range(4):
                    c = half * 4 + j
                    P.op("pe", lambda e, ps=ps, j=j, c=c, xin=xin: e.transpose(
                        ps.ap[:, j * 128:(j + 1) * 128], xin.ap[:, c * 128:(c + 1) * 128], self.ident.ap),
                        reads=[xin, self.ident], writes=[(ps, j)])
                dst = self.xT3[:, half * 4:half * 4 + 4, tt * 128:(tt + 1) * 128]
                P.op("act", lambda e, ps=ps, dst=dst: e.copy(dst, ps.ap.rearrange("p (c t) -> p c t", c=4)),
                     reads=[ps], writes=[(self.xT, tt)])
            P.release(xin)

    def store_x(self):
        P = self.P
        fin = []
        for tt in range(S // 128):
            xo = P.alloc("xo", D)
            for half in range(2):
                ps = P.ps()
                for j in range(4):
                    c = half * 4 + j
                    P.op("pe", lambda e, ps=ps, j=j, c=c, tt=tt: e.transpose(
                        ps.ap[:, j * 128:(j + 1) * 128], self.xT3[:, c, tt * 128:(tt + 1) * 128], self.ident.ap),
                        reads=[self.xT, self.ident], writes=[(ps, j)])
                P.op("act", lambda e, ps=ps, xo=xo, half=half: e.copy(xo.ap[:, half * 512:(half + 1) * 512], ps.ap),
                     reads=[ps], writes=[(xo, half)])
            fin.append(P.dma(self.out.ap[tt * 128:(tt + 1) * 128, :], xo.ap, reads=[xo], writes=[(self.out, tt)]))
            P.release(xo)
        return fin

    def rstd_of(self, src3, src_tile, n, rstd, eps=1e-6):
        P = self.P
        ps = P.ps()
        for c in range(NCH):
            sq = P.alloc("sq", n)
            P.op("act", lambda e, sq=sq, c=c: e.activation(sq.ap, src3[:, c, :], AF.Square),
                 reads=[src_tile], writes=[sq])
            P.op("pe", lambda e, sq=sq, c=c, ps=ps: e.matmul(ps.ap[:, 0:n], self.ones.ap, sq.ap,
                                                         start=(c == 0), stop=(c == NCH - 1)),
                 reads=[sq, self.ones], writes=[ps])
            P.release(sq)
        P.op("act", lambda e, ps=ps: e.activation(rstd.ap[:, 0:n], ps.ap[:, 0:n], AF.Sqrt, bias=self.epsb(eps), scale=1.0 / D),
             reads=[ps, self.epst], writes=[rstd])
        P.op("dve", lambda e: e.reciprocal(rstd.ap[:, 0:n], rstd.ap[:, 0:n]), reads=[rstd], writes=[rstd])

    def epsb(self, eps):
        return self.epst.ap[:, 0:1]

    def setup_eps(self):
        P = self.P
        self.epst = P.alloc("eps", 4)
        P.op("dve", lambda e: e.memset(self.epst.ap[:, 0:1], 1e-6), writes=[self.epst])
        P.op("dve", lambda e: e.memset(self.epst.ap[:, 1:2], 1e-5), writes=[self.epst])
        P.op("dve", lambda e: e.memset(self.epst.ap[:, 2:3], 64e-5), writes=[self.epst])

    def pre_norm_block(self, l, which, t0, n, hblk):
        h3 = hblk.ap.rearrange("p (c t) -> p c t", c=NCH)
        self.norm_to(l, which, self.xT3[:, :, t0:t0 + n], self.xT, n, h3, hblk)

    def norm_to(self, l, which, src3, src_tile, n, dst3, dst_tile):
        P = self.P
        rstd = P.alloc("rstd", n)
        self.rstd_of(src3, src_tile, n, rstd)
        for c in range(NCH):
            P.op("dve", lambda e, c=c: e.scalar_tensor_tensor(
                out=dst3[:, c, :], in0=src3[:, c, :], scalar=self.gain(l, which, c), in1=rstd.ap[:, 0:n],
                op0=ALU.mult, op1=ALU.mult), reads=[src_tile, rstd, self.gains], writes=[dst_tile])
        P.release(rstd)

    def post_norm_residual(self, l, which, t0, n, yblk):
        P = self.P
        rstd = P.alloc("rstd", n)
        y3 = yblk.ap.rearrange("p (c t) -> p c t", c=NCH)
        self.rstd_of(y3, yblk, n, rstd)
        for c in range(NCH):
            P.op("dve", lambda e, c=c: e.tensor_tensor(out=y3[:, c, :], in0=y3[:, c, :], in1=rstd.ap[:, 0:n], op=ALU.mult),
                 reads=[yblk, rstd], writes=[(yblk, c)])
            dst = self.xT3[:, c, t0:t0 + n]
            P.op("dve", lambda e, c=c, dst=dst: e.scalar_tensor_tensor(
                out=dst, in0=y3[:, c, :], scalar=self.gain(l, which, c), in1=dst, op0=ALU.mult, op1=ALU.add),
                reads=[(yblk, c), self.xT, self.gains], writes=[self.xT])
        P.release(rstd)

    def mlp(self, l):
        P = self.P
        TB = 256
        w1 = self.w1.ap[l]
        w2 = self.w2.ap[l]
        for tb in range(S // TB):
            self.mlp_blk(l, tb, TB, w1, w2)

    def mlp_blk(self, l, tb, TB, w1, w2):
        P = self.P
        if True:
            t0 = tb * TB
            hblk = P.alloc("hblk", NCH * TB)
            self.pre_norm_block(l, 4, t0, TB, hblk)
            h3 = hblk.ap.rearrange("p (c t) -> p c t", c=NCH)
            ablk = P.alloc("ablk", (DFF // 128) * TB)
            a3 = ablk.ap.rearrange("p (c t) -> p c t", c=DFF // 128)
            w1ring = [P.alloc("w1t", NCH * 512) for _ in range(2)]
            for fg in range(DFF // 512):
                wt = w1ring[fg % 2]
                wt3 = wt.ap.rearrange("p (k n) -> p k n", k=NCH)
                P.dma(wt3, w1[:, fg * 512:(fg + 1) * 512].rearrange("(k p) n -> p k n", p=128), reads=[self.w1], writes=[wt])
                for j in range(4):
                    f = fg * 4 + j
                    ps = P.ps()
                    for k in range(NCH):
                        P.op("pe", lambda e, ps=ps, k=k, j=j, wt3=wt3: e.matmul(
                            ps.ap[:, 0:TB], wt3[:, k, j * 128:(j + 1) * 128], h3[:, k, :], start=(k == 0), stop=(k == NCH - 1)),
                            reads=[wt, hblk], writes=[ps])
                    r = P.alloc("relu", TB)
                    P.op("act", lambda e, ps=ps, r=r: e.activation(r.ap, ps.ap[:, 0:TB], AF.Relu), reads=[ps], writes=[r])
                    P.op("dve", lambda e, r=r, f=f: e.tensor_tensor(out=a3[:, f, :], in0=r.ap, in1=r.ap, op=ALU.mult),
                         reads=[r], writes=[(ablk, f)])
                    P.release(r)
            P.release(*w1ring)
            yblk = P.alloc("yblk", NCH * TB)
            y3 = yblk.ap.rearrange("p (c t) -> p c t", c=NCH)
            KG = 4
            pss = [P.ps(hold=True) for _ in range(4)]
            w2ring = [P.alloc("w2t", KG * D) for _ in range(2)]
            for kg in range(DFF // 128 // KG):
                wt = w2ring[kg % 2]
                wt3 = wt.ap.rearrange("p (k n) -> p k n", k=KG)
                P.dma(wt3, w2[kg * KG * 128:(kg + 1) * KG * 128, :].rearrange("(k p) n -> p k n", p=128), reads=[self.w2], writes=[wt])
                for f in range(NCH):
                    ps = pss[f // 2]
                    o = (f % 2) * TB
                    for k in range(KG):
                        kk = kg * KG + k
                        first = (kk == 0 and f % 2 == 0)
                        P.op("pe", lambda e, ps=ps, o=o, k=k, kk=kk, f=f, wt3=wt3, first=first: e.matmul(
                            ps.ap[:, o:o + TB], wt3[:, k, f * 128:(f + 1) * 128], a3[:, kk, :], start=first,
                            stop=(kk == DFF // 128 - 1), skip_group_check=True),
                            reads=[wt, ablk], writes=[(ps, f % 2)])
            P.release(*w2ring)
            for f in range(NCH):
                ps = pss[f // 2]
                o = (f % 2) * TB
                P.op("act", lambda e, ps=ps, o=o, f=f: e.copy(y3[:, f, :], ps.ap[:, o:o + TB]), reads=[(ps, f % 2)], writes=[(yblk, f)])
            P.ps_free(*pss)
            P.release(ablk, hblk)
            self.post_norm_residual(l, 5, t0, TB, yblk)
            P.release(yblk)


    ZT_ROWS = 3456
    ZQ, ZKC, ZVC, ZKS, ZKW, ZRQ, ZRQS, ZRK, ZRKS, ZRG, ZRW, ZCV = 0, 256, 320, 384, 448, 512, 768, 1024, 1280, 1536, 1792, 2688
    ZN_COLS = 396
    NVS, NVW, NG, NRV = 0, 64, 128, 140

    def setup_mixer(self):
        L = self.depth
        self.wt_d = self.din("w_T", [L, D, self.ZT_ROWS])
        self.wn_d = self.din("w_N", [L, D, self.ZN_COLS])
        self.wgate_d = self.din("w_gate", [L, D, 4 * D])
        self.wbr_d = self.din("w_branch", [L, 4, 256, D])
        self.wmo_d = self.din("w_mix_out", [L, D, D])
        self.convw_d = self.din("conv_wT", [L, 128, 6])
        self.rot_d = self.din("rot_tab", [64, 2, S])
        self.rdec_d = self.din("ret_dec", [128, 4 * 2 * 512])
        self.retg_d = self.din("ret_gT", [L, 64, 4])
        self.ZT = self.P.dram("ZT", [self.ZT_ROWS, S])
        self.ZN = self.P.dram("ZN", [S, self.ZN_COLS])
        self.OBR = self.P.dram("OBR", [D, S])

    def project(self, l, hT):
        P = self.P
        h3 = hT.ap.rearrange("p (c t) -> p c t", c=NCH)
        wn = P.alloc("wn", NCH * self.ZN_COLS)
        wn3 = wn.ap.rearrange("p (k n) -> p k n", k=NCH)
        P.dma(wn3, self.wn_d.ap[l].rearrange("(k p) n -> p k n", p=128), reads=[self.wn_d], writes=[wn])
        for tt in range(S // 128):
            ps = P.ps()
            for k in range(NCH):
                P.op("pe", lambda e, ps=ps, k=k, tt=tt: e.matmul(ps.ap[:, 0:self.ZN_COLS], h3[:, k, tt * 128:(tt + 1) * 128], wn3[:, k, :],
                                                             start=(k == 0), stop=(k == NCH - 1)), reads=[hT, wn], writes=[ps])
            stg = P.alloc("stgn", self.ZN_COLS)
            P.op("act", lambda e, ps=ps, stg=stg: e.copy(stg.ap, ps.ap[:, 0:self.ZN_COLS]), reads=[ps], writes=[stg])
            P.dma(self.ZN.ap[tt * 128:(tt + 1) * 128, :], stg.ap, reads=[stg], writes=[(self.ZN, tt)], q="gq")
            P.release(stg)
        P.release(wn)
        for ch in range(self.ZT_ROWS // 128):
            wt = P.alloc("wt", NCH * 128)
            wt3 = wt.ap.rearrange("p (k n) -> p k n", k=NCH)
            P.dma(wt3, self.wt_d.ap[l][:, ch * 128:(ch + 1) * 128].rearrange("(k p) n -> p k n", p=128), reads=[self.wt_d], writes=[wt])
            for tb in range(S // 512):
                ps = P.ps()
                for k in range(NCH):
                    P.op("pe", lambda e, ps=ps, k=k, tb=tb, wt3=wt3: e.matmul(ps.ap, wt3[:, k, :], h3[:, k, tb * 512:(tb + 1) * 512],
                                                                       start=(k == 0), stop=(k == NCH - 1)), reads=[hT, wt], writes=[ps])
                stg = P.alloc("stgt", 512)
                eng = "act" if tb % 2 == 0 else "dve"
                if eng == "act":
                    P.op("act", lambda e, ps=ps, stg=stg: e.copy(stg.ap, ps.ap), reads=[ps], writes=[stg])
                else:
                    P.op("dve", lambda e, ps=ps, stg=stg: e.tensor_copy(stg.ap, ps.ap), reads=[ps], writes=[stg])
                P.dma(self.ZT.ap[ch * 128:(ch + 1) * 128, tb * 512:(tb + 1) * 512], stg.ap, reads=[stg], writes=[(self.ZT, ch)], q="gq")
                P.release(stg)
            P.release(wt)

    def conv_branch(self, l):
        P = self.P
        cw = P.alloc("convw", 6)
        P.dma(cw.ap, self.convw_d.ap[l], reads=[self.convw_d], writes=[cw])
        for c in range(2):
            self.conv_chunk(cw, c)
        P.release(cw)

    def conv_chunk(self, cw, c):
        P = self.P
        if True:
            bg = P.alloc("cv_b", S)
            cg = P.alloc("cv_c", S)
            xt = P.alloc("cv_x", S)
            for j, t in enumerate((bg, cg, xt)):
                r0 = self.ZCV + j * 256 + c * 128
                P.dma(t.ap, self.ZT.ap[r0:r0 + 128, :], reads=[(self.ZT, r0 // 128)], writes=[t])
            w = lambda j: cw.ap[:, c * 3 + j:c * 3 + j + 1]
            P.op("dve", lambda e: e.tensor_tensor(out=cg.ap, in0=cg.ap, in1=xt.ap, op=ALU.mult), reads=[cg, xt], writes=[cg])
            P.op("dve", lambda e, w=w: e.tensor_scalar(out=xt.ap, in0=cg.ap, scalar1=w(2), scalar2=None, op0=ALU.mult), reads=[cg, cw], writes=[xt])
            P.op("dve", lambda e, w=w: e.scalar_tensor_tensor(out=xt.ap[:, 1:S], in0=cg.ap[:, 0:S - 1], scalar=w(1), in1=xt.ap[:, 1:S],
                                                              op0=ALU.mult, op1=ALU.add), reads=[cg, cw, xt], writes=[xt])
            P.op("dve", lambda e, w=w: e.scalar_tensor_tensor(out=xt.ap[:, 2:S], in0=cg.ap[:, 0:S - 2], scalar=w(0), in1=xt.ap[:, 2:S],
                                                              op0=ALU.mult, op1=ALU.add), reads=[cg, cw, xt], writes=[xt])
            P.op("dve", lambda e: e.tensor_tensor(out=bg.ap, in0=bg.ap, in1=xt.ap, op=ALU.mult), reads=[bg, xt], writes=[bg])
            P.dma(self.OBR.ap[768 + c * 128:768 + (c + 1) * 128, :], bg.ap, reads=[bg], writes=[(self.OBR, 6 + c)], q="gq")
            P.release(bg, cg, xt)

    def retention_branch(self, l):
        P = self.P
        rot = P.alloc("rot", 2 * S)
        rot3 = rot.ap.rearrange("p (a t) -> p a t", a=2)
        P.dma(rot3[0:64], self.rot_d.ap, reads=[self.rot_d], writes=[rot])
        dec = P.alloc("rdec", 4 * 2 * 512)
        dec4 = dec.ap.rearrange("p (h a q) -> p h a q", h=4, a=2)
        P.dma(dec.ap, self.rdec_d.ap, reads=[self.rdec_d], writes=[dec])
        rg = P.alloc("retg", 4)
        P.dma(rg.ap[0:64], self.retg_d.ap[l], reads=[self.retg_d], writes=[rg])
        for h in range(4):
            self.ret_head(l, h, rot, rot3, dec, dec4, rg)
        P.release(rot, dec, rg)

    def ret_head(self, l, h, rot, rot3, dec, dec4, rg):
        P = self.P
        if True:
            lg = float(np.log(1.0 - 2.0 ** (-5.0 - h)))
            qk = []
            for base, bsw in ((self.ZRQ, self.ZRQS), (self.ZRK, self.ZRKS)):
                u = P.alloc("ru", S)
                us = P.alloc("rus", S)
                P.dma(u.ap[0:64], self.ZT.ap[base + h * 64:base + (h + 1) * 64, :], reads=[(self.ZT, (base + h * 64) // 128)], writes=[u])
                P.dma(us.ap[0:64], self.ZT.ap[bsw + h * 64:bsw + (h + 1) * 64, :], reads=[(self.ZT, (bsw + h * 64) // 128)], writes=[us])
                P.op("dve", lambda e, u=u: e.tensor_tensor(out=u.ap[0:64], in0=u.ap[0:64], in1=rot3[0:64, 0, :], op=ALU.mult), reads=[u, rot], writes=[u])
                P.op("dve", lambda e, us=us: e.tensor_tensor(out=us.ap[0:64], in0=us.ap[0:64], in1=rot3[0:64, 1, :], op=ALU.mult), reads=[us, rot], writes=[us])
                P.op("dve", lambda e, u=u, us=us: e.tensor_tensor(out=u.ap[0:64], in0=u.ap[0:64], in1=us.ap[0:64], op=ALU.add), reads=[u, us], writes=[u])
                P.release(us)
                qk.append(u)
            qT, kT = qk
            vh = P.alloc("rv", 16 * 64)
            vh3 = vh.ap.rearrange("p (t d) -> p t d", t=16)
            P.dma(vh3, self.ZN.ap[:, self.NRV + h * 64:self.NRV + (h + 1) * 64].rearrange("(t p) d -> p t d", p=128), reads=[self.ZN], writes=[vh])
            for Q in range(4):
                pso = P.ps(hold=True)
                first = True
                nkb = 4 * Q + 4
                for kb in range(nkb):
                    j = kb - 4 * Q
                    c0 = 128 * j if j > 0 else 0
                    nq = 512 - c0
                    pss = P.ps()
                    P.op("pe", lambda e, pss=pss, kb=kb, Q=Q, c0=c0, nq=nq: e.matmul(
                        pss.ap[:, 0:nq], kT.ap[0:64, kb * 128:(kb + 1) * 128], qT.ap[0:64, Q * 512 + c0:(Q + 1) * 512], start=True, stop=True),
                        reads=[kT, qT], writes=[pss])
                    pT = P.alloc("rp", 512)
                    if j < 0:
                        sc = float(np.exp(lg * 128.0 * (4 * Q - kb)))
                        P.op("dve", lambda e, pss=pss, pT=pT, sc=sc, h=h: e.scalar_tensor_tensor(
                            out=pT.ap, in0=pss.ap, scalar=sc, in1=dec4[:, h, 0, :], op0=ALU.mult, op1=ALU.mult), reads=[pss, dec], writes=[pT])
                    else:
                        P.op("dve", lambda e, pss=pss, pT=pT, nq=nq, h=h: e.tensor_tensor(
                            out=pT.ap[:, 0:nq], in0=pss.ap[:, 0:nq], in1=dec4[:, h, 1, 0:nq], op=ALU.mult), reads=[pss, dec], writes=[pT])
                    P.op("pe", lambda e, pso=pso, pT=pT, kb=kb, c0=c0, nq=nq, first=first, last=(kb == nkb - 1): e.matmul(
                        pso.ap[0:64, c0:512], vh3[:, kb, :], pT.ap[:, 0:nq], start=first, stop=last, skip_group_check=True),
                        reads=[vh, pT], writes=[pso])
                    first = False
                    P.release(pT)
                self.ret_epilogue(l, h, Q, pso, rg)
                P.ps_free(pso)
            P.release(qT, kT, vh)

    def ret_epilogue(self, l, h, Q, pso, rg):
        P = self.P
        n = 512
        o = P.alloc("ro", n)
        sq = P.alloc("rsq", n)
        P.op("act", lambda e: e.copy(o.ap[0:64], pso.ap[0:64, :]), reads=[pso], writes=[o])
        P.op("act", lambda e: e.activation(sq.ap[0:64], pso.ap[0:64, :], AF.Square), reads=[pso], writes=[sq])
        p1 = P.ps()
        p2 = P.ps()
        P.op("pe", lambda e: e.matmul(p1.ap[0:64, :], self.ones.ap[0:64, 0:64], o.ap[0:64], start=True, stop=True), reads=[o, self.ones], writes=[p1])
        P.op("pe", lambda e: e.matmul(p2.ap[0:64, :], self.ones.ap[0:64, 0:64], sq.ap[0:64], start=True, stop=True), reads=[sq, self.ones], writes=[p2])
        mean = P.alloc("rmean", n)
        P.op("dve", lambda e: e.tensor_scalar(out=mean.ap[0:64], in0=p1.ap[0:64, :], scalar1=1.0 / 64, scalar2=None, op0=ALU.mult), reads=[p1], writes=[mean])
        P.op("dve", lambda e: e.tensor_tensor(out=o.ap[0:64], in0=o.ap[0:64], in1=mean.ap[0:64], op=ALU.subtract), reads=[o, mean], writes=[o])
        P.op("dve", lambda e: e.tensor_tensor(out=mean.ap[0:64], in0=mean.ap[0:64], in1=mean.ap[0:64], op=ALU.mult), reads=[mean], writes=[mean])
        P.op("dve", lambda e: e.scalar_tensor_tensor(out=sq.ap[0:64], in0=p2.ap[0:64, :], scalar=1.0 / 64, in1=mean.ap[0:64],
                                                     op0=ALU.mult, op1=ALU.subtract), reads=[p2, mean], writes=[sq])
        P.op("act", lambda e: e.activation(sq.ap[0:64], sq.ap[0:64], AF.Sqrt, bias=self.epst.ap[0:64, 1:2], scale=1.0), reads=[sq, self.epst], writes=[sq])
        P.op("dve", lambda e: e.reciprocal(sq.ap[0:64], sq.ap[0:64]), reads=[sq], writes=[sq])
        P.op("dve", lambda e: e.tensor_tensor(out=o.ap[0:64], in0=o.ap[0:64], in1=sq.ap[0:64], op=ALU.mult), reads=[o, sq], writes=[o])
        g = P.alloc("rgate", n)
        r0 = self.ZRG + h * 64
        P.dma(g.ap[0:64], self.ZT.ap[r0:r0 + 64, Q * n:(Q + 1) * n], reads=[(self.ZT, r0 // 128)], writes=[g])
        P.op("act", lambda e: e.activation(g.ap[0:64], g.ap[0:64], AF.Silu), reads=[g], writes=[g])
        P.op("dve", lambda e: e.scalar_tensor_tensor(out=o.ap[0:64], in0=o.ap[0:64], scalar=rg.ap[0:64, h:h + 1], in1=g.ap[0:64],
                                                     op0=ALU.mult, op1=ALU.mult), reads=[o, rg, g], writes=[o])
        P.dma(self.OBR.ap[256 + h * 64:256 + (h + 1) * 64, Q * n:(Q + 1) * n], o.ap[0:64], reads=[o], writes=[(self.OBR, 2 + h // 2)], q="gq")
        P.release(o, sq, mean, g)

    def merge(self, l, hT):
        P = self.P
        TB = 256
        for tb in range(S // TB):
            self.merge_blk(l, hT, tb, TB)

    def merge_blk(self, l, hT, tb, TB):
        P = self.P
        h3 = hT.ap.rearrange("p (c t) -> p c t", c=NCH)
        if True:
            t0 = tb * TB
            obr = P.alloc("obr", NCH * TB)
            obr3 = obr.ap.rearrange("p (c t) -> p c t", c=NCH)
            P.dma(obr3, self.OBR.ap[:, t0:t0 + TB].rearrange("(c p) t -> p c t", p=128), reads=[self.OBR], writes=[obr])
            mrg = P.alloc("mrg", NCH * TB)
            m3 = mrg.ap.rearrange("p (c t) -> p c t", c=NCH)
            for f in range(NCH):
                for m in range(4):
                    wg = P.alloc("wg", NCH * 128)
                    wg3 = wg.ap.rearrange("p (k n) -> p k n", k=NCH)
                    c0 = m * D + f * 128
                    P.dma(wg3, self.wgate_d.ap[l][:, c0:c0 + 128].rearrange("(k p) n -> p k n", p=128), reads=[self.wgate_d], writes=[wg])
                    wb = P.alloc("wb", 2 * 128)
                    wb3 = wb.ap.rearrange("p (k n) -> p k n", k=2)
                    P.dma(wb3, self.wbr_d.ap[l, m][:, f * 128:(f + 1) * 128].rearrange("(k p) n -> p k n", p=128), reads=[self.wbr_d], writes=[wb])
                    ps1 = P.ps()
                    for k in range(NCH):
                        P.op("pe", lambda e, ps1=ps1, k=k, wg3=wg3: e.matmul(ps1.ap[:, 0:TB], wg3[:, k, :], h3[:, k, t0:t0 + TB],
                                                                          start=(k == 0), stop=(k == NCH - 1)), reads=[wg, hT], writes=[ps1])
                    ps2 = P.ps()
                    for k in range(2):
                        P.op("pe", lambda e, ps2=ps2, k=k, m=m, wb3=wb3: e.matmul(ps2.ap[:, 0:TB], wb3[:, k, :], obr3[:, 2 * m + k, :],
                                                                               start=(k == 0), stop=(k == 1)), reads=[wb, obr], writes=[ps2])
                    g = P.alloc("mg", TB)
                    P.op("act", lambda e, ps1=ps1, g=g: e.activation(g.ap, ps1.ap[:, 0:TB], AF.Sigmoid), reads=[ps1], writes=[g])
                    if m == 0:
                        P.op("dve", lambda e, g=g, ps2=ps2, f=f: e.tensor_tensor(out=m3[:, f, :], in0=g.ap, in1=ps2.ap[:, 0:TB], op=ALU.mult),
                             reads=[g, ps2], writes=[(mrg, f)])
                    else:
                        P.op("dve", lambda e, g=g, ps2=ps2: e.tensor_tensor(out=g.ap, in0=g.ap, in1=ps2.ap[:, 0:TB], op=ALU.mult),
                             reads=[g, ps2], writes=[g])
                        P.op("dve", lambda e, g=g, f=f: e.tensor_tensor(out=m3[:, f, :], in0=m3[:, f, :], in1=g.ap, op=ALU.add),
                             reads=[g, (mrg, f)], writes=[(mrg, f)])
                    P.release(g, wg, wb)
            P.release(obr)
            yblk = P.alloc("yblk", NCH * TB)
            y3 = yblk.ap.rearrange("p (c t) -> p c t", c=NCH)
            for f in range(NCH):
                wo = P.alloc("wmo", NCH * 128)
                wo3 = wo.ap.rearrange("p (k n) -> p k n", k=NCH)
                P.dma(wo3, self.wmo_d.ap[l][:, f * 128:(f + 1) * 128].rearrange("(k p) n -> p k n", p=128), reads=[self.wmo_d], writes=[wo])
                ps = P.ps()
                for k in range(NCH):
                    P.op("pe", lambda e, ps=ps, k=k, wo3=wo3: e.matmul(ps.ap[:, 0:TB], wo3[:, k, :], m3[:, k, :], start=(k == 0), stop=(k == NCH - 1)),
                         reads=[wo, mrg], writes=[ps])
                P.op("act", lambda e, ps=ps, f=f: e.copy(y3[:, f, :], ps.ap[:, 0:TB]), reads=[ps], writes=[(yblk, f)])
                P.release(wo)
            P.release(mrg)
            self.post_norm_residual(l, 1, t0, TB, yblk)
            P.release(yblk)

    def zero_obr(self, r0, r1):
        P = self.P
        z = P.alloc("zero", S)
        P.op("dve", lambda e: e.memset(z.ap, 0.0), writes=[z])
        for r in range(r0, r1, 128):
            P.dma(self.OBR.ap[r:r + 128, :], z.ap, reads=[z], writes=[(self.OBR, r // 128)], q="gq")
        P.release(z)

    def mixer(self, l):
        P = self.P
        hT = P.alloc("hT", NCH * S)
        h3 = hT.ap.rearrange("p (c t) -> p c t", c=NCH)
        for tb in range(4):
            self.norm_to(l, 0, self.xT3[:, :, tb * 512:(tb + 1) * 512], self.xT, 512, h3[:, :, tb * 512:(tb + 1) * 512], hT)
        self.project(l, hT)
        P.release(hT)
        if "nsa" in self.branches:
            self.nsa_branch(l)
        else:
            self.zero_obr(0, 256)
        if "ret" in self.branches:
            self.retention_branch(l)
        else:
            self.zero_obr(256, 512)
        if "rwkv" in self.branches:
            self.rwkv_branch(l)
        else:
            self.zero_obr(512, 768)
        if "conv" in self.branches:
            self.conv_branch(l)
        else:
            self.zero_obr(768, 1024)
        if "nomerge" not in self.phases:
            hT = P.alloc("hT", NCH * S)
            h3 = hT.ap.rearrange("p (c t) -> p c t", c=NCH)
            for tb in range(4):
                self.norm_to(l, 0, self.xT3[:, :, tb * 512:(tb + 1) * 512], self.xT, 512, h3[:, :, tb * 512:(tb + 1) * 512], hT)
            self.merge(l, hT)
            P.release(hT)


    def setup_nsa(self):
        L = self.depth
        self.cmpw_d = self.din("nsa_cmp_w", [L, 2, 32, 64, 64])
        self.peT_d = self.din("nsa_peT", [L, 64, 32])
        self.biasc_d = self.din("nsa_biasc", [128, 4, S])
        self.ntab_d = self.din("nsa_tab", [128, 4 * 512])
        self.e2_d = self.din("nsa_e2", [32, S])
        self.ovl_d = self.din("nsa_ovl", [128, 32])
        self.addt_d = self.din("nsa_addtab", [S, 32])

    def nsa_branch(self, l):
        P = self.P
        W = P.alloc("cmpw", 2 * 32 * 64)
        W3 = W.ap.rearrange("p (a e) -> p a e", a=64)
        P.dma(W3[0:64], self.cmpw_d.ap[l].rearrange("a l d e -> d (a l) e"), reads=[self.cmpw_d], writes=[W])
        peT = P.alloc("peT", 32)
        P.dma(peT.ap[0:64], self.peT_d.ap[l], reads=[self.peT_d], writes=[peT])
        kc = P.alloc("kcT", S)
        vc = P.alloc("vcT", S)
        P.dma(kc.ap[0:64], self.ZT.ap[self.ZKC:self.ZKC + 64, :], reads=[(self.ZT, 2)], writes=[kc])
        P.dma(vc.ap[0:64], self.ZT.ap[self.ZVC:self.ZVC + 64, :], reads=[(self.ZT, 2)], writes=[vc])
        kcmp = P.alloc("kcmpT", 128)
        vaug = P.alloc("vcmp_aug", 97)
        P.op("dve", lambda e: e.memset(kcmp.ap, 0.0), writes=[kcmp])
        P.op("dve", lambda e: e.memset(vaug.ap, 0.0), writes=[vaug])
        P.op("dve", lambda e: e.memset(vaug.ap[0:127, 64:65], 1.0), reads=[vaug], writes=[vaug])
        P.dma(vaug.ap[:, 65:97], self.ovl_d.ap, reads=[vaug, self.ovl_d], writes=[vaug])
        kc3 = kc.ap.rearrange("p (n s) -> p n s", s=16)
        vc3 = vc.ap.rearrange("p (n s) -> p n s", s=16)
        psk = P.ps()
        psb = P.ps()
        for li in range(32):
            rhs = kc3[0:64, li // 16:li // 16 + 127, li % 16]
            P.op("pe", lambda e, li=li, rhs=rhs: e.matmul(psk.ap[0:64, 0:127], W3[0:64, li, :], rhs, start=(li == 0), stop=(li == 31)),
                 reads=[W, kc], writes=[psk])
        for li in range(32):
            P.op("pe", lambda e, li=li: e.matmul(psb.ap[0:64, 0:1], W3[0:64, li, :], peT.ap[0:64, li:li + 1], start=(li == 0), stop=(li == 31)),
                 reads=[W, peT], writes=[psb])
        bk = P.alloc("bk", 1)
        P.op("act", lambda e: e.copy(bk.ap[0:64], psb.ap[0:64, 0:1]), reads=[psb], writes=[bk])
        P.op("dve", lambda e: e.tensor_scalar(out=kcmp.ap[0:64, 0:127], in0=psk.ap[0:64, 0:127], scalar1=bk.ap[0:64, 0:1], scalar2=None, op0=ALU.add),
             reads=[psk, bk, kcmp], writes=[kcmp])
        psv = P.ps()
        psbv = P.ps()
        for li in range(32):
            P.op("pe", lambda e, li=li: e.matmul(psbv.ap[0:1, 0:64], peT.ap[0:64, li:li + 1], W3[0:64, 32 + li, :], start=(li == 0), stop=(li == 31)),
                 reads=[W, peT], writes=[psbv])
        bv = P.alloc("bv", 64)
        P.op("act", lambda e: e.copy(bv.ap[0:1], psbv.ap[0:1, 0:64]), reads=[psbv], writes=[bv])
        for li in range(32):
            lhs = vc3[0:64, li // 16:li // 16 + 127, li % 16]
            P.op("pe", lambda e, li=li, lhs=lhs: e.matmul(psv.ap[0:127, 0:64], lhs, W3[0:64, 32 + li, :], start=(li == 0), stop=False),
                 reads=[W, vc], writes=[psv])
        P.op("pe", lambda e: e.matmul(psv.ap[0:127, 0:64], self.ones.ap[0:1, 0:127], bv.ap[0:1, 0:64], start=False, stop=True),
             reads=[bv, self.ones], writes=[psv])
        P.op("act", lambda e: e.copy(vaug.ap[0:127, 0:64], psv.ap[0:127, 0:64]), reads=[psv, vaug], writes=[vaug])
        P.release(W, peT, kc, vc, bk, bv)
        ks = P.alloc("ksT", S)
        kw = P.alloc("kwT", S)
        P.dma(ks.ap[0:64], self.ZT.ap[self.ZKS:self.ZKS + 64, :], reads=[(self.ZT, 3)], writes=[ks])
        P.dma(kw.ap[0:64], self.ZT.ap[self.ZKW:self.ZKW + 64, :], reads=[(self.ZT, 3)], writes=[kw])
        e2 = P.alloc("e2", S)
        P.dma(e2.ap[0:32], self.e2_d.ap, reads=[self.e2_d], writes=[e2])
        tab = P.alloc("ntab", 4 * 512)
        P.dma(tab.ap, self.ntab_d.ap, reads=[self.ntab_d], writes=[tab])
        vaugs = []
        for c0 in (self.NVS, self.NVW):
            va = P.alloc("vaug", 16 * 65)
            va3 = va.ap.rearrange("p (t d) -> p t d", t=16)
            P.op("dve", lambda e, va=va: e.memset(va.ap, 1.0), writes=[va])
            P.dma(va3[:, :, 0:64], self.ZN.ap[:, c0:c0 + 64].rearrange("(t p) d -> p t d", p=128), reads=[self.ZN, va], writes=[va])
            vaugs.append((va, va3))
        gl = P.alloc("ngl", 16 * 12)
        gl3 = gl.ap.rearrange("p (t g) -> p t g", t=16)
        P.dma(gl3, self.ZN.ap[:, self.NG:self.NG + 12].rearrange("(t p) g -> p t g", p=128), reads=[self.ZN], writes=[gl])
        P.op("act", lambda e: e.activation(gl.ap, gl.ap, AF.Sigmoid), reads=[gl], writes=[gl])
        for qb in range(S // 128):
            self.nsa_qblock(l, qb, kcmp, vaug, ks, kw, e2, tab, vaugs, gl3, gl)
        P.release(kcmp, vaug, ks, kw, e2, tab, vaugs[0][0], vaugs[1][0], gl)

    def nsa_scores(self, ps_s, tabsl, tab, pso, vaug_ap, vaug_tile, first, width):
        P = self.P
        tmp = P.alloc("ntmp", 512)
        src_tiles = [ps_s, tab]
        P.op("dve", lambda e: e.scalar_tensor_tensor(out=tmp.ap, in0=ps_s.ap, scalar=0.125, in1=tabsl, op0=ALU.mult, op1=ALU.add),
             reads=src_tiles, writes=[tmp])
        P.op("act", lambda e: e.activation(tmp.ap, tmp.ap, AF.Exp), reads=[tmp], writes=[tmp])
        for h in range(4):
            P.op("pe", lambda e, h=h: e.matmul(pso.ap[:, h * width:(h + 1) * width], tmp.ap[:, h * 128:(h + 1) * 128], vaug_ap,
                                              start=(first and h == 0), stop=True, skip_group_check=True),
                 reads=[tmp, vaug_tile], writes=[pso])
        P.release(tmp)

    def nsa_qblock(self, l, qb, kcmp, vaug, ks, kw, e2, tab, vaugs, gl3, gl):
        P = self.P
        q0 = qb * 128
        q4 = P.alloc("q4", 512)
        P.dma(q4.ap[0:64].rearrange("p (h t) -> p h t", h=4), self.ZT.ap[0:256, q0:q0 + 128].rearrange("(h d) t -> d h t", d=64),
              reads=[(self.ZT, 0), (self.ZT, 1)], writes=[q4])
        bc = P.alloc("bc", 512)
        P.dma(bc.ap.rearrange("p (h t) -> p h t", h=4), self.biasc_d.ap[:, :, q0:q0 + 128], reads=[self.biasc_d], writes=[bc])
        ps_c = P.ps()
        P.op("pe", lambda e: e.matmul(ps_c.ap, kcmp.ap[0:64, :], q4.ap[0:64, :], start=True, stop=True), reads=[kcmp, q4], writes=[ps_c])
        ps_oc = P.ps(hold=True)
        self.nsa_scores(ps_c, bc.ap, bc, ps_oc, vaug.ap, vaug, True, 97)
        P.release(bc)
        oc3 = ps_oc.ap[:, 0:388].rearrange("p (h w) -> p h w", h=4)
        rdc = P.alloc("rdc", 4)
        P.op("dve", lambda e: e.tensor_scalar(out=rdc.ap, in0=oc3[:, :, 64], scalar1=1e-30, scalar2=None, op0=ALU.max), reads=[ps_oc], writes=[rdc])
        P.op("dve", lambda e: e.reciprocal(rdc.ap, rdc.ap), reads=[rdc], writes=[rdc])
        imp = P.alloc("imp", 32)
        P.dma(imp.ap, self.addt_d.ap[q0:q0 + 128, :], reads=[self.addt_d], writes=[imp])
        for h in range(4):
            P.op("dve", lambda e, h=h: e.scalar_tensor_tensor(out=imp.ap, in0=oc3[:, h, 65:97], scalar=rdc.ap[:, h:h + 1], in1=imp.ap,
                                                              op0=ALU.mult, op1=ALU.add), reads=[ps_oc, rdc, imp], writes=[imp])
        top8 = P.alloc("top8", 8)
        P.op("dve", lambda e: e.max(out=top8.ap, in_=imp.ap), reads=[imp], writes=[top8])
        P.op("dve", lambda e: e.tensor_scalar(out=imp.ap, in0=imp.ap, scalar1=top8.ap[:, 7:8], scalar2=1.0, op0=ALU.is_ge, op1=ALU.subtract),
             reads=[imp, top8], writes=[imp])
        ps_t = P.ps()
        P.op("pe", lambda e: e.transpose(ps_t.ap[0:32, 0:128], imp.ap, self.ident.ap), reads=[imp, self.ident], writes=[ps_t])
        ns4 = P.alloc("ns4", 512)
        P.op("dve", lambda e: e.tensor_copy(ns4.ap[0:32].rearrange("p (h t) -> p h t", h=4),
                                            ps_t.ap[0:32, 0:128].rearrange("p (o t) -> p o t", o=1).broadcast_to([32, 4, 128])),
             reads=[ps_t], writes=[ns4])
        P.release(imp, top8)
        ps_os = P.ps(hold=True)
        for kb in range(qb + 1):
            dlt = qb - kb
            ti = min(dlt, 2)
            ps_s = P.ps()
            P.op("pe", lambda e, kb=kb, ps_s=ps_s: e.matmul(ps_s.ap, ks.ap[0:64, kb * 128:(kb + 1) * 128], q4.ap[0:64, :], start=True, stop=False),
                 reads=[ks, q4], writes=[ps_s])
            P.op("pe", lambda e, kb=kb, ps_s=ps_s: e.matmul(ps_s.ap, e2.ap[0:32, kb * 128:(kb + 1) * 128], ns4.ap[0:32, :], start=False, stop=True),
                 reads=[e2, ns4], writes=[ps_s])
            self.nsa_scores(ps_s, tab.ap[:, ti * 512:(ti + 1) * 512], tab, ps_os, vaugs[0][1][:, kb, :], vaugs[0][0], kb == 0, 65)
        ps_ow = P.ps(hold=True)
        kb0 = max(0, qb - 4)
        for kb in range(kb0, qb + 1):
            dlt = qb - kb
            ti = (0, 1, 2, 2, 3)[dlt]
            ps_s = P.ps()
            P.op("pe", lambda e, kb=kb, ps_s=ps_s: e.matmul(ps_s.ap, kw.ap[0:64, kb * 128:(kb + 1) * 128], q4.ap[0:64, :], start=True, stop=True),
                 reads=[kw, q4], writes=[ps_s])
            self.nsa_scores(ps_s, tab.ap[:, ti * 512:(ti + 1) * 512], tab, ps_ow, vaugs[1][1][:, kb, :], vaugs[1][0], kb == kb0, 65)
        P.release(q4, ns4)
        acc = P.alloc("nacc", 256)
        acc3 = acc.ap.rearrange("p (h d) -> p h d", h=4)
        g3 = gl3[:, qb, :].rearrange("p (h b) -> p h b", b=3)
        for b, (pso, w) in enumerate(((ps_oc, 97), (ps_os, 65), (ps_ow, 65))):
            o3 = pso.ap[:, 0:4 * w].rearrange("p (h w) -> p h w", h=4)
            scl = P.alloc("nscl", 4)
            P.op("dve", lambda e, o3=o3, scl=scl: e.tensor_scalar(out=scl.ap, in0=o3[:, :, 64], scalar1=1e-30, scalar2=None, op0=ALU.max),
                 reads=[pso], writes=[scl])
            P.op("dve", lambda e, scl=scl: e.reciprocal(scl.ap, scl.ap), reads=[scl], writes=[scl])
            P.op("dve", lambda e, scl=scl, b=b: e.tensor_tensor(out=scl.ap, in0=scl.ap, in1=g3[:, :, b], op=ALU.mult), reads=[scl, gl], writes=[scl])
            sb = scl.ap.rearrange("p (h o) -> p h o", o=1).broadcast_to([128, 4, 64])
            if b == 0:
                P.op("dve", lambda e, o3=o3, sb=sb: e.tensor_tensor(out=acc3, in0=o3[:, :, 0:64], in1=sb, op=ALU.mult), reads=[pso, scl], writes=[acc])
            else:
                t2 = P.alloc("nt2", 256)
                t23 = t2.ap.rearrange("p (h d) -> p h d", h=4)
                P.op("dve", lambda e, o3=o3, sb=sb, t23=t23: e.tensor_tensor(out=t23, in0=o3[:, :, 0:64], in1=sb, op=ALU.mult), reads=[pso, scl], writes=[t2])
                P.op("dve", lambda e, t2=t2: e.tensor_tensor(out=acc.ap, in0=acc.ap, in1=t2.ap, op=ALU.add), reads=[acc, t2], writes=[acc])
                P.release(t2)
            P.release(scl)
        P.ps_free(ps_oc, ps_os, ps_ow)
        P.release(rdc)
        ps_o = P.ps()
        for c in range(2):
            P.op("pe", lambda e, c=c: e.transpose(ps_o.ap[:, c * 128:(c + 1) * 128], acc.ap[:, c * 128:(c + 1) * 128], self.ident.ap),
                 reads=[acc, self.ident], writes=[ps_o])
        stg = P.alloc("nstg", 256)
        P.op("act", lambda e: e.copy(stg.ap, ps_o.ap[:, 0:256]), reads=[ps_o], writes=[stg])
        P.dma(self.OBR.ap[0:256, q0:q0 + 128].rearrange("(c p) t -> p c t", p=128), stg.ap.rearrange("p (c t) -> p c t", c=2),
              reads=[stg], writes=[(self.OBR, 0), (self.OBR, 1)], q="gq")
        P.release(acc, stg)


    RC = 64

    def setup_rwkv(self):
        L = self.depth
        self.rwpar_d = self.din("rw_par", [L, 64, 43])
        self.rww2_d = self.din("rwkv_w2", [L, 32, 256])
        self.rwa2_d = self.din("rwkv_a2", [L, 32, 256])
        self.rwg2_d = self.din("rwkv_g2", [L, 64, 256])
        self.rwmask_d = self.din("rw_mask5", [64, 320])

    def rw_shift(self, z, n, mu_ap, par):
        P = self.P
        d = P.alloc("rwd", S)
        P.op("dve", lambda e: e.tensor_tensor(out=d.ap[0:n, 1:S], in0=z.ap[0:n, 0:S - 1], in1=z.ap[0:n, 1:S], op=ALU.subtract), reads=[z], writes=[d])
        P.op("dve", lambda e: e.tensor_scalar(out=d.ap[0:n, 0:1], in0=z.ap[0:n, 0:1], scalar1=-1.0, scalar2=None, op0=ALU.mult), reads=[z, d], writes=[d])
        P.op("dve", lambda e: e.scalar_tensor_tensor(out=z.ap[0:n], in0=d.ap[0:n], scalar=mu_ap, in1=z.ap[0:n], op0=ALU.mult, op1=ALU.add),
             reads=[d, z, par], writes=[z])
        P.release(d)

    def rwkv_branch(self, l):
        P = self.P
        par = P.alloc("rwpar", 43)
        P.dma(par.ap[0:64], self.rwpar_d.ap[l], reads=[self.rwpar_d], writes=[par])
        omk = P.alloc("rwomk", 4)
        P.op("dve", lambda e: e.tensor_scalar(out=omk.ap[0:64], in0=par.ap[0:64, 15 + 3 * 4:15 + 4 * 4], scalar1=-1.0, scalar2=1.0, op0=ALU.mult, op1=ALU.add),
             reads=[par], writes=[omk])
        lw = P.alloc("rwlw", 3 * 256)
        P.dma(lw.ap[0:32, 0:256], self.rww2_d.ap[l], reads=[self.rww2_d], writes=[(lw, 0)])
        P.dma(lw.ap[0:32, 256:512], self.rwa2_d.ap[l], reads=[self.rwa2_d], writes=[(lw, 1)])
        P.dma(lw.ap[0:64, 512:768], self.rwg2_d.ap[l], reads=[self.rwg2_d], writes=[(lw, 2)])
        m5 = P.alloc("rwm5", 320)
        P.dma(m5.ap[0:64], self.rwmask_d.ap, reads=[self.rwmask_d], writes=[m5])
        smask = P.alloc("rwsm", S)
        P.op("dve", lambda e: e.memset(smask.ap, 1.0), writes=[smask])
        P.op("dve", lambda e: e.memset(smask.ap.rearrange("p (n c) -> p n c", c=self.RC)[:, :, 0:1], 0.0), reads=[smask], writes=[smask])
        base = self.ZRW + 768
        twl = P.alloc("rwtwl", S)
        tal = P.alloc("rwtal", S)
        tgl = P.alloc("rwtgl", S)
        for t, r0, n, mc, fn in ((twl, base, 32, 12, AF.Tanh), (tal, base + 32, 32, 13, None), (tgl, base + 64, 64, 14, AF.Sigmoid)):
            self.rw_lora_in(t, r0, n, mc, fn, par)
        import os
        for h in range(4 if int(os.environ.get("RWDBG", "9")) >= 9 else 1):
            self.rwkv_head(l, h, par, omk, lw, m5, smask, twl, tal, tgl)
        P.release(par, omk, lw, m5, smask, twl, tal, tgl)

    def rw_lora_in(self, t, r0, n, mc, fn, par):
        P = self.P
        P.dma(t.ap[0:n], self.ZT.ap[r0:r0 + n, :], reads=[(self.ZT, r0 // 128)], writes=[t])
        self.rw_shift(t, n, par.ap[0:n, mc:mc + 1], par)
        if fn is not None:
            P.op("act", lambda e: e.activation(t.ap[0:n], t.ap[0:n], fn), reads=[t], writes=[t])

    def rwkv_head(self, l, h, par, omk, lw, m5, smask, twl, tal, tgl):
        P = self.P
        C = self.RC
        NCK = S // C
        pc = lambda which: par.ap[0:64, 15 + which * 4 + h:15 + which * 4 + h + 1]
        hc = slice(h * 64, (h + 1) * 64)
        r = P.alloc("rw_r", S)
        k = P.alloc("rw_k", S)
        v = P.alloc("rw_v", S)
        for j, t in enumerate((r, k, v)):
            r0 = self.ZRW + j * 256 + h * 64
            P.dma(t.ap[0:64], self.ZT.ap[r0:r0 + 64, :], reads=[(self.ZT, r0 // 128)], writes=[t])
            self.rw_shift(t, 64, par.ap[0:64, j * 4 + h:j * 4 + h + 1], par)
        a = P.alloc("rw_a", S)
        logw = P.alloc("rw_lw", S)
        kkn = P.alloc("rw_kkn", S)
        P.op("dve", lambda e: e.tensor_scalar(out=kkn.ap[0:64], in0=k.ap[0:64], scalar1=pc(2), scalar2=None, op0=ALU.mult), reads=[k, par], writes=[kkn])
        for tb in range(4):
            self.rw_prep_blk(h, tb, par, pc, lw, twl, tal, a, logw, kkn)
        kt = P.alloc("rw_kt", S)
        P.op("dve", lambda e: e.tensor_scalar(out=kt.ap[0:64], in0=a.ap[0:64], scalar1=pc(3), scalar2=omk.ap[0:64, h:h + 1], op0=ALU.mult, op1=ALU.add),
             reads=[a, par, omk], writes=[kt])
        P.op("dve", lambda e: e.tensor_tensor(out=kt.ap[0:64], in0=kt.ap[0:64], in1=k.ap[0:64], op=ALU.mult), reads=[kt, k], writes=[kt])
        P.release(k)
        P.op("dve", lambda e: e.tensor_tensor(out=a.ap[0:64], in0=a.ap[0:64], in1=kkn.ap[0:64], op=ALU.mult), reads=[a, kkn], writes=[a])
        bb = a
        import os
        dbg = int(os.environ.get("RWDBG", "9"))
        bonv = P.alloc("rw_bon", S)
        P.op("dve", lambda e: e.scalar_tensor_tensor(out=bonv.ap[0:64], in0=r.ap[0:64], scalar=pc(4), in1=kt.ap[0:64], op0=ALU.mult, op1=ALU.mult),
             reads=[r, kt, par], writes=[bonv])
        for tb in range(4):
            ps = P.ps()
            P.op("pe", lambda e, ps=ps, tb=tb: e.matmul(ps.ap[0:64, :], self.ones.ap[0:64, 0:64], bonv.ap[0:64, tb * 512:(tb + 1) * 512], start=True, stop=True),
                 reads=[bonv, self.ones], writes=[ps])
            P.op("dve", lambda e, ps=ps, tb=tb: e.tensor_tensor(out=bonv.ap[0:64, tb * 512:(tb + 1) * 512], in0=ps.ap[0:64, :], in1=v.ap[0:64, tb * 512:(tb + 1) * 512], op=ALU.mult),
                 reads=[ps, v, bonv], writes=[bonv])
        if dbg <= 1:
            return
        cum = P.alloc("rw_cum", S)
        P.op("dve", lambda e: e.tensor_tensor_scan(out=cum.ap[0:64], data0=smask.ap[0:64], data1=logw.ap[0:64], initial=0.0, op0=ALU.mult, op1=ALU.add),
             reads=[smask, logw], writes=[cum])
        eg = P.alloc("rw_eg", S)
        P.op("act", lambda e: e.activation(eg.ap[0:64], cum.ap[0:64], AF.Exp), reads=[cum], writes=[eg])
        gC = P.alloc("rw_gC", NCK)
        P.op("dve", lambda e: e.tensor_copy(gC.ap[0:64], eg.ap[0:64].rearrange("p (n c) -> p n c", c=C)[:, :, C - 1]), reads=[eg], writes=[gC])
        P.op("dve", lambda e: e.tensor_tensor(out=r.ap[0:64], in0=r.ap[0:64], in1=eg.ap[0:64], op=ALU.mult), reads=[r, eg], writes=[r])
        P.release(eg)
        RH = r
        P.op("dve", lambda e: e.tensor_tensor(out=logw.ap[0:64], in0=cum.ap[0:64], in1=logw.ap[0:64], op=ALU.subtract), reads=[cum, logw], writes=[logw])
        P.op("act", lambda e: e.activation(logw.ap[0:64], logw.ap[0:64], AF.Exp), reads=[logw], writes=[logw])
        P.op("dve", lambda e: e.tensor_tensor(out=kkn.ap[0:64], in0=kkn.ap[0:64], in1=logw.ap[0:64], op=ALU.mult), reads=[kkn, logw], writes=[kkn])
        P.release(logw)
        KH = kkn
        P.op("act", lambda e: e.activation(cum.ap[0:64], cum.ap[0:64], AF.Exp, scale=-1.0), reads=[cum], writes=[cum])
        P.op("dve", lambda e: e.tensor_tensor(out=kt.ap[0:64], in0=kt.ap[0:64], in1=cum.ap[0:64], op=ALU.mult), reads=[kt, cum], writes=[kt])
        P.op("dve", lambda e: e.tensor_tensor(out=bb.ap[0:64], in0=bb.ap[0:64], in1=cum.ap[0:64], op=ALU.mult), reads=[bb, cum], writes=[bb])
        P.release(cum)
        KG, BG = kt, bb
        if dbg <= 2:
            return
        tms = []
        for src in (v, KG, BG):
            tm = P.alloc("rw_tm", NCK * 64)
            tm3 = tm.ap.rearrange("p (n c) -> p n c", c=64)
            for g8 in range(NCK // 8):
                ps = P.ps()
                for j in range(8):
                    n = g8 * 8 + j
                    P.op("pe", lambda e, ps=ps, j=j, n=n, src=src: e.transpose(ps.ap[0:64, j * 64:(j + 1) * 64], src.ap[0:64, n * C:(n + 1) * C], self.ident.ap[0:64, 0:64]),
                         reads=[src, self.ident], writes=[ps])
                P.op("act", lambda e, ps=ps, g8=g8, tm=tm: e.copy(tm.ap[0:64, g8 * 512:(g8 + 1) * 512], ps.ap[0:64, :]), reads=[ps], writes=[(tm, g8)])
            tms.append((tm, tm3))
        P.release(v)
        (Vt, Vt3), (KGt, KGt3), (BGt, BGt3) = tms
        if dbg <= 3:
            return
        yT = P.alloc("rw_y", S)
        ST = P.alloc("rw_ST", 64)
        P.op("dve", lambda e: e.memset(ST.ap[0:64], 0.0), writes=[ST])
        G = 4
        for g0 in range(0, NCK, G):
            As, TTs = self.rw_group_prep(g0, G, KH, RH, KG, BG, m5)
            for gi in range(G):
                if dbg > 4:
                    self.rw_chunk(g0 + gi, As[gi], TTs[gi], KH, RH, Vt3, Vt, KGt3, KGt, BGt3, BGt, ST, gC, yT)
            P.release(*As)
            P.release(*TTs)
        P.release(RH, KH, KG, BG, Vt, KGt, BGt, ST, gC)
        self.rw_epilogue(l, h, yT, bonv, pc, par, lw, tgl)
        P.release(yT, bonv)

    def rw_prep_blk(self, h, tb, par, pc, lw, twl, tal, a, logw, kkn):
        P = self.P
        ts = slice(tb * 512, (tb + 1) * 512)
        ps = P.ps()
        P.op("pe", lambda e: e.matmul(ps.ap[0:64, :], lw.ap[0:32, h * 64:(h + 1) * 64], twl.ap[0:32, ts], start=True, stop=True), reads=[lw, twl], writes=[ps])
        P.op("act", lambda e: e.activation(logw.ap[0:64, ts], ps.ap[0:64, :], AF.Sigmoid, bias=pc(0), scale=1.0), reads=[ps, par], writes=[logw])
        P.op("dve", lambda e: e.tensor_scalar(out=logw.ap[0:64, ts], in0=logw.ap[0:64, ts], scalar1=-0.6065306597126334, scalar2=None, op0=ALU.mult),
             reads=[logw], writes=[logw])
        ps2 = P.ps()
        P.op("pe", lambda e: e.matmul(ps2.ap[0:64, :], lw.ap[0:32, 256 + h * 64:256 + (h + 1) * 64], tal.ap[0:32, ts], start=True, stop=True), reads=[lw, tal], writes=[ps2])
        P.op("act", lambda e: e.activation(a.ap[0:64, ts], ps2.ap[0:64, :], AF.Sigmoid, bias=pc(1), scale=1.0), reads=[ps2, par], writes=[a])
        sq = P.alloc("rw_sq", 512)
        P.op("act", lambda e: e.activation(sq.ap[0:64], kkn.ap[0:64, ts], AF.Square), reads=[kkn], writes=[sq])
        ps3 = P.ps()
        P.op("pe", lambda e: e.matmul(ps3.ap[0:64, :], self.ones.ap[0:64, 0:64], sq.ap[0:64], start=True, stop=True), reads=[sq, self.ones], writes=[ps3])
        P.op("act", lambda e: e.activation(sq.ap[0:64], ps3.ap[0:64, :], AF.Sqrt), reads=[ps3], writes=[sq])
        P.op("dve", lambda e: e.tensor_scalar(out=sq.ap[0:64], in0=sq.ap[0:64], scalar1=1e-12, scalar2=None, op0=ALU.max), reads=[sq], writes=[sq])
        P.op("dve", lambda e: e.reciprocal(sq.ap[0:64], sq.ap[0:64]), reads=[sq], writes=[sq])
        P.op("dve", lambda e: e.tensor_tensor(out=kkn.ap[0:64, ts], in0=kkn.ap[0:64, ts], in1=sq.ap[0:64], op=ALU.mult), reads=[kkn, sq], writes=[kkn])
        P.release(sq)

    def rw_group_prep(self, g0, G, KH, RH, KG, BG, m5):
        P = self.P
        C = self.RC
        As, Ms, Ps = [], [], []
        for gi in range(G):
            cs = slice((g0 + gi) * C, (g0 + gi + 1) * C)
            ps = P.ps()
            for j, (lh, rh) in enumerate(((KG, KH), (KG, RH), (BG, KH), (BG, RH), (KH, BG))):
                P.op("pe", lambda e, ps=ps, j=j, lh=lh, rh=rh, cs=cs: e.matmul(ps.ap[0:64, j * 64:(j + 1) * 64], lh.ap[0:64, cs], rh.ap[0:64, cs], start=True, stop=True),
                     reads=[lh, rh], writes=[ps])
            A = P.alloc("rw_A", 320)
            P.op("dve", lambda e, ps=ps, A=A: e.tensor_tensor(out=A.ap[0:64], in0=ps.ap[0:64, 0:320], in1=m5.ap[0:64], op=ALU.mult), reads=[ps, m5], writes=[A])
            As.append(A)
            Pm = P.alloc("rw_P", 64)
            P.op("dve", lambda e, A=A, Pm=Pm: e.tensor_tensor(out=Pm.ap[0:64], in0=self.ident.ap[0:64, 0:64], in1=A.ap[0:64, 128:192], op=ALU.subtract),
                 reads=[A, self.ident], writes=[Pm])
            Ps.append(Pm)
            Ms.append((A.ap[0:64, 128:192], A.ap[0:64, 256:320], A))
        import os
        nsteps = int(os.environ.get("RWSTEPS", "6"))
        for step in range(6):
            if step >= nsteps:
                for gi in range(G):
                    if Ms[gi][2] is not As[gi]:
                        P.release(Ms[gi][2])
                break
            pss = []
            for gi in range(G):
                M, MT, Mt = Ms[gi]
                ps = P.ps()
                if step < 5:
                    P.op("pe", lambda e, ps=ps, M=M, MT=MT: e.matmul(ps.ap[0:64, 0:64], MT, M, start=True, stop=True), reads=[Mt], writes=[ps])
                    P.op("pe", lambda e, ps=ps, M=M, MT=MT: e.matmul(ps.ap[0:64, 64:128], M, MT, start=True, stop=True), reads=[Mt], writes=[ps])
                if step > 0:
                    P.op("pe", lambda e, ps=ps, MT=MT, Pm=Ps[gi]: e.matmul(ps.ap[0:64, 128:192], MT, Pm.ap[0:64], start=True, stop=True), reads=[Mt, Ps[gi]], writes=[ps])
                pss.append(ps)
            for gi in range(G):
                ps = pss[gi]
                if step > 0:
                    P.op("dve", lambda e, ps=ps, Pm=Ps[gi]: e.tensor_tensor(out=Pm.ap[0:64], in0=Pm.ap[0:64], in1=ps.ap[0:64, 128:192], op=ALU.add),
                         reads=[ps, Ps[gi]], writes=[Ps[gi]])
                if step < 5:
                    Mn = P.alloc("rw_M", 128)
                    P.op("act", lambda e, ps=ps, Mn=Mn: e.copy(Mn.ap[0:64], ps.ap[0:64, 0:128]), reads=[ps], writes=[Mn])
                    old = Ms[gi][2]
                    Ms[gi] = (Mn.ap[0:64, 0:64], Mn.ap[0:64, 64:128], Mn)
                    if old is not As[gi]:
                        P.release(old)
                elif Ms[gi][2] is not As[gi]:
                    P.release(Ms[gi][2])
        return As, Ps

    def rw_chunk(self, n, A, TT, KH, RH, Vt3, Vt, KGt3, KGt, BGt3, BGt, ST, gC, yT):
        P = self.P
        C = self.RC
        cs = slice(n * C, (n + 1) * C)
        psx = P.ps()
        P.op("pe", lambda e: e.matmul(psx.ap[0:64, 0:64], KH.ap[0:64, cs], ST.ap[0:64], start=True, stop=False), reads=[KH, ST], writes=[psx])
        P.op("pe", lambda e: e.matmul(psx.ap[0:64, 0:64], A.ap[0:64, 0:64], Vt3[0:64, n, :], start=False, stop=True), reads=[A, Vt], writes=[psx])
        nx = P.alloc("rw_nx", 64)
        P.op("act", lambda e: e.mul(nx.ap[0:64], psx.ap[0:64, 0:64], -1.0), reads=[psx], writes=[nx])
        psu = P.ps()
        P.op("pe", lambda e: e.matmul(psu.ap[0:64, 0:64], TT.ap[0:64], nx.ap[0:64], start=True, stop=True), reads=[TT, nx], writes=[psu])
        U = P.alloc("rw_U", 64)
        P.op("act", lambda e: e.copy(U.ap[0:64], psu.ap[0:64, 0:64]), reads=[psu], writes=[U])
        psy = P.ps()
        P.op("pe", lambda e: e.matmul(psy.ap[0:64, 0:64], ST.ap[0:64], RH.ap[0:64, cs], start=True, stop=False), reads=[ST, RH], writes=[psy])
        P.op("pe", lambda e: e.matmul(psy.ap[0:64, 0:64], Vt3[0:64, n, :], A.ap[0:64, 64:128], start=False, stop=False), reads=[Vt, A], writes=[psy])
        P.op("pe", lambda e: e.matmul(psy.ap[0:64, 0:64], U.ap[0:64], A.ap[0:64, 192:256], start=False, stop=True), reads=[U, A], writes=[psy])
        P.op("act", lambda e: e.copy(yT.ap[0:64, cs], psy.ap[0:64, 0:64]), reads=[psy], writes=[(yT, n)])
        pss = P.ps()
        P.op("pe", lambda e: e.matmul(pss.ap[0:64, 0:64], KGt3[0:64, n, :], Vt3[0:64, n, :], start=True, stop=False), reads=[KGt, Vt], writes=[pss])
        P.op("pe", lambda e: e.matmul(pss.ap[0:64, 0:64], BGt3[0:64, n, :], U.ap[0:64], start=False, stop=True), reads=[BGt, U], writes=[pss])
        P.op("dve", lambda e: e.tensor_tensor(out=ST.ap[0:64], in0=ST.ap[0:64], in1=pss.ap[0:64, 0:64], op=ALU.add), reads=[ST, pss], writes=[ST])
        P.op("dve", lambda e: e.tensor_scalar(out=ST.ap[0:64], in0=ST.ap[0:64], scalar1=gC.ap[0:64, n:n + 1], scalar2=None, op0=ALU.mult),
             reads=[ST, gC], writes=[ST])
        P.release(nx, U)

    def rw_epilogue(self, l, h, yT, bonv, pc, par, lw, tgl):
        P = self.P
        n = 512
        for Q in range(S // n):
            self.rw_epi_blk(h, Q, n, yT, bonv, pc, par, lw, tgl)

    def rw_epi_blk(self, h, Q, n, yT, bonv, pc, par, lw, tgl):
        P = self.P
        ts = slice(Q * n, (Q + 1) * n)
        o = P.alloc("ro", n)
        sq = P.alloc("rsq", n)
        P.op("act", lambda e: e.activation(sq.ap[0:64], yT.ap[0:64, ts], AF.Square), reads=[yT], writes=[sq])
        p1 = P.ps()
        p2 = P.ps()
        P.op("pe", lambda e: e.matmul(p1.ap[0:64, :], self.ones.ap[0:64, 0:64], yT.ap[0:64, ts], start=True, stop=True), reads=[yT, self.ones], writes=[p1])
        P.op("pe", lambda e: e.matmul(p2.ap[0:64, :], self.ones.ap[0:64, 0:64], sq.ap[0:64], start=True, stop=True), reads=[sq, self.ones], writes=[p2])
        mean = P.alloc("rmean", n)
        P.op("dve", lambda e: e.tensor_scalar(out=mean.ap[0:64], in0=p1.ap[0:64, :], scalar1=1.0 / 64, scalar2=None, op0=ALU.mult), reads=[p1], writes=[mean])
        P.op("dve", lambda e: e.tensor_tensor(out=o.ap[0:64], in0=yT.ap[0:64, ts], in1=mean.ap[0:64], op=ALU.subtract), reads=[yT, mean], writes=[o])
        P.op("dve", lambda e: e.tensor_tensor(out=mean.ap[0:64], in0=mean.ap[0:64], in1=mean.ap[0:64], op=ALU.mult), reads=[mean], writes=[mean])
        P.op("dve", lambda e: e.scalar_tensor_tensor(out=sq.ap[0:64], in0=p2.ap[0:64, :], scalar=1.0 / 64, in1=mean.ap[0:64],
                                                     op0=ALU.mult, op1=ALU.subtract), reads=[p2, mean], writes=[sq])
        P.op("act", lambda e: e.activation(sq.ap[0:64], sq.ap[0:64], AF.Sqrt, bias=self.epst.ap[0:64, 2:3], scale=1.0), reads=[sq, self.epst], writes=[sq])
        P.op("dve", lambda e: e.reciprocal(sq.ap[0:64], sq.ap[0:64]), reads=[sq], writes=[sq])
        P.op("dve", lambda e: e.tensor_tensor(out=o.ap[0:64], in0=o.ap[0:64], in1=sq.ap[0:64], op=ALU.mult), reads=[o, sq], writes=[o])
        P.op("dve", lambda e: e.tensor_scalar(out=o.ap[0:64], in0=o.ap[0:64], scalar1=pc(5), scalar2=pc(6), op0=ALU.mult, op1=ALU.add), reads=[o, par], writes=[o])
        P.op("dve", lambda e: e.tensor_tensor(out=o.ap[0:64], in0=o.ap[0:64], in1=bonv.ap[0:64, ts], op=ALU.add), reads=[o, bonv], writes=[o])
        pg = P.ps()
        P.op("pe", lambda e: e.matmul(pg.ap[0:64, :], lw.ap[0:64, 512 + h * 64:512 + (h + 1) * 64], tgl.ap[0:64, ts], start=True, stop=True), reads=[lw, tgl], writes=[pg])
        P.op("dve", lambda e: e.tensor_tensor(out=o.ap[0:64], in0=o.ap[0:64], in1=pg.ap[0:64, :], op=ALU.mult), reads=[o, pg], writes=[o])
        P.dma(self.OBR.ap[512 + h * 64:512 + (h + 1) * 64, ts], o.ap[0:64], reads=[o], writes=[(self.OBR, 4 + h // 2)], q="gq")
        P.release(o, sq, mean)

    def setup_xa(self):
        P = self.P
        L = self.depth
        self.mem_d = self.din("mem", [MEM, D])
        self.wq_d = self.din("xa_wq", [L, D, D])
        self.wkv_d = self.din("xa_wkv", [L, D, 2 * D])
        self.wo_d = self.din("xa_wo", [L, D, D])
        self.memT = P.alloc("memT", NCH * MEM)
        memT3 = self.memT.ap.rearrange("p (c t) -> p c t", c=NCH)
        for tt in range(MEM // 128):
            self.xa_load_mem(tt, memT3)

    def xa_load_mem(self, tt, memT3):
        P = self.P
        xin = P.alloc("xin", D)
        P.dma(xin.ap, self.mem_d.ap[tt * 128:(tt + 1) * 128, :], reads=[self.mem_d], writes=[xin])
        for half in range(2):
            ps = P.ps()
            for j in range(4):
                c = half * 4 + j
                P.op("pe", lambda e, ps=ps, j=j, c=c: e.transpose(ps.ap[:, j * 128:(j + 1) * 128], xin.ap[:, c * 128:(c + 1) * 128], self.ident.ap),
                     reads=[xin, self.ident], writes=[ps])
            dst = memT3[:, half * 4:half * 4 + 4, tt * 128:(tt + 1) * 128]
            P.op("act", lambda e, ps=ps, dst=dst: e.copy(dst, ps.ap.rearrange("p (c t) -> p c t", c=4)), reads=[ps], writes=[self.memT])
        P.release(xin)

    def xattn(self, l):
        P = self.P
        mnT = P.alloc("mnT", NCH * MEM)
        mn3 = mnT.ap.rearrange("p (c t) -> p c t", c=NCH)
        self.norm_to(l, 6, self.memT.ap.rearrange("p (c t) -> p c t", c=NCH), self.memT, MEM, mn3, mnT)
        kT = P.alloc("xkT", NCH * MEM)
        kT3 = kT.ap.rearrange("p (c t) -> p c t", c=NCH)
        vN = P.alloc("xv", 2 * D)
        vN3 = vN.ap.rearrange("p (m n) -> p m n", m=2)
        for f in range(NCH):
            self.xa_k(l, f, mn3, mnT, kT3, kT)
        for half in range(2):
            self.xa_v(l, half, mn3, mnT, vN3, vN)
        P.release(mnT)
        TB = 256
        for tb in range(S // TB):
            self.xa_blk(l, tb, TB, kT3, kT, vN3, vN)
        P.release(kT, vN)

    def xa_k(self, l, f, mn3, mnT, kT3, kT):
        P = self.P
        w = P.alloc("xw", NCH * 128)
        w3 = w.ap.rearrange("p (k n) -> p k n", k=NCH)
        P.dma(w3, self.wkv_d.ap[l][:, f * 128:(f + 1) * 128].rearrange("(k p) n -> p k n", p=128), reads=[self.wkv_d], writes=[w])
        ps = P.ps()
        for k in range(NCH):
            P.op("pe", lambda e, k=k: e.matmul(ps.ap[:, 0:MEM], w3[:, k, :], mn3[:, k, :], start=(k == 0), stop=(k == NCH - 1)),
                 reads=[w, mnT], writes=[ps])
        P.op("act", lambda e: e.copy(kT3[:, f, :], ps.ap[:, 0:MEM]), reads=[ps], writes=[(kT, f)])
        P.release(w)

    def xa_v(self, l, half, mn3, mnT, vN3, vN):
        P = self.P
        w = P.alloc("xwv", NCH * 512)
        w3 = w.ap.rearrange("p (k n) -> p k n", k=NCH)
        c0 = D + half * 512
        P.dma(w3, self.wkv_d.ap[l][:, c0:c0 + 512].rearrange("(k p) n -> p k n", p=128), reads=[self.wkv_d], writes=[w])
        for mc in range(2):
            ps = P.ps()
            for k in range(NCH):
                P.op("pe", lambda e, k=k, ps=ps, mc=mc: e.matmul(ps.ap, mn3[:, k, mc * 128:(mc + 1) * 128], w3[:, k, :], start=(k == 0), stop=(k == NCH - 1)),
                     reads=[w, mnT], writes=[ps])
            P.op("act", lambda e, ps=ps, mc=mc: e.copy(vN3[:, mc, half * 512:(half + 1) * 512], ps.ap), reads=[ps], writes=[(vN, (mc, half))])
        P.release(w)

    def lin8(self, w_ap, src3, src_tile, dst3, dst_tile, TB):
        P = self.P
        for f in range(NCH):
            self.lin8_f(w_ap, f, src3, src_tile, dst3, dst_tile, TB)

    def lin8_f(self, w_ap, f, src3, src_tile, dst3, dst_tile, TB):
        P = self.P
        w = P.alloc("xw", NCH * 128)
        w3 = w.ap.rearrange("p (k n) -> p k n", k=NCH)
        P.dma(w3, w_ap[:, f * 128:(f + 1) * 128].rearrange("(k p) n -> p k n", p=128), reads=[], writes=[w])
        ps = P.ps()
        for k in range(NCH):
            P.op("pe", lambda e, k=k: e.matmul(ps.ap[:, 0:TB], w3[:, k, :], src3[:, k, :], start=(k == 0), stop=(k == NCH - 1)),
                 reads=[w, src_tile], writes=[ps])
        P.op("act", lambda e: e.copy(dst3[:, f, :], ps.ap[:, 0:TB]), reads=[ps], writes=[(dst_tile, f)])
        P.release(w)

    def xa_blk(self, l, tb, TB, kT3, kT, vN3, vN):
        P = self.P
        t0 = tb * TB
        hblk = P.alloc("hblk", NCH * TB)
        self.pre_norm_block(l, 2, t0, TB, hblk)
        h3 = hblk.ap.rearrange("p (c t) -> p c t", c=NCH)
        qT = P.alloc("xq", NCH * TB)
        q3 = qT.ap.rearrange("p (c t) -> p c t", c=NCH)
        self.lin8(self.wq_d.ap[l], h3, hblk, q3, qT, TB)
        P.release(hblk)
        at = P.alloc("xat", NCH * TB)
        at3 = at.ap.rearrange("p (c t) -> p c t", c=NCH)
        for h in range(4):
            self.xa_head(h, TB, kT3, kT, vN3, vN, q3, qT, at3, at)
        P.release(qT)
        yblk = P.alloc("yblk", NCH * TB)
        y3 = yblk.ap.rearrange("p (c t) -> p c t", c=NCH)
        self.lin8(self.wo_d.ap[l], at3, at, y3, yblk, TB)
        P.release(at)
        self.post_norm_residual(l, 3, t0, TB, yblk)
        P.release(yblk)

    def xa_head(self, h, TB, kT3, kT, vN3, vN, q3, qT, at3, at):
        P = self.P
        pT = P.alloc("xp", 2 * TB)
        p3 = pT.ap.rearrange("p (m t) -> p m t", m=2)
        for mc in range(2):
            ps = P.ps()
            for c in range(2):
                P.op("pe", lambda e, ps=ps, c=c, mc=mc: e.matmul(ps.ap[:, 0:TB], kT3[:, 2 * h + c, mc * 128:(mc + 1) * 128], q3[:, 2 * h + c, :],
                                                             start=(c == 0), stop=(c == 1)), reads=[kT, qT], writes=[ps])
            P.op("act", lambda e, ps=ps, mc=mc: e.activation(p3[:, mc, :], ps.ap[:, 0:TB], AF.Exp, scale=1.0 / 16.0), reads=[ps], writes=[(pT, mc)])
        psd = P.ps()
        for mc in range(2):
            P.op("pe", lambda e, mc=mc: e.matmul(psd.ap[:, 0:TB], self.ones.ap, p3[:, mc, :], start=(mc == 0), stop=(mc == 1)),
                 reads=[pT, self.ones], writes=[psd])
        rden = P.alloc("xrd", TB)
        P.op("dve", lambda e: e.reciprocal(rden.ap, psd.ap[:, 0:TB]), reads=[psd], writes=[rden])
        for dc in range(2):
            pso = P.ps()
            for mc in range(2):
                P.op("pe", lambda e, pso=pso, mc=mc, dc=dc: e.matmul(pso.ap[:, 0:TB], vN3[:, mc, h * 256 + dc * 128:h * 256 + (dc + 1) * 128], p3[:, mc, :],
                                                                 start=(mc == 0), stop=(mc == 1)), reads=[vN, pT], writes=[pso])
            P.op("dve", lambda e, pso=pso, dc=dc: e.tensor_tensor(out=at3[:, 2 * h + dc, :], in0=pso.ap[:, 0:TB], in1=rden.ap, op=ALU.mult),
                 reads=[pso, rden], writes=[(at, 2 * h + dc)])
        P.release(pT, rden)

    def build(self):
        self.setup()
        self.setup_eps()
        if "mix" in self.phases:
            self.setup_mixer()
            if "nsa" in self.branches:
                self.setup_nsa()
            if "rwkv" in self.branches:
                self.setup_rwkv()
        self.load_x()
        if "xa" in self.phases:
            self.setup_xa()
        for l in range(self.depth):
            if "mix" in self.phases:
                self.mixer(l)
            if "xa" in self.phases:
                self.xattn(l)
            if "mlp" in self.phases:
                self.mlp(l)
        fin = self.store_x()
        self.P.finalize(fin)
        return self.nc


def host_inputs(inputs, depth=DEPTH):
    f = np.float32
    g = np.stack([np.asarray(inputs[k], f)[:depth] for k in
                  ("ln_mix_pre", "ln_mix_post", "ln_xa_pre", "ln_xa_post", "ln_mlp_pre", "ln_mlp_post", "ln_mem")], axis=1)
    gains = np.ascontiguousarray(g.reshape(depth, 7, NCH, 128).transpose(3, 0, 1, 2).reshape(128, depth * 7 * NCH))
    common = {
        "ident": np.eye(128, dtype=f),
        "gains": gains,
        "mlp_w1": np.ascontiguousarray(np.asarray(inputs["mlp_w1"], f)[:depth]),
        "mlp_w2": np.ascontiguousarray(np.asarray(inputs["mlp_w2"], f)[:depth]),
    }
    w_in = np.asarray(inputs["w_in"], f)[:depth]
    q = w_in[:, :, 0:256]
    kv = w_in[:, :, 256:640]
    gl = w_in[:, :, 640:652]
    ret = w_in[:, :, 652:1676]
    rw = w_in[:, :, 1676:2572]
    cv = w_in[:, :, 2572:3340]

    def swp(w):
        w4 = w.reshape(w.shape[0], w.shape[1], 4, 2, 32)
        return w4[:, :, :, ::-1, :].reshape(w.shape)
    rq, rk, rv, rgt = ret[..., 0:256], ret[..., 256:512], ret[..., 512:768], ret[..., 768:1024]
    common["w_T"] = np.ascontiguousarray(np.concatenate(
        [q, kv[..., 0:64], kv[..., 64:128], kv[..., 128:192], kv[..., 256:320], rq, swp(rq), rk, swp(rk), rgt, rw, cv], axis=-1))
    common["w_N"] = np.ascontiguousarray(np.concatenate([kv[..., 192:256], kv[..., 320:384], gl, rv], axis=-1))
    common["w_gate"] = np.ascontiguousarray(w_in[:, :, 3340:7436])
    common["w_branch"] = np.ascontiguousarray(np.asarray(inputs["w_branch"], f)[:depth])
    common["w_mix_out"] = np.ascontiguousarray(np.asarray(inputs["w_mix_out"], f)[:depth])
    cw = np.asarray(inputs["conv_w"], f)[:depth]
    common["conv_wT"] = np.ascontiguousarray(cw.reshape(depth, 3, 2, 128).transpose(0, 3, 2, 1).reshape(depth, 128, 6))
    common["ret_gT"] = np.ascontiguousarray(np.asarray(inputs["ret_norm_g"], f)[:depth].reshape(depth, 4, 64).transpose(0, 2, 1))
    half = 32
    inv_freq = (10000.0 ** (-np.arange(half, dtype=np.float32) / half)).astype(f)
    ang = np.arange(S, dtype=f)[:, None] * inv_freq[None, :]
    cosT = np.concatenate([np.cos(ang), np.cos(ang)], axis=1).T
    sinT = np.concatenate([-np.sin(ang), np.sin(ang)], axis=1).T
    common["rot_tab"] = np.ascontiguousarray(np.stack([cosT, sinT], axis=1).astype(f))
    kk = np.arange(128)[:, None]
    qq = np.arange(512)[None, :]
    dec = np.zeros((128, 4, 2, 512), np.float64)
    for h in range(4):
        lg = np.log(1.0 - 2.0 ** (-5.0 - h))
        full = np.exp(lg * (qq - kk)) * 0.125
        dec[:, h, 0] = full
        dec[:, h, 1] = np.where(qq >= kk, full, 0.0)
    common["ret_dec"] = np.ascontiguousarray(dec.reshape(128, -1).astype(f))
    import math
    common["nsa_cmp_w"] = np.ascontiguousarray(np.asarray(inputs["nsa_cmp_w"], f)[:depth])
    common["nsa_peT"] = np.ascontiguousarray(np.asarray(inputs["nsa_cmp_pe"], f)[:depth].transpose(0, 2, 1))
    rb = np.asarray(inputs["rel_bias"], f)

    def bucket(dist):
        n = np.maximum(dist, 0)
        nf = np.maximum(n, 1).astype(np.float32)
        large = 16 + (np.log(nf / np.float32(16)) / np.float32(math.log(128 / 16)) * np.float32(16)).astype(np.int32)
        large = np.minimum(large, 31)
        return np.where(n < 16, n, large)
    NEGB = np.float32(-30000.0)
    tpos = np.arange(S)
    nidx = np.arange(128)
    d_c = tpos[None, :] - (16 * nidx[:, None] + 31)
    bc = rb[bucket(d_c)]
    bc = np.where(((d_c >= 0) & (nidx[:, None] < 127))[:, :, None], bc, NEGB).transpose(0, 2, 1)
    common["nsa_biasc"] = np.ascontiguousarray(bc.astype(f))
    kk = np.arange(128)[:, None]
    qq = np.arange(128)[None, :]
    tabs = np.zeros((128, 4, 4, 128), f)
    d0 = qq - kk
    tabs[:, 0] = np.where((d0 >= 0)[:, None, :], rb[bucket(d0)].transpose(0, 2, 1), NEGB)
    tabs[:, 1] = rb[bucket(d0 + 128)].transpose(0, 2, 1)
    tabs[:, 2] = rb[31][None, :, None]
    tabs[:, 3] = np.where((d0 < 0)[:, None, :], rb[31][None, :, None], NEGB)
    common["nsa_tab"] = np.ascontiguousarray(tabs.reshape(128, -1))
    keys = np.arange(S)
    common["nsa_e2"] = np.ascontiguousarray(((keys[None, :] // 64) == np.arange(32)[:, None]).astype(f) * f(240000.0))
    cs = np.arange(127) * 16
    ss = np.arange(32) * 64
    ov = np.clip(np.minimum(cs[:, None] + 32, ss[None, :] + 64) - np.maximum(cs[:, None], ss[None, :]), 0, None).astype(f) / f(32)
    common["nsa_ovl"] = np.ascontiguousarray(np.concatenate([ov, np.zeros((1, 32), f)], axis=0))
    cur = tpos // 64
    blk = np.arange(32)
    forced = (blk[None, :] == 0) | (blk[None, :] == cur[:, None]) | (blk[None, :] == cur[:, None] - 1)
    addt = np.where(blk[None, :] <= cur[:, None], np.where(forced, f(1e4), f(0.0)), f(-1e30)).astype(f)
    common["nsa_addtab"] = np.ascontiguousarray(addt)
    mu = np.asarray(inputs["rwkv_mu"], f)[:depth]
    par = np.zeros((depth, 64, 43), f)
    par[:, :, 0:12] = mu[:, 0:768].reshape(depth, 3, 4, 64).transpose(0, 3, 1, 2).reshape(depth, 64, 12)
    par[:, 0:32, 12] = mu[:, 768:800]
    par[:, 0:32, 13] = mu[:, 800:832]
    par[:, 0:64, 14] = mu[:, 832:896]
    for wi, nm in enumerate(("rwkv_w0", "rwkv_a0", "rwkv_k_k", "rwkv_k_a", "rwkv_r_k", "rwkv_ln_g", "rwkv_ln_b")):
        par[:, :, 15 + wi * 4:15 + (wi + 1) * 4] = np.asarray(inputs[nm], f)[:depth].reshape(depth, 4, 64).transpose(0, 2, 1)
    common["rw_par"] = par
    for nm in ("rwkv_w2", "rwkv_a2", "rwkv_g2"):
        common[nm] = np.ascontiguousarray(np.asarray(inputs[nm], f)[:depth])
    ii = np.arange(64)[:, None]
    tt2 = np.arange(64)[None, :]
    mu_ = (ii < tt2).astype(f)
    mui = (ii <= tt2).astype(f)
    ml = (ii > tt2).astype(f)
    common["rw_mask5"] = np.ascontiguousarray(np.concatenate([mu_, mui, mu_, mui, ml], axis=1))
    for k in ("xa_wq", "xa_wkv", "xa_wo"):
        common[k] = np.ascontiguousarray(np.asarray(inputs[k], f)[:depth])
    maps = []
    for b in range(8):
        m = dict(common)
        m["mem"] = np.ascontiguousarray(np.asarray(inputs["mem"], f)[b])
        m["x"] = np.ascontiguousarray(np.asarray(inputs["x"], f)[b])
        maps.append(m)
    return maps


def kernel(**inputs):
    bld = Builder()
    nc = bld.build()
    maps = host_inputs(inputs)
    maps = [{k: v for k, v in m.items() if k in bld.inp} for m in maps]
    res = run_bass_kernel_spmd(nc, maps, core_ids=list(range(8)))
    return np.stack([np.asarray(r["out"], np.float32) for r in res.results], axis=0)
```

```python
import numpy as np
import concourse.bass as bass
import concourse.mybir as mybir
from concourse.bass_utils import run_bass_kernel_spmd

F32 = mybir.dt.float32
AF = mybir.ActivationFunctionType
ALU = mybir.AluOpType
AX = mybir.AxisListType

D = 1024
S = 2048
DEPTH = 4
MEM = 256
NCH = D // 128
HD = 64
DFF = 4096


class Op:
    __slots__ = ("eng", "emit", "deps", "idx", "flag", "val", "dma", "slot", "dval", "prev_slot_op")

    def __init__(self, eng, emit, dma=False):
        self.eng = eng
        self.emit = emit
        self.deps = []
        self.idx = -1
        self.flag = False
        self.val = 0
        self.dma = dma
        self.slot = -1
        self.dval = 0
        self.prev_slot_op = None


class TState:
    __slots__ = ("w", "r")

    def __init__(self):
        self.w = None
        self.r = {}

    def add_reader(self, o):
        k = o.slot if o.dma else o.eng
        p = self.r.get(k)
        if p is None or (o.dval > p.dval if o.dma else o.idx > p.idx):
            self.r[k] = o

    def all_ops(self):
        o = list(self.r.values())
        if self.w is not None:
            o.append(self.w)
        return o


class Tile:
    def __init__(self, name, ap, start=0, size=0):
        self.name = name
        self.ap = ap
        self.st = {}
        self.start = start
        self.size = size

    def __getitem__(self, k):
        return self.ap[k]


ND = 16


class Prog:
    SMALL = 1100
    COMPUTE = ("pe", "act", "dve", "pool")
    QUEUES = ("sp", "gq")

    def __init__(self, nc, arena_cols):
        self.nc = nc
        self.ops = {e: [] for e in ("pe", "act", "dve", "pool", "sp")}
        self.ndma = {"sp": 0, "gq": 0}
        self.slot_last = {"sp": [None] * ND, "gq": [None] * ND}
        self.arena = nc.alloc_sbuf_tensor("arena", [128, arena_cols], F32)
        self.free = [[0, arena_cols, []]]
        self.ncols = arena_cols
        self.cursor = arena_cols
        self.psum = [Tile(f"ps{i}", nc.alloc_psum_tensor(f"ps{i}", [128, 512], F32).ap()) for i in range(8)]
        self.ps_rr = 0
        self.held = set()

    def _take(self, i, name, cols, from_end):
        st, sz, pend = self.free[i]
        a = st + sz - cols if from_end else st
        t = Tile(name, self.arena[:, a:a + cols], a, cols)
        if pend:
            s = TState()
            for o in pend:
                s.add_reader(o)
            t.st[None] = s
        rest = []
        if a > st:
            rest.append([st, a - st, pend])
        if a + cols < st + sz:
            rest.append([a + cols, st + sz - a - cols, pend])
        self.free[i:i + 1] = rest
        return t

    def alloc(self, name, cols):
        if cols <= self.SMALL:
            for attempt in range(2):
                for i in range(len(self.free) - 1, -1, -1):
                    st, sz, _ = self.free[i]
                    if sz >= cols and st + cols <= self.cursor:
                        end = min(st + sz, self.cursor)
                        if end - st >= cols:
                            if end < st + sz:
                                pend = self.free[i][2]
                                self.free[i:i + 1] = [[st, end - st, pend], [end, st + sz - end, pend]]
                            t = self._take(i, name, cols, True)
                            self.cursor = t.start
                            return t
                self.cursor = self.ncols
        for i, (st, sz, pend) in enumerate(self.free):
            if sz >= cols:
                return self._take(i, name, cols, False)
        raise RuntimeError(f"SBUF arena full allocating {name} ({cols} cols); free={[(a, b) for a, b, _ in self.free]}")

    def release(self, *tiles):
        for t in tiles:
            tmp = TState()
            for s in t.st.values():
                for o in s.all_ops():
                    tmp.add_reader(o)
            self.free.append([t.start, t.size, list(tmp.r.values())])
        self.free.sort(key=lambda x: x[0])
        m = []
        for blk in self.free:
            if m and m[-1][0] + m[-1][1] == blk[0]:
                m[-1][1] += blk[1]
                tmp = TState()
                for o in m[-1][2] + blk[2]:
                    tmp.add_reader(o)
                m[-1][2] = list(tmp.r.values())
            else:
                m.append(blk)
        self.free = m

    def dram(self, name, shape, kind="Internal"):
        h = self.nc.dram_tensor(name, list(shape), F32, kind=kind)
        return Tile(name, h.ap())

    def ps(self, hold=False):
        while True:
            t = self.psum[self.ps_rr % 8]
            self.ps_rr += 1
            if t.name not in self.held:
                break
        if hold:
            self.held.add(t.name)
        return t

    def ps_free(self, *ts):
        for t in ts:
            self.held.discard(t.name)

    @staticmethod
    def _norm(x):
        if isinstance(x, tuple):
            return (x[0], None) if x[0].name.startswith("ps") else x
        return (x, None)

    def _track(self, op, reads, writes):
        pr = [x for x in reads if self._norm(x)[0].name.startswith("ps")]
        if pr:
            reads = [x for x in reads if not self._norm(x)[0].name.startswith("ps")]
            writes = list(writes) + [x for x in pr if all(self._norm(x)[0] is not self._norm(w)[0] for w in writes)]
        deps = []
        for x in reads:
            t, k = self._norm(x)
            for kk, s in t.st.items():
                if k is None or kk is None or kk == k:
                    if s.w is not None:
                        deps.append(s.w)
        for x in writes:
            t, k = self._norm(x)
            for kk, s in t.st.items():
                if k is None or kk is None or kk == k:
                    deps.extend(s.all_ops())
        for x in reads:
            t, k = self._norm(x)
            s = t.st.get(k)
            if s is None:
                s = t.st[k] = TState()
            s.add_reader(op)
        for x in writes:
            t, k = self._norm(x)
            if k is None:
                t.st.clear()
            s = t.st[k] = TState()
            s.w = op
        seen = set()
        for d in deps:
            if d is op or id(d) in seen:
                continue
            if op.eng == "pe" and d.eng == "pe" and not d.dma:
                continue
            seen.add(id(d))
            d.flag = True
            op.deps.append(d)

    def op(self, eng, emit, reads=(), writes=()):
        o = Op(eng, emit)
        o.idx = len(self.ops[eng])
        self._track(o, reads, writes)
        self.ops[eng].append(o)
        return o

    def dma(self, out_ap, in_ap, reads=(), writes=(), q="sp"):
        eng = "sp" if q == "sp" else "pool"
        o = Op(eng, None, dma=True)
        o.emit = lambda e: e.dma_start(out=out_ap, in_=in_ap)
        n = self.ndma[q]
        self.ndma[q] = n + 1
        o.slot = (q, n % ND)
        o.dval = 16 * (n // ND + 1)
        o.prev_slot_op = self.slot_last[q][n % ND]
        self.slot_last[q][n % ND] = o
        o.idx = len(self.ops[eng])
        self._track(o, reads, writes)
        self.ops[eng].append(o)
        return o

    def finalize(self, final_wait_ops):
        nc = self.nc
        esem = {e: nc.alloc_semaphore(f"sem_{e}") for e in self.COMPUTE}
        dsem = {q: [nc.alloc_semaphore(f"dsem_{q}{i}") for i in range(ND)] for q in self.QUEUES}
        for e in self.COMPUTE:
            c = 0
            for o in self.ops[e]:
                if o.dma:
                    continue
                if o.flag:
                    c += 1
                    o.val = c

        def token(d):
            if d.dma:
                return dsem[d.slot[0]][d.slot[1]], d.dval
            return esem[d.eng], d.val

        ops = self.ops

        def run(ename, eng):
            waited = {}
            for o in ops[ename]:
                deps = list(o.deps)
                if o.dma and o.prev_slot_op is not None:
                    deps.append(o.prev_slot_op)
                need = {}
                for d in deps:
                    sem, v = token(d)
                    if waited.get(sem.num, 0) >= v:
                        continue
                    if need.get(sem.num, (None, 0))[1] < v:
                        need[sem.num] = (sem, v)
                for num, (sem, v) in need.items():
                    eng.wait_ge(sem, v)
                    waited[num] = v
                ins = o.emit(eng)
                if o.dma:
                    ins.then_inc(dsem[o.slot[0]][o.slot[1]], 16)
                elif o.flag:
                    ins.then_inc(esem[o.eng], 1)
            if ename == "sp":
                for d in final_wait_ops:
                    sem, v = token(d)
                    if waited.get(sem.num, 0) < v:
                        eng.wait_ge(sem, v)
                        waited[sem.num] = v

        with nc.Block() as block:
            @block.tensor
            def _(e):
                run("pe", e)

            @block.scalar
            def _(e):
                run("act", e)

            @block.vector
            def _(e):
                run("dve", e)

            @block.gpsimd
            def _(e):
                run("pool", e)

            @block.sync
            def _(e):
                run("sp", e)


class Builder:
    def __init__(self, depth=DEPTH, phases=("mix", "xa", "mlp"), debug_outs=(), branches=("nsa", "ret", "rwkv", "conv")):
        self.depth = depth
        self.branches = branches
        self.phases = phases
        nc = self.nc = bass.Bass("TRN2", target_bir_lowering=False)
        P = self.P = Prog(nc, 51200)
        self.inp = {}
        self.debug_outs = debug_outs

    def din(self, name, shape):
        t = self.P.dram(name, shape, kind="ExternalInput")
        self.inp[name] = t
        return t

    def setup(self):
        P = self.P
        self.x_in = self.din("x", [S, D])
        self.out = self.P.dram("out", [S, D], kind="ExternalOutput")
        self.ident_d = self.din("ident", [128, 128])
        self.gains_d = self.din("gains", [128, self.depth * 7 * NCH])
        self.w1 = self.din("mlp_w1", [self.depth, D, DFF])
        self.w2 = self.din("mlp_w2", [self.depth, DFF, D])

        self.xT = P.alloc("xT", NCH * S)
        self.xT3 = self.xT.ap.rearrange("p (c t) -> p c t", c=NCH)
        self.ident = P.alloc("ident", 128)
        self.ones = P.alloc("ones", 128)
        self.gains = P.alloc("gains", self.depth * 7 * NCH)
        P.dma(self.ident.ap, self.ident_d.ap, reads=[self.ident_d], writes=[self.ident])
        P.dma(self.gains.ap, self.gains_d.ap, reads=[self.gains_d], writes=[self.gains])
        P.op("dve", lambda e: e.memset(self.ones.ap, 1.0), writes=[self.ones])

    def gain(self, l, which, c):
        i = (l * 7 + which) * NCH + c
        return self.gains.ap[:, i:i + 1]

    def load_x(self):
        P = self.P
        for tt in range(S // 128):
            xin = P.alloc("xin", D)
            P.dma(xin.ap, self.x_in.ap[tt * 128:(tt + 1) * 128, :], reads=[self.x_in], writes=[xin])
            for half in range(2):
                ps = P.ps()
                for j in range(4):
                    c = half * 4 + j
                    P.op("pe", lambda e, ps=ps, j=j, c=c, xin=xin: e.transpose(
                        ps.ap[:, j * 128:(j + 1) * 128], xin.ap[:, c * 128:(c + 1) * 128], self.ident.ap),
                        reads=[xin, self.ident], writes=[(ps, j)])
                dst = self.xT3[:, half * 4:half * 4 + 4, tt * 128:(tt + 1) * 128]
                P.op("act", lambda e, ps=ps, dst=dst: e.copy(dst, ps.ap.rearrange("p (c t) -> p c t", c=4)),
                     reads=[ps], writes=[(self.xT, tt)])
            P.release(xin)

    def store_x(self):
        P = self.P
        fin = []
        for tt in range(S // 128):
            xo = P.alloc("xo", D)
            for half in range(2):
                ps = P.ps()
                for j in range(4):
                    c = half * 4 + j
                    P.op("pe", lambda e, ps=ps, j=j, c=c, tt=tt: e.transpose(
                        ps.ap[:, j * 128:(j + 1) * 128], self.xT3[:, c, tt * 128:(tt + 1) * 128], self.ident.ap),
                        reads=[self.xT, self.ident], writes=[(ps, j)])
                P.op("act", lambda e, ps=ps, xo=xo, half=half: e.copy(xo.ap[:, half * 512:(half + 1) * 512], ps.ap),
                     reads=[ps], writes=[(xo, half)])
            fin.append(P.dma(self.out.ap[tt * 128:(tt + 1) * 128, :], xo.ap, reads=[xo], writes=[(self.out, tt)]))
            P.release(xo)
        return fin

    def rstd_of(self, src3, src_tile, n, rstd, eps=1e-6):
        P = self.P
        ps = P.ps()
        for c in range(NCH):
            sq = P.alloc("sq", n)
            P.op("act", lambda e, sq=sq, c=c: e.activation(sq.ap, src3[:, c, :], AF.Square),
                 reads=[src_tile], writes=[sq])
            P.op("pe", lambda e, sq=sq, c=c, ps=ps: e.matmul(ps.ap[:, 0:n], self.ones.ap, sq.ap,
                                                         start=(c == 0), stop=(c == NCH - 1)),
                 reads=[sq, self.ones], writes=[ps])
            P.release(sq)
        P.op("act", lambda e, ps=ps: e.activation(rstd.ap[:, 0:n], ps.ap[:, 0:n], AF.Sqrt, bias=self.epsb(eps), scale=1.0 / D),
             reads=[ps, self.epst], writes=[rstd])
        P.op("dve", lambda e: e.reciprocal(rstd.ap[:, 0:n], rstd.ap[:, 0:n]), reads=[rstd], writes=[rstd])

    def epsb(self, eps):
        return self.epst.ap[:, 0:1]

    def setup_eps(self):
        P = self.P
        self.epst = P.alloc("eps", 4)
        P.op("dve", lambda e: e.memset(self.epst.ap[:, 0:1], 1e-6), writes=[self.epst])
        P.op("dve", lambda e: e.memset(self.epst.ap[:, 1:2], 1e-5), writes=[self.epst])
        P.op("dve", lambda e: e.memset(self.epst.ap[:, 2:3], 64e-5), writes=[self.epst])

    def pre_norm_block(self, l, which, t0, n, hblk):
        h3 = hblk.ap.rearrange("p (c t) -> p c t", c=NCH)
        self.norm_to(l, which, self.xT3[:, :, t0:t0 + n], self.xT, n, h3, hblk)

    def norm_to(self, l, which, src3, src_tile, n, dst3, dst_tile):
        P = self.P
        rstd = P.alloc("rstd", n)
        self.rstd_of(src3, src_tile, n, rstd)
        for c in range(NCH):
            P.op("dve", lambda e, c=c: e.scalar_tensor_tensor(
                out=dst3[:, c, :], in0=src3[:, c, :], scalar=self.gain(l, which, c), in1=rstd.ap[:, 0:n],
                op0=ALU.mult, op1=ALU.mult), reads=[src_tile, rstd, self.gains], writes=[dst_tile])
        P.release(rstd)

    def post_norm_residual(self, l, which, t0, n, yblk):
        P = self.P
        rstd = P.alloc("rstd", n)
        y3 = yblk.ap.rearrange("p (c t) -> p c t", c=NCH)
        self.rstd_of(y3, yblk, n, rstd)
        for c in range(NCH):
            P.op("dve", lambda e, c=c: e.tensor_tensor(out=y3[:, c, :], in0=y3[:, c, :], in1=rstd.ap[:, 0:n], op=ALU.mult),
                 reads=[yblk, rstd], writes=[(yblk, c)])
            dst = self.xT3[:, c, t0:t0 + n]
            P.op("dve", lambda e, c=c, dst=dst: e.scalar_tensor_tensor(
                out=dst, in0=y3[:, c, :], scalar=self.gain(l, which, c), in1=dst, op0=ALU.mult, op1=ALU.add),
                reads=[(yblk, c), self.xT, self.gains], writes=[self.xT])
        P.release(rstd)

    def mlp(self, l):
        P = self.P
        TB = 256
        w1 = self.w1.ap[l]
        w2 = self.w2.ap[l]
        for tb in range(S // TB):
            self.mlp_blk(l, tb, TB, w1, w2)

    def mlp_blk(self, l, tb, TB, w1, w2):
        P = self.P
        if True:
            t0 = tb * TB
            hblk = P.alloc("hblk", NCH * TB)
            self.pre_norm_block(l, 4, t0, TB, hblk)
            h3 = hblk.ap.rearrange("p (c t) -> p c t", c=NCH)
            ablk = P.alloc("ablk", (DFF // 128) * TB)
            a3 = ablk.ap.rearrange("p (c t) -> p c t", c=DFF // 128)
            w1ring = [P.alloc("w1t", NCH * 512) for _ in range(2)]
            for fg in range(DFF // 512):
                wt = w1ring[fg % 2]
                wt3 = wt.ap.rearrange("p (k n) -> p k n", k=NCH)
                P.dma(wt3, w1[:, fg * 512:(fg + 1) * 512].rearrange("(k p) n -> p k n", p=128), reads=[self.w1], writes=[wt])
                for j in range(4):
                    f = fg * 4 + j
                    ps = P.ps()
                    for k in range(NCH):
                        P.op("pe", lambda e, ps=ps, k=k, j=j, wt3=wt3: e.matmul(
                            ps.ap[:, 0:TB], wt3[:, k, j * 128:(j + 1) * 128], h3[:, k, :], start=(k == 0), stop=(k == NCH - 1)),
                            reads=[wt, hblk], writes=[ps])
                    r = P.alloc("relu", TB)
                    P.op("act", lambda e, ps=ps, r=r: e.activation(r.ap, ps.ap[:, 0:TB], AF.Relu), reads=[ps], writes=[r])
                    P.op("dve", lambda e, r=r, f=f: e.tensor_tensor(out=a3[:, f, :], in0=r.ap, in1=r.ap, op=ALU.mult),
                         reads=[r], writes=[(ablk, f)])
                    P.release(r)
            P.release(*w1ring)
            yblk = P.alloc("yblk", NCH * TB)
            y3 = yblk.ap.rearrange("p (c t) -> p c t", c=NCH)
            KG = 4
            pss = [P.ps(hold=True) for _ in range(4)]
            w2ring = [P.alloc("w2t", KG * D) for _ in range(2)]
            for kg in range(DFF // 128 // KG):
                wt = w2ring[kg % 2]
                wt3 = wt.ap.rearrange("p (k n) -> p k n", k=KG)
                P.dma(wt3, w2[kg * KG * 128:(kg + 1) * KG * 128, :].rearrange("(k p) n -> p k n", p=128), reads=[self.w2], writes=[wt])
                for f in range(NCH):
                    ps = pss[f // 2]
                    o = (f % 2) * TB
                    for k in range(KG):
                        kk = kg * KG + k
                        first = (kk == 0 and f % 2 == 0)
                        P.op("pe", lambda e, ps=ps, o=o, k=k, kk=kk, f=f, wt3=wt3, first=first: e.matmul(
                            ps.ap[:, o:o + TB], wt3[:, k, f * 128:(f + 1) * 128], a3[:, kk, :], start=first,
                            stop=(kk == DFF // 128 - 1), skip_group_check=True),
                            reads=[wt, ablk], writes=[(ps, f % 2)])
            P.release(*w2ring)
            for f in range(NCH):
                ps = pss[f // 2]
                o = (f % 2) * TB
                P.op("act", lambda e, ps=ps, o=o, f=f: e.copy(y3[:, f, :], ps.ap[:, o:o + TB]), reads=[(ps, f % 2)], writes=[(yblk, f)])
            P.ps_free(*pss)
            P.release(ablk, hblk)
            self.post_norm_residual(l, 5, t0, TB, yblk)
            P.release(yblk)


    ZT_ROWS = 3456
    ZQ, ZKC, ZVC, ZKS, ZKW, ZRQ, ZRQS, ZRK, ZRKS, ZRG, ZRW, ZCV = 0, 256, 320, 384, 448, 512, 768, 1024, 1280, 1536, 1792, 2688
    ZN_COLS = 396
    NVS, NVW, NG, NRV = 0, 64, 128, 140

    def setup_mixer(self):
        L = self.depth
        self.wt_d = self.din("w_T", [L, D, self.ZT_ROWS])
        self.wn_d = self.din("w_N", [L, D, self.ZN_COLS])
        self.wgate_d = self.din("w_gate", [L, D, 4 * D])
        self.wbr_d = self.din("w_branch", [L, 4, 256, D])
        self.wmo_d = self.din("w_mix_out", [L, D, D])
        self.convw_d = self.din("conv_wT", [L, 128, 6])
        self.rot_d = self.din("rot_tab", [64, 2, S])
        self.rdec_d = self.din("ret_dec", [128, 4 * 2 * 512])
        self.retg_d = self.din("ret_gT", [L, 64, 4])
        self.ZT = self.P.dram("ZT", [self.ZT_ROWS, S])
        self.ZN = self.P.dram("ZN", [S, self.ZN_COLS])
        self.OBR = self.P.dram("OBR", [D, S])
        self.MRG = self.P.dram("MRG", [D, S])

    def project(self, l, hT):
        P = self.P
        h3 = hT.ap.rearrange("p (c t) -> p c t", c=NCH)
        wn = P.alloc("wn", NCH * self.ZN_COLS)
        wn3 = wn.ap.rearrange("p (k n) -> p k n", k=NCH)
        P.dma(wn3, self.wn_d.ap[l].rearrange("(k p) n -> p k n", p=128), reads=[self.wn_d], writes=[wn])
        for tt in range(S // 128):
            ps = P.ps()
            for k in range(NCH):
                P.op("pe", lambda e, ps=ps, k=k, tt=tt: e.matmul(ps.ap[:, 0:self.ZN_COLS], h3[:, k, tt * 128:(tt + 1) * 128], wn3[:, k, :],
                                                             start=(k == 0), stop=(k == NCH - 1)), reads=[hT, wn], writes=[ps])
            stg = P.alloc("stgn", self.ZN_COLS)
            P.op("act", lambda e, ps=ps, stg=stg: e.copy(stg.ap, ps.ap[:, 0:self.ZN_COLS]), reads=[ps], writes=[stg])
            P.dma(self.ZN.ap[tt * 128:(tt + 1) * 128, :], stg.ap, reads=[stg], writes=[(self.ZN, tt)], q="gq")
            P.release(stg)
        P.release(wn)
        for ch in range(self.ZT_ROWS // 128):
            wt = P.alloc("wt", NCH * 128)
            wt3 = wt.ap.rearrange("p (k n) -> p k n", k=NCH)
            P.dma(wt3, self.wt_d.ap[l][:, ch * 128:(ch + 1) * 128].rearrange("(k p) n -> p k n", p=128), reads=[self.wt_d], writes=[wt])
            for tb in range(S // 512):
                ps = P.ps()
                for k in range(NCH):
                    P.op("pe", lambda e, ps=ps, k=k, tb=tb, wt3=wt3: e.matmul(ps.ap, wt3[:, k, :], h3[:, k, tb * 512:(tb + 1) * 512],
                                                                       start=(k == 0), stop=(k == NCH - 1)), reads=[hT, wt], writes=[ps])
                stg = P.alloc("stgt", 512)
                eng = "act" if tb % 2 == 0 else "dve"
                if eng == "act":
                    P.op("act", lambda e, ps=ps, stg=stg: e.copy(stg.ap, ps.ap), reads=[ps], writes=[stg])
                else:
                    P.op("dve", lambda e, ps=ps, stg=stg: e.tensor_copy(stg.ap, ps.ap), reads=[ps], writes=[stg])
                P.dma(self.ZT.ap[ch * 128:(ch + 1) * 128, tb * 512:(tb + 1) * 512], stg.ap, reads=[stg], writes=[(self.ZT, ch)], q="gq")
                P.release(stg)
            P.release(wt)

    def conv_branch(self, l):
        P = self.P
        cw = P.alloc("convw", 6)
        P.dma(cw.ap, self.convw_d.ap[l], reads=[self.convw_d], writes=[cw])
        for c in range(2):
            self.conv_chunk(cw, c)
        P.release(cw)

    def conv_chunk(self, cw, c):
        P = self.P
        if True:
            bg = P.alloc("cv_b", S)
            cg = P.alloc("cv_c", S)
            xt = P.alloc("cv_x", S)
            for j, t in enumerate((bg, cg, xt)):
                r0 = self.ZCV + j * 256 + c * 128
                P.dma(t.ap, self.ZT.ap[r0:r0 + 128, :], reads=[(self.ZT, r0 // 128)], writes=[t])
            w = lambda j: cw.ap[:, c * 3 + j:c * 3 + j + 1]
            P.op("dve", lambda e: e.tensor_tensor(out=cg.ap, in0=cg.ap, in1=xt.ap, op=ALU.mult), reads=[cg, xt], writes=[cg])
            P.op("dve", lambda e, w=w: e.tensor_scalar(out=xt.ap, in0=cg.ap, scalar1=w(2), scalar2=None, op0=ALU.mult), reads=[cg, cw], writes=[xt])
            P.op("dve", lambda e, w=w: e.scalar_tensor_tensor(out=xt.ap[:, 1:S], in0=cg.ap[:, 0:S - 1], scalar=w(1), in1=xt.ap[:, 1:S],
                                                              op0=ALU.mult, op1=ALU.add), reads=[cg, cw, xt], writes=[xt])
            P.op("dve", lambda e, w=w: e.scalar_tensor_tensor(out=xt.ap[:, 2:S], in0=cg.ap[:, 0:S - 2], scalar=w(0), in1=xt.ap[:, 2:S],
                                                              op0=ALU.mult, op1=ALU.add), reads=[cg, cw, xt], writes=[xt])
            P.op("dve", lambda e: e.tensor_tensor(out=bg.ap, in0=bg.ap, in1=xt.ap, op=ALU.mult), reads=[bg, xt], writes=[bg])
            P.dma(self.OBR.ap[768 + c * 128:768 + (c + 1) * 128, :], bg.ap, reads=[bg], writes=[(self.OBR, 6 + c)], q="gq")
            P.release(bg, cg, xt)

    def retention_branch(self, l):
        P = self.P
        rot = P.alloc("rot", 2 * S)
        rot3 = rot.ap.rearrange("p (a t) -> p a t", a=2)
        P.dma(rot3[0:64], self.rot_d.ap, reads=[self.rot_d], writes=[rot])
        dec = P.alloc("rdec", 4 * 2 * 512)
        dec4 = dec.ap.rearrange("p (h a q) -> p h a q", h=4, a=2)
        P.dma(dec.ap, self.rdec_d.ap, reads=[self.rdec_d], writes=[dec])
        rg = P.alloc("retg", 4)
        P.dma(rg.ap[0:64], self.retg_d.ap[l], reads=[self.retg_d], writes=[rg])
        for h in range(4):
            self.ret_head(l, h, rot, rot3, dec, dec4, rg)
        P.release(rot, dec, rg)

    def ret_head(self, l, h, rot, rot3, dec, dec4, rg):
        P = self.P
        if True:
            lg = float(np.log(1.0 - 2.0 ** (-5.0 - h)))
            qk = []
            for base, bsw in ((self.ZRQ, self.ZRQS), (self.ZRK, self.ZRKS)):
                u = P.alloc("ru", S)
                us = P.alloc("rus", S)
                P.dma(u.ap[0:64], self.ZT.ap[base + h * 64:base + (h + 1) * 64, :], reads=[(self.ZT, (base + h * 64) // 128)], writes=[u])
                P.dma(us.ap[0:64], self.ZT.ap[bsw + h * 64:bsw + (h + 1) * 64, :], reads=[(self.ZT, (bsw + h * 64) // 128)], writes=[us])
                P.op("dve", lambda e, u=u: e.tensor_tensor(out=u.ap[0:64], in0=u.ap[0:64], in1=rot3[0:64, 0, :], op=ALU.mult), reads=[u, rot], writes=[u])
                P.op("dve", lambda e, us=us: e.tensor_tensor(out=us.ap[0:64], in0=us.ap[0:64], in1=rot3[0:64, 1, :], op=ALU.mult), reads=[us, rot], writes=[us])
                P.op("dve", lambda e, u=u, us=us: e.tensor_tensor(out=u.ap[0:64], in0=u.ap[0:64], in1=us.ap[0:64], op=ALU.add), reads=[u, us], writes=[u])
                P.release(us)
                qk.append(u)
            qT, kT = qk
            vh = P.alloc("rv", 16 * 64)
            vh3 = vh.ap.rearrange("p (t d) -> p t d", t=16)
            P.dma(vh3, self.ZN.ap[:, self.NRV + h * 64:self.NRV + (h + 1) * 64].rearrange("(t p) d -> p t d", p=128), reads=[self.ZN], writes=[vh])
            for Q in range(4):
                pso = P.ps(hold=True)
                first = True
                nkb = 4 * Q + 4
                for kb in range(nkb):
                    j = kb - 4 * Q
                    c0 = 128 * j if j > 0 else 0
                    nq = 512 - c0
                    pss = P.ps()
                    P.op("pe", lambda e, pss=pss, kb=kb, Q=Q, c0=c0, nq=nq: e.matmul(
                        pss.ap[:, 0:nq], kT.ap[0:64, kb * 128:(kb + 1) * 128], qT.ap[0:64, Q * 512 + c0:(Q + 1) * 512], start=True, stop=True),
                        reads=[kT, qT], writes=[pss])
                    pT = P.alloc("rp", 512)
                    if j < 0:
                        sc = float(np.exp(lg * 128.0 * (4 * Q - kb)))
                        P.op("dve", lambda e, pss=pss, pT=pT, sc=sc, h=h: e.scalar_tensor_tensor(
                            out=pT.ap, in0=pss.ap, scalar=sc, in1=dec4[:, h, 0, :], op0=ALU.mult, op1=ALU.mult), reads=[pss, dec], writes=[pT])
                    else:
                        P.op("dve", lambda e, pss=pss, pT=pT, nq=nq, h=h: e.tensor_tensor(
                            out=pT.ap[:, 0:nq], in0=pss.ap[:, 0:nq], in1=dec4[:, h, 1, 0:nq], op=ALU.mult), reads=[pss, dec], writes=[pT])
                    P.op("pe", lambda e, pso=pso, pT=pT, kb=kb, c0=c0, nq=nq, first=first, last=(kb == nkb - 1): e.matmul(
                        pso.ap[0:64, c0:512], vh3[:, kb, :], pT.ap[:, 0:nq], start=first, stop=last, skip_group_check=True),
                        reads=[vh, pT], writes=[pso])
                    first = False
                    P.release(pT)
                self.ret_epilogue(l, h, Q, pso, rg)
                P.ps_free(pso)
            P.release(qT, kT, vh)

    def ret_epilogue(self, l, h, Q, pso, rg):
        P = self.P
        n = 512
        o = P.alloc("ro", n)
        sq = P.alloc("rsq", n)
        P.op("act", lambda e: e.copy(o.ap[0:64], pso.ap[0:64, :]), reads=[pso], writes=[o])
        P.op("act", lambda e: e.activation(sq.ap[0:64], pso.ap[0:64, :], AF.Square), reads=[pso], writes=[sq])
        p1 = P.ps()
        p2 = P.ps()
        P.op("pe", lambda e: e.matmul(p1.ap[0:64, :], self.ones.ap[0:64, 0:64], o.ap[0:64], start=True, stop=True), reads=[o, self.ones], writes=[p1])
        P.op("pe", lambda e: e.matmul(p2.ap[0:64, :], self.ones.ap[0:64, 0:64], sq.ap[0:64], start=True, stop=True), reads=[sq, self.ones], writes=[p2])
        mean = P.alloc("rmean", n)
        P.op("dve", lambda e: e.tensor_scalar(out=mean.ap[0:64], in0=p1.ap[0:64, :], scalar1=1.0 / 64, scalar2=None, op0=ALU.mult), reads=[p1], writes=[mean])
        P.op("dve", lambda e: e.tensor_tensor(out=o.ap[0:64], in0=o.ap[0:64], in1=mean.ap[0:64], op=ALU.subtract), reads=[o, mean], writes=[o])
        P.op("dve", lambda e: e.tensor_tensor(out=mean.ap[0:64], in0=mean.ap[0:64], in1=mean.ap[0:64], op=ALU.mult), reads=[mean], writes=[mean])
        P.op("dve", lambda e: e.scalar_tensor_tensor(out=sq.ap[0:64], in0=p2.ap[0:64, :], scalar=1.0 / 64, in1=mean.ap[0:64],
                                                     op0=ALU.mult, op1=ALU.subtract), reads=[p2, mean], writes=[sq])
        P.op("act", lambda e: e.activation(sq.ap[0:64], sq.ap[0:64], AF.Sqrt, bias=self.epst.ap[0:64, 1:2], scale=1.0), reads=[sq, self.epst], writes=[sq])
        P.op("dve", lambda e: e.reciprocal(sq.ap[0:64], sq.ap[0:64]), reads=[sq], writes=[sq])
        P.op("dve", lambda e: e.tensor_tensor(out=o.ap[0:64], in0=o.ap[0:64], in1=sq.ap[0:64], op=ALU.mult), reads=[o, sq], writes=[o])
        g = P.alloc("rgate", n)
        r0 = self.ZRG + h * 64
        P.dma(g.ap[0:64], self.ZT.ap[r0:r0 + 64, Q * n:(Q + 1) * n], reads=[(self.ZT, r0 // 128)], writes=[g])
        P.op("act", lambda e: e.activation(g.ap[0:64], g.ap[0:64], AF.Silu), reads=[g], writes=[g])
        P.op("dve", lambda e: e.scalar_tensor_tensor(out=o.ap[0:64], in0=o.ap[0:64], scalar=rg.ap[0:64, h:h + 1], in1=g.ap[0:64],
                                                     op0=ALU.mult, op1=ALU.mult), reads=[o, rg, g], writes=[o])
        P.dma(self.OBR.ap[256 + h * 64:256 + (h + 1) * 64, Q * n:(Q + 1) * n], o.ap[0:64], reads=[o], writes=[(self.OBR, 2 + h // 2)], q="gq")
        P.release(o, sq, mean, g)

    def merge(self, l, hT):
        P = self.P
        TB = 256
        for tb in range(S // TB):
            self.merge_blk(l, hT, tb, TB)

    def merge_blk(self, l, hT, tb, TB):
        P = self.P
        h3 = hT.ap.rearrange("p (c t) -> p c t", c=NCH)
        if True:
            t0 = tb * TB
            obr = P.alloc("obr", NCH * TB)
            obr3 = obr.ap.rearrange("p (c t) -> p c t", c=NCH)
            P.dma(obr3, self.OBR.ap[:, t0:t0 + TB].rearrange("(c p) t -> p c t", p=128), reads=[self.OBR], writes=[obr])
            mrg = P.alloc("mrg", NCH * TB)
            m3 = mrg.ap.rearrange("p (c t) -> p c t", c=NCH)
            for f in range(NCH):
                for m in range(4):
                    wg = P.alloc("wg", NCH * 128)
                    wg3 = wg.ap.rearrange("p (k n) -> p k n", k=NCH)
                    c0 = m * D + f * 128
                    P.dma(wg3, self.wgate_d.ap[l][:, c0:c0 + 128].rearrange("(k p) n -> p k n", p=128), reads=[self.wgate_d], writes=[wg])
                    wb = P.alloc("wb", 2 * 128)
                    wb3 = wb.ap.rearrange("p (k n) -> p k n", k=2)
                    P.dma(wb3, self.wbr_d.ap[l, m][:, f * 128:(f + 1) * 128].rearrange("(k p) n -> p k n", p=128), reads=[self.wbr_d], writes=[wb])
                    ps1 = P.ps()
                    for k in range(NCH):
                        P.op("pe", lambda e, ps1=ps1, k=k, wg3=wg3: e.matmul(ps1.ap[:, 0:TB], wg3[:, k, :], h3[:, k, t0:t0 + TB],
                                                                          start=(k == 0), stop=(k == NCH - 1)), reads=[wg, hT], writes=[ps1])
                    ps2 = P.ps()
                    for k in range(2):
                        P.op("pe", lambda e, ps2=ps2, k=k, m=m, wb3=wb3: e.matmul(ps2.ap[:, 0:TB], wb3[:, k, :], obr3[:, 2 * m + k, :],
                                                                               start=(k == 0), stop=(k == 1)), reads=[wb, obr], writes=[ps2])
                    g = P.alloc("mg", TB)
                    P.op("act", lambda e, ps1=ps1, g=g: e.activation(g.ap, ps1.ap[:, 0:TB], AF.Sigmoid), reads=[ps1], writes=[g])
                    if m == 0:
                        P.op("dve", lambda e, g=g, ps2=ps2, f=f: e.tensor_tensor(out=m3[:, f, :], in0=g.ap, in1=ps2.ap[:, 0:TB], op=ALU.mult),
                             reads=[g, ps2], writes=[(mrg, f)])
                    else:
                        P.op("dve", lambda e, g=g, ps2=ps2: e.tensor_tensor(out=g.ap, in0=g.ap, in1=ps2.ap[:, 0:TB], op=ALU.mult),
                             reads=[g, ps2], writes=[g])
                        P.op("dve", lambda e, g=g, f=f: e.tensor_tensor(out=m3[:, f, :], in0=m3[:, f, :], in1=g.ap, op=ALU.add),
                             reads=[g, (mrg, f)], writes=[(mrg, f)])
                    P.release(g, wg, wb)
            P.release(obr)
            yblk = P.alloc("yblk", NCH * TB)
            y3 = yblk.ap.rearrange("p (c t) -> p c t", c=NCH)
            for f in range(NCH):
                wo = P.alloc("wmo", NCH * 128)
                wo3 = wo.ap.rearrange("p (k n) -> p k n", k=NCH)
                P.dma(wo3, self.wmo_d.ap[l][:, f * 128:(f + 1) * 128].rearrange("(k p) n -> p k n", p=128), reads=[self.wmo_d], writes=[wo])
                ps = P.ps()
                for k in range(NCH):
                    P.op("pe", lambda e, ps=ps, k=k, wo3=wo3: e.matmul(ps.ap[:, 0:TB], wo3[:, k, :], m3[:, k, :], start=(k == 0), stop=(k == NCH - 1)),
                         reads=[wo, mrg], writes=[ps])
                P.op("act", lambda e, ps=ps, f=f: e.copy(y3[:, f, :], ps.ap[:, 0:TB]), reads=[ps], writes=[(yblk, f)])
                P.release(wo)
            P.release(mrg)
            self.post_norm_residual(l, 1, t0, TB, yblk)
            P.release(yblk)


    def merge2(self, l, hT):
        P = self.P
        h3 = hT.ap.rearrange("p (c t) -> p c t", c=NCH)
        obr_ring = [P.alloc("obrm", 2 * S) for _ in range(2)]
        it = 0
        for f in range(NCH):
            mf = P.alloc("mrgf", S)
            for m in range(4):
                self.merge_fm(l, f, m, h3, hT, mf, obr_ring[it % 2])
                it += 1
            P.dma(self.MRG.ap[f * 128:(f + 1) * 128, :], mf.ap, reads=[mf], writes=[(self.MRG, f)], q="gq")
            P.release(mf)
        P.release(*obr_ring)

    def merge_fm(self, l, f, m, h3, hT, mf, obr):
        P = self.P
        obr3 = obr.ap.rearrange("p (k t) -> p k t", k=2)
        P.dma(obr3, self.OBR.ap[m * 256:(m + 1) * 256, :].rearrange("(k p) t -> p k t", p=128),
              reads=[(self.OBR, 2 * m), (self.OBR, 2 * m + 1)], writes=[obr])
        wg = P.alloc("wg", NCH * 128)
        wg3 = wg.ap.rearrange("p (k n) -> p k n", k=NCH)
        c0 = m * D + f * 128
        P.dma(wg3, self.wgate_d.ap[l][:, c0:c0 + 128].rearrange("(k p) n -> p k n", p=128), reads=[self.wgate_d], writes=[wg])
        wb = P.alloc("wb", 2 * 128)
        wb3 = wb.ap.rearrange("p (k n) -> p k n", k=2)
        P.dma(wb3, self.wbr_d.ap[l, m][:, f * 128:(f + 1) * 128].rearrange("(k p) n -> p k n", p=128), reads=[self.wbr_d], writes=[wb])
        for tb in range(S // 512):
            ts = slice(tb * 512, (tb + 1) * 512)
            ps1 = P.ps()
            for k in range(NCH):
                P.op("pe", lambda e, ps1=ps1, k=k, ts=ts: e.matmul(ps1.ap, wg3[:, k, :], h3[:, k, ts], start=(k == 0), stop=(k == NCH - 1)),
                     reads=[wg, hT], writes=[ps1])
            ps2 = P.ps()
            for k in range(2):
                P.op("pe", lambda e, ps2=ps2, k=k, ts=ts: e.matmul(ps2.ap, wb3[:, k, :], obr3[:, k, ts], start=(k == 0), stop=(k == 1)),
                     reads=[wb, obr], writes=[ps2])
            g = P.alloc("mg", 512)
            P.op("act", lambda e, ps1=ps1, g=g: e.activation(g.ap, ps1.ap, AF.Sigmoid), reads=[ps1], writes=[g])
            if m == 0:
                P.op("dve", lambda e, g=g, ps2=ps2, ts=ts: e.tensor_tensor(out=mf.ap[:, ts], in0=g.ap, in1=ps2.ap, op=ALU.mult),
                     reads=[g, ps2], writes=[(mf, tb)])
            else:
                P.op("dve", lambda e, g=g, ps2=ps2: e.tensor_tensor(out=g.ap, in0=g.ap, in1=ps2.ap, op=ALU.mult), reads=[g, ps2], writes=[g])
                P.op("dve", lambda e, g=g, ts=ts: e.tensor_tensor(out=mf.ap[:, ts], in0=mf.ap[:, ts], in1=g.ap, op=ALU.add),
                     reads=[g, (mf, tb)], writes=[(mf, tb)])
            P.release(g)
        P.release(wg, wb)

    def mixout(self, l):
        for tb in range(S // 512):
            self.mixout_blk(l, tb, 512)

    def mixout_blk(self, l, tb, TB):
        P = self.P
        t0 = tb * TB
        mrg = P.alloc("mrg", NCH * TB)
        m3 = mrg.ap.rearrange("p (c t) -> p c t", c=NCH)
        P.dma(m3, self.MRG.ap[:, t0:t0 + TB].rearrange("(c p) t -> p c t", p=128), reads=[self.MRG], writes=[mrg])
        yblk = P.alloc("yblk", NCH * TB)
        y3 = yblk.ap.rearrange("p (c t) -> p c t", c=NCH)
        self.lin8(self.wmo_d.ap[l], m3, mrg, y3, yblk, TB)
        P.release(mrg)
        self.post_norm_residual(l, 1, t0, TB, yblk)
        P.release(yblk)

    def zero_obr(self, r0, r1):
        P = self.P
        z = P.alloc("zero", S)
        P.op("dve", lambda e: e.memset(z.ap, 0.0), writes=[z])
        for r in range(r0, r1, 128):
            P.dma(self.OBR.ap[r:r + 128, :], z.ap, reads=[z], writes=[(self.OBR, r // 128)], q="gq")
        P.release(z)

    def mixer(self, l):
        P = self.P
        hT = P.alloc("hT", NCH * S)
        h3 = hT.ap.rearrange("p (c t) -> p c t", c=NCH)
        for tb in range(4):
            self.norm_to(l, 0, self.xT3[:, :, tb * 512:(tb + 1) * 512], self.xT, 512, h3[:, :, tb * 512:(tb + 1) * 512], hT)
        self.project(l, hT)
        P.release(hT)
        if "nsa" in self.branches:
            self.nsa_branch(l)
        else:
            self.zero_obr(0, 256)
        if "ret" in self.branches:
            self.retention_branch(l)
        else:
            self.zero_obr(256, 512)
        if "rwkv" in self.branches:
            self.rwkv_branch(l)
        else:
            self.zero_obr(512, 768)
        if "conv" in self.branches:
            self.conv_branch(l)
        else:
            self.zero_obr(768, 1024)
        if "nomerge" not in self.phases:
            hT = P.alloc("hT", NCH * S)
            h3 = hT.ap.rearrange("p (c t) -> p c t", c=NCH)
            for tb in range(4):
                self.norm_to(l, 0, self.xT3[:, :, tb * 512:(tb + 1) * 512], self.xT, 512, h3[:, :, tb * 512:(tb + 1) * 512], hT)
            self.merge2(l, hT)
            P.release(hT)
            self.mixout(l)


    def setup_nsa(self):
        L = self.depth
        self.cmpw_d = self.din("nsa_cmp_w", [L, 2, 32, 64, 64])
        self.peT_d = self.din("nsa_peT", [L, 64, 32])
        self.biasc_d = self.din("nsa_biasc", [128, 4, S])
        self.ntab_d = self.din("nsa_tab", [128, 4 * 512])
        self.e2_d = self.din("nsa_e2", [32, S])
        self.ovl_d = self.din("nsa_ovl", [128, 32])
        self.addt_d = self.din("nsa_addtab", [S, 32])

    def nsa_branch(self, l):
        P = self.P
        W = P.alloc("cmpw", 2 * 32 * 64)
        W3 = W.ap.rearrange("p (a e) -> p a e", a=64)
        P.dma(W3[0:64], self.cmpw_d.ap[l].rearrange("a l d e -> d (a l) e"), reads=[self.cmpw_d], writes=[W])
        peT = P.alloc("peT", 32)
        P.dma(peT.ap[0:64], self.peT_d.ap[l], reads=[self.peT_d], writes=[peT])
        kc = P.alloc("kcT", S)
        vc = P.alloc("vcT", S)
        P.dma(kc.ap[0:64], self.ZT.ap[self.ZKC:self.ZKC + 64, :], reads=[(self.ZT, 2)], writes=[kc])
        P.dma(vc.ap[0:64], self.ZT.ap[self.ZVC:self.ZVC + 64, :], reads=[(self.ZT, 2)], writes=[vc])
        kcmp = P.alloc("kcmpT", 128)
        vaug = P.alloc("vcmp_aug", 97)
        P.op("dve", lambda e: e.memset(kcmp.ap, 0.0), writes=[kcmp])
        P.op("dve", lambda e: e.memset(vaug.ap, 0.0), writes=[vaug])
        P.op("dve", lambda e: e.memset(vaug.ap[0:127, 64:65], 1.0), reads=[vaug], writes=[vaug])
        P.dma(vaug.ap[:, 65:97], self.ovl_d.ap, reads=[vaug, self.ovl_d], writes=[vaug])
        kc3 = kc.ap.rearrange("p (n s) -> p n s", s=16)
        vc3 = vc.ap.rearrange("p (n s) -> p n s", s=16)
        psk = P.ps()
        psb = P.ps()
        for li in range(32):
            rhs = kc3[0:64, li // 16:li // 16 + 127, li % 16]
            P.op("pe", lambda e, li=li, rhs=rhs: e.matmul(psk.ap[0:64, 0:127], W3[0:64, li, :], rhs, start=(li == 0), stop=(li == 31)),
                 reads=[W, kc], writes=[psk])
        for li in range(32):
            P.op("pe", lambda e, li=li: e.matmul(psb.ap[0:64, 0:1], W3[0:64, li, :], peT.ap[0:64, li:li + 1], start=(li == 0), stop=(li == 31)),
                 reads=[W, peT], writes=[psb])
        bk = P.alloc("bk", 1)
        P.op("act", lambda e: e.copy(bk.ap[0:64], psb.ap[0:64, 0:1]), reads=[psb], writes=[bk])
        P.op("dve", lambda e: e.tensor_scalar(out=kcmp.ap[0:64, 0:127], in0=psk.ap[0:64, 0:127], scalar1=bk.ap[0:64, 0:1], scalar2=None, op0=ALU.add),
             reads=[psk, bk, kcmp], writes=[kcmp])
        psv = P.ps()
        psbv = P.ps()
        for li in range(32):
            P.op("pe", lambda e, li=li: e.matmul(psbv.ap[0:1, 0:64], peT.ap[0:64, li:li + 1], W3[0:64, 32 + li, :], start=(li == 0), stop=(li == 31)),
                 reads=[W, peT], writes=[psbv])
        bv = P.alloc("bv", 64)
        P.op("act", lambda e: e.copy(bv.ap[0:1], psbv.ap[0:1, 0:64]), reads=[psbv], writes=[bv])
        for li in range(32):
            lhs = vc3[0:64, li // 16:li // 16 + 127, li % 16]
            P.op("pe", lambda e, li=li, lhs=lhs: e.matmul(psv.ap[0:127, 0:64], lhs, W3[0:64, 32 + li, :], start=(li == 0), stop=False),
                 reads=[W, vc], writes=[psv])
        P.op("pe", lambda e: e.matmul(psv.ap[0:127, 0:64], self.ones.ap[0:1, 0:127], bv.ap[0:1, 0:64], start=False, stop=True),
             reads=[bv, self.ones], writes=[psv])
        P.op("act", lambda e: e.copy(vaug.ap[0:127, 0:64], psv.ap[0:127, 0:64]), reads=[psv, vaug], writes=[vaug])
        P.release(W, peT, kc, vc, bk, bv)
        ks = P.alloc("ksT", S)
        kw = P.alloc("kwT", S)
        P.dma(ks.ap[0:64], self.ZT.ap[self.ZKS:self.ZKS + 64, :], reads=[(self.ZT, 3)], writes=[ks])
        P.dma(kw.ap[0:64], self.ZT.ap[self.ZKW:self.ZKW + 64, :], reads=[(self.ZT, 3)], writes=[kw])
        e2 = P.alloc("e2", S)
        P.dma(e2.ap[0:32], self.e2_d.ap, reads=[self.e2_d], writes=[e2])
        tab = P.alloc("ntab", 4 * 512)
        P.dma(tab.ap, self.ntab_d.ap, reads=[self.ntab_d], writes=[tab])
        vaugs = []
        for c0 in (self.NVS, self.NVW):
            va = P.alloc("vaug", 16 * 65)
            va3 = va.ap.rearrange("p (t d) -> p t d", t=16)
            P.op("dve", lambda e, va=va: e.memset(va.ap, 1.0), writes=[va])
            P.dma(va3[:, :, 0:64], self.ZN.ap[:, c0:c0 + 64].rearrange("(t p) d -> p t d", p=128), reads=[self.ZN, va], writes=[va])
            vaugs.append((va, va3))
        gl = P.alloc("ngl", 16 * 12)
        gl3 = gl.ap.rearrange("p (t g) -> p t g", t=16)
        P.dma(gl3, self.ZN.ap[:, self.NG:self.NG + 12].rearrange("(t p) g -> p t g", p=128), reads=[self.ZN], writes=[gl])
        P.op("act", lambda e: e.activation(gl.ap, gl.ap, AF.Sigmoid), reads=[gl], writes=[gl])
        for qb in range(S // 128):
            self.nsa_qblock(l, qb, kcmp, vaug, ks, kw, e2, tab, vaugs, gl3, gl)
        P.release(kcmp, vaug, ks, kw, e2, tab, vaugs[0][0], vaugs[1][0], gl)

    def nsa_scores(self, ps_s, tabsl, tab, pso, vaug_ap, vaug_tile, first, width):
        P = self.P
        tmp = P.alloc("ntmp", 512)
        src_tiles = [ps_s, tab]
        P.op("dve", lambda e: e.scalar_tensor_tensor(out=tmp.ap, in0=ps_s.ap, scalar=0.125, in1=tabsl, op0=ALU.mult, op1=ALU.add),
             reads=src_tiles, writes=[tmp])
        P.op("act", lambda e: e.activation(tmp.ap, tmp.ap, AF.Exp), reads=[tmp], writes=[tmp])
        for h in range(4):
            P.op("pe", lambda e, h=h: e.matmul(pso.ap[:, h * width:(h + 1) * width], tmp.ap[:, h * 128:(h + 1) * 128], vaug_ap,
                                              start=(first and h == 0), stop=True, skip_group_check=True),
                 reads=[tmp, vaug_tile], writes=[pso])
        P.release(tmp)

    def nsa_qblock(self, l, qb, kcmp, vaug, ks, kw, e2, tab, vaugs, gl3, gl):
        P = self.P
        q0 = qb * 128
        q4 = P.alloc("q4", 512)
        P.dma(q4.ap[0:64].rearrange("p (h t) -> p h t", h=4), self.ZT.ap[0:256, q0:q0 + 128].rearrange("(h d) t -> d h t", d=64),
              reads=[(self.ZT, 0), (self.ZT, 1)], writes=[q4])
        bc = P.alloc("bc", 512)
        P.dma(bc.ap.rearrange("p (h t) -> p h t", h=4), self.biasc_d.ap[:, :, q0:q0 + 128], reads=[self.biasc_d], writes=[bc])
        ps_c = P.ps()
        P.op("pe", lambda e: e.matmul(ps_c.ap, kcmp.ap[0:64, :], q4.ap[0:64, :], start=True, stop=True), reads=[kcmp, q4], writes=[ps_c])
        ps_oc = P.ps(hold=True)
        self.nsa_scores(ps_c, bc.ap, bc, ps_oc, vaug.ap, vaug, True, 97)
        P.release(bc)
        oc3 = ps_oc.ap[:, 0:388].rearrange("p (h w) -> p h w", h=4)
        rdc = P.alloc("rdc", 4)
        P.op("dve", lambda e: e.tensor_scalar(out=rdc.ap, in0=oc3[:, :, 64], scalar1=1e-30, scalar2=None, op0=ALU.max), reads=[ps_oc], writes=[rdc])
        P.op("dve", lambda e: e.reciprocal(rdc.ap, rdc.ap), reads=[rdc], writes=[rdc])
        imp = P.alloc("imp", 32)
        P.dma(imp.ap, self.addt_d.ap[q0:q0 + 128, :], reads=[self.addt_d], writes=[imp])
        for h in range(4):
            P.op("dve", lambda e, h=h: e.scalar_tensor_tensor(out=imp.ap, in0=oc3[:, h, 65:97], scalar=rdc.ap[:, h:h + 1], in1=imp.ap,
                                                              op0=ALU.mult, op1=ALU.add), reads=[ps_oc, rdc, imp], writes=[imp])
        top8 = P.alloc("top8", 8)
        P.op("dve", lambda e: e.max(out=top8.ap, in_=imp.ap), reads=[imp], writes=[top8])
        P.op("dve", lambda e: e.tensor_scalar(out=imp.ap, in0=imp.ap, scalar1=top8.ap[:, 7:8], scalar2=1.0, op0=ALU.is_ge, op1=ALU.subtract),
             reads=[imp, top8], writes=[imp])
        ps_t = P.ps()
        P.op("pe", lambda e: e.transpose(ps_t.ap[0:32, 0:128], imp.ap, self.ident.ap), reads=[imp, self.ident], writes=[ps_t])
        ns4 = P.alloc("ns4", 512)
        P.op("dve", lambda e: e.tensor_copy(ns4.ap[0:32].rearrange("p (h t) -> p h t", h=4),
                                            ps_t.ap[0:32, 0:128].rearrange("p (o t) -> p o t", o=1).broadcast_to([32, 4, 128])),
             reads=[ps_t], writes=[ns4])
        P.release(imp, top8)
        ps_os = P.ps(hold=True)
        for kb in range(qb + 1):
            dlt = qb - kb
            ti = min(dlt, 2)
            ps_s = P.ps()
            P.op("pe", lambda e, kb=kb, ps_s=ps_s: e.matmul(ps_s.ap, ks.ap[0:64, kb * 128:(kb + 1) * 128], q4.ap[0:64, :], start=True, stop=False),
                 reads=[ks, q4], writes=[ps_s])
            P.op("pe", lambda e, kb=kb, ps_s=ps_s: e.matmul(ps_s.ap, e2.ap[0:32, kb * 128:(kb + 1) * 128], ns4.ap[0:32, :], start=False, stop=True),
                 reads=[e2, ns4], writes=[ps_s])
            self.nsa_scores(ps_s, tab.ap[:, ti * 512:(ti + 1) * 512], tab, ps_os, vaugs[0][1][:, kb, :], vaugs[0][0], kb == 0, 65)
        ps_ow = P.ps(hold=True)
        kb0 = max(0, qb - 4)
        for kb in range(kb0, qb + 1):
            dlt = qb - kb
            ti = (0, 1, 2, 2, 3)[dlt]
            ps_s = P.ps()
            P.op("pe", lambda e, kb=kb, ps_s=ps_s: e.matmul(ps_s.ap, kw.ap[0:64, kb * 128:(kb + 1) * 128], q4.ap[0:64, :], start=True, stop=True),
                 reads=[kw, q4], writes=[ps_s])
            self.nsa_scores(ps_s, tab.ap[:, ti * 512:(ti + 1) * 512], tab, ps_ow, vaugs[1][1][:, kb, :], vaugs[1][0], kb == kb0, 65)
        P.release(q4, ns4)
        acc = P.alloc("nacc", 256)
        acc3 = acc.ap.rearrange("p (h d) -> p h d", h=4)
        g3 = gl3[:, qb, :].rearrange("p (h b) -> p h b", b=3)
        for b, (pso, w) in enumerate(((ps_oc, 97), (ps_os, 65), (ps_ow, 65))):
            o3 = pso.ap[:, 0:4 * w].rearrange("p (h w) -> p h w", h=4)
            scl = P.alloc("nscl", 4)
            P.op("dve", lambda e, o3=o3, scl=scl: e.tensor_scalar(out=scl.ap, in0=o3[:, :, 64], scalar1=1e-30, scalar2=None, op0=ALU.max),
                 reads=[pso], writes=[scl])
            P.op("dve", lambda e, scl=scl: e.reciprocal(scl.ap, scl.ap), reads=[scl], writes=[scl])
            P.op("dve", lambda e, scl=scl, b=b: e.tensor_tensor(out=scl.ap, in0=scl.ap, in1=g3[:, :, b], op=ALU.mult), reads=[scl, gl], writes=[scl])
            sb = scl.ap.rearrange("p (h o) -> p h o", o=1).broadcast_to([128, 4, 64])
            if b == 0:
                P.op("dve", lambda e, o3=o3, sb=sb: e.tensor_tensor(out=acc3, in0=o3[:, :, 0:64], in1=sb, op=ALU.mult), reads=[pso, scl], writes=[acc])
            else:
                t2 = P.alloc("nt2", 256)
                t23 = t2.ap.rearrange("p (h d) -> p h d", h=4)
                P.op("dve", lambda e, o3=o3, sb=sb, t23=t23: e.tensor_tensor(out=t23, in0=o3[:, :, 0:64], in1=sb, op=ALU.mult), reads=[pso, scl], writes=[t2])
                P.op("dve", lambda e, t2=t2: e.tensor_tensor(out=acc.ap, in0=acc.ap, in1=t2.ap, op=ALU.add), reads=[acc, t2], writes=[acc])
                P.release(t2)
            P.release(scl)
        P.ps_free(ps_oc, ps_os, ps_ow)
        P.release(rdc)
        ps_o = P.ps()
        for c in range(2):
            P.op("pe", lambda e, c=c: e.transpose(ps_o.ap[:, c * 128:(c + 1) * 128], acc.ap[:, c * 128:(c + 1) * 128], self.ident.ap),
                 reads=[acc, self.ident], writes=[ps_o])
        stg = P.alloc("nstg", 256)
        P.op("act", lambda e: e.copy(stg.ap, ps_o.ap[:, 0:256]), reads=[ps_o], writes=[stg])
        P.dma(self.OBR.ap[0:256, q0:q0 + 128].rearrange("(c p) t -> p c t", p=128), stg.ap.rearrange("p (c t) -> p c t", c=2),
              reads=[stg], writes=[(self.OBR, 0), (self.OBR, 1)], q="gq")
        P.release(acc, stg)


    RC = 64

    def setup_rwkv(self):
        L = self.depth
        self.rwpar_d = self.din("rw_par", [L, 64, 43])
        self.rww2_d = self.din("rwkv_w2", [L, 32, 256])
        self.rwa2_d = self.din("rwkv_a2", [L, 32, 256])
        self.rwg2_d = self.din("rwkv_g2", [L, 64, 256])
        self.rwmask_d = self.din("rw_mask5", [64, 320])

    def rw_shift(self, z, n, mu_ap, par):
        P = self.P
        d = P.alloc("rwd", S)
        P.op("dve", lambda e: e.tensor_tensor(out=d.ap[0:n, 1:S], in0=z.ap[0:n, 0:S - 1], in1=z.ap[0:n, 1:S], op=ALU.subtract), reads=[z], writes=[d])
        P.op("dve", lambda e: e.tensor_scalar(out=d.ap[0:n, 0:1], in0=z.ap[0:n, 0:1], scalar1=-1.0, scalar2=None, op0=ALU.mult), reads=[z, d], writes=[d])
        P.op("dve", lambda e: e.scalar_tensor_tensor(out=z.ap[0:n], in0=d.ap[0:n], scalar=mu_ap, in1=z.ap[0:n], op0=ALU.mult, op1=ALU.add),
             reads=[d, z, par], writes=[z])
        P.release(d)

    def rwkv_branch(self, l):
        P = self.P
        par = P.alloc("rwpar", 43)
        P.dma(par.ap[0:64], self.rwpar_d.ap[l], reads=[self.rwpar_d], writes=[par])
        omk = P.alloc("rwomk", 4)
        P.op("dve", lambda e: e.tensor_scalar(out=omk.ap[0:64], in0=par.ap[0:64, 15 + 3 * 4:15 + 4 * 4], scalar1=-1.0, scalar2=1.0, op0=ALU.mult, op1=ALU.add),
             reads=[par], writes=[omk])
        lw = P.alloc("rwlw", 3 * 256)
        P.dma(lw.ap[0:32, 0:256], self.rww2_d.ap[l], reads=[self.rww2_d], writes=[(lw, 0)])
        P.dma(lw.ap[0:32, 256:512], self.rwa2_d.ap[l], reads=[self.rwa2_d], writes=[(lw, 1)])
        P.dma(lw.ap[0:64, 512:768], self.rwg2_d.ap[l], reads=[self.rwg2_d], writes=[(lw, 2)])
        m5 = P.alloc("rwm5", 320)
        P.dma(m5.ap[0:64], self.rwmask_d.ap, reads=[self.rwmask_d], writes=[m5])
        smask = P.alloc("rwsm", S)
        P.op("dve", lambda e: e.memset(smask.ap, 1.0), writes=[smask])
        P.op("dve", lambda e: e.memset(smask.ap.rearrange("p (n c) -> p n c", c=self.RC)[:, :, 0:1], 0.0), reads=[smask], writes=[smask])
        base = self.ZRW + 768
        twl = P.alloc("rwtwl", S)
        tal = P.alloc("rwtal", S)
        tgl = P.alloc("rwtgl", S)
        for t, r0, n, mc, fn in ((twl, base, 32, 12, AF.Tanh), (tal, base + 32, 32, 13, None), (tgl, base + 64, 64, 14, AF.Sigmoid)):
            self.rw_lora_in(t, r0, n, mc, fn, par)
        import os
        for h in range(4 if int(os.environ.get("RWDBG", "9")) >= 9 else 1):
            self.rwkv_head(l, h, par, omk, lw, m5, smask, twl, tal, tgl)
        P.release(par, omk, lw, m5, smask, twl, tal, tgl)

    def rw_lora_in(self, t, r0, n, mc, fn, par):
        P = self.P
        P.dma(t.ap[0:n], self.ZT.ap[r0:r0 + n, :], reads=[(self.ZT, r0 // 128)], writes=[t])
        self.rw_shift(t, n, par.ap[0:n, mc:mc + 1], par)
        if fn is not None:
            P.op("act", lambda e: e.activation(t.ap[0:n], t.ap[0:n], fn), reads=[t], writes=[t])

    def rwkv_head(self, l, h, par, omk, lw, m5, smask, twl, tal, tgl):
        P = self.P
        C = self.RC
        NCK = S // C
        pc = lambda which: par.ap[0:64, 15 + which * 4 + h:15 + which * 4 + h + 1]
        hc = slice(h * 64, (h + 1) * 64)
        r = P.alloc("rw_r", S)
        k = P.alloc("rw_k", S)
        v = P.alloc("rw_v", S)
        for j, t in enumerate((r, k, v)):
            r0 = self.ZRW + j * 256 + h * 64
            P.dma(t.ap[0:64], self.ZT.ap[r0:r0 + 64, :], reads=[(self.ZT, r0 // 128)], writes=[t])
            self.rw_shift(t, 64, par.ap[0:64, j * 4 + h:j * 4 + h + 1], par)
        a = P.alloc("rw_a", S)
        logw = P.alloc("rw_lw", S)
        kkn = P.alloc("rw_kkn", S)
        P.op("dve", lambda e: e.tensor_scalar(out=kkn.ap[0:64], in0=k.ap[0:64], scalar1=pc(2), scalar2=None, op0=ALU.mult), reads=[k, par], writes=[kkn])
        for tb in range(4):
            self.rw_prep_blk(h, tb, par, pc, lw, twl, tal, a, logw, kkn)
        kt = P.alloc("rw_kt", S)
        P.op("dve", lambda e: e.tensor_scalar(out=kt.ap[0:64], in0=a.ap[0:64], scalar1=pc(3), scalar2=omk.ap[0:64, h:h + 1], op0=ALU.mult, op1=ALU.add),
             reads=[a, par, omk], writes=[kt])
        P.op("dve", lambda e: e.tensor_tensor(out=kt.ap[0:64], in0=kt.ap[0:64], in1=k.ap[0:64], op=ALU.mult), reads=[kt, k], writes=[kt])
        P.release(k)
        P.op("dve", lambda e: e.tensor_tensor(out=a.ap[0:64], in0=a.ap[0:64], in1=kkn.ap[0:64], op=ALU.mult), reads=[a, kkn], writes=[a])
        bb = a
        import os
        dbg = int(os.environ.get("RWDBG", "9"))
        bonv = P.alloc("rw_bon", S)
        P.op("dve", lambda e: e.scalar_tensor_tensor(out=bonv.ap[0:64], in0=r.ap[0:64], scalar=pc(4), in1=kt.ap[0:64], op0=ALU.mult, op1=ALU.mult),
             reads=[r, kt, par], writes=[bonv])
        for tb in range(4):
            ps = P.ps()
            P.op("pe", lambda e, ps=ps, tb=tb: e.matmul(ps.ap[0:64, :], self.ones.ap[0:64, 0:64], bonv.ap[0:64, tb * 512:(tb + 1) * 512], start=True, stop=True),
                 reads=[bonv, self.ones], writes=[ps])
            P.op("dve", lambda e, ps=ps, tb=tb: e.tensor_tensor(out=bonv.ap[0:64, tb * 512:(tb + 1) * 512], in0=ps.ap[0:64, :], in1=v.ap[0:64, tb * 512:(tb + 1) * 512], op=ALU.mult),
                 reads=[ps, v, bonv], writes=[bonv])
        if dbg <= 1:
            return
        cum = P.alloc("rw_cum", S)
        P.op("dve", lambda e: e.tensor_tensor_scan(out=cum.ap[0:64], data0=smask.ap[0:64], data1=logw.ap[0:64], initial=0.0, op0=ALU.mult, op1=ALU.add),
             reads=[smask, logw], writes=[cum])
        eg = P.alloc("rw_eg", S)
        P.op("act", lambda e: e.activation(eg.ap[0:64], cum.ap[0:64], AF.Exp), reads=[cum], writes=[eg])
        gC = P.alloc("rw_gC", NCK)
        P.op("dve", lambda e: e.tensor_copy(gC.ap[0:64], eg.ap[0:64].rearrange("p (n c) -> p n c", c=C)[:, :, C - 1]), reads=[eg], writes=[gC])
        P.op("dve", lambda e: e.tensor_tensor(out=r.ap[0:64], in0=r.ap[0:64], in1=eg.ap[0:64], op=ALU.mult), reads=[r, eg], writes=[r])
        P.release(eg)
        RH = r
        P.op("dve", lambda e: e.tensor_tensor(out=logw.ap[0:64], in0=cum.ap[0:64], in1=logw.ap[0:64], op=ALU.subtract), reads=[cum, logw], writes=[logw])
        P.op("act", lambda e: e.activation(logw.ap[0:64], logw.ap[0:64], AF.Exp), reads=[logw], writes=[logw])
        P.op("dve", lambda e: e.tensor_tensor(out=kkn.ap[0:64], in0=kkn.ap[0:64], in1=logw.ap[0:64], op=ALU.mult), reads=[kkn, logw], writes=[kkn])
        P.release(logw)
        KH = kkn
        P.op("act", lambda e: e.activation(cum.ap[0:64], cum.ap[0:64], AF.Exp, scale=-1.0), reads=[cum], writes=[cum])
        P.op("dve", lambda e: e.tensor_tensor(out=kt.ap[0:64], in0=kt.ap[0:64], in1=cum.ap[0:64], op=ALU.mult), reads=[kt, cum], writes=[kt])
        P.op("dve", lambda e: e.tensor_tensor(out=bb.ap[0:64], in0=bb.ap[0:64], in1=cum.ap[0:64], op=ALU.mult), reads=[bb, cum], writes=[bb])
        P.release(cum)
        KG, BG = kt, bb
        if dbg <= 2:
            return
        tms = []
        for src in (v, KG, BG):
            tm = P.alloc("rw_tm", NCK * 64)
            tm3 = tm.ap.rearrange("p (n c) -> p n c", c=64)
            for g8 in range(NCK // 8):
                ps = P.ps()
                for j in range(8):
                    n = g8 * 8 + j
                    P.op("pe", lambda e, ps=ps, j=j, n=n, src=src: e.transpose(ps.ap[0:64, j * 64:(j + 1) * 64], src.ap[0:64, n * C:(n + 1) * C], self.ident.ap[0:64, 0:64]),
                         reads=[src, self.ident], writes=[ps])
                P.op("act", lambda e, ps=ps, g8=g8, tm=tm: e.copy(tm.ap[0:64, g8 * 512:(g8 + 1) * 512], ps.ap[0:64, :]), reads=[ps], writes=[(tm, g8)])
            tms.append((tm, tm3))
        P.release(v)
        (Vt, Vt3), (KGt, KGt3), (BGt, BGt3) = tms
        if dbg <= 3:
            return
        yT = P.alloc("rw_y", S)
        ST = P.alloc("rw_ST", 64)
        P.op("dve", lambda e: e.memset(ST.ap[0:64], 0.0), writes=[ST])
        G = 4
        for g0 in range(0, NCK, G):
            As, TTs = self.rw_group_prep(g0, G, KH, RH, KG, BG, m5)
            for gi in range(G):
                if dbg > 4:
                    self.rw_chunk(g0 + gi, As[gi], TTs[gi], KH, RH, Vt3, Vt, KGt3, KGt, BGt3, BGt, ST, gC, yT)
            P.release(*As)
            P.release(*TTs)
        P.release(RH, KH, KG, BG, Vt, KGt, BGt, ST, gC)
        self.rw_epilogue(l, h, yT, bonv, pc, par, lw, tgl)
        P.release(yT, bonv)

    def rw_prep_blk(self, h, tb, par, pc, lw, twl, tal, a, logw, kkn):
        P = self.P
        ts = slice(tb * 512, (tb + 1) * 512)
        ps = P.ps()
        P.op("pe", lambda e: e.matmul(ps.ap[0:64, :], lw.ap[0:32, h * 64:(h + 1) * 64], twl.ap[0:32, ts], start=True, stop=True), reads=[lw, twl], writes=[ps])
        P.op("act", lambda e: e.activation(logw.ap[0:64, ts], ps.ap[0:64, :], AF.Sigmoid, bias=pc(0), scale=1.0), reads=[ps, par], writes=[logw])
        P.op("dve", lambda e: e.tensor_scalar(out=logw.ap[0:64, ts], in0=logw.ap[0:64, ts], scalar1=-0.6065306597126334, scalar2=None, op0=ALU.mult),
             reads=[logw], writes=[logw])
        ps2 = P.ps()
        P.op("pe", lambda e: e.matmul(ps2.ap[0:64, :], lw.ap[0:32, 256 + h * 64:256 + (h + 1) * 64], tal.ap[0:32, ts], start=True, stop=True), reads=[lw, tal], writes=[ps2])
        P.op("act", lambda e: e.activation(a.ap[0:64, ts], ps2.ap[0:64, :], AF.Sigmoid, bias=pc(1), scale=1.0), reads=[ps2, par], writes=[a])
        sq = P.alloc("rw_sq", 512)
        P.op("act", lambda e: e.activation(sq.ap[0:64], kkn.ap[0:64, ts], AF.Square), reads=[kkn], writes=[sq])
        ps3 = P.ps()
        P.op("pe", lambda e: e.matmul(ps3.ap[0:64, :], self.ones.ap[0:64, 0:64], sq.ap[0:64], start=True, stop=True), reads=[sq, self.ones], writes=[ps3])
        P.op("act", lambda e: e.activation(sq.ap[0:64], ps3.ap[0:64, :], AF.Sqrt), reads=[ps3], writes=[sq])
        P.op("dve", lambda e: e.tensor_scalar(out=sq.ap[0:64], in0=sq.ap[0:64], scalar1=1e-12, scalar2=None, op0=ALU.max), reads=[sq], writes=[sq])
        P.op("dve", lambda e: e.reciprocal(sq.ap[0:64], sq.ap[0:64]), reads=[sq], writes=[sq])
        P.op("dve", lambda e: e.tensor_tensor(out=kkn.ap[0:64, ts], in0=kkn.ap[0:64, ts], in1=sq.ap[0:64], op=ALU.mult), reads=[kkn, sq], writes=[kkn])
        P.release(sq)

    def rw_group_prep(self, g0, G, KH, RH, KG, BG, m5):
        P = self.P
        C = self.RC
        As, Ms, Ps = [], [], []
        for gi in range(G):
            cs = slice((g0 + gi) * C, (g0 + gi + 1) * C)
            ps = P.ps()
            for j, (lh, rh) in enumerate(((KG, KH), (KG, RH), (BG, KH), (BG, RH), (KH, BG))):
                P.op("pe", lambda e, ps=ps, j=j, lh=lh, rh=rh, cs=cs: e.matmul(ps.ap[0:64, j * 64:(j + 1) * 64], lh.ap[0:64, cs], rh.ap[0:64, cs], start=True, stop=True),
                     reads=[lh, rh], writes=[ps])
            A = P.alloc("rw_A", 320)
            P.op("dve", lambda e, ps=ps, A=A: e.tensor_tensor(out=A.ap[0:64], in0=ps.ap[0:64, 0:320], in1=m5.ap[0:64], op=ALU.mult), reads=[ps, m5], writes=[A])
            As.append(A)
            Pm = P.alloc("rw_P", 64)
            P.op("dve", lambda e, A=A, Pm=Pm: e.tensor_tensor(out=Pm.ap[0:64], in0=self.ident.ap[0:64, 0:64], in1=A.ap[0:64, 128:192], op=ALU.subtract),
                 reads=[A, self.ident], writes=[Pm])
            Ps.append(Pm)
            Ms.append((A.ap[0:64, 128:192], A.ap[0:64, 256:320], A))
        import os
        nsteps = int(os.environ.get("RWSTEPS", "6"))
        for step in range(6):
            if step >= nsteps:
                for gi in range(G):
                    if Ms[gi][2] is not As[gi]:
                        P.release(Ms[gi][2])
                break
            pss = []
            for gi in range(G):
                M, MT, Mt = Ms[gi]
                ps = P.ps()
                if step < 5:
                    P.op("pe", lambda e, ps=ps, M=M, MT=MT: e.matmul(ps.ap[0:64, 0:64], MT, M, start=True, stop=True), reads=[Mt], writes=[ps])
                    P.op("pe", lambda e, ps=ps, M=M, MT=MT: e.matmul(ps.ap[0:64, 64:128], M, MT, start=True, stop=True), reads=[Mt], writes=[ps])
                if step > 0:
                    P.op("pe", lambda e, ps=ps, MT=MT, Pm=Ps[gi]: e.matmul(ps.ap[0:64, 128:192], MT, Pm.ap[0:64], start=True, stop=True), reads=[Mt, Ps[gi]], writes=[ps])
                pss.append(ps)
            for gi in range(G):
                ps = pss[gi]
                if step > 0:
                    P.op("dve", lambda e, ps=ps, Pm=Ps[gi]: e.tensor_tensor(out=Pm.ap[0:64], in0=Pm.ap[0:64], in1=ps.ap[0:64, 128:192], op=ALU.add),
                         reads=[ps, Ps[gi]], writes=[Ps[gi]])
                if step < 5:
                    Mn = P.alloc("rw_M", 128)
                    P.op("act", lambda e, ps=ps, Mn=Mn: e.copy(Mn.ap[0:64], ps.ap[0:64, 0:128]), reads=[ps], writes=[Mn])
                    old = Ms[gi][2]
                    Ms[gi] = (Mn.ap[0:64, 0:64], Mn.ap[0:64, 64:128], Mn)
                    if old is not As[gi]:
                        P.release(old)
                elif Ms[gi][2] is not As[gi]:
                    P.release(Ms[gi][2])
        return As, Ps

    def rw_chunk(self, n, A, TT, KH, RH, Vt3, Vt, KGt3, KGt, BGt3, BGt, ST, gC, yT):
        P = self.P
        C = self.RC
        cs = slice(n * C, (n + 1) * C)
        psx = P.ps()
        P.op("pe", lambda e: e.matmul(psx.ap[0:64, 0:64], KH.ap[0:64, cs], ST.ap[0:64], start=True, stop=False), reads=[KH, ST], writes=[psx])
        P.op("pe", lambda e: e.matmul(psx.ap[0:64, 0:64], A.ap[0:64, 0:64], Vt3[0:64, n, :], start=False, stop=True), reads=[A, Vt], writes=[psx])
        nx = P.alloc("rw_nx", 64)
        P.op("act", lambda e: e.mul(nx.ap[0:64], psx.ap[0:64, 0:64], -1.0), reads=[psx], writes=[nx])
        psu = P.ps()
        P.op("pe", lambda e: e.matmul(psu.ap[0:64, 0:64], TT.ap[0:64], nx.ap[0:64], start=True, stop=True), reads=[TT, nx], writes=[psu])
        U = P.alloc("rw_U", 64)
        P.op("act", lambda e: e.copy(U.ap[0:64], psu.ap[0:64, 0:64]), reads=[psu], writes=[U])
        psy = P.ps()
        P.op("pe", lambda e: e.matmul(psy.ap[0:64, 0:64], ST.ap[0:64], RH.ap[0:64, cs], start=True, stop=False), reads=[ST, RH], writes=[psy])
        P.op("pe", lambda e: e.matmul(psy.ap[0:64, 0:64], Vt3[0:64, n, :], A.ap[0:64, 64:128], start=False, stop=False), reads=[Vt, A], writes=[psy])
        P.op("pe", lambda e: e.matmul(psy.ap[0:64, 0:64], U.ap[0:64], A.ap[0:64, 192:256], start=False, stop=True), reads=[U, A], writes=[psy])
        P.op("act", lambda e: e.copy(yT.ap[0:64, cs], psy.ap[0:64, 0:64]), reads=[psy], writes=[(yT, n)])
        pss = P.ps()
        P.op("pe", lambda e: e.matmul(pss.ap[0:64, 0:64], KGt3[0:64, n, :], Vt3[0:64, n, :], start=True, stop=False), reads=[KGt, Vt], writes=[pss])
        P.op("pe", lambda e: e.matmul(pss.ap[0:64, 0:64], BGt3[0:64, n, :], U.ap[0:64], start=False, stop=True), reads=[BGt, U], writes=[pss])
        P.op("dve", lambda e: e.tensor_tensor(out=ST.ap[0:64], in0=ST.ap[0:64], in1=pss.ap[0:64, 0:64], op=ALU.add), reads=[ST, pss], writes=[ST])
        P.op("dve", lambda e: e.tensor_scalar(out=ST.ap[0:64], in0=ST.ap[0:64], scalar1=gC.ap[0:64, n:n + 1], scalar2=None, op0=ALU.mult),
             reads=[ST, gC], writes=[ST])
        P.release(nx, U)

    def rw_epilogue(self, l, h, yT, bonv, pc, par, lw, tgl):
        P = self.P
        n = 512
        for Q in range(S // n):
            self.rw_epi_blk(h, Q, n, yT, bonv, pc, par, lw, tgl)

    def rw_epi_blk(self, h, Q, n, yT, bonv, pc, par, lw, tgl):
        P = self.P
        ts = slice(Q * n, (Q + 1) * n)
        o = P.alloc("ro", n)
        sq = P.alloc("rsq", n)
        P.op("act", lambda e: e.activation(sq.ap[0:64], yT.ap[0:64, ts], AF.Square), reads=[yT], writes=[sq])
        p1 = P.ps()
        p2 = P.ps()
        P.op("pe", lambda e: e.matmul(p1.ap[0:64, :], self.ones.ap[0:64, 0:64], yT.ap[0:64, ts], start=True, stop=True), reads=[yT, self.ones], writes=[p1])
        P.op("pe", lambda e: e.matmul(p2.ap[0:64, :], self.ones.ap[0:64, 0:64], sq.ap[0:64], start=True, stop=True), reads=[sq, self.ones], writes=[p2])
        mean = P.alloc("rmean", n)
        P.op("dve", lambda e: e.tensor_scalar(out=mean.ap[0:64], in0=p1.ap[0:64, :], scalar1=1.0 / 64, scalar2=None, op0=ALU.mult), reads=[p1], writes=[mean])
        P.op("dve", lambda e: e.tensor_tensor(out=o.ap[0:64], in0=yT.ap[0:64, ts], in1=mean.ap[0:64], op=ALU.subtract), reads=[yT, mean], writes=[o])
        P.op("dve", lambda e: e.tensor_tensor(out=mean.ap[0:64], in0=mean.ap[0:64], in1=mean.ap[0:64], op=ALU.mult), reads=[mean], writes=[mean])
        P.op("dve", lambda e: e.scalar_tensor_tensor(out=sq.ap[0:64], in0=p2.ap[0:64, :], scalar=1.0 / 64, in1=mean.ap[0:64],
                                                     op0=ALU.mult, op1=ALU.subtract), reads=[p2, mean], writes=[sq])
        P.op("act", lambda e: e.activation(sq.ap[0:64], sq.ap[0:64], AF.Sqrt, bias=self.epst.ap[0:64, 2:3], scale=1.0), reads=[sq, self.epst], writes=[sq])
        P.op("dve", lambda e: e.reciprocal(sq.ap[0:64], sq.ap[0:64]), reads=[sq], writes=[sq])
        P.op("dve", lambda e: e.tensor_tensor(out=o.ap[0:64], in0=o.ap[0:64], in1=sq.ap[0:64], op=ALU.mult), reads=[o, sq], writes=[o])
        P.op("dve", lambda e: e.tensor_scalar(out=o.ap[0:64], in0=o.ap[0:64], scalar1=pc(5), scalar2=pc(6), op0=ALU.mult, op1=ALU.add), reads=[o, par], writes=[o])
        P.op("dve", lambda e: e.tensor_tensor(out=o.ap[0:64], in0=o.ap[0:64], in1=bonv.ap[0:64, ts], op=ALU.add), reads=[o, bonv], writes=[o])
        pg = P.ps()
        P.op("pe", lambda e: e.matmul(pg.ap[0:64, :], lw.ap[0:64, 512 + h * 64:512 + (h + 1) * 64], tgl.ap[0:64, ts], start=True, stop=True), reads=[lw, tgl], writes=[pg])
        P.op("dve", lambda e: e.tensor_tensor(out=o.ap[0:64], in0=o.ap[0:64], in1=pg.ap[0:64, :], op=ALU.mult), reads=[o, pg], writes=[o])
        P.dma(self.OBR.ap[512 + h * 64:512 + (h + 1) * 64, ts], o.ap[0:64], reads=[o], writes=[(self.OBR, 4 + h // 2)], q="gq")
        P.release(o, sq, mean)

    def setup_xa(self):
        P = self.P
        L = self.depth
        self.mem_d = self.din("mem", [MEM, D])
        self.wq_d = self.din("xa_wq", [L, D, D])
        self.wkv_d = self.din("xa_wkv", [L, D, 2 * D])
        self.wo_d = self.din("xa_wo", [L, D, D])
        self.memT = P.alloc("memT", NCH * MEM)
        memT3 = self.memT.ap.rearrange("p (c t) -> p c t", c=NCH)
        for tt in range(MEM // 128):
            self.xa_load_mem(tt, memT3)

    def xa_load_mem(self, tt, memT3):
        P = self.P
        xin = P.alloc("xin", D)
        P.dma(xin.ap, self.mem_d.ap[tt * 128:(tt + 1) * 128, :], reads=[self.mem_d], writes=[xin])
        for half in range(2):
            ps = P.ps()
            for j in range(4):
                c = half * 4 + j
                P.op("pe", lambda e, ps=ps, j=j, c=c: e.transpose(ps.ap[:, j * 128:(j + 1) * 128], xin.ap[:, c * 128:(c + 1) * 128], self.ident.ap),
                     reads=[xin, self.ident], writes=[ps])
            dst = memT3[:, half * 4:half * 4 + 4, tt * 128:(tt + 1) * 128]
            P.op("act", lambda e, ps=ps, dst=dst: e.copy(dst, ps.ap.rearrange("p (c t) -> p c t", c=4)), reads=[ps], writes=[self.memT])
        P.release(xin)

    def xattn(self, l):
        P = self.P
        mnT = P.alloc("mnT", NCH * MEM)
        mn3 = mnT.ap.rearrange("p (c t) -> p c t", c=NCH)
        self.norm_to(l, 6, self.memT.ap.rearrange("p (c t) -> p c t", c=NCH), self.memT, MEM, mn3, mnT)
        kT = P.alloc("xkT", NCH * MEM)
        kT3 = kT.ap.rearrange("p (c t) -> p c t", c=NCH)
        vN = P.alloc("xv", 2 * D)
        vN3 = vN.ap.rearrange("p (m n) -> p m n", m=2)
        for f in range(NCH):
            self.xa_k(l, f, mn3, mnT, kT3, kT)
        for half in range(2):
            self.xa_v(l, half, mn3, mnT, vN3, vN)
        P.release(mnT)
        TB = 512
        for tb in range(S // TB):
            self.xa_blk(l, tb, TB, kT3, kT, vN3, vN)
        P.release(kT, vN)

    def xa_k(self, l, f, mn3, mnT, kT3, kT):
        P = self.P
        w = P.alloc("xw", NCH * 128)
        w3 = w.ap.rearrange("p (k n) -> p k n", k=NCH)
        P.dma(w3, self.wkv_d.ap[l][:, f * 128:(f + 1) * 128].rearrange("(k p) n -> p k n", p=128), reads=[self.wkv_d], writes=[w])
        ps = P.ps()
        for k in range(NCH):
            P.op("pe", lambda e, k=k: e.matmul(ps.ap[:, 0:MEM], w3[:, k, :], mn3[:, k, :], start=(k == 0), stop=(k == NCH - 1)),
                 reads=[w, mnT], writes=[ps])
        P.op("act", lambda e: e.copy(kT3[:, f, :], ps.ap[:, 0:MEM]), reads=[ps], writes=[(kT, f)])
        P.release(w)

    def xa_v(self, l, half, mn3, mnT, vN3, vN):
        P = self.P
        w = P.alloc("xwv", NCH * 512)
        w3 = w.ap.rearrange("p (k n) -> p k n", k=NCH)
        c0 = D + half * 512
        P.dma(w3, self.wkv_d.ap[l][:, c0:c0 + 512].rearrange("(k p) n -> p k n", p=128), reads=[self.wkv_d], writes=[w])
        for mc in range(2):
            ps = P.ps()
            for k in range(NCH):
                P.op("pe", lambda e, k=k, ps=ps, mc=mc: e.matmul(ps.ap, mn3[:, k, mc * 128:(mc + 1) * 128], w3[:, k, :], start=(k == 0), stop=(k == NCH - 1)),
                     reads=[w, mnT], writes=[ps])
            P.op("act", lambda e, ps=ps, mc=mc: e.copy(vN3[:, mc, half * 512:(half + 1) * 512], ps.ap), reads=[ps], writes=[(vN, (mc, half))])
        P.release(w)

    def lin8(self, w_ap, src3, src_tile, dst3, dst_tile, TB):
        P = self.P
        for f in range(NCH):
            self.lin8_f(w_ap, f, src3, src_tile, dst3, dst_tile, TB)

    def lin8_f(self, w_ap, f, src3, src_tile, dst3, dst_tile, TB):
        P = self.P
        w = P.alloc("xw", NCH * 128)
        w3 = w.ap.rearrange("p (k n) -> p k n", k=NCH)
        P.dma(w3, w_ap[:, f * 128:(f + 1) * 128].rearrange("(k p) n -> p k n", p=128), reads=[], writes=[w])
        ps = P.ps()
        for k in range(NCH):
            P.op("pe", lambda e, k=k: e.matmul(ps.ap[:, 0:TB], w3[:, k, :], src3[:, k, :], start=(k == 0), stop=(k == NCH - 1)),
                 reads=[w, src_tile], writes=[ps])
        P.op("act", lambda e: e.copy(dst3[:, f, :], ps.ap[:, 0:TB]), reads=[ps], writes=[(dst_tile, f)])
        P.release(w)

    def xa_blk(self, l, tb, TB, kT3, kT, vN3, vN):
        P = self.P
        t0 = tb * TB
        hblk = P.alloc("hblk", NCH * TB)
        self.pre_norm_block(l, 2, t0, TB, hblk)
        h3 = hblk.ap.rearrange("p (c t) -> p c t", c=NCH)
        qT = P.alloc("xq", NCH * TB)
        q3 = qT.ap.rearrange("p (c t) -> p c t", c=NCH)
        self.lin8(self.wq_d.ap[l], h3, hblk, q3, qT, TB)
        P.release(hblk)
        at = P.alloc("xat", NCH * TB)
        at3 = at.ap.rearrange("p (c t) -> p c t", c=NCH)
        for h in range(4):
            self.xa_head(h, TB, kT3, kT, vN3, vN, q3, qT, at3, at)
        P.release(qT)
        yblk = P.alloc("yblk", NCH * TB)
        y3 = yblk.ap.rearrange("p (c t) -> p c t", c=NCH)
        self.lin8(self.wo_d.ap[l], at3, at, y3, yblk, TB)
        P.release(at)
        self.post_norm_residual(l, 3, t0, TB, yblk)
        P.release(yblk)

    def xa_head(self, h, TB, kT3, kT, vN3, vN, q3, qT, at3, at):
        P = self.P
        pT = P.alloc("xp", 2 * TB)
        p3 = pT.ap.rearrange("p (m t) -> p m t", m=2)
        for mc in range(2):
            ps = P.ps()
            for c in range(2):
                P.op("pe", lambda e, ps=ps, c=c, mc=mc: e.matmul(ps.ap[:, 0:TB], kT3[:, 2 * h + c, mc * 128:(mc + 1) * 128], q3[:, 2 * h + c, :],
                                                             start=(c == 0), stop=(c == 1)), reads=[kT, qT], writes=[ps])
            P.op("act", lambda e, ps=ps, mc=mc: e.activation(p3[:, mc, :], ps.ap[:, 0:TB], AF.Exp, scale=1.0 / 16.0), reads=[ps], writes=[(pT, mc)])
        psd = P.ps()
        for mc in range(2):
            P.op("pe", lambda e, mc=mc: e.matmul(psd.ap[:, 0:TB], self.ones.ap, p3[:, mc, :], start=(mc == 0), stop=(mc == 1)),
                 reads=[pT, self.ones], writes=[psd])
        rden = P.alloc("xrd", TB)
        P.op("dve", lambda e: e.reciprocal(rden.ap, psd.ap[:, 0:TB]), reads=[psd], writes=[rden])
        for dc in range(2):
            pso = P.ps()
            for mc in range(2):
                P.op("pe", lambda e, pso=pso, mc=mc, dc=dc: e.matmul(pso.ap[:, 0:TB], vN3[:, mc, h * 256 + dc * 128:h * 256 + (dc + 1) * 128], p3[:, mc, :],
                                                                 start=(mc == 0), stop=(mc == 1)), reads=[vN, pT], writes=[pso])
            P.op("dve", lambda e, pso=pso, dc=dc: e.tensor_tensor(out=at3[:, 2 * h + dc, :], in0=pso.ap[:, 0:TB], in1=rden.ap, op=ALU.mult),
                 reads=[pso, rden], writes=[(at, 2 * h + dc)])
        P.release(pT, rden)

    def build(self):
        self.setup()
        self.setup_eps()
        if "mix" in self.phases:
            self.setup_mixer()
            if "nsa" in self.branches:
                self.setup_nsa()
            if "rwkv" in self.branches:
                self.setup_rwkv()
        self.load_x()
        if "xa" in self.phases:
            self.setup_xa()
        for l in range(self.depth):
            if "mix" in self.phases:
                self.mixer(l)
            if "xa" in self.phases:
                self.xattn(l)
            if "mlp" in self.phases:
                self.mlp(l)
        fin = self.store_x()
        self.P.finalize(fin)
        return self.nc


def host_inputs(inputs, depth=DEPTH):
    f = np.float32
    g = np.stack([np.asarray(inputs[k], f)[:depth] for k in
                  ("ln_mix_pre", "ln_mix_post", "ln_xa_pre", "ln_xa_post", "ln_mlp_pre", "ln_mlp_post", "ln_mem")], axis=1)
    gains = np.ascontiguousarray(g.reshape(depth, 7, NCH, 128).transpose(3, 0, 1, 2).reshape(128, depth * 7 * NCH))
    common = {
        "ident": np.eye(128, dtype=f),
        "gains": gains,
        "mlp_w1": np.ascontiguousarray(np.asarray(inputs["mlp_w1"], f)[:depth]),
        "mlp_w2": np.ascontiguousarray(np.asarray(inputs["mlp_w2"], f)[:depth]),
    }
    w_in = np.asarray(inputs["w_in"], f)[:depth]
    q = w_in[:, :, 0:256]
    kv = w_in[:, :, 256:640]
    gl = w_in[:, :, 640:652]
    ret = w_in[:, :, 652:1676]
    rw = w_in[:, :, 1676:2572]
    cv = w_in[:, :, 2572:3340]

    def swp(w):
        w4 = w.reshape(w.shape[0], w.shape[1], 4, 2, 32)
        return w4[:, :, :, ::-1, :].reshape(w.shape)
    rq, rk, rv, rgt = ret[..., 0:256], ret[..., 256:512], ret[..., 512:768], ret[..., 768:1024]
    common["w_T"] = np.ascontiguousarray(np.concatenate(
        [q, kv[..., 0:64], kv[..., 64:128], kv[..., 128:192], kv[..., 256:320], rq, swp(rq), rk, swp(rk), rgt, rw, cv], axis=-1))
    common["w_N"] = np.ascontiguousarray(np.concatenate([kv[..., 192:256], kv[..., 320:384], gl, rv], axis=-1))
    common["w_gate"] = np.ascontiguousarray(w_in[:, :, 3340:7436])
    common["w_branch"] = np.ascontiguousarray(np.asarray(inputs["w_branch"], f)[:depth])
    common["w_mix_out"] = np.ascontiguousarray(np.asarray(inputs["w_mix_out"], f)[:depth])
    cw = np.asarray(inputs["conv_w"], f)[:depth]
    common["conv_wT"] = np.ascontiguousarray(cw.reshape(depth, 3, 2, 128).transpose(0, 3, 2, 1).reshape(depth, 128, 6))
    common["ret_gT"] = np.ascontiguousarray(np.asarray(inputs["ret_norm_g"], f)[:depth].reshape(depth, 4, 64).transpose(0, 2, 1))
    half = 32
    inv_freq = (10000.0 ** (-np.arange(half, dtype=np.float32) / half)).astype(f)
    ang = np.arange(S, dtype=f)[:, None] * inv_freq[None, :]
    cosT = np.concatenate([np.cos(ang), np.cos(ang)], axis=1).T
    sinT = np.concatenate([-np.sin(ang), np.sin(ang)], axis=1).T
    common["rot_tab"] = np.ascontiguousarray(np.stack([cosT, sinT], axis=1).astype(f))
    kk = np.arange(128)[:, None]
    qq = np.arange(512)[None, :]
    dec = np.zeros((128, 4, 2, 512), np.float64)
    for h in range(4):
        lg = np.log(1.0 - 2.0 ** (-5.0 - h))
        full = np.exp(lg * (qq - kk)) * 0.125
        dec[:, h, 0] = full
        dec[:, h, 1] = np.where(qq >= kk, full, 0.0)
    common["ret_dec"] = np.ascontiguousarray(dec.reshape(128, -1).astype(f))
    import math
    common["nsa_cmp_w"] = np.ascontiguousarray(np.asarray(inputs["nsa_cmp_w"], f)[:depth])
    common["nsa_peT"] = np.ascontiguousarray(np.asarray(inputs["nsa_cmp_pe"], f)[:depth].transpose(0, 2, 1))
    rb = np.asarray(inputs["rel_bias"], f)

    def bucket(dist):
        n = np.maximum(dist, 0)
        nf = np.maximum(n, 1).astype(np.float32)
        large = 16 + (np.log(nf / np.float32(16)) / np.float32(math.log(128 / 16)) * np.float32(16)).astype(np.int32)
        large = np.minimum(large, 31)
        return np.where(n < 16, n, large)
    NEGB = np.float32(-30000.0)
    tpos = np.arange(S)
    nidx = np.arange(128)
    d_c = tpos[None, :] - (16 * nidx[:, None] + 31)
    bc = rb[bucket(d_c)]
    bc = np.where(((d_c >= 0) & (nidx[:, None] < 127))[:, :, None], bc, NEGB).transpose(0, 2, 1)
    common["nsa_biasc"] = np.ascontiguousarray(bc.astype(f))
    kk = np.arange(128)[:, None]
    qq = np.arange(128)[None, :]
    tabs = np.zeros((128, 4, 4, 128), f)
    d0 = qq - kk
    tabs[:, 0] = np.where((d0 >= 0)[:, None, :], rb[bucket(d0)].transpose(0, 2, 1), NEGB)
    tabs[:, 1] = rb[bucket(d0 + 128)].transpose(0, 2, 1)
    tabs[:, 2] = rb[31][None, :, None]
    tabs[:, 3] = np.where((d0 < 0)[:, None, :], rb[31][None, :, None], NEGB)
    common["nsa_tab"] = np.ascontiguousarray(tabs.reshape(128, -1))
    keys = np.arange(S)
    common["nsa_e2"] = np.ascontiguousarray(((keys[None, :] // 64) == np.arange(32)[:, None]).astype(f) * f(240000.0))
    cs = np.arange(127) * 16
    ss = np.arange(32) * 64
    ov = np.clip(np.minimum(cs[:, None] + 32, ss[None, :] + 64) - np.maximum(cs[:, None], ss[None, :]), 0, None).astype(f) / f(32)
    common["nsa_ovl"] = np.ascontiguousarray(np.concatenate([ov, np.zeros((1, 32), f)], axis=0))
    cur = tpos // 64
    blk = np.arange(32)
    forced = (blk[None, :] == 0) | (blk[None, :] == cur[:, None]) | (blk[None, :] == cur[:, None] - 1)
    addt = np.where(blk[None, :] <= cur[:, None], np.where(forced, f(1e4), f(0.0)), f(-1e30)).astype(f)
    common["nsa_addtab"] = np.ascontiguousarray(addt)
    mu = np.asarray(inputs["rwkv_mu"], f)[:depth]
    par = np.zeros((depth, 64, 43), f)
    par[:, :, 0:12] = mu[:, 0:768].reshape(depth, 3, 4, 64).transpose(0, 3, 1, 2).reshape(depth, 64, 12)
    par[:, 0:32, 12] = mu[:, 768:800]
    par[:, 0:32, 13] = mu[:, 800:832]
    par[:, 0:64, 14] = mu[:, 832:896]
    for wi, nm in enumerate(("rwkv_w0", "rwkv_a0", "rwkv_k_k", "rwkv_k_a", "rwkv_r_k", "rwkv_ln_g", "rwkv_ln_b")):
        par[:, :, 15 + wi * 4:15 + (wi + 1) * 4] = np.asarray(inputs[nm], f)[:depth].reshape(depth, 4, 64).transpose(0, 2, 1)
    common["rw_par"] = par
    for nm in ("rwkv_w2", "rwkv_a2", "rwkv_g2"):
        common[nm] = np.ascontiguousarray(np.asarray(inputs[nm], f)[:depth])
    ii = np.arange(64)[:, None]
    tt2 = np.arange(64)[None, :]
    mu_ = (ii < tt2).astype(f)
    mui = (ii <= tt2).astype(f)
    ml = (ii > tt2).astype(f)
    common["rw_mask5"] = np.ascontiguousarray(np.concatenate([mu_, mui, mu_, mui, ml], axis=1))
    for k in ("xa_wq", "xa_wkv", "xa_wo"):
        common[k] = np.ascontiguousarray(np.asarray(inputs[k], f)[:depth])
    maps = []
    for b in range(8):
        m = dict(common)
        m["mem"] = np.ascontiguousarray(np.asarray(inputs["mem"], f)[b])
        m["x"] = np.ascontiguousarray(np.asarray(inputs["x"], f)[b])
        maps.append(m)
    return maps


def kernel(**inputs):
    bld = Builder()
    nc = bld.build()
    maps = host_inputs(inputs)
    maps = [{k: v for k, v in m.items() if k in bld.inp} for m in maps]
    res = run_bass_kernel_spmd(nc, maps, core_ids=list(range(8)))
    return np.stack([np.asarray(r["out"], np.float32) for r in res.results], axis=0)
```

```python
import numpy as np
import concourse.bass as bass
import concourse.mybir as mybir
from concourse.bass_utils import run_bass_kernel_spmd

F32 = mybir.dt.float32
BF16 = mybir.dt.bfloat16
AF = mybir.ActivationFunctionType
ALU = mybir.AluOpType
AX = mybir.AxisListType

D = 1024
S = 2048
DEPTH = 4
MEM = 256
NCH = D // 128
HD = 64
DFF = 4096


class Op:
    __slots__ = ("eng", "emit", "deps", "idx", "flag", "val", "dma", "slot", "dval", "prev_slot_op")

    def __init__(self, eng, emit, dma=False):
        self.eng = eng
        self.emit = emit
        self.deps = []
        self.idx = -1
        self.flag = False
        self.val = 0
        self.dma = dma
        self.slot = -1
        self.dval = 0
        self.prev_slot_op = None


class TState:
    __slots__ = ("w", "r")

    def __init__(self):
        self.w = None
        self.r = {}

    def add_reader(self, o):
        k = o.slot if o.dma else o.eng
        p = self.r.get(k)
        if p is None or (o.dval > p.dval if o.dma else o.idx > p.idx):
            self.r[k] = o

    def all_ops(self):
        o = list(self.r.values())
        if self.w is not None:
            o.append(self.w)
        return o


class Tile:
    def __init__(self, name, ap, start=0, size=0):
        self.name = name
        self.ap = ap
        self.st = {}
        self.start = start
        self.size = size

    def __getitem__(self, k):
        return self.ap[k]


ND = 16


class Prog:
    SMALL = 1100
    COMPUTE = ("pe", "act", "dve", "pool")
    QUEUES = ("sp", "gq")

    def __init__(self, nc, arena_cols):
        self.nc = nc
        self.ops = {e: [] for e in ("pe", "act", "dve", "pool", "sp")}
        self.ndma = {"sp": 0, "gq": 0}
        self.slot_last = {"sp": [None] * ND, "gq": [None] * ND}
        self.arena = nc.alloc_sbuf_tensor("arena", [128, arena_cols], F32)
        self.free = [[0, arena_cols, []]]
        self.ncols = arena_cols
        self.cursor = arena_cols
        self.psum = [Tile(f"ps{i}", nc.alloc_psum_tensor(f"ps{i}", [128, 512], F32).ap()) for i in range(8)]
        self.ps_rr = 0
        self.held = set()

    def _take(self, i, name, cols, from_end):
        st, sz, pend = self.free[i]
        a = st + sz - cols if from_end else st
        t = Tile(name, self.arena[:, a:a + cols], a, cols)
        if pend:
            s = TState()
            for o in pend:
                s.add_reader(o)
            t.st[None] = s
        rest = []
        if a > st:
            rest.append([st, a - st, pend])
        if a + cols < st + sz:
            rest.append([a + cols, st + sz - a - cols, pend])
        self.free[i:i + 1] = rest
        return t

    def alloc(self, name, cols):
        if cols <= self.SMALL:
            for attempt in range(2):
                for i in range(len(self.free) - 1, -1, -1):
                    st, sz, _ = self.free[i]
                    if sz >= cols and st + cols <= self.cursor:
                        end = min(st + sz, self.cursor)
                        if end - st >= cols:
                            if end < st + sz:
                                pend = self.free[i][2]
                                self.free[i:i + 1] = [[st, end - st, pend], [end, st + sz - end, pend]]
                            t = self._take(i, name, cols, True)
                            self.cursor = t.start
                            return t
                self.cursor = self.ncols
        for i, (st, sz, pend) in enumerate(self.free):
            if sz >= cols:
                return self._take(i, name, cols, False)
        raise RuntimeError(f"SBUF arena full allocating {name} ({cols} cols); free={[(a, b) for a, b, _ in self.free]}")

    def release(self, *tiles):
        for t in tiles:
            tmp = TState()
            for s in t.st.values():
                for o in s.all_ops():
                    tmp.add_reader(o)
            self.free.append([t.start, t.size, list(tmp.r.values())])
        self.free.sort(key=lambda x: x[0])
        m = []
        for blk in self.free:
            if m and m[-1][0] + m[-1][1] == blk[0]:
                m[-1][1] += blk[1]
                tmp = TState()
                for o in m[-1][2] + blk[2]:
                    tmp.add_reader(o)
                m[-1][2] = list(tmp.r.values())
            else:
                m.append(blk)
        self.free = m

    def dram(self, name, shape, kind="Internal"):
        h = self.nc.dram_tensor(name, list(shape), F32, kind=kind)
        return Tile(name, h.ap())

    def ps(self, hold=False):
        while True:
            t = self.psum[self.ps_rr % 8]
            self.ps_rr += 1
            if t.name not in self.held:
                break
        if hold:
            self.held.add(t.name)
        return t

    def ps_free(self, *ts):
        for t in ts:
            self.held.discard(t.name)

    @staticmethod
    def _norm(x):
        if isinstance(x, tuple):
            return (x[0], None) if x[0].name.startswith("ps") else x
        return (x, None)

    def _track(self, op, reads, writes):
        pr = [x for x in reads if self._norm(x)[0].name.startswith("ps")]
        if pr:
            reads = [x for x in reads if not self._norm(x)[0].name.startswith("ps")]
            writes = list(writes) + [x for x in pr if all(self._norm(x)[0] is not self._norm(w)[0] for w in writes)]
        deps = []
        for x in reads:
            t, k = self._norm(x)
            for kk, s in t.st.items():
                if k is None or kk is None or kk == k:
                    if s.w is not None:
                        deps.append(s.w)
        for x in writes:
            t, k = self._norm(x)
            for kk, s in t.st.items():
                if k is None or kk is None or kk == k:
                    deps.extend(s.all_ops())
        for x in reads:
            t, k = self._norm(x)
            s = t.st.get(k)
            if s is None:
                s = t.st[k] = TState()
            s.add_reader(op)
        for x in writes:
            t, k = self._norm(x)
            if k is None:
                t.st.clear()
            s = t.st[k] = TState()
            s.w = op
        seen = set()
        for d in deps:
            if d is op or id(d) in seen:
                continue
            if op.eng == "pe" and d.eng == "pe" and not d.dma:
                continue
            seen.add(id(d))
            d.flag = True
            op.deps.append(d)

    def op(self, eng, emit, reads=(), writes=()):
        o = Op(eng, emit)
        o.idx = len(self.ops[eng])
        self._track(o, reads, writes)
        self.ops[eng].append(o)
        return o

    def dma(self, out_ap, in_ap, reads=(), writes=(), q="sp"):
        eng = "sp" if q == "sp" else "pool"
        o = Op(eng, None, dma=True)
        o.emit = lambda e: e.dma_start(out=out_ap, in_=in_ap)
        n = self.ndma[q]
        self.ndma[q] = n + 1
        o.slot = (q, n % ND)
        o.dval = 16 * (n // ND + 1)
        o.prev_slot_op = self.slot_last[q][n % ND]
        self.slot_last[q][n % ND] = o
        o.idx = len(self.ops[eng])
        self._track(o, reads, writes)
        self.ops[eng].append(o)
        return o

    def finalize(self, final_wait_ops):
        nc = self.nc
        esem = {e: nc.alloc_semaphore(f"sem_{e}") for e in self.COMPUTE}
        dsem = {q: [nc.alloc_semaphore(f"dsem_{q}{i}") for i in range(ND)] for q in self.QUEUES}
        for e in self.COMPUTE:
            c = 0
            for o in self.ops[e]:
                if o.dma:
                    continue
                if o.flag:
                    c += 1
                    o.val = c

        def token(d):
            if d.dma:
                return dsem[d.slot[0]][d.slot[1]], d.dval
            return esem[d.eng], d.val

        ops = self.ops

        def run(ename, eng):
            waited = {}
            for o in ops[ename]:
                deps = list(o.deps)
                if o.dma and o.prev_slot_op is not None:
                    deps.append(o.prev_slot_op)
                need = {}
                for d in deps:
                    sem, v = token(d)
                    if waited.get(sem.num, 0) >= v:
                        continue
                    if need.get(sem.num, (None, 0))[1] < v:
                        need[sem.num] = (sem, v)
                for num, (sem, v) in need.items():
                    eng.wait_ge(sem, v)
                    waited[num] = v
                ins = o.emit(eng)
                if o.dma:
                    ins.then_inc(dsem[o.slot[0]][o.slot[1]], 16)
                elif o.flag:
                    ins.then_inc(esem[o.eng], 1)
            if ename == "sp":
                for d in final_wait_ops:
                    sem, v = token(d)
                    if waited.get(sem.num, 0) < v:
                        eng.wait_ge(sem, v)
                        waited[sem.num] = v

        with nc.Block() as block:
            @block.tensor
            def _(e):
                run("pe", e)

            @block.scalar
            def _(e):
                run("act", e)

            @block.vector
            def _(e):
                run("dve", e)

            @block.gpsimd
            def _(e):
                run("pool", e)

            @block.sync
            def _(e):
                run("sp", e)


class Builder:
    def __init__(self, depth=DEPTH, phases=("mix", "xa", "mlp"), debug_outs=(), branches=("nsa", "ret", "rwkv", "conv")):
        self.depth = depth
        self.branches = branches
        self.phases = phases
        nc = self.nc = bass.Bass("TRN2", target_bir_lowering=False)
        P = self.P = Prog(nc, 51200)
        self.inp = {}
        self.debug_outs = debug_outs

    def din(self, name, shape):
        t = self.P.dram(name, shape, kind="ExternalInput")
        self.inp[name] = t
        return t

    def setup(self):
        P = self.P
        self.x_in = self.din("x", [S, D])
        self.out = self.P.dram("out", [S, D], kind="ExternalOutput")
        self.ident_d = self.din("ident", [128, 128])
        self.gains_d = self.din("gains", [128, self.depth * 7 * NCH])
        self.w1 = self.din("mlp_w1", [self.depth, D, DFF])
        self.w2 = self.din("mlp_w2", [self.depth, DFF, D])

        self.xT = P.alloc("xT", NCH * S)
        self.xT3 = self.xT.ap.rearrange("p (c t) -> p c t", c=NCH)
        self.ident = P.alloc("ident", 128)
        self.ones = P.alloc("ones", 128)
        self.gains = P.alloc("gains", self.depth * 7 * NCH)
        P.dma(self.ident.ap, self.ident_d.ap, reads=[self.ident_d], writes=[self.ident])
        P.dma(self.gains.ap, self.gains_d.ap, reads=[self.gains_d], writes=[self.gains])
        P.op("dve", lambda e: e.memset(self.ones.ap, 1.0), writes=[self.ones])

    def gain(self, l, which, c):
        i = (l * 7 + which) * NCH + c
        return self.gains.ap[:, i:i + 1]

    def load_x(self):
        P = self.P
        for tt in range(S // 128):
            xin = P.alloc("xin", D)
            P.dma(xin.ap, self.x_in.ap[tt * 128:(tt + 1) * 128, :], reads=[self.x_in], writes=[xin])
            for half in range(2):
                ps = P.ps()
                for j in range(4):
                    c = half * 4 + j
                    P.op("pe", lambda e, ps=ps, j=j, c=c, xin=xin: e.transpose(
                        ps.ap[:, j * 128:(j + 1) * 128], xin.ap[:, c * 128:(c + 1) * 128], self.ident.ap),
                        reads=[xin, self.ident], writes=[(ps, j)])
                dst = self.xT3[:, half * 4:half * 4 + 4, tt * 128:(tt + 1) * 128]
                P.op("act", lambda e, ps=ps, dst=dst: e.copy(dst, ps.ap.rearrange("p (c t) -> p c t", c=4)),
                     reads=[ps], writes=[(self.xT, tt)])
            P.release(xin)

    def store_x(self):
        P = self.P
        fin = []
        for tt in range(S // 128):
            xo = P.alloc("xo", D)
            for half in range(2):
                ps = P.ps()
                for j in range(4):
                    c = half * 4 + j
                    P.op("pe", lambda e, ps=ps, j=j, c=c, tt=tt: e.transpose(
                        ps.ap[:, j * 128:(j + 1) * 128], self.xT3[:, c, tt * 128:(tt + 1) * 128], self.ident.ap),
                        reads=[self.xT, self.ident], writes=[(ps, j)])
                P.op("act", lambda e, ps=ps, xo=xo, half=half: e.copy(xo.ap[:, half * 512:(half + 1) * 512], ps.ap),
                     reads=[ps], writes=[(xo, half)])
            fin.append(P.dma(self.out.ap[tt * 128:(tt + 1) * 128, :], xo.ap, reads=[xo], writes=[(self.out, tt)]))
            P.release(xo)
        return fin

    def rstd_of(self, src3, src_tile, n, rstd, eps=1e-6):
        P = self.P
        ps = P.ps()
        for c in range(NCH):
            sq = P.alloc("sq", n)
            P.op("act", lambda e, sq=sq, c=c: e.activation(sq.ap, src3[:, c, :], AF.Square),
                 reads=[src_tile], writes=[sq])
            P.op("pe", lambda e, sq=sq, c=c, ps=ps: e.matmul(ps.ap[:, 0:n], self.ones.ap, sq.ap,
                                                         start=(c == 0), stop=(c == NCH - 1)),
                 reads=[sq, self.ones], writes=[ps])
            P.release(sq)
        P.op("act", lambda e, ps=ps: e.activation(rstd.ap[:, 0:n], ps.ap[:, 0:n], AF.Sqrt, bias=self.epsb(eps), scale=1.0 / D),
             reads=[ps, self.epst], writes=[rstd])
        P.op("dve", lambda e: e.reciprocal(rstd.ap[:, 0:n], rstd.ap[:, 0:n]), reads=[rstd], writes=[rstd])

    def epsb(self, eps):
        return self.epst.ap[:, 0:1]

    def setup_eps(self):
        P = self.P
        self.epst = P.alloc("eps", 4)
        P.op("dve", lambda e: e.memset(self.epst.ap[:, 0:1], 1e-6), writes=[self.epst])
        P.op("dve", lambda e: e.memset(self.epst.ap[:, 1:2], 1e-5), writes=[self.epst])
        P.op("dve", lambda e: e.memset(self.epst.ap[:, 2:3], 64e-5), writes=[self.epst])

    def pre_norm_block(self, l, which, t0, n, hblk):
        h3 = hblk.ap.rearrange("p (c t) -> p c t", c=NCH)
        self.norm_to(l, which, self.xT3[:, :, t0:t0 + n], self.xT, n, h3, hblk)

    def norm_to(self, l, which, src3, src_tile, n, dst3, dst_tile):
        P = self.P
        rstd = P.alloc("rstd", n)
        self.rstd_of(src3, src_tile, n, rstd)
        for c in range(NCH):
            P.op("dve", lambda e, c=c: e.scalar_tensor_tensor(
                out=dst3[:, c, :], in0=src3[:, c, :], scalar=self.gain(l, which, c), in1=rstd.ap[:, 0:n],
                op0=ALU.mult, op1=ALU.mult), reads=[src_tile, rstd, self.gains], writes=[dst_tile])
        P.release(rstd)

    def post_norm_residual(self, l, which, t0, n, yblk):
        P = self.P
        rstd = P.alloc("rstd", n)
        y3 = yblk.ap.rearrange("p (c t) -> p c t", c=NCH)
        self.rstd_of(y3, yblk, n, rstd)
        for c in range(NCH):
            P.op("dve", lambda e, c=c: e.tensor_tensor(out=y3[:, c, :], in0=y3[:, c, :], in1=rstd.ap[:, 0:n], op=ALU.mult),
                 reads=[yblk, rstd], writes=[(yblk, c)])
            dst = self.xT3[:, c, t0:t0 + n]
            P.op("dve", lambda e, c=c, dst=dst: e.scalar_tensor_tensor(
                out=dst, in0=y3[:, c, :], scalar=self.gain(l, which, c), in1=dst, op0=ALU.mult, op1=ALU.add),
                reads=[(yblk, c), self.xT, self.gains], writes=[self.xT])
        P.release(rstd)

    def mlp(self, l):
        P = self.P
        TB = 512
        w1 = self.w1.ap[l]
        w2 = self.w2.ap[l]
        for tb in range(S // TB):
            self.mlp_blk(l, tb, TB, w1, w2)

    def mlp_blk(self, l, tb, TB, w1, w2):
        P = self.P
        t0 = tb * TB
        hblk = P.alloc("hblk", NCH * TB // 2)
        h3 = hblk.ap.bitcast(BF16).rearrange("p (c t) -> p c t", c=NCH)
        self.norm_to(l, 4, self.xT3[:, :, t0:t0 + TB], self.xT, TB, h3, hblk)
        ablk = P.alloc("ablk", (DFF // 128) * TB // 2)
        a3 = ablk.ap.bitcast(BF16).rearrange("p (c t) -> p c t", c=DFF // 128)
        w1ring = [P.alloc("w1t", NCH * 512 // 2) for _ in range(2)]
        for fg in range(DFF // 512):
            wt = w1ring[fg % 2]
            wt3 = wt.ap.bitcast(BF16).rearrange("p (k n) -> p k n", k=NCH)
            P.dma(wt3, w1[:, fg * 512:(fg + 1) * 512].rearrange("(k p) n -> p k n", p=128), reads=[self.w1], writes=[wt], q="gq")
            for j in range(4):
                f = fg * 4 + j
                ps = P.ps()
                for k in range(NCH):
                    P.op("pe", lambda e, ps=ps, k=k, j=j, wt3=wt3: e.matmul(
                        ps.ap[:, 0:TB], wt3[:, k, j * 128:(j + 1) * 128], h3[:, k, :], start=(k == 0), stop=(k == NCH - 1)),
                        reads=[wt, hblk], writes=[ps])
                r = P.alloc("relu", TB)
                P.op("act", lambda e, ps=ps, r=r: e.activation(r.ap, ps.ap[:, 0:TB], AF.Relu), reads=[ps], writes=[r])
                P.op("dve", lambda e, r=r, f=f: e.tensor_tensor(out=a3[:, f, :], in0=r.ap, in1=r.ap, op=ALU.mult),
                     reads=[r], writes=[(ablk, f)])
                P.release(r)
        P.release(*w1ring)
        yblk = P.alloc("yblk", NCH * TB)
        y3 = yblk.ap.rearrange("p (c t) -> p c t", c=NCH)
        KG = 8
        w2ring = [P.alloc("w2t", KG * 512 // 2) for _ in range(2)]
        it = 0
        for half in range(2):
            pss = [P.ps(hold=True) for _ in range(4)]
            for kg in range(DFF // 128 // KG):
                wt = w2ring[it % 2]
                it += 1
                wt3 = wt.ap.bitcast(BF16).rearrange("p (k n) -> p k n", k=KG)
                P.dma(wt3, w2[kg * KG * 128:(kg + 1) * KG * 128, half * 512:(half + 1) * 512].rearrange("(k p) n -> p k n", p=128),
                      reads=[self.w2], writes=[wt], q="gq")
                for fj in range(4):
                    ps = pss[fj]
                    for k in range(KG):
                        kk = kg * KG + k
                        P.op("pe", lambda e, ps=ps, k=k, kk=kk, fj=fj, wt3=wt3: e.matmul(
                            ps.ap[:, 0:TB], wt3[:, k, fj * 128:(fj + 1) * 128], a3[:, kk, :], start=(kk == 0),
                            stop=(kk == DFF // 128 - 1)), reads=[wt, ablk], writes=[ps])
            for fj in range(4):
                f = half * 4 + fj
                P.op("act", lambda e, ps=pss[fj], f=f: e.copy(y3[:, f, :], ps.ap[:, 0:TB]), reads=[ps], writes=[(yblk, f)])
            P.ps_free(*pss)
        P.release(*w2ring)
        P.release(ablk, hblk)
        self.post_norm_residual(l, 5, t0, TB, yblk)
        P.release(yblk)

    ZT_ROWS = 3456
    ZQ, ZKC, ZVC, ZKS, ZKW, ZRQ, ZRQS, ZRK, ZRKS, ZRG, ZRW, ZCV = 0, 256, 320, 384, 448, 512, 768, 1024, 1280, 1536, 1792, 2688
    ZN_COLS = 396
    NVS, NVW, NG, NRV = 0, 64, 128, 140

    def setup_mixer(self):
        L = self.depth
        self.wt_d = self.din("w_T", [L, D, self.ZT_ROWS])
        self.wn_d = self.din("w_N", [L, D, self.ZN_COLS])
        self.wgate_d = self.din("w_gate", [L, D, 4 * D])
        self.wbr_d = self.din("w_branch", [L, 4, 256, D])
        self.wmo_d = self.din("w_mix_out", [L, D, D])
        self.convw_d = self.din("conv_wT", [L, 128, 6])
        self.rot_d = self.din("rot_tab", [64, 2, S])
        self.rdec_d = self.din("ret_dec", [128, 4 * 2 * 512])
        self.retg_d = self.din("ret_gT", [L, 64, 4])
        self.ZT = self.P.dram("ZT", [self.ZT_ROWS, S])
        self.ZN = self.P.dram("ZN", [S, self.ZN_COLS])
        self.OBR = self.P.dram("OBR", [D, S])
        self.MRG = self.P.dram("MRG", [D, S])

    def project(self, l, hT):
        P = self.P
        h3 = hT.ap.bitcast(BF16).rearrange("p (c t) -> p c t", c=NCH)
        wn = P.alloc("wn", NCH * self.ZN_COLS // 2)
        wn3 = wn.ap.bitcast(BF16).rearrange("p (k n) -> p k n", k=NCH)
        P.dma(wn3, self.wn_d.ap[l].rearrange("(k p) n -> p k n", p=128), reads=[self.wn_d], writes=[wn], q="gq")
        for tt in range(S // 128):
            ps = P.ps()
            for k in range(NCH):
                P.op("pe", lambda e, ps=ps, k=k, tt=tt: e.matmul(ps.ap[:, 0:self.ZN_COLS], h3[:, k, tt * 128:(tt + 1) * 128], wn3[:, k, :],
                                                             start=(k == 0), stop=(k == NCH - 1)), reads=[hT, wn], writes=[ps])
            stg = P.alloc("stgn", self.ZN_COLS)
            P.op("act", lambda e, ps=ps, stg=stg: e.copy(stg.ap, ps.ap[:, 0:self.ZN_COLS]), reads=[ps], writes=[stg])
            P.dma(self.ZN.ap[tt * 128:(tt + 1) * 128, :], stg.ap, reads=[stg], writes=[(self.ZN, tt)], q="gq")
            P.release(stg)
        P.release(wn)
        for ch in range(self.ZT_ROWS // 128):
            wt = P.alloc("wt", NCH * 128 // 2)
            wt3 = wt.ap.bitcast(BF16).rearrange("p (k n) -> p k n", k=NCH)
            P.dma(wt3, self.wt_d.ap[l][:, ch * 128:(ch + 1) * 128].rearrange("(k p) n -> p k n", p=128), reads=[self.wt_d], writes=[wt], q="gq")
            for tb in range(S // 512):
                ps = P.ps()
                for k in range(NCH):
                    P.op("pe", lambda e, ps=ps, k=k, tb=tb, wt3=wt3: e.matmul(ps.ap, wt3[:, k, :], h3[:, k, tb * 512:(tb + 1) * 512],
                                                                       start=(k == 0), stop=(k == NCH - 1)), reads=[hT, wt], writes=[ps])
                stg = P.alloc("stgt", 512)
                eng = "act" if tb % 2 == 0 else "dve"
                if eng == "act":
                    P.op("act", lambda e, ps=ps, stg=stg: e.copy(stg.ap, ps.ap), reads=[ps], writes=[stg])
                else:
                    P.op("dve", lambda e, ps=ps, stg=stg: e.tensor_copy(stg.ap, ps.ap), reads=[ps], writes=[stg])
                P.dma(self.ZT.ap[ch * 128:(ch + 1) * 128, tb * 512:(tb + 1) * 512], stg.ap, reads=[stg], writes=[(self.ZT, ch)], q="gq")
                P.release(stg)
            P.release(wt)

    def conv_branch(self, l):
        P = self.P
        cw = P.alloc("convw", 6)
        P.dma(cw.ap, self.convw_d.ap[l], reads=[self.convw_d], writes=[cw])
        for c in range(2):
            self.conv_chunk(cw, c)
        P.release(cw)

    def conv_chunk(self, cw, c):
        P = self.P
        if True:
            bg = P.alloc("cv_b", S)
            cg = P.alloc("cv_c", S)
            xt = P.alloc("cv_x", S)
            for j, t in enumerate((bg, cg, xt)):
                r0 = self.ZCV + j * 256 + c * 128
                P.dma(t.ap, self.ZT.ap[r0:r0 + 128, :], reads=[(self.ZT, r0 // 128)], writes=[t])
            w = lambda j: cw.ap[:, c * 3 + j:c * 3 + j + 1]
            P.op("dve", lambda e: e.tensor_tensor(out=cg.ap, in0=cg.ap, in1=xt.ap, op=ALU.mult), reads=[cg, xt], writes=[cg])
            P.op("dve", lambda e, w=w: e.tensor_scalar(out=xt.ap, in0=cg.ap, scalar1=w(2), scalar2=None, op0=ALU.mult), reads=[cg, cw], writes=[xt])
            P.op("dve", lambda e, w=w: e.scalar_tensor_tensor(out=xt.ap[:, 1:S], in0=cg.ap[:, 0:S - 1], scalar=w(1), in1=xt.ap[:, 1:S],
                                                              op0=ALU.mult, op1=ALU.add), reads=[cg, cw, xt], writes=[xt])
            P.op("dve", lambda e, w=w: e.scalar_tensor_tensor(out=xt.ap[:, 2:S], in0=cg.ap[:, 0:S - 2], scalar=w(0), in1=xt.ap[:, 2:S],
                                                              op0=ALU.mult, op1=ALU.add), reads=[cg, cw, xt], writes=[xt])
            P.op("dve", lambda e: e.tensor_tensor(out=bg.ap, in0=bg.ap, in1=xt.ap, op=ALU.mult), reads=[bg, xt], writes=[bg])
            P.dma(self.OBR.ap[768 + c * 128:768 + (c + 1) * 128, :], bg.ap, reads=[bg], writes=[(self.OBR, 6 + c)], q="gq")
            P.release(bg, cg, xt)

    def retention_branch(self, l):
        P = self.P
        rot = P.alloc("rot", 2 * S)
        rot3 = rot.ap.rearrange("p (a t) -> p a t", a=2)
        P.dma(rot3[0:64], self.rot_d.ap, reads=[self.rot_d], writes=[rot])
        dec = P.alloc("rdec", 4 * 2 * 512)
        dec4 = dec.ap.rearrange("p (h a q) -> p h a q", h=4, a=2)
        P.dma(dec.ap, self.rdec_d.ap, reads=[self.rdec_d], writes=[dec])
        rg = P.alloc("retg", 4)
        P.dma(rg.ap[0:64], self.retg_d.ap[l], reads=[self.retg_d], writes=[rg])
        for h in range(4):
            self.ret_head(l, h, rot, rot3, dec, dec4, rg)
        P.release(rot, dec, rg)

    def ret_head(self, l, h, rot, rot3, dec, dec4, rg):
        P = self.P
        if True:
            lg = float(np.log(1.0 - 2.0 ** (-5.0 - h)))
            qk = []
            for base, bsw in ((self.ZRQ, self.ZRQS), (self.ZRK, self.ZRKS)):
                u = P.alloc("ru", S)
                us = P.alloc("rus", S)
                P.dma(u.ap[0:64], self.ZT.ap[base + h * 64:base + (h + 1) * 64, :], reads=[(self.ZT, (base + h * 64) // 128)], writes=[u])
                P.dma(us.ap[0:64], self.ZT.ap[bsw + h * 64:bsw + (h + 1) * 64, :], reads=[(self.ZT, (bsw + h * 64) // 128)], writes=[us])
                P.op("dve", lambda e, u=u: e.tensor_tensor(out=u.ap[0:64], in0=u.ap[0:64], in1=rot3[0:64, 0, :], op=ALU.mult), reads=[u, rot], writes=[u])
                P.op("dve", lambda e, us=us: e.tensor_tensor(out=us.ap[0:64], in0=us.ap[0:64], in1=rot3[0:64, 1, :], op=ALU.mult), reads=[us, rot], writes=[us])
                P.op("dve", lambda e, u=u, us=us: e.tensor_tensor(out=u.ap[0:64], in0=u.ap[0:64], in1=us.ap[0:64], op=ALU.add), reads=[u, us], writes=[u])
                P.release(us)
                qk.append(u)
            qT, kT = qk
            vh = P.alloc("rv", 16 * 64)
            vh3 = vh.ap.rearrange("p (t d) -> p t d", t=16)
            P.dma(vh3, self.ZN.ap[:, self.NRV + h * 64:self.NRV + (h + 1) * 64].rearrange("(t p) d -> p t d", p=128), reads=[self.ZN], writes=[vh])
            for Q in range(4):
                pso = P.ps(hold=True)
                first = True
                nkb = 4 * Q + 4
                for kb in range(nkb):
                    j = kb - 4 * Q
                    c0 = 128 * j if j > 0 else 0
                    nq = 512 - c0
                    pss = P.ps()
                    P.op("pe", lambda e, pss=pss, kb=kb, Q=Q, c0=c0, nq=nq: e.matmul(
                        pss.ap[:, 0:nq], kT.ap[0:64, kb * 128:(kb + 1) * 128], qT.ap[0:64, Q * 512 + c0:(Q + 1) * 512], start=True, stop=True),
                        reads=[kT, qT], writes=[pss])
                    pT = P.alloc("rp", 512)
                    if j < 0:
                        sc = float(np.exp(lg * 128.0 * (4 * Q - kb)))
                        P.op("dve", lambda e, pss=pss, pT=pT, sc=sc, h=h: e.scalar_tensor_tensor(
                            out=pT.ap, in0=pss.ap, scalar=sc, in1=dec4[:, h, 0, :], op0=ALU.mult, op1=ALU.mult), reads=[pss, dec], writes=[pT])
                    else:
                        P.op("dve", lambda e, pss=pss, pT=pT, nq=nq, h=h: e.tensor_tensor(
                            out=pT.ap[:, 0:nq], in0=pss.ap[:, 0:nq], in1=dec4[:, h, 1, 0:nq], op=ALU.mult), reads=[pss, dec], writes=[pT])
                    P.op("pe", lambda e, pso=pso, pT=pT, kb=kb, c0=c0, nq=nq, first=first, last=(kb == nkb - 1): e.matmul(
                        pso.ap[0:64, c0:512], vh3[:, kb, :], pT.ap[:, 0:nq], start=first, stop=last, skip_group_check=True),
                        reads=[vh, pT], writes=[pso])
                    first = False
                    P.release(pT)
                self.ret_epilogue(l, h, Q, pso, rg)
                P.ps_free(pso)
            P.release(qT, kT, vh)

    def ret_epilogue(self, l, h, Q, pso, rg):
        P = self.P
        n = 512
        o = P.alloc("ro", n)
        sq = P.alloc("rsq", n)
        P.op("act", lambda e: e.copy(o.ap[0:64], pso.ap[0:64, :]), reads=[pso], writes=[o])
        P.op("act", lambda e: e.activation(sq.ap[0:64], pso.ap[0:64, :], AF.Square), reads=[pso], writes=[sq])
        p1 = P.ps()
        p2 = P.ps()
        P.op("pe", lambda e: e.matmul(p1.ap[0:64, :], self.ones.ap[0:64, 0:64], o.ap[0:64], start=True, stop=True), reads=[o, self.ones], writes=[p1])
        P.op("pe", lambda e: e.matmul(p2.ap[0:64, :], self.ones.ap[0:64, 0:64], sq.ap[0:64], start=True, stop=True), reads=[sq, self.ones], writes=[p2])
        mean = P.alloc("rmean", n)
        P.op("dve", lambda e: e.tensor_scalar(out=mean.ap[0:64], in0=p1.ap[0:64, :], scalar1=1.0 / 64, scalar2=None, op0=ALU.mult), reads=[p1], writes=[mean])
        P.op("dve", lambda e: e.tensor_tensor(out=o.ap[0:64], in0=o.ap[0:64], in1=mean.ap[0:64], op=ALU.subtract), reads=[o, mean], writes=[o])
        P.op("dve", lambda e: e.tensor_tensor(out=mean.ap[0:64], in0=mean.ap[0:64], in1=mean.ap[0:64], op=ALU.mult), reads=[mean], writes=[mean])
        P.op("dve", lambda e: e.scalar_tensor_tensor(out=sq.ap[0:64], in0=p2.ap[0:64, :], scalar=1.0 / 64, in1=mean.ap[0:64],
                                                     op0=ALU.mult, op1=ALU.subtract), reads=[p2, mean], writes=[sq])
        P.op("act", lambda e: e.activation(sq.ap[0:64], sq.ap[0:64], AF.Sqrt, bias=self.epst.ap[0:64, 1:2], scale=1.0), reads=[sq, self.epst], writes=[sq])
        P.op("dve", lambda e: e.reciprocal(sq.ap[0:64], sq.ap[0:64]), reads=[sq], writes=[sq])
        P.op("dve", lambda e: e.tensor_tensor(out=o.ap[0:64], in0=o.ap[0:64], in1=sq.ap[0:64], op=ALU.mult), reads=[o, sq], writes=[o])
        g = P.alloc("rgate", n)
        r0 = self.ZRG + h * 64
        P.dma(g.ap[0:64], self.ZT.ap[r0:r0 + 64, Q * n:(Q + 1) * n], reads=[(self.ZT, r0 // 128)], writes=[g])
        P.op("act", lambda e: e.activation(g.ap[0:64], g.ap[0:64], AF.Silu), reads=[g], writes=[g])
        P.op("dve", lambda e: e.scalar_tensor_tensor(out=o.ap[0:64], in0=o.ap[0:64], scalar=rg.ap[0:64, h:h + 1], in1=g.ap[0:64],
                                                     op0=ALU.mult, op1=ALU.mult), reads=[o, rg, g], writes=[o])
        P.dma(self.OBR.ap[256 + h * 64:256 + (h + 1) * 64, Q * n:(Q + 1) * n], o.ap[0:64], reads=[o], writes=[(self.OBR, 2 + h // 2)], q="gq")
        P.release(o, sq, mean, g)

    def merge(self, l, hT):
        P = self.P
        TB = 256
        for tb in range(S // TB):
            self.merge_blk(l, hT, tb, TB)

    def merge_blk(self, l, hT, tb, TB):
        P = self.P
        h3 = hT.ap.rearrange("p (c t) -> p c t", c=NCH)
        if True:
            t0 = tb * TB
            obr = P.alloc("obr", NCH * TB)
            obr3 = obr.ap.rearrange("p (c t) -> p c t", c=NCH)
            P.dma(obr3, self.OBR.ap[:, t0:t0 + TB].rearrange("(c p) t -> p c t", p=128), reads=[self.OBR], writes=[obr])
            mrg = P.alloc("mrg", NCH * TB)
            m3 = mrg.ap.rearrange("p (c t) -> p c t", c=NCH)
            for f in range(NCH):
                for m in range(4):
                    wg = P.alloc("wg", NCH * 128)
                    wg3 = wg.ap.rearrange("p (k n) -> p k n", k=NCH)
                    c0 = m * D + f * 128
                    P.dma(wg3, self.wgate_d.ap[l][:, c0:c0 + 128].rearrange("(k p) n -> p k n", p=128), reads=[self.wgate_d], writes=[wg])
                    wb = P.alloc("wb", 2 * 128)
                    wb3 = wb.ap.rearrange("p (k n) -> p k n", k=2)
                    P.dma(wb3, self.wbr_d.ap[l, m][:, f * 128:(f + 1) * 128].rearrange("(k p) n -> p k n", p=128), reads=[self.wbr_d], writes=[wb])
                    ps1 = P.ps()
                    for k in range(NCH):
                        P.op("pe", lambda e, ps1=ps1, k=k, wg3=wg3: e.matmul(ps1.ap[:, 0:TB], wg3[:, k, :], h3[:, k, t0:t0 + TB],
                                                                          start=(k == 0), stop=(k == NCH - 1)), reads=[wg, hT], writes=[ps1])
                    ps2 = P.ps()
                    for k in range(2):
                        P.op("pe", lambda e, ps2=ps2, k=k, m=m, wb3=wb3: e.matmul(ps2.ap[:, 0:TB], wb3[:, k, :], obr3[:, 2 * m + k, :],
                                                                               start=(k == 0), stop=(k == 1)), reads=[wb, obr], writes=[ps2])
                    g = P.alloc("mg", TB)
                    P.op("act", lambda e, ps1=ps1, g=g: e.activation(g.ap, ps1.ap[:, 0:TB], AF.Sigmoid), reads=[ps1], writes=[g])
                    if m == 0:
                        P.op("dve", lambda e, g=g, ps2=ps2, f=f: e.tensor_tensor(out=m3[:, f, :], in0=g.ap, in1=ps2.ap[:, 0:TB], op=ALU.mult),
                             reads=[g, ps2], writes=[(mrg, f)])
                    else:
                        P.op("dve", lambda e, g=g, ps2=ps2: e.tensor_tensor(out=g.ap, in0=g.ap, in1=ps2.ap[:, 0:TB], op=ALU.mult),
                             reads=[g, ps2], writes=[g])
                        P.op("dve", lambda e, g=g, f=f: e.tensor_tensor(out=m3[:, f, :], in0=m3[:, f, :], in1=g.ap, op=ALU.add),
                             reads=[g, (mrg, f)], writes=[(mrg, f)])
                    P.release(g, wg, wb)
            P.release(obr)
            yblk = P.alloc("yblk", NCH * TB)
            y3 = yblk.ap.rearrange("p (c t) -> p c t", c=NCH)
            for f in range(NCH):
                wo = P.alloc("wmo", NCH * 128)
                wo3 = wo.ap.rearrange("p (k n) -> p k n", k=NCH)
                P.dma(wo3, self.wmo_d.ap[l][:, f * 128:(f + 1) * 128].rearrange("(k p) n -> p k n", p=128), reads=[self.wmo_d], writes=[wo])
                ps = P.ps()
                for k in range(NCH):
                    P.op("pe", lambda e, ps=ps, k=k, wo3=wo3: e.matmul(ps.ap[:, 0:TB], wo3[:, k, :], m3[:, k, :], start=(k == 0), stop=(k == NCH - 1)),
                         reads=[wo, mrg], writes=[ps])
                P.op("act", lambda e, ps=ps, f=f: e.copy(y3[:, f, :], ps.ap[:, 0:TB]), reads=[ps], writes=[(yblk, f)])
                P.release(wo)
            P.release(mrg)
            self.post_norm_residual(l, 1, t0, TB, yblk)
            P.release(yblk)


    def merge2(self, l, hT):
        P = self.P
        h3 = hT.ap.bitcast(BF16).rearrange("p (c t) -> p c t", c=NCH)
        obr_ring = [P.alloc("obrm", 2 * S // 2) for _ in range(2)]
        it = 0
        for f in range(NCH):
            mf = P.alloc("mrgf", S)
            for m in range(4):
                self.merge_fm(l, f, m, h3, hT, mf, obr_ring[it % 2])
                it += 1
            P.dma(self.MRG.ap[f * 128:(f + 1) * 128, :], mf.ap, reads=[mf], writes=[(self.MRG, f)], q="gq")
            P.release(mf)
        P.release(*obr_ring)

    def merge_fm(self, l, f, m, h3, hT, mf, obr):
        P = self.P
        obr3 = obr.ap.bitcast(BF16).rearrange("p (k t) -> p k t", k=2)
        P.dma(obr3, self.OBR.ap[m * 256:(m + 1) * 256, :].rearrange("(k p) t -> p k t", p=128),
              reads=[(self.OBR, 2 * m), (self.OBR, 2 * m + 1)], writes=[obr], q="gq")
        wg = P.alloc("wg", NCH * 128 // 2)
        wg3 = wg.ap.bitcast(BF16).rearrange("p (k n) -> p k n", k=NCH)
        c0 = m * D + f * 128
        P.dma(wg3, self.wgate_d.ap[l][:, c0:c0 + 128].rearrange("(k p) n -> p k n", p=128), reads=[self.wgate_d], writes=[wg], q="gq")
        wb = P.alloc("wb", 2 * 128 // 2)
        wb3 = wb.ap.bitcast(BF16).rearrange("p (k n) -> p k n", k=2)
        P.dma(wb3, self.wbr_d.ap[l, m][:, f * 128:(f + 1) * 128].rearrange("(k p) n -> p k n", p=128), reads=[self.wbr_d], writes=[wb], q="gq")
        for tb in range(S // 512):
            ts = slice(tb * 512, (tb + 1) * 512)
            ps1 = P.ps()
            for k in range(NCH):
                P.op("pe", lambda e, ps1=ps1, k=k, ts=ts: e.matmul(ps1.ap, wg3[:, k, :], h3[:, k, ts], start=(k == 0), stop=(k == NCH - 1)),
                     reads=[wg, hT], writes=[ps1])
            ps2 = P.ps()
            for k in range(2):
                P.op("pe", lambda e, ps2=ps2, k=k, ts=ts: e.matmul(ps2.ap, wb3[:, k, :], obr3[:, k, ts], start=(k == 0), stop=(k == 1)),
                     reads=[wb, obr], writes=[ps2])
            g = P.alloc("mg", 512)
            P.op("act", lambda e, ps1=ps1, g=g: e.activation(g.ap, ps1.ap, AF.Sigmoid), reads=[ps1], writes=[g])
            if m == 0:
                P.op("dve", lambda e, g=g, ps2=ps2, ts=ts: e.tensor_tensor(out=mf.ap[:, ts], in0=g.ap, in1=ps2.ap, op=ALU.mult),
                     reads=[g, ps2], writes=[(mf, tb)])
            else:
                P.op("dve", lambda e, g=g, ps2=ps2: e.tensor_tensor(out=g.ap, in0=g.ap, in1=ps2.ap, op=ALU.mult), reads=[g, ps2], writes=[g])
                P.op("dve", lambda e, g=g, ts=ts: e.tensor_tensor(out=mf.ap[:, ts], in0=mf.ap[:, ts], in1=g.ap, op=ALU.add),
                     reads=[g, (mf, tb)], writes=[(mf, tb)])
            P.release(g)
        P.release(wg, wb)

    def mixout(self, l):
        for tb in range(S // 512):
            self.mixout_blk(l, tb, 512)

    def mixout_blk(self, l, tb, TB):
        P = self.P
        t0 = tb * TB
        mrg = P.alloc("mrg", NCH * TB // 2)
        m3 = mrg.ap.bitcast(BF16).rearrange("p (c t) -> p c t", c=NCH)
        P.dma(m3, self.MRG.ap[:, t0:t0 + TB].rearrange("(c p) t -> p c t", p=128), reads=[self.MRG], writes=[mrg], q="gq")
        yblk = P.alloc("yblk", NCH * TB)
        y3 = yblk.ap.rearrange("p (c t) -> p c t", c=NCH)
        self.lin8(self.wmo_d.ap[l], m3, mrg, y3, yblk, TB)
        P.release(mrg)
        self.post_norm_residual(l, 1, t0, TB, yblk)
        P.release(yblk)

    def zero_obr(self, r0, r1):
        P = self.P
        z = P.alloc("zero", S)
        P.op("dve", lambda e: e.memset(z.ap, 0.0), writes=[z])
        for r in range(r0, r1, 128):
            P.dma(self.OBR.ap[r:r + 128, :], z.ap, reads=[z], writes=[(self.OBR, r // 128)], q="gq")
        P.release(z)

    def mixer(self, l):
        P = self.P
        hT = P.alloc("hT", NCH * S // 2)
        h3 = hT.ap.bitcast(BF16).rearrange("p (c t) -> p c t", c=NCH)
        for tb in range(4):
            self.norm_to(l, 0, self.xT3[:, :, tb * 512:(tb + 1) * 512], self.xT, 512, h3[:, :, tb * 512:(tb + 1) * 512], hT)
        self.project(l, hT)
        P.release(hT)
        if "nsa" in self.branches:
            self.nsa_branch(l)
        else:
            self.zero_obr(0, 256)
        if "ret" in self.branches:
            self.retention_branch(l)
        else:
            self.zero_obr(256, 512)
        if "rwkv" in self.branches:
            self.rwkv_branch(l)
        else:
            self.zero_obr(512, 768)
        if "conv" in self.branches:
            self.conv_branch(l)
        else:
            self.zero_obr(768, 1024)
        if "nomerge" not in self.phases:
            hT = P.alloc("hT", NCH * S // 2)
            h3 = hT.ap.bitcast(BF16).rearrange("p (c t) -> p c t", c=NCH)
            for tb in range(4):
                self.norm_to(l, 0, self.xT3[:, :, tb * 512:(tb + 1) * 512], self.xT, 512, h3[:, :, tb * 512:(tb + 1) * 512], hT)
            self.merge2(l, hT)
            P.release(hT)
            self.mixout(l)


    def setup_nsa(self):
        L = self.depth
        self.cmpw_d = self.din("nsa_cmp_w", [L, 2, 32, 64, 64])
        self.peT_d = self.din("nsa_peT", [L, 64, 32])
        self.biasc_d = self.din("nsa_biasc", [128, 4, S])
        self.ntab_d = self.din("nsa_tab", [128, 4 * 512])
        self.e2_d = self.din("nsa_e2", [32, S])
        self.ovl_d = self.din("nsa_ovl", [128, 32])
        self.addt_d = self.din("nsa_addtab", [S, 32])

    def nsa_branch(self, l):
        P = self.P
        W = P.alloc("cmpw", 2 * 32 * 64)
        W3 = W.ap.rearrange("p (a e) -> p a e", a=64)
        P.dma(W3[0:64], self.cmpw_d.ap[l].rearrange("a l d e -> d (a l) e"), reads=[self.cmpw_d], writes=[W])
        peT = P.alloc("peT", 32)
        P.dma(peT.ap[0:64], self.peT_d.ap[l], reads=[self.peT_d], writes=[peT])
        kc = P.alloc("kcT", S)
        vc = P.alloc("vcT", S)
        P.dma(kc.ap[0:64], self.ZT.ap[self.ZKC:self.ZKC + 64, :], reads=[(self.ZT, 2)], writes=[kc])
        P.dma(vc.ap[0:64], self.ZT.ap[self.ZVC:self.ZVC + 64, :], reads=[(self.ZT, 2)], writes=[vc])
        kcmp = P.alloc("kcmpT", 128)
        vaug = P.alloc("vcmp_aug", 97)
        P.op("dve", lambda e: e.memset(kcmp.ap, 0.0), writes=[kcmp])
        P.op("dve", lambda e: e.memset(vaug.ap, 0.0), writes=[vaug])
        P.op("dve", lambda e: e.memset(vaug.ap[0:127, 64:65], 1.0), reads=[vaug], writes=[vaug])
        P.dma(vaug.ap[:, 65:97], self.ovl_d.ap, reads=[vaug, self.ovl_d], writes=[vaug])
        kc3 = kc.ap.rearrange("p (n s) -> p n s", s=16)
        vc3 = vc.ap.rearrange("p (n s) -> p n s", s=16)
        psk = P.ps()
        psb = P.ps()
        for li in range(32):
            rhs = kc3[0:64, li // 16:li // 16 + 127, li % 16]
            P.op("pe", lambda e, li=li, rhs=rhs: e.matmul(psk.ap[0:64, 0:127], W3[0:64, li, :], rhs, start=(li == 0), stop=(li == 31)),
                 reads=[W, kc], writes=[psk])
        for li in range(32):
            P.op("pe", lambda e, li=li: e.matmul(psb.ap[0:64, 0:1], W3[0:64, li, :], peT.ap[0:64, li:li + 1], start=(li == 0), stop=(li == 31)),
                 reads=[W, peT], writes=[psb])
        bk = P.alloc("bk", 1)
        P.op("act", lambda e: e.copy(bk.ap[0:64], psb.ap[0:64, 0:1]), reads=[psb], writes=[bk])
        P.op("dve", lambda e: e.tensor_scalar(out=kcmp.ap[0:64, 0:127], in0=psk.ap[0:64, 0:127], scalar1=bk.ap[0:64, 0:1], scalar2=None, op0=ALU.add),
             reads=[psk, bk, kcmp], writes=[kcmp])
        psv = P.ps()
        psbv = P.ps()
        for li in range(32):
            P.op("pe", lambda e, li=li: e.matmul(psbv.ap[0:1, 0:64], peT.ap[0:64, li:li + 1], W3[0:64, 32 + li, :], start=(li == 0), stop=(li == 31)),
                 reads=[W, peT], writes=[psbv])
        bv = P.alloc("bv", 64)
        P.op("act", lambda e: e.copy(bv.ap[0:1], psbv.ap[0:1, 0:64]), reads=[psbv], writes=[bv])
        for li in range(32):
            lhs = vc3[0:64, li // 16:li // 16 + 127, li % 16]
            P.op("pe", lambda e, li=li, lhs=lhs: e.matmul(psv.ap[0:127, 0:64], lhs, W3[0:64, 32 + li, :], start=(li == 0), stop=False),
                 reads=[W, vc], writes=[psv])
        P.op("pe", lambda e: e.matmul(psv.ap[0:127, 0:64], self.ones.ap[0:1, 0:127], bv.ap[0:1, 0:64], start=False, stop=True),
             reads=[bv, self.ones], writes=[psv])
        P.op("act", lambda e: e.copy(vaug.ap[0:127, 0:64], psv.ap[0:127, 0:64]), reads=[psv, vaug], writes=[vaug])
        P.release(W, peT, kc, vc, bk, bv)
        ks = P.alloc("ksT", S)
        kw = P.alloc("kwT", S)
        P.dma(ks.ap[0:64], self.ZT.ap[self.ZKS:self.ZKS + 64, :], reads=[(self.ZT, 3)], writes=[ks])
        P.dma(kw.ap[0:64], self.ZT.ap[self.ZKW:self.ZKW + 64, :], reads=[(self.ZT, 3)], writes=[kw])
        e2 = P.alloc("e2", S)
        P.dma(e2.ap[0:32], self.e2_d.ap, reads=[self.e2_d], writes=[e2])
        tab = P.alloc("ntab", 4 * 512)
        P.dma(tab.ap, self.ntab_d.ap, reads=[self.ntab_d], writes=[tab])
        vaugs = []
        for c0 in (self.NVS, self.NVW):
            va = P.alloc("vaug", 16 * 65)
            va3 = va.ap.rearrange("p (t d) -> p t d", t=16)
            P.op("dve", lambda e, va=va: e.memset(va.ap, 1.0), writes=[va])
            P.dma(va3[:, :, 0:64], self.ZN.ap[:, c0:c0 + 64].rearrange("(t p) d -> p t d", p=128), reads=[self.ZN, va], writes=[va])
            vaugs.append((va, va3))
        gl = P.alloc("ngl", 16 * 12)
        gl3 = gl.ap.rearrange("p (t g) -> p t g", t=16)
        P.dma(gl3, self.ZN.ap[:, self.NG:self.NG + 12].rearrange("(t p) g -> p t g", p=128), reads=[self.ZN], writes=[gl])
        P.op("act", lambda e: e.activation(gl.ap, gl.ap, AF.Sigmoid), reads=[gl], writes=[gl])
        for qb in range(S // 128):
            self.nsa_qblock(l, qb, kcmp, vaug, ks, kw, e2, tab, vaugs, gl3, gl)
        P.release(kcmp, vaug, ks, kw, e2, tab, vaugs[0][0], vaugs[1][0], gl)

    def nsa_scores(self, ps_s, tabsl, tab, pso, vaug_ap, vaug_tile, first, width):
        P = self.P
        tmp = P.alloc("ntmp", 512)
        src_tiles = [ps_s, tab]
        P.op("dve", lambda e: e.scalar_tensor_tensor(out=tmp.ap, in0=ps_s.ap, scalar=0.125, in1=tabsl, op0=ALU.mult, op1=ALU.add),
             reads=src_tiles, writes=[tmp])
        P.op("act", lambda e: e.activation(tmp.ap, tmp.ap, AF.Exp), reads=[tmp], writes=[tmp])
        for h in range(4):
            P.op("pe", lambda e, h=h: e.matmul(pso.ap[:, h * width:(h + 1) * width], tmp.ap[:, h * 128:(h + 1) * 128], vaug_ap,
                                              start=(first and h == 0), stop=True, skip_group_check=True),
                 reads=[tmp, vaug_tile], writes=[pso])
        P.release(tmp)

    def nsa_qblock(self, l, qb, kcmp, vaug, ks, kw, e2, tab, vaugs, gl3, gl):
        P = self.P
        q0 = qb * 128
        q4 = P.alloc("q4", 512)
        P.dma(q4.ap[0:64].rearrange("p (h t) -> p h t", h=4), self.ZT.ap[0:256, q0:q0 + 128].rearrange("(h d) t -> d h t", d=64),
              reads=[(self.ZT, 0), (self.ZT, 1)], writes=[q4])
        bc = P.alloc("bc", 512)
        P.dma(bc.ap.rearrange("p (h t) -> p h t", h=4), self.biasc_d.ap[:, :, q0:q0 + 128], reads=[self.biasc_d], writes=[bc])
        ps_c = P.ps()
        P.op("pe", lambda e: e.matmul(ps_c.ap, kcmp.ap[0:64, :], q4.ap[0:64, :], start=True, stop=True), reads=[kcmp, q4], writes=[ps_c])
        ps_oc = P.ps(hold=True)
        self.nsa_scores(ps_c, bc.ap, bc, ps_oc, vaug.ap, vaug, True, 97)
        P.release(bc)
        oc3 = ps_oc.ap[:, 0:388].rearrange("p (h w) -> p h w", h=4)
        rdc = P.alloc("rdc", 4)
        P.op("dve", lambda e: e.tensor_scalar(out=rdc.ap, in0=oc3[:, :, 64], scalar1=1e-30, scalar2=None, op0=ALU.max), reads=[ps_oc], writes=[rdc])
        P.op("dve", lambda e: e.reciprocal(rdc.ap, rdc.ap), reads=[rdc], writes=[rdc])
        imp = P.alloc("imp", 32)
        P.dma(imp.ap, self.addt_d.ap[q0:q0 + 128, :], reads=[self.addt_d], writes=[imp])
        for h in range(4):
            P.op("dve", lambda e, h=h: e.scalar_tensor_tensor(out=imp.ap, in0=oc3[:, h, 65:97], scalar=rdc.ap[:, h:h + 1], in1=imp.ap,
                                                              op0=ALU.mult, op1=ALU.add), reads=[ps_oc, rdc, imp], writes=[imp])
        top8 = P.alloc("top8", 8)
        P.op("dve", lambda e: e.max(out=top8.ap, in_=imp.ap), reads=[imp], writes=[top8])
        P.op("dve", lambda e: e.tensor_scalar(out=imp.ap, in0=imp.ap, scalar1=top8.ap[:, 7:8], scalar2=1.0, op0=ALU.is_ge, op1=ALU.subtract),
             reads=[imp, top8], writes=[imp])
        ps_t = P.ps()
        P.op("pe", lambda e: e.transpose(ps_t.ap[0:32, 0:128], imp.ap, self.ident.ap), reads=[imp, self.ident], writes=[ps_t])
        ns4 = P.alloc("ns4", 512)
        P.op("dve", lambda e: e.tensor_copy(ns4.ap[0:32].rearrange("p (h t) -> p h t", h=4),
                                            ps_t.ap[0:32, 0:128].rearrange("p (o t) -> p o t", o=1).broadcast_to([32, 4, 128])),
             reads=[ps_t], writes=[ns4])
        P.release(imp, top8)
        ps_os = P.ps(hold=True)
        for kb in range(qb + 1):
            dlt = qb - kb
            ti = min(dlt, 2)
            ps_s = P.ps()
            P.op("pe", lambda e, kb=kb, ps_s=ps_s: e.matmul(ps_s.ap, ks.ap[0:64, kb * 128:(kb + 1) * 128], q4.ap[0:64, :], start=True, stop=False),
                 reads=[ks, q4], writes=[ps_s])
            P.op("pe", lambda e, kb=kb, ps_s=ps_s: e.matmul(ps_s.ap, e2.ap[0:32, kb * 128:(kb + 1) * 128], ns4.ap[0:32, :], start=False, stop=True),
                 reads=[e2, ns4], writes=[ps_s])
            self.nsa_scores(ps_s, tab.ap[:, ti * 512:(ti + 1) * 512], tab, ps_os, vaugs[0][1][:, kb, :], vaugs[0][0], kb == 0, 65)
        ps_ow = P.ps(hold=True)
        kb0 = max(0, qb - 4)
        for kb in range(kb0, qb + 1):
            dlt = qb - kb
            ti = (0, 1, 2, 2, 3)[dlt]
            ps_s = P.ps()
            P.op("pe", lambda e, kb=kb, ps_s=ps_s: e.matmul(ps_s.ap, kw.ap[0:64, kb * 128:(kb + 1) * 128], q4.ap[0:64, :], start=True, stop=True),
                 reads=[kw, q4], writes=[ps_s])
            self.nsa_scores(ps_s, tab.ap[:, ti * 512:(ti + 1) * 512], tab, ps_ow, vaugs[1][1][:, kb, :], vaugs[1][0], kb == kb0, 65)
        P.release(q4, ns4)
        acc = P.alloc("nacc", 256)
        acc3 = acc.ap.rearrange("p (h d) -> p h d", h=4)
        g3 = gl3[:, qb, :].rearrange("p (h b) -> p h b", b=3)
        for b, (pso, w) in enumerate(((ps_oc, 97), (ps_os, 65), (ps_ow, 65))):
            o3 = pso.ap[:, 0:4 * w].rearrange("p (h w) -> p h w", h=4)
            scl = P.alloc("nscl", 4)
            P.op("dve", lambda e, o3=o3, scl=scl: e.tensor_scalar(out=scl.ap, in0=o3[:, :, 64], scalar1=1e-30, scalar2=None, op0=ALU.max),
                 reads=[pso], writes=[scl])
            P.op("dve", lambda e, scl=scl: e.reciprocal(scl.ap, scl.ap), reads=[scl], writes=[scl])
            P.op("dve", lambda e, scl=scl, b=b: e.tensor_tensor(out=scl.ap, in0=scl.ap, in1=g3[:, :, b], op=ALU.mult), reads=[scl, gl], writes=[scl])
            sb = scl.ap.rearrange("p (h o) -> p h o", o=1).broadcast_to([128, 4, 64])
            if b == 0:
                P.op("dve", lambda e, o3=o3, sb=sb: e.tensor_tensor(out=acc3, in0=o3[:, :, 0:64], in1=sb, op=ALU.mult), reads=[pso, scl], writes=[acc])
            else:
                t2 = P.alloc("nt2", 256)
                t23 = t2.ap.rearrange("p (h d) -> p h d", h=4)
                P.op("dve", lambda e, o3=o3, sb=sb, t23=t23: e.tensor_tensor(out=t23, in0=o3[:, :, 0:64], in1=sb, op=ALU.mult), reads=[pso, scl], writes=[t2])
                P.op("dve", lambda e, t2=t2: e.tensor_tensor(out=acc.ap, in0=acc.ap, in1=t2.ap, op=ALU.add), reads=[acc, t2], writes=[acc])
                P.release(t2)
            P.release(scl)
        P.ps_free(ps_oc, ps_os, ps_ow)
        P.release(rdc)
        ps_o = P.ps()
        for c in range(2):
            P.op("pe", lambda e, c=c: e.transpose(ps_o.ap[:, c * 128:(c + 1) * 128], acc.ap[:, c * 128:(c + 1) * 128], self.ident.ap),
                 reads=[acc, self.ident], writes=[ps_o])
        stg = P.alloc("nstg", 256)
        P.op("act", lambda e: e.copy(stg.ap, ps_o.ap[:, 0:256]), reads=[ps_o], writes=[stg])
        P.dma(self.OBR.ap[0:256, q0:q0 + 128].rearrange("(c p) t -> p c t", p=128), stg.ap.rearrange("p (c t) -> p c t", c=2),
              reads=[stg], writes=[(self.OBR, 0), (self.OBR, 1)], q="gq")
        P.release(acc, stg)


    RC = 64

    def setup_rwkv(self):
        L = self.depth
        self.rwpar_d = self.din("rw_par", [L, 64, 43])
        self.rww2_d = self.din("rwkv_w2", [L, 32, 256])
        self.rwa2_d = self.din("rwkv_a2", [L, 32, 256])
        self.rwg2_d = self.din("rwkv_g2", [L, 64, 256])
        self.rwmask_d = self.din("rw_mask5", [64, 320])

    def rw_shift(self, z, n, mu_ap, par):
        P = self.P
        d = P.alloc("rwd", S)
        P.op("dve", lambda e: e.tensor_tensor(out=d.ap[0:n, 1:S], in0=z.ap[0:n, 0:S - 1], in1=z.ap[0:n, 1:S], op=ALU.subtract), reads=[z], writes=[d])
        P.op("dve", lambda e: e.tensor_scalar(out=d.ap[0:n, 0:1], in0=z.ap[0:n, 0:1], scalar1=-1.0, scalar2=None, op0=ALU.mult), reads=[z, d], writes=[d])
        P.op("dve", lambda e: e.scalar_tensor_tensor(out=z.ap[0:n], in0=d.ap[0:n], scalar=mu_ap, in1=z.ap[0:n], op0=ALU.mult, op1=ALU.add),
             reads=[d, z, par], writes=[z])
        P.release(d)

    def rwkv_branch(self, l):
        P = self.P
        par = P.alloc("rwpar", 43)
        P.dma(par.ap[0:64], self.rwpar_d.ap[l], reads=[self.rwpar_d], writes=[par])
        omk = P.alloc("rwomk", 4)
        P.op("dve", lambda e: e.tensor_scalar(out=omk.ap[0:64], in0=par.ap[0:64, 15 + 3 * 4:15 + 4 * 4], scalar1=-1.0, scalar2=1.0, op0=ALU.mult, op1=ALU.add),
             reads=[par], writes=[omk])
        lw = P.alloc("rwlw", 3 * 256)
        P.dma(lw.ap[0:32, 0:256], self.rww2_d.ap[l], reads=[self.rww2_d], writes=[(lw, 0)])
        P.dma(lw.ap[0:32, 256:512], self.rwa2_d.ap[l], reads=[self.rwa2_d], writes=[(lw, 1)])
        P.dma(lw.ap[0:64, 512:768], self.rwg2_d.ap[l], reads=[self.rwg2_d], writes=[(lw, 2)])
        m5 = P.alloc("rwm5", 320)
        P.dma(m5.ap[0:64], self.rwmask_d.ap, reads=[self.rwmask_d], writes=[m5])
        smask = P.alloc("rwsm", S)
        P.op("dve", lambda e: e.memset(smask.ap, 1.0), writes=[smask])
        P.op("dve", lambda e: e.memset(smask.ap.rearrange("p (n c) -> p n c", c=self.RC)[:, :, 0:1], 0.0), reads=[smask], writes=[smask])
        base = self.ZRW + 768
        twl = P.alloc("rwtwl", S)
        tal = P.alloc("rwtal", S)
        tgl = P.alloc("rwtgl", S)
        for t, r0, n, mc, fn in ((twl, base, 32, 12, AF.Tanh), (tal, base + 32, 32, 13, None), (tgl, base + 64, 64, 14, AF.Sigmoid)):
            self.rw_lora_in(t, r0, n, mc, fn, par)
        import os
        for h in range(4 if int(os.environ.get("RWDBG", "9")) >= 9 else 1):
            self.rwkv_head(l, h, par, omk, lw, m5, smask, twl, tal, tgl)
        P.release(par, omk, lw, m5, smask, twl, tal, tgl)

    def rw_lora_in(self, t, r0, n, mc, fn, par):
        P = self.P
        P.dma(t.ap[0:n], self.ZT.ap[r0:r0 + n, :], reads=[(self.ZT, r0 // 128)], writes=[t])
        self.rw_shift(t, n, par.ap[0:n, mc:mc + 1], par)
        if fn is not None:
            P.op("act", lambda e: e.activation(t.ap[0:n], t.ap[0:n], fn), reads=[t], writes=[t])

    def rwkv_head(self, l, h, par, omk, lw, m5, smask, twl, tal, tgl):
        P = self.P
        C = self.RC
        NCK = S // C
        pc = lambda which: par.ap[0:64, 15 + which * 4 + h:15 + which * 4 + h + 1]
        hc = slice(h * 64, (h + 1) * 64)
        r = P.alloc("rw_r", S)
        k = P.alloc("rw_k", S)
        v = P.alloc("rw_v", S)
        for j, t in enumerate((r, k, v)):
            r0 = self.ZRW + j * 256 + h * 64
            P.dma(t.ap[0:64], self.ZT.ap[r0:r0 + 64, :], reads=[(self.ZT, r0 // 128)], writes=[t])
            self.rw_shift(t, 64, par.ap[0:64, j * 4 + h:j * 4 + h + 1], par)
        a = P.alloc("rw_a", S)
        logw = P.alloc("rw_lw", S)
        kkn = P.alloc("rw_kkn", S)
        P.op("dve", lambda e: e.tensor_scalar(out=kkn.ap[0:64], in0=k.ap[0:64], scalar1=pc(2), scalar2=None, op0=ALU.mult), reads=[k, par], writes=[kkn])
        for tb in range(4):
            self.rw_prep_blk(h, tb, par, pc, lw, twl, tal, a, logw, kkn)
        kt = P.alloc("rw_kt", S)
        P.op("dve", lambda e: e.tensor_scalar(out=kt.ap[0:64], in0=a.ap[0:64], scalar1=pc(3), scalar2=omk.ap[0:64, h:h + 1], op0=ALU.mult, op1=ALU.add),
             reads=[a, par, omk], writes=[kt])
        P.op("dve", lambda e: e.tensor_tensor(out=kt.ap[0:64], in0=kt.ap[0:64], in1=k.ap[0:64], op=ALU.mult), reads=[kt, k], writes=[kt])
        P.release(k)
        P.op("dve", lambda e: e.tensor_tensor(out=a.ap[0:64], in0=a.ap[0:64], in1=kkn.ap[0:64], op=ALU.mult), reads=[a, kkn], writes=[a])
        bb = a
        import os
        dbg = int(os.environ.get("RWDBG", "9"))
        bonv = P.alloc("rw_bon", S)
        P.op("dve", lambda e: e.scalar_tensor_tensor(out=bonv.ap[0:64], in0=r.ap[0:64], scalar=pc(4), in1=kt.ap[0:64], op0=ALU.mult, op1=ALU.mult),
             reads=[r, kt, par], writes=[bonv])
        for tb in range(4):
            ps = P.ps()
            P.op("pe", lambda e, ps=ps, tb=tb: e.matmul(ps.ap[0:64, :], self.ones.ap[0:64, 0:64], bonv.ap[0:64, tb * 512:(tb + 1) * 512], start=True, stop=True),
                 reads=[bonv, self.ones], writes=[ps])
            P.op("dve", lambda e, ps=ps, tb=tb: e.tensor_tensor(out=bonv.ap[0:64, tb * 512:(tb + 1) * 512], in0=ps.ap[0:64, :], in1=v.ap[0:64, tb * 512:(tb + 1) * 512], op=ALU.mult),
                 reads=[ps, v, bonv], writes=[bonv])
        if dbg <= 1:
            return
        cum = P.alloc("rw_cum", S)
        P.op("dve", lambda e: e.tensor_tensor_scan(out=cum.ap[0:64], data0=smask.ap[0:64], data1=logw.ap[0:64], initial=0.0, op0=ALU.mult, op1=ALU.add),
             reads=[smask, logw], writes=[cum])
        eg = P.alloc("rw_eg", S)
        P.op("act", lambda e: e.activation(eg.ap[0:64], cum.ap[0:64], AF.Exp), reads=[cum], writes=[eg])
        gC = P.alloc("rw_gC", NCK)
        P.op("dve", lambda e: e.tensor_copy(gC.ap[0:64], eg.ap[0:64].rearrange("p (n c) -> p n c", c=C)[:, :, C - 1]), reads=[eg], writes=[gC])
        P.op("dve", lambda e: e.tensor_tensor(out=r.ap[0:64], in0=r.ap[0:64], in1=eg.ap[0:64], op=ALU.mult), reads=[r, eg], writes=[r])
        P.release(eg)
        RH = r
        P.op("dve", lambda e: e.tensor_tensor(out=logw.ap[0:64], in0=cum.ap[0:64], in1=logw.ap[0:64], op=ALU.subtract), reads=[cum, logw], writes=[logw])
        P.op("act", lambda e: e.activation(logw.ap[0:64], logw.ap[0:64], AF.Exp), reads=[logw], writes=[logw])
        P.op("dve", lambda e: e.tensor_tensor(out=kkn.ap[0:64], in0=kkn.ap[0:64], in1=logw.ap[0:64], op=ALU.mult), reads=[kkn, logw], writes=[kkn])
        P.release(logw)
        KH = kkn
        P.op("act", lambda e: e.activation(cum.ap[0:64], cum.ap[0:64], AF.Exp, scale=-1.0), reads=[cum], writes=[cum])
        P.op("dve", lambda e: e.tensor_tensor(out=kt.ap[0:64], in0=kt.ap[0:64], in1=cum.ap[0:64], op=ALU.mult), reads=[kt, cum], writes=[kt])
        P.op("dve", lambda e: e.tensor_tensor(out=bb.ap[0:64], in0=bb.ap[0:64], in1=cum.ap[0:64], op=ALU.mult), reads=[bb, cum], writes=[bb])
        P.release(cum)
        KG, BG = kt, bb
        if dbg <= 2:
            return
        tms = []
        for src in (v, KG, BG):
            tm = P.alloc("rw_tm", NCK * 64)
            tm3 = tm.ap.rearrange("p (n c) -> p n c", c=64)
            for g8 in range(NCK // 8):
                ps = P.ps()
                for j in range(8):
                    n = g8 * 8 + j
                    P.op("pe", lambda e, ps=ps, j=j, n=n, src=src: e.transpose(ps.ap[0:64, j * 64:(j + 1) * 64], src.ap[0:64, n * C:(n + 1) * C], self.ident.ap[0:64, 0:64]),
                         reads=[src, self.ident], writes=[ps])
                P.op("act", lambda e, ps=ps, g8=g8, tm=tm: e.copy(tm.ap[0:64, g8 * 512:(g8 + 1) * 512], ps.ap[0:64, :]), reads=[ps], writes=[(tm, g8)])
            tms.append((tm, tm3))
        P.release(v)
        (Vt, Vt3), (KGt, KGt3), (BGt, BGt3) = tms
        if dbg <= 3:
            return
        yT = P.alloc("rw_y", S)
        ST = P.alloc("rw_ST", 64)
        P.op("dve", lambda e: e.memset(ST.ap[0:64], 0.0), writes=[ST])
        G = 4
        for g0 in range(0, NCK, G):
            As, TTs = self.rw_group_prep(g0, G, KH, RH, KG, BG, m5)
            for gi in range(G):
                if dbg > 4:
                    self.rw_chunk(g0 + gi, As[gi], TTs[gi], KH, RH, Vt3, Vt, KGt3, KGt, BGt3, BGt, ST, gC, yT)
            P.release(*As)
            P.release(*TTs)
        P.release(RH, KH, KG, BG, Vt, KGt, BGt, ST, gC)
        self.rw_epilogue(l, h, yT, bonv, pc, par, lw, tgl)
        P.release(yT, bonv)

    def rw_prep_blk(self, h, tb, par, pc, lw, twl, tal, a, logw, kkn):
        P = self.P
        ts = slice(tb * 512, (tb + 1) * 512)
        ps = P.ps()
        P.op("pe", lambda e: e.matmul(ps.ap[0:64, :], lw.ap[0:32, h * 64:(h + 1) * 64], twl.ap[0:32, ts], start=True, stop=True), reads=[lw, twl], writes=[ps])
        P.op("act", lambda e: e.activation(logw.ap[0:64, ts], ps.ap[0:64, :], AF.Sigmoid, bias=pc(0), scale=1.0), reads=[ps, par], writes=[logw])
        P.op("dve", lambda e: e.tensor_scalar(out=logw.ap[0:64, ts], in0=logw.ap[0:64, ts], scalar1=-0.6065306597126334, scalar2=None, op0=ALU.mult),
             reads=[logw], writes=[logw])
        ps2 = P.ps()
        P.op("pe", lambda e: e.matmul(ps2.ap[0:64, :], lw.ap[0:32, 256 + h * 64:256 + (h + 1) * 64], tal.ap[0:32, ts], start=True, stop=True), reads=[lw, tal], writes=[ps2])
        P.op("act", lambda e: e.activation(a.ap[0:64, ts], ps2.ap[0:64, :], AF.Sigmoid, bias=pc(1), scale=1.0), reads=[ps2, par], writes=[a])
        sq = P.alloc("rw_sq", 512)
        P.op("act", lambda e: e.activation(sq.ap[0:64], kkn.ap[0:64, ts], AF.Square), reads=[kkn], writes=[sq])
        ps3 = P.ps()
        P.op("pe", lambda e: e.matmul(ps3.ap[0:64, :], self.ones.ap[0:64, 0:64], sq.ap[0:64], start=True, stop=True), reads=[sq, self.ones], writes=[ps3])
        P.op("act", lambda e: e.activation(sq.ap[0:64], ps3.ap[0:64, :], AF.Sqrt), reads=[ps3], writes=[sq])
        P.op("dve", lambda e: e.tensor_scalar(out=sq.ap[0:64], in0=sq.ap[0:64], scalar1=1e-12, scalar2=None, op0=ALU.max), reads=[sq], writes=[sq])
        P.op("dve", lambda e: e.reciprocal(sq.ap[0:64], sq.ap[0:64]), reads=[sq], writes=[sq])
        P.op("dve", lambda e: e.tensor_tensor(out=kkn.ap[0:64, ts], in0=kkn.ap[0:64, ts], in1=sq.ap[0:64], op=ALU.mult), reads=[kkn, sq], writes=[kkn])
        P.release(sq)

    def rw_group_prep(self, g0, G, KH, RH, KG, BG, m5):
        P = self.P
        C = self.RC
        As, Ms, Ps = [], [], []
        for gi in range(G):
            cs = slice((g0 + gi) * C, (g0 + gi + 1) * C)
            ps = P.ps()
            for j, (lh, rh) in enumerate(((KG, KH), (KG, RH), (BG, KH), (BG, RH), (KH, BG))):
                P.op("pe", lambda e, ps=ps, j=j, lh=lh, rh=rh, cs=cs: e.matmul(ps.ap[0:64, j * 64:(j + 1) * 64], lh.ap[0:64, cs], rh.ap[0:64, cs], start=True, stop=True),
                     reads=[lh, rh], writes=[ps])
            A = P.alloc("rw_A", 320)
            P.op("dve", lambda e, ps=ps, A=A: e.tensor_tensor(out=A.ap[0:64], in0=ps.ap[0:64, 0:320], in1=m5.ap[0:64], op=ALU.mult), reads=[ps, m5], writes=[A])
            As.append(A)
            Pm = P.alloc("rw_P", 64)
            P.op("dve", lambda e, A=A, Pm=Pm: e.tensor_tensor(out=Pm.ap[0:64], in0=self.ident.ap[0:64, 0:64], in1=A.ap[0:64, 128:192], op=ALU.subtract),
                 reads=[A, self.ident], writes=[Pm])
            Ps.append(Pm)
            Ms.append((A.ap[0:64, 128:192], A.ap[0:64, 256:320], A))
        import os
        nsteps = int(os.environ.get("RWSTEPS", "6"))
        for step in range(6):
            if step >= nsteps:
                for gi in range(G):
                    if Ms[gi][2] is not As[gi]:
                        P.release(Ms[gi][2])
                break
            pss = []
            for gi in range(G):
                M, MT, Mt = Ms[gi]
                ps = P.ps()
                if step < 5:
                    P.op("pe", lambda e, ps=ps, M=M, MT=MT: e.matmul(ps.ap[0:64, 0:64], MT, M, start=True, stop=True), reads=[Mt], writes=[ps])
                    P.op("pe", lambda e, ps=ps, M=M, MT=MT: e.matmul(ps.ap[0:64, 64:128], M, MT, start=True, stop=True), reads=[Mt], writes=[ps])
                if step > 0:
                    P.op("pe", lambda e, ps=ps, MT=MT, Pm=Ps[gi]: e.matmul(ps.ap[0:64, 128:192], MT, Pm.ap[0:64], start=True, stop=True), reads=[Mt, Ps[gi]], writes=[ps])
                pss.append(ps)
            for gi in range(G):
                ps = pss[gi]
                if step > 0:
                    P.op("dve", lambda e, ps=ps, Pm=Ps[gi]: e.tensor_tensor(out=Pm.ap[0:64], in0=Pm.ap[0:64], in1=ps.ap[0:64, 128:192], op=ALU.add),
                         reads=[ps, Ps[gi]], writes=[Ps[gi]])
                if step < 5:
                    Mn = P.alloc("rw_M", 128)
                    P.op("act", lambda e, ps=ps, Mn=Mn: e.copy(Mn.ap[0:64], ps.ap[0:64, 0:128]), reads=[ps], writes=[Mn])
                    old = Ms[gi][2]
                    Ms[gi] = (Mn.ap[0:64, 0:64], Mn.ap[0:64, 64:128], Mn)
                    if old is not As[gi]:
                        P.release(old)
                elif Ms[gi][2] is not As[gi]:
                    P.release(Ms[gi][2])
        return As, Ps

    def rw_chunk(self, n, A, TT, KH, RH, Vt3, Vt, KGt3, KGt, BGt3, BGt, ST, gC, yT):
        P = self.P
        C = self.RC
        cs = slice(n * C, (n + 1) * C)
        psx = P.ps()
        P.op("pe", lambda e: e.matmul(psx.ap[0:64, 0:64], KH.ap[0:64, cs], ST.ap[0:64], start=True, stop=False), reads=[KH, ST], writes=[psx])
        P.op("pe", lambda e: e.matmul(psx.ap[0:64, 0:64], A.ap[0:64, 0:64], Vt3[0:64, n, :], start=False, stop=True), reads=[A, Vt], writes=[psx])
        nx = P.alloc("rw_nx", 64)
        P.op("act", lambda e: e.mul(nx.ap[0:64], psx.ap[0:64, 0:64], -1.0), reads=[psx], writes=[nx])
        psu = P.ps()
        P.op("pe", lambda e: e.matmul(psu.ap[0:64, 0:64], TT.ap[0:64], nx.ap[0:64], start=True, stop=True), reads=[TT, nx], writes=[psu])
        U = P.alloc("rw_U", 64)
        P.op("act", lambda e: e.copy(U.ap[0:64], psu.ap[0:64, 0:64]), reads=[psu], writes=[U])
        psy = P.ps()
        P.op("pe", lambda e: e.matmul(psy.ap[0:64, 0:64], ST.ap[0:64], RH.ap[0:64, cs], start=True, stop=False), reads=[ST, RH], writes=[psy])
        P.op("pe", lambda e: e.matmul(psy.ap[0:64, 0:64], Vt3[0:64, n, :], A.ap[0:64, 64:128], start=False, stop=False), reads=[Vt, A], writes=[psy])
        P.op("pe", lambda e: e.matmul(psy.ap[0:64, 0:64], U.ap[0:64], A.ap[0:64, 192:256], start=False, stop=True), reads=[U, A], writes=[psy])
        P.op("act", lambda e: e.copy(yT.ap[0:64, cs], psy.ap[0:64, 0:64]), reads=[psy], writes=[(yT, n)])
        pss = P.ps()
        P.op("pe", lambda e: e.matmul(pss.ap[0:64, 0:64], KGt3[0:64, n, :], Vt3[0:64, n, :], start=True, stop=False), reads=[KGt, Vt], writes=[pss])
        P.op("pe", lambda e: e.matmul(pss.ap[0:64, 0:64], BGt3[0:64, n, :], U.ap[0:64], start=False, stop=True), reads=[BGt, U], writes=[pss])
        P.op("dve", lambda e: e.tensor_tensor(out=ST.ap[0:64], in0=ST.ap[0:64], in1=pss.ap[0:64, 0:64], op=ALU.add), reads=[ST, pss], writes=[ST])
        P.op("dve", lambda e: e.tensor_scalar(out=ST.ap[0:64], in0=ST.ap[0:64], scalar1=gC.ap[0:64, n:n + 1], scalar2=None, op0=ALU.mult),
             reads=[ST, gC], writes=[ST])
        P.release(nx, U)

    def rw_epilogue(self, l, h, yT, bonv, pc, par, lw, tgl):
        P = self.P
        n = 512
        for Q in range(S // n):
            self.rw_epi_blk(h, Q, n, yT, bonv, pc, par, lw, tgl)

    def rw_epi_blk(self, h, Q, n, yT, bonv, pc, par, lw, tgl):
        P = self.P
        ts = slice(Q * n, (Q + 1) * n)
        o = P.alloc("ro", n)
        sq = P.alloc("rsq", n)
        P.op("act", lambda e: e.activation(sq.ap[0:64], yT.ap[0:64, ts], AF.Square), reads=[yT], writes=[sq])
        p1 = P.ps()
        p2 = P.ps()
        P.op("pe", lambda e: e.matmul(p1.ap[0:64, :], self.ones.ap[0:64, 0:64], yT.ap[0:64, ts], start=True, stop=True), reads=[yT, self.ones], writes=[p1])
        P.op("pe", lambda e: e.matmul(p2.ap[0:64, :], self.ones.ap[0:64, 0:64], sq.ap[0:64], start=True, stop=True), reads=[sq, self.ones], writes=[p2])
        mean = P.alloc("rmean", n)
        P.op("dve", lambda e: e.tensor_scalar(out=mean.ap[0:64], in0=p1.ap[0:64, :], scalar1=1.0 / 64, scalar2=None, op0=ALU.mult), reads=[p1], writes=[mean])
        P.op("dve", lambda e: e.tensor_tensor(out=o.ap[0:64], in0=yT.ap[0:64, ts], in1=mean.ap[0:64], op=ALU.subtract), reads=[yT, mean], writes=[o])
        P.op("dve", lambda e: e.tensor_tensor(out=mean.ap[0:64], in0=mean.ap[0:64], in1=mean.ap[0:64], op=ALU.mult), reads=[mean], writes=[mean])
        P.op("dve", lambda e: e.scalar_tensor_tensor(out=sq.ap[0:64], in0=p2.ap[0:64, :], scalar=1.0 / 64, in1=mean.ap[0:64],
                                                     op0=ALU.mult, op1=ALU.subtract), reads=[p2, mean], writes=[sq])
        P.op("act", lambda e: e.activation(sq.ap[0:64], sq.ap[0:64], AF.Sqrt, bias=self.epst.ap[0:64, 2:3], scale=1.0), reads=[sq, self.epst], writes=[sq])
        P.op("dve", lambda e: e.reciprocal(sq.ap[0:64], sq.ap[0:64]), reads=[sq], writes=[sq])
        P.op("dve", lambda e: e.tensor_tensor(out=o.ap[0:64], in0=o.ap[0:64], in1=sq.ap[0:64], op=ALU.mult), reads=[o, sq], writes=[o])
        P.op("dve", lambda e: e.tensor_scalar(out=o.ap[0:64], in0=o.ap[0:64], scalar1=pc(5), scalar2=pc(6), op0=ALU.mult, op1=ALU.add), reads=[o, par], writes=[o])
        P.op("dve", lambda e: e.tensor_tensor(out=o.ap[0:64], in0=o.ap[0:64], in1=bonv.ap[0:64, ts], op=ALU.add), reads=[o, bonv], writes=[o])
        pg = P.ps()
        P.op("pe", lambda e: e.matmul(pg.ap[0:64, :], lw.ap[0:64, 512 + h * 64:512 + (h + 1) * 64], tgl.ap[0:64, ts], start=True, stop=True), reads=[lw, tgl], writes=[pg])
        P.op("dve", lambda e: e.tensor_tensor(out=o.ap[0:64], in0=o.ap[0:64], in1=pg.ap[0:64, :], op=ALU.mult), reads=[o, pg], writes=[o])
        P.dma(self.OBR.ap[512 + h * 64:512 + (h + 1) * 64, ts], o.ap[0:64], reads=[o], writes=[(self.OBR, 4 + h // 2)], q="gq")
        P.release(o, sq, mean)

    def setup_xa(self):
        P = self.P
        L = self.depth
        self.mem_d = self.din("mem", [MEM, D])
        self.wq_d = self.din("xa_wq", [L, D, D])
        self.wkv_d = self.din("xa_wkv", [L, D, 2 * D])
        self.wo_d = self.din("xa_wo", [L, D, D])
        self.memT = P.alloc("memT", NCH * MEM)
        memT3 = self.memT.ap.rearrange("p (c t) -> p c t", c=NCH)
        for tt in range(MEM // 128):
            self.xa_load_mem(tt, memT3)

    def xa_load_mem(self, tt, memT3):
        P = self.P
        xin = P.alloc("xin", D)
        P.dma(xin.ap, self.mem_d.ap[tt * 128:(tt + 1) * 128, :], reads=[self.mem_d], writes=[xin])
        for half in range(2):
            ps = P.ps()
            for j in range(4):
                c = half * 4 + j
                P.op("pe", lambda e, ps=ps, j=j, c=c: e.transpose(ps.ap[:, j * 128:(j + 1) * 128], xin.ap[:, c * 128:(c + 1) * 128], self.ident.ap),
                     reads=[xin, self.ident], writes=[ps])
            dst = memT3[:, half * 4:half * 4 + 4, tt * 128:(tt + 1) * 128]
            P.op("act", lambda e, ps=ps, dst=dst: e.copy(dst, ps.ap.rearrange("p (c t) -> p c t", c=4)), reads=[ps], writes=[self.memT])
        P.release(xin)

    def xattn(self, l):
        P = self.P
        mnT = P.alloc("mnT", NCH * MEM)
        mn3 = mnT.ap.rearrange("p (c t) -> p c t", c=NCH)
        self.norm_to(l, 6, self.memT.ap.rearrange("p (c t) -> p c t", c=NCH), self.memT, MEM, mn3, mnT)
        kT = P.alloc("xkT", NCH * MEM)
        kT3 = kT.ap.rearrange("p (c t) -> p c t", c=NCH)
        vN = P.alloc("xv", 2 * D)
        vN3 = vN.ap.rearrange("p (m n) -> p m n", m=2)
        for f in range(NCH):
            self.xa_k(l, f, mn3, mnT, kT3, kT)
        for half in range(2):
            self.xa_v(l, half, mn3, mnT, vN3, vN)
        P.release(mnT)
        TB = 512
        for tb in range(S // TB):
            self.xa_blk(l, tb, TB, kT3, kT, vN3, vN)
        P.release(kT, vN)

    def xa_k(self, l, f, mn3, mnT, kT3, kT):
        P = self.P
        w = P.alloc("xw", NCH * 128)
        w3 = w.ap.rearrange("p (k n) -> p k n", k=NCH)
        P.dma(w3, self.wkv_d.ap[l][:, f * 128:(f + 1) * 128].rearrange("(k p) n -> p k n", p=128), reads=[self.wkv_d], writes=[w])
        ps = P.ps()
        for k in range(NCH):
            P.op("pe", lambda e, k=k: e.matmul(ps.ap[:, 0:MEM], w3[:, k, :], mn3[:, k, :], start=(k == 0), stop=(k == NCH - 1)),
                 reads=[w, mnT], writes=[ps])
        P.op("act", lambda e: e.copy(kT3[:, f, :], ps.ap[:, 0:MEM]), reads=[ps], writes=[(kT, f)])
        P.release(w)

    def xa_v(self, l, half, mn3, mnT, vN3, vN):
        P = self.P
        w = P.alloc("xwv", NCH * 512)
        w3 = w.ap.rearrange("p (k n) -> p k n", k=NCH)
        c0 = D + half * 512
        P.dma(w3, self.wkv_d.ap[l][:, c0:c0 + 512].rearrange("(k p) n -> p k n", p=128), reads=[self.wkv_d], writes=[w])
        for mc in range(2):
            ps = P.ps()
            for k in range(NCH):
                P.op("pe", lambda e, k=k, ps=ps, mc=mc: e.matmul(ps.ap, mn3[:, k, mc * 128:(mc + 1) * 128], w3[:, k, :], start=(k == 0), stop=(k == NCH - 1)),
                     reads=[w, mnT], writes=[ps])
            P.op("act", lambda e, ps=ps, mc=mc: e.copy(vN3[:, mc, half * 512:(half + 1) * 512], ps.ap), reads=[ps], writes=[(vN, (mc, half))])
        P.release(w)

    def lin8(self, w_ap, src3, src_tile, dst3, dst_tile, TB):
        P = self.P
        for f in range(NCH):
            self.lin8_f(w_ap, f, src3, src_tile, dst3, dst_tile, TB)

    def lin8_f(self, w_ap, f, src3, src_tile, dst3, dst_tile, TB):
        P = self.P
        w = P.alloc("xw", NCH * 128 // 2)
        w3 = w.ap.bitcast(BF16).rearrange("p (k n) -> p k n", k=NCH)
        P.dma(w3, w_ap[:, f * 128:(f + 1) * 128].rearrange("(k p) n -> p k n", p=128), reads=[], writes=[w], q="gq")
        ps = P.ps()
        for k in range(NCH):
            P.op("pe", lambda e, k=k: e.matmul(ps.ap[:, 0:TB], w3[:, k, :], src3[:, k, :], start=(k == 0), stop=(k == NCH - 1)),
                 reads=[w, src_tile], writes=[ps])
        P.op("act", lambda e: e.copy(dst3[:, f, :], ps.ap[:, 0:TB]), reads=[ps], writes=[(dst_tile, f)])
        P.release(w)

    def xa_blk(self, l, tb, TB, kT3, kT, vN3, vN):
        P = self.P
        t0 = tb * TB
        hblk = P.alloc("hblk", NCH * TB // 2)
        h3 = hblk.ap.bitcast(BF16).rearrange("p (c t) -> p c t", c=NCH)
        self.norm_to(l, 2, self.xT3[:, :, t0:t0 + TB], self.xT, TB, h3, hblk)
        qT = P.alloc("xq", NCH * TB)
        q3 = qT.ap.rearrange("p (c t) -> p c t", c=NCH)
        self.lin8(self.wq_d.ap[l], h3, hblk, q3, qT, TB)
        P.release(hblk)
        at = P.alloc("xat", NCH * TB // 2)
        at3 = at.ap.bitcast(BF16).rearrange("p (c t) -> p c t", c=NCH)
        for h in range(4):
            self.xa_head(h, TB, kT3, kT, vN3, vN, q3, qT, at3, at)
        P.release(qT)
        yblk = P.alloc("yblk", NCH * TB)
        y3 = yblk.ap.rearrange("p (c t) -> p c t", c=NCH)
        self.lin8(self.wo_d.ap[l], at3, at, y3, yblk, TB)
        P.release(at)
        self.post_norm_residual(l, 3, t0, TB, yblk)
        P.release(yblk)

    def xa_head(self, h, TB, kT3, kT, vN3, vN, q3, qT, at3, at):
        P = self.P
        pT = P.alloc("xp", 2 * TB)
        p3 = pT.ap.rearrange("p (m t) -> p m t", m=2)
        for mc in range(2):
            ps = P.ps()
            for c in range(2):
                P.op("pe", lambda e, ps=ps, c=c, mc=mc: e.matmul(ps.ap[:, 0:TB], kT3[:, 2 * h + c, mc * 128:(mc + 1) * 128], q3[:, 2 * h + c, :],
                                                             start=(c == 0), stop=(c == 1)), reads=[kT, qT], writes=[ps])
            P.op("act", lambda e, ps=ps, mc=mc: e.activation(p3[:, mc, :], ps.ap[:, 0:TB], AF.Exp, scale=1.0 / 16.0), reads=[ps], writes=[(pT, mc)])
        psd = P.ps()
        for mc in range(2):
            P.op("pe", lambda e, mc=mc: e.matmul(psd.ap[:, 0:TB], self.ones.ap, p3[:, mc, :], start=(mc == 0), stop=(mc == 1)),
                 reads=[pT, self.ones], writes=[psd])
        rden = P.alloc("xrd", TB)
        P.op("dve", lambda e: e.reciprocal(rden.ap, psd.ap[:, 0:TB]), reads=[psd], writes=[rden])
        for dc in range(2):
            pso = P.ps()
            for mc in range(2):
                P.op("pe", lambda e, pso=pso, mc=mc, dc=dc: e.matmul(pso.ap[:, 0:TB], vN3[:, mc, h * 256 + dc * 128:h * 256 + (dc + 1) * 128], p3[:, mc, :],
                                                                 start=(mc == 0), stop=(mc == 1)), reads=[vN, pT], writes=[pso])
            P.op("dve", lambda e, pso=pso, dc=dc: e.tensor_tensor(out=at3[:, 2 * h + dc, :], in0=pso.ap[:, 0:TB], in1=rden.ap, op=ALU.mult),
                 reads=[pso, rden], writes=[(at, 2 * h + dc)])
        P.release(pT, rden)

    def build(self):
        self.setup()
        self.setup_eps()
        if "mix" in self.phases:
            self.setup_mixer()
            if "nsa" in self.branches:
                self.setup_nsa()
            if "rwkv" in self.branches:
                self.setup_rwkv()
        self.load_x()
        if "xa" in self.phases:
            self.setup_xa()
        for l in range(self.depth):
            if "mix" in self.phases:
                self.mixer(l)
            if "xa" in self.phases:
                self.xattn(l)
            if "mlp" in self.phases:
                self.mlp(l)
        fin = self.store_x()
        self.P.finalize(fin)
        return self.nc


def host_inputs(inputs, depth=DEPTH):
    f = np.float32
    g = np.stack([np.asarray(inputs[k], f)[:depth] for k in
                  ("ln_mix_pre", "ln_mix_post", "ln_xa_pre", "ln_xa_post", "ln_mlp_pre", "ln_mlp_post", "ln_mem")], axis=1)
    gains = np.ascontiguousarray(g.reshape(depth, 7, NCH, 128).transpose(3, 0, 1, 2).reshape(128, depth * 7 * NCH))
    common = {
        "ident": np.eye(128, dtype=f),
        "gains": gains,
        "mlp_w1": np.ascontiguousarray(np.asarray(inputs["mlp_w1"], f)[:depth]),
        "mlp_w2": np.ascontiguousarray(np.asarray(inputs["mlp_w2"], f)[:depth]),
    }
    w_in = np.asarray(inputs["w_in"], f)[:depth]
    q = w_in[:, :, 0:256]
    kv = w_in[:, :, 256:640]
    gl = w_in[:, :, 640:652]
    ret = w_in[:, :, 652:1676]
    rw = w_in[:, :, 1676:2572]
    cv = w_in[:, :, 2572:3340]

    def swp(w):
        w4 = w.reshape(w.shape[0], w.shape[1], 4, 2, 32)
        return w4[:, :, :, ::-1, :].reshape(w.shape)
    rq, rk, rv, rgt = ret[..., 0:256], ret[..., 256:512], ret[..., 512:768], ret[..., 768:1024]
    common["w_T"] = np.ascontiguousarray(np.concatenate(
        [q, kv[..., 0:64], kv[..., 64:128], kv[..., 128:192], kv[..., 256:320], rq, swp(rq), rk, swp(rk), rgt, rw, cv], axis=-1))
    common["w_N"] = np.ascontiguousarray(np.concatenate([kv[..., 192:256], kv[..., 320:384], gl, rv], axis=-1))
    common["w_gate"] = np.ascontiguousarray(w_in[:, :, 3340:7436])
    common["w_branch"] = np.ascontiguousarray(np.asarray(inputs["w_branch"], f)[:depth])
    common["w_mix_out"] = np.ascontiguousarray(np.asarray(inputs["w_mix_out"], f)[:depth])
    cw = np.asarray(inputs["conv_w"], f)[:depth]
    common["conv_wT"] = np.ascontiguousarray(cw.reshape(depth, 3, 2, 128).transpose(0, 3, 2, 1).reshape(depth, 128, 6))
    common["ret_gT"] = np.ascontiguousarray(np.asarray(inputs["ret_norm_g"], f)[:depth].reshape(depth, 4, 64).transpose(0, 2, 1))
    half = 32
    inv_freq = (10000.0 ** (-np.arange(half, dtype=np.float32) / half)).astype(f)
    ang = np.arange(S, dtype=f)[:, None] * inv_freq[None, :]
    cosT = np.concatenate([np.cos(ang), np.cos(ang)], axis=1).T
    sinT = np.concatenate([-np.sin(ang), np.sin(ang)], axis=1).T
    common["rot_tab"] = np.ascontiguousarray(np.stack([cosT, sinT], axis=1).astype(f))
    kk = np.arange(128)[:, None]
    qq = np.arange(512)[None, :]
    dec = np.zeros((128, 4, 2, 512), np.float64)
    for h in range(4):
        lg = np.log(1.0 - 2.0 ** (-5.0 - h))
        full = np.exp(lg * (qq - kk)) * 0.125
        dec[:, h, 0] = full
        dec[:, h, 1] = np.where(qq >= kk, full, 0.0)
    common["ret_dec"] = np.ascontiguousarray(dec.reshape(128, -1).astype(f))
    import math
    common["nsa_cmp_w"] = np.ascontiguousarray(np.asarray(inputs["nsa_cmp_w"], f)[:depth])
    common["nsa_peT"] = np.ascontiguousarray(np.asarray(inputs["nsa_cmp_pe"], f)[:depth].transpose(0, 2, 1))
    rb = np.asarray(inputs["rel_bias"], f)

    def bucket(dist):
        n = np.maximum(dist, 0)
        nf = np.maximum(n, 1).astype(np.float32)
        large = 16 + (np.log(nf / np.float32(16)) / np.float32(math.log(128 / 16)) * np.float32(16)).astype(np.int32)
        large = np.minimum(large, 31)
        return np.where(n < 16, n, large)
    NEGB = np.float32(-30000.0)
    tpos = np.arange(S)
    nidx = np.arange(128)
    d_c = tpos[None, :] - (16 * nidx[:, None] + 31)
    bc = rb[bucket(d_c)]
    bc = np.where(((d_c >= 0) & (nidx[:, None] < 127))[:, :, None], bc, NEGB).transpose(0, 2, 1)
    common["nsa_biasc"] = np.ascontiguousarray(bc.astype(f))
    kk = np.arange(128)[:, None]
    qq = np.arange(128)[None, :]
    tabs = np.zeros((128, 4, 4, 128), f)
    d0 = qq - kk
    tabs[:, 0] = np.where((d0 >= 0)[:, None, :], rb[bucket(d0)].transpose(0, 2, 1), NEGB)
    tabs[:, 1] = rb[bucket(d0 + 128)].transpose(0, 2, 1)
    tabs[:, 2] = rb[31][None, :, None]
    tabs[:, 3] = np.where((d0 < 0)[:, None, :], rb[31][None, :, None], NEGB)
    common["nsa_tab"] = np.ascontiguousarray(tabs.reshape(128, -1))
    keys = np.arange(S)
    common["nsa_e2"] = np.ascontiguousarray(((keys[None, :] // 64) == np.arange(32)[:, None]).astype(f) * f(240000.0))
    cs = np.arange(127) * 16
    ss = np.arange(32) * 64
    ov = np.clip(np.minimum(cs[:, None] + 32, ss[None, :] + 64) - np.maximum(cs[:, None], ss[None, :]), 0, None).astype(f) / f(32)
    common["nsa_ovl"] = np.ascontiguousarray(np.concatenate([ov, np.zeros((1, 32), f)], axis=0))
    cur = tpos // 64
    blk = np.arange(32)
    forced = (blk[None, :] == 0) | (blk[None, :] == cur[:, None]) | (blk[None, :] == cur[:, None] - 1)
    addt = np.where(blk[None, :] <= cur[:, None], np.where(forced, f(1e4), f(0.0)), f(-1e30)).astype(f)
    common["nsa_addtab"] = np.ascontiguousarray(addt)
    mu = np.asarray(inputs["rwkv_mu"], f)[:depth]
    par = np.zeros((depth, 64, 43), f)
    par[:, :, 0:12] = mu[:, 0:768].reshape(depth, 3, 4, 64).transpose(0, 3, 1, 2).reshape(depth, 64, 12)
    par[:, 0:32, 12] = mu[:, 768:800]
    par[:, 0:32, 13] = mu[:, 800:832]
    par[:, 0:64, 14] = mu[:, 832:896]
    for wi, nm in enumerate(("rwkv_w0", "rwkv_a0", "rwkv_k_k", "rwkv_k_a", "rwkv_r_k", "rwkv_ln_g", "rwkv_ln_b")):
        par[:, :, 15 + wi * 4:15 + (wi + 1) * 4] = np.asarray(inputs[nm], f)[:depth].reshape(depth, 4, 64).transpose(0, 2, 1)
    common["rw_par"] = par
    for nm in ("rwkv_w2", "rwkv_a2", "rwkv_g2"):
        common[nm] = np.ascontiguousarray(np.asarray(inputs[nm], f)[:depth])
    ii = np.arange(64)[:, None]
    tt2 = np.arange(64)[None, :]
    mu_ = (ii < tt2).astype(f)
    mui = (ii <= tt2).astype(f)
    ml = (ii > tt2).astype(f)
    common["rw_mask5"] = np.ascontiguousarray(np.concatenate([mu_, mui, mu_, mui, ml], axis=1))
    for k in ("xa_wq", "xa_wkv", "xa_wo"):
        common[k] = np.ascontiguousarray(np.asarray(inputs[k], f)[:depth])
    maps = []
    for b in range(8):
        m = dict(common)
        m["mem"] = np.ascontiguousarray(np.asarray(inputs["mem"], f)[b])
        m["x"] = np.ascontiguousarray(np.asarray(inputs["x"], f)[b])
        maps.append(m)
    return maps


def kernel(**inputs):
    bld = Builder()
    nc = bld.build()
    maps = host_inputs(inputs)
    maps = [{k: v for k, v in m.items() if k in bld.inp} for m in maps]
    res = run_bass_kernel_spmd(nc, maps, core_ids=list(range(8)))
    return np.stack([np.asarray(r["out"], np.float32) for r in res.results], axis=0)
```

```python
import numpy as np
import concourse.bass as bass
import concourse.mybir as mybir
from concourse.bass_utils import run_bass_kernel_spmd

F32 = mybir.dt.float32
BF16 = mybir.dt.bfloat16
AF = mybir.ActivationFunctionType
ALU = mybir.AluOpType
AX = mybir.AxisListType

D = 1024
S = 2048
DEPTH = 4
MEM = 256
NCH = D // 128
HD = 64
DFF = 4096


class Op:
    __slots__ = ("eng", "emit", "deps", "idx", "flag", "val", "dma", "slot", "dval", "prev_slot_op")

    def __init__(self, eng, emit, dma=False):
        self.eng = eng
        self.emit = emit
        self.deps = []
        self.idx = -1
        self.flag = False
        self.val = 0
        self.dma = dma
        self.slot = -1
        self.dval = 0
        self.prev_slot_op = None


class TState:
    __slots__ = ("w", "r")

    def __init__(self):
        self.w = None
        self.r = {}

    def add_reader(self, o):
        k = o.slot if o.dma else o.eng
        p = self.r.get(k)
        if p is None or (o.dval > p.dval if o.dma else o.idx > p.idx):
            self.r[k] = o

    def all_ops(self):
        o = list(self.r.values())
        if self.w is not None:
            o.append(self.w)
        return o


class Tile:
    def __init__(self, name, ap, start=0, size=0):
        self.name = name
        self.ap = ap
        self.st = {}
        self.start = start
        self.size = size

    def __getitem__(self, k):
        return self.ap[k]


ND = 16


class Prog:
    SMALL = 1100
    COMPUTE = ("pe", "act", "dve", "pool")
    QUEUES = ("sp", "gq")

    def __init__(self, nc, arena_cols):
        self.nc = nc
        self.ops = {e: [] for e in ("pe", "act", "dve", "pool", "sp")}
        self.ndma = {"sp": 0, "gq": 0}
        self.slot_last = {"sp": [None] * ND, "gq": [None] * ND}
        self.arena = nc.alloc_sbuf_tensor("arena", [128, arena_cols], F32)
        self.free = [[0, arena_cols, []]]
        self.ncols = arena_cols
        self.cursor = arena_cols
        self.psum = [Tile(f"ps{i}", nc.alloc_psum_tensor(f"ps{i}", [128, 512], F32).ap()) for i in range(8)]
        self.ps_rr = 0
        self.held = set()

    def _take(self, i, name, cols, from_end):
        st, sz, pend = self.free[i]
        a = st + sz - cols if from_end else st
        t = Tile(name, self.arena[:, a:a + cols], a, cols)
        if pend:
            s = TState()
            for o in pend:
                s.add_reader(o)
            t.st[None] = s
        rest = []
        if a > st:
            rest.append([st, a - st, pend])
        if a + cols < st + sz:
            rest.append([a + cols, st + sz - a - cols, pend])
        self.free[i:i + 1] = rest
        return t

    def alloc(self, name, cols):
        if cols <= self.SMALL:
            for attempt in range(2):
                for i in range(len(self.free) - 1, -1, -1):
                    st, sz, _ = self.free[i]
                    if sz >= cols and st + cols <= self.cursor:
                        end = min(st + sz, self.cursor)
                        if end - st >= cols:
                            if end < st + sz:
                                pend = self.free[i][2]
                                self.free[i:i + 1] = [[st, end - st, pend], [end, st + sz - end, pend]]
                            t = self._take(i, name, cols, True)
                            self.cursor = t.start
                            return t
                self.cursor = self.ncols
        for i, (st, sz, pend) in enumerate(self.free):
            if sz >= cols:
                return self._take(i, name, cols, False)
        raise RuntimeError(f"SBUF arena full allocating {name} ({cols} cols); free={[(a, b) for a, b, _ in self.free]}")

    def release(self, *tiles):
        for t in tiles:
            tmp = TState()
            for s in t.st.values():
                for o in s.all_ops():
                    tmp.add_reader(o)
            self.free.append([t.start, t.size, list(tmp.r.values())])
        self.free.sort(key=lambda x: x[0])
        m = []
        for blk in self.free:
            if m and m[-1][0] + m[-1][1] == blk[0]:
                m[-1][1] += blk[1]
                tmp = TState()
                for o in m[-1][2] + blk[2]:
                    tmp.add_reader(o)
                m[-1][2] = list(tmp.r.values())
            else:
                m.append(blk)
        self.free = m

    def dram(self, name, shape, kind="Internal"):
        h = self.nc.dram_tensor(name, list(shape), F32, kind=kind)
        return Tile(name, h.ap())

    def ps(self, hold=False):
        while True:
            t = self.psum[self.ps_rr % 8]
            self.ps_rr += 1
            if t.name not in self.held:
                break
        if hold:
            self.held.add(t.name)
        return t

    def ps_free(self, *ts):
        for t in ts:
            self.held.discard(t.name)

    @staticmethod
    def _norm(x):
        if isinstance(x, tuple):
            return (x[0], None) if x[0].name.startswith("ps") else x
        return (x, None)

    def _track(self, op, reads, writes):
        pr = [x for x in reads if self._norm(x)[0].name.startswith("ps")]
        if pr:
            reads = [x for x in reads if not self._norm(x)[0].name.startswith("ps")]
            writes = list(writes) + [x for x in pr if all(self._norm(x)[0] is not self._norm(w)[0] for w in writes)]
        deps = []
        for x in reads:
            t, k = self._norm(x)
            for kk, s in t.st.items():
                if k is None or kk is None or kk == k:
                    if s.w is not None:
                        deps.append(s.w)
        for x in writes:
            t, k = self._norm(x)
            for kk, s in t.st.items():
                if k is None or kk is None or kk == k:
                    deps.extend(s.all_ops())
        for x in reads:
            t, k = self._norm(x)
            s = t.st.get(k)
            if s is None:
                s = t.st[k] = TState()
            s.add_reader(op)
        for x in writes:
            t, k = self._norm(x)
            if k is None:
                t.st.clear()
            s = t.st[k] = TState()
            s.w = op
        seen = set()
        for d in deps:
            if d is op or id(d) in seen:
                continue
            if op.eng == "pe" and d.eng == "pe" and not d.dma:
                continue
            seen.add(id(d))
            d.flag = True
            op.deps.append(d)

    def op(self, eng, emit, reads=(), writes=()):
        o = Op(eng, emit)
        o.idx = len(self.ops[eng])
        self._track(o, reads, writes)
        self.ops[eng].append(o)
        return o

    def dma(self, out_ap, in_ap, reads=(), writes=(), q="sp"):
        q = "gq" if out_ap.dtype != in_ap.dtype else "sp"
        eng = "sp" if q == "sp" else "pool"
        o = Op(eng, None, dma=True)
        o.emit = lambda e: e.dma_start(out=out_ap, in_=in_ap)
        n = self.ndma[q]
        self.ndma[q] = n + 1
        o.slot = (q, n % ND)
        o.dval = 16 * (n // ND + 1)
        o.prev_slot_op = self.slot_last[q][n % ND]
        self.slot_last[q][n % ND] = o
        o.idx = len(self.ops[eng])
        self._track(o, reads, writes)
        self.ops[eng].append(o)
        return o

    def finalize(self, final_wait_ops):
        nc = self.nc
        esem = {e: nc.alloc_semaphore(f"sem_{e}") for e in self.COMPUTE}
        dsem = {q: [nc.alloc_semaphore(f"dsem_{q}{i}") for i in range(ND)] for q in self.QUEUES}
        for e in self.COMPUTE:
            c = 0
            for o in self.ops[e]:
                if o.dma:
                    continue
                if o.flag:
                    c += 1
                    o.val = c

        def token(d):
            if d.dma:
                return dsem[d.slot[0]][d.slot[1]], d.dval
            return esem[d.eng], d.val

        ops = self.ops

        def run(ename, eng):
            waited = {}
            for o in ops[ename]:
                deps = list(o.deps)
                if o.dma and o.prev_slot_op is not None:
                    deps.append(o.prev_slot_op)
                need = {}
                for d in deps:
                    sem, v = token(d)
                    if waited.get(sem.num, 0) >= v:
                        continue
                    if need.get(sem.num, (None, 0))[1] < v:
                        need[sem.num] = (sem, v)
                for num, (sem, v) in need.items():
                    eng.wait_ge(sem, v)
                    waited[num] = v
                ins = o.emit(eng)
                if o.dma:
                    ins.then_inc(dsem[o.slot[0]][o.slot[1]], 16)
                elif o.flag:
                    ins.then_inc(esem[o.eng], 1)
            if ename == "sp":
                for d in final_wait_ops:
                    sem, v = token(d)
                    if waited.get(sem.num, 0) < v:
                        eng.wait_ge(sem, v)
                        waited[sem.num] = v

        with nc.Block() as block:
            @block.tensor
            def _(e):
                run("pe", e)

            @block.scalar
            def _(e):
                run("act", e)

            @block.vector
            def _(e):
                run("dve", e)

            @block.gpsimd
            def _(e):
                run("pool", e)

            @block.sync
            def _(e):
                run("sp", e)


class Builder:
    def __init__(self, depth=DEPTH, phases=("mix", "xa", "mlp"), debug_outs=(), branches=("nsa", "ret", "rwkv", "conv")):
        self.depth = depth
        self.branches = branches
        self.phases = phases
        nc = self.nc = bass.Bass("TRN2", target_bir_lowering=False)
        P = self.P = Prog(nc, 51200)
        self.inp = {}
        self.debug_outs = debug_outs

    def din(self, name, shape):
        t = self.P.dram(name, shape, kind="ExternalInput")
        self.inp[name] = t
        return t

    def setup(self):
        P = self.P
        self.x_in = self.din("x", [S, D])
        self.out = self.P.dram("out", [S, D], kind="ExternalOutput")
        self.ident_d = self.din("ident", [128, 128])
        self.gains_d = self.din("gains", [128, self.depth * 7 * NCH])
        self.w1 = self.din("mlp_w1", [self.depth, D, DFF])
        self.w2 = self.din("mlp_w2", [self.depth, DFF, D])

        self.xT = P.alloc("xT", NCH * S)
        self.xT3 = self.xT.ap.rearrange("p (c t) -> p c t", c=NCH)
        self.ident = P.alloc("ident", 128)
        self.ones = P.alloc("ones", 128)
        self.gains = P.alloc("gains", self.depth * 7 * NCH)
        P.dma(self.ident.ap, self.ident_d.ap, reads=[self.ident_d], writes=[self.ident])
        P.dma(self.gains.ap, self.gains_d.ap, reads=[self.gains_d], writes=[self.gains])
        P.op("dve", lambda e: e.memset(self.ones.ap, 1.0), writes=[self.ones])

    def gain(self, l, which, c):
        i = (l * 7 + which) * NCH + c
        return self.gains.ap[:, i:i + 1]

    def load_x(self):
        P = self.P
        for tt in range(S // 128):
            xin = P.alloc("xin", D)
            P.dma(xin.ap, self.x_in.ap[tt * 128:(tt + 1) * 128, :], reads=[self.x_in], writes=[xin])
            for half in range(2):
                ps = P.ps()
                for j in range(4):
                    c = half * 4 + j
                    P.op("pe", lambda e, ps=ps, j=j, c=c, xin=xin: e.transpose(
                        ps.ap[:, j * 128:(j + 1) * 128], xin.ap[:, c * 128:(c + 1) * 128], self.ident.ap),
                        reads=[xin, self.ident], writes=[(ps, j)])
                dst = self.xT3[:, half * 4:half * 4 + 4, tt * 128:(tt + 1) * 128]
                P.op("act", lambda e, ps=ps, dst=dst: e.copy(dst, ps.ap.rearrange("p (c t) -> p c t", c=4)),
                     reads=[ps], writes=[(self.xT, tt)])
            P.release(xin)

    def store_x(self):
        P = self.P
        fin = []
        for tt in range(S // 128):
            xo = P.alloc("xo", D)
            for half in range(2):
                ps = P.ps()
                for j in range(4):
                    c = half * 4 + j
                    P.op("pe", lambda e, ps=ps, j=j, c=c, tt=tt: e.transpose(
                        ps.ap[:, j * 128:(j + 1) * 128], self.xT3[:, c, tt * 128:(tt + 1) * 128], self.ident.ap),
                        reads=[self.xT, self.ident], writes=[(ps, j)])
                P.op("act", lambda e, ps=ps, xo=xo, half=half: e.copy(xo.ap[:, half * 512:(half + 1) * 512], ps.ap),
                     reads=[ps], writes=[(xo, half)])
            fin.append(P.dma(self.out.ap[tt * 128:(tt + 1) * 128, :], xo.ap, reads=[xo], writes=[(self.out, tt)]))
            P.release(xo)
        return fin

    def rstd_of(self, src3, src_tile, n, rstd, eps=1e-6):
        P = self.P
        ps = P.ps()
        for c in range(NCH):
            sq = P.alloc("sq", n)
            P.op("act", lambda e, sq=sq, c=c: e.activation(sq.ap, src3[:, c, :], AF.Square),
                 reads=[src_tile], writes=[sq])
            P.op("pe", lambda e, sq=sq, c=c, ps=ps: e.matmul(ps.ap[:, 0:n], self.ones.ap, sq.ap,
                                                         start=(c == 0), stop=(c == NCH - 1)),
                 reads=[sq, self.ones], writes=[ps])
            P.release(sq)
        P.op("act", lambda e, ps=ps: e.activation(rstd.ap[:, 0:n], ps.ap[:, 0:n], AF.Sqrt, bias=self.epsb(eps), scale=1.0 / D),
             reads=[ps, self.epst], writes=[rstd])
        P.op("dve", lambda e: e.reciprocal(rstd.ap[:, 0:n], rstd.ap[:, 0:n]), reads=[rstd], writes=[rstd])

    def epsb(self, eps):
        return self.epst.ap[:, 0:1]

    def setup_eps(self):
        P = self.P
        self.epst = P.alloc("eps", 4)
        P.op("dve", lambda e: e.memset(self.epst.ap[:, 0:1], 1e-6), writes=[self.epst])
        P.op("dve", lambda e: e.memset(self.epst.ap[:, 1:2], 1e-5), writes=[self.epst])
        P.op("dve", lambda e: e.memset(self.epst.ap[:, 2:3], 64e-5), writes=[self.epst])

    def pre_norm_block(self, l, which, t0, n, hblk):
        h3 = hblk.ap.rearrange("p (c t) -> p c t", c=NCH)
        self.norm_to(l, which, self.xT3[:, :, t0:t0 + n], self.xT, n, h3, hblk)

    def norm_to(self, l, which, src3, src_tile, n, dst3, dst_tile):
        P = self.P
        rstd = P.alloc("rstd", n)
        self.rstd_of(src3, src_tile, n, rstd)
        for c in range(NCH):
            P.op("dve", lambda e, c=c: e.scalar_tensor_tensor(
                out=dst3[:, c, :], in0=src3[:, c, :], scalar=self.gain(l, which, c), in1=rstd.ap[:, 0:n],
                op0=ALU.mult, op1=ALU.mult), reads=[src_tile, rstd, self.gains], writes=[dst_tile])
        P.release(rstd)

    def post_norm_residual(self, l, which, t0, n, yblk):
        P = self.P
        rstd = P.alloc("rstd", n)
        y3 = yblk.ap.rearrange("p (c t) -> p c t", c=NCH)
        self.rstd_of(y3, yblk, n, rstd)
        for c in range(NCH):
            P.op("dve", lambda e, c=c: e.tensor_tensor(out=y3[:, c, :], in0=y3[:, c, :], in1=rstd.ap[:, 0:n], op=ALU.mult),
                 reads=[yblk, rstd], writes=[(yblk, c)])
            dst = self.xT3[:, c, t0:t0 + n]
            P.op("dve", lambda e, c=c, dst=dst: e.scalar_tensor_tensor(
                out=dst, in0=y3[:, c, :], scalar=self.gain(l, which, c), in1=dst, op0=ALU.mult, op1=ALU.add),
                reads=[(yblk, c), self.xT, self.gains], writes=[self.xT])
        P.release(rstd)

    def mlp(self, l):
        P = self.P
        TB = 512
        w1 = self.w1.ap[l]
        w2 = self.w2.ap[l]
        for tb in range(S // TB):
            self.mlp_blk(l, tb, TB, w1, w2)

    def mlp_blk(self, l, tb, TB, w1, w2):
        P = self.P
        t0 = tb * TB
        hblk = P.alloc("hblk", NCH * TB // 2)
        h3 = hblk.ap.bitcast(BF16).rearrange("p (c t) -> p c t", c=NCH)
        self.norm_to(l, 4, self.xT3[:, :, t0:t0 + TB], self.xT, TB, h3, hblk)
        ablk = P.alloc("ablk", (DFF // 128) * TB // 2)
        a3 = ablk.ap.bitcast(BF16).rearrange("p (c t) -> p c t", c=DFF // 128)
        w1ring = [P.alloc("w1t", NCH * 512 // 2) for _ in range(2)]
        for fg in range(DFF // 512):
            wt = w1ring[fg % 2]
            wt3 = wt.ap.bitcast(BF16).rearrange("p (k n) -> p k n", k=NCH)
            P.dma(wt3, w1[:, fg * 512:(fg + 1) * 512].rearrange("(k p) n -> p k n", p=128), reads=[self.w1], writes=[wt], q="gq")
            for j in range(4):
                f = fg * 4 + j
                ps = P.ps()
                for k in range(NCH):
                    P.op("pe", lambda e, ps=ps, k=k, j=j, wt3=wt3: e.matmul(
                        ps.ap[:, 0:TB], wt3[:, k, j * 128:(j + 1) * 128], h3[:, k, :], start=(k == 0), stop=(k == NCH - 1)),
                        reads=[wt, hblk], writes=[ps])
                r = P.alloc("relu", TB)
                P.op("act", lambda e, ps=ps, r=r: e.activation(r.ap, ps.ap[:, 0:TB], AF.Relu), reads=[ps], writes=[r])
                P.op("dve", lambda e, r=r, f=f: e.tensor_tensor(out=a3[:, f, :], in0=r.ap, in1=r.ap, op=ALU.mult),
                     reads=[r], writes=[(ablk, f)])
                P.release(r)
        P.release(*w1ring)
        yblk = P.alloc("yblk", NCH * TB)
        y3 = yblk.ap.rearrange("p (c t) -> p c t", c=NCH)
        KG = 8
        w2ring = [P.alloc("w2t", KG * 512 // 2) for _ in range(2)]
        it = 0
        for half in range(2):
            pss = [P.ps(hold=True) for _ in range(4)]
            for kg in range(DFF // 128 // KG):
                wt = w2ring[it % 2]
                it += 1
                wt3 = wt.ap.bitcast(BF16).rearrange("p (k n) -> p k n", k=KG)
                P.dma(wt3, w2[kg * KG * 128:(kg + 1) * KG * 128, half * 512:(half + 1) * 512].rearrange("(k p) n -> p k n", p=128),
                      reads=[self.w2], writes=[wt], q="gq")
                for fj in range(4):
                    ps = pss[fj]
                    for k in range(KG):
                        kk = kg * KG + k
                        P.op("pe", lambda e, ps=ps, k=k, kk=kk, fj=fj, wt3=wt3: e.matmul(
                            ps.ap[:, 0:TB], wt3[:, k, fj * 128:(fj + 1) * 128], a3[:, kk, :], start=(kk == 0),
                            stop=(kk == DFF // 128 - 1)), reads=[wt, ablk], writes=[ps])
            for fj in range(4):
                f = half * 4 + fj
                P.op("act", lambda e, ps=pss[fj], f=f: e.copy(y3[:, f, :], ps.ap[:, 0:TB]), reads=[ps], writes=[(yblk, f)])
            P.ps_free(*pss)
        P.release(*w2ring)
        P.release(ablk, hblk)
        self.post_norm_residual(l, 5, t0, TB, yblk)
        P.release(yblk)

    ZT_ROWS = 3456
    ZQ, ZKC, ZVC, ZKS, ZKW, ZRQ, ZRQS, ZRK, ZRKS, ZRG, ZRW, ZCV = 0, 256, 320, 384, 448, 512, 768, 1024, 1280, 1536, 1792, 2688
    ZN_COLS = 396
    NVS, NVW, NG, NRV = 0, 64, 128, 140

    def setup_mixer(self):
        L = self.depth
        self.wt_d = self.din("w_T", [L, D, self.ZT_ROWS])
        self.wn_d = self.din("w_N", [L, D, self.ZN_COLS])
        self.wgate_d = self.din("w_gate", [L, D, 4 * D])
        self.wbr_d = self.din("w_branch", [L, 4, 256, D])
        self.wmo_d = self.din("w_mix_out", [L, D, D])
        self.convw_d = self.din("conv_wT", [L, 128, 6])
        self.rot_d = self.din("rot_tab", [64, 2, S])
        self.rdec_d = self.din("ret_dec", [128, 4 * 2 * 512])
        self.retg_d = self.din("ret_gT", [L, 64, 4])
        self.ZT = self.P.dram("ZT", [self.ZT_ROWS, S])
        self.ZN = self.P.dram("ZN", [S, self.ZN_COLS])
        self.OBR = self.P.dram("OBR", [D, S])
        self.MRG = self.P.dram("MRG", [D, S])

    def project(self, l, hT):
        P = self.P
        h3 = hT.ap.bitcast(BF16).rearrange("p (c t) -> p c t", c=NCH)
        wn = P.alloc("wn", NCH * self.ZN_COLS // 2)
        wn3 = wn.ap.bitcast(BF16).rearrange("p (k n) -> p k n", k=NCH)
        P.dma(wn3, self.wn_d.ap[l].rearrange("(k p) n -> p k n", p=128), reads=[self.wn_d], writes=[wn], q="gq")
        for tt in range(S // 128):
            ps = P.ps()
            for k in range(NCH):
                P.op("pe", lambda e, ps=ps, k=k, tt=tt: e.matmul(ps.ap[:, 0:self.ZN_COLS], h3[:, k, tt * 128:(tt + 1) * 128], wn3[:, k, :],
                                                             start=(k == 0), stop=(k == NCH - 1)), reads=[hT, wn], writes=[ps])
            stg = P.alloc("stgn", self.ZN_COLS)
            P.op("act", lambda e, ps=ps, stg=stg: e.copy(stg.ap, ps.ap[:, 0:self.ZN_COLS]), reads=[ps], writes=[stg])
            P.dma(self.ZN.ap[tt * 128:(tt + 1) * 128, :], stg.ap, reads=[stg], writes=[(self.ZN, tt)], q="gq")
            P.release(stg)
        P.release(wn)
        for ch in range(self.ZT_ROWS // 128):
            wt = P.alloc("wt", NCH * 128 // 2)
            wt3 = wt.ap.bitcast(BF16).rearrange("p (k n) -> p k n", k=NCH)
            P.dma(wt3, self.wt_d.ap[l][:, ch * 128:(ch + 1) * 128].rearrange("(k p) n -> p k n", p=128), reads=[self.wt_d], writes=[wt], q="gq")
            for tb in range(S // 512):
                ps = P.ps()
                for k in range(NCH):
                    P.op("pe", lambda e, ps=ps, k=k, tb=tb, wt3=wt3: e.matmul(ps.ap, wt3[:, k, :], h3[:, k, tb * 512:(tb + 1) * 512],
                                                                       start=(k == 0), stop=(k == NCH - 1)), reads=[hT, wt], writes=[ps])
                stg = P.alloc("stgt", 512)
                eng = "act" if tb % 2 == 0 else "dve"
                if eng == "act":
                    P.op("act", lambda e, ps=ps, stg=stg: e.copy(stg.ap, ps.ap), reads=[ps], writes=[stg])
                else:
                    P.op("dve", lambda e, ps=ps, stg=stg: e.tensor_copy(stg.ap, ps.ap), reads=[ps], writes=[stg])
                P.dma(self.ZT.ap[ch * 128:(ch + 1) * 128, tb * 512:(tb + 1) * 512], stg.ap, reads=[stg], writes=[(self.ZT, ch)], q="gq")
                P.release(stg)
            P.release(wt)

    def conv_branch(self, l):
        P = self.P
        cw = P.alloc("convw", 6)
        P.dma(cw.ap, self.convw_d.ap[l], reads=[self.convw_d], writes=[cw])
        for c in range(2):
            self.conv_chunk(cw, c)
        P.release(cw)

    def conv_chunk(self, cw, c):
        P = self.P
        if True:
            bg = P.alloc("cv_b", S)
            cg = P.alloc("cv_c", S)
            xt = P.alloc("cv_x", S)
            for j, t in enumerate((bg, cg, xt)):
                r0 = self.ZCV + j * 256 + c * 128
                P.dma(t.ap, self.ZT.ap[r0:r0 + 128, :], reads=[(self.ZT, r0 // 128)], writes=[t])
            w = lambda j: cw.ap[:, c * 3 + j:c * 3 + j + 1]
            P.op("dve", lambda e: e.tensor_tensor(out=cg.ap, in0=cg.ap, in1=xt.ap, op=ALU.mult), reads=[cg, xt], writes=[cg])
            P.op("dve", lambda e, w=w: e.tensor_scalar(out=xt.ap, in0=cg.ap, scalar1=w(2), scalar2=None, op0=ALU.mult), reads=[cg, cw], writes=[xt])
            P.op("dve", lambda e, w=w: e.scalar_tensor_tensor(out=xt.ap[:, 1:S], in0=cg.ap[:, 0:S - 1], scalar=w(1), in1=xt.ap[:, 1:S],
                                                              op0=ALU.mult, op1=ALU.add), reads=[cg, cw, xt], writes=[xt])
            P.op("dve", lambda e, w=w: e.scalar_tensor_tensor(out=xt.ap[:, 2:S], in0=cg.ap[:, 0:S - 2], scalar=w(0), in1=xt.ap[:, 2:S],
                                                              op0=ALU.mult, op1=ALU.add), reads=[cg, cw, xt], writes=[xt])
            P.op("dve", lambda e: e.tensor_tensor(out=bg.ap, in0=bg.ap, in1=xt.ap, op=ALU.mult), reads=[bg, xt], writes=[bg])
            P.dma(self.OBR.ap[768 + c * 128:768 + (c + 1) * 128, :], bg.ap, reads=[bg], writes=[(self.OBR, 6 + c)], q="gq")
            P.release(bg, cg, xt)

    def retention_branch(self, l):
        P = self.P
        rot = P.alloc("rot", 2 * S)
        rot3 = rot.ap.rearrange("p (a t) -> p a t", a=2)
        P.dma(rot3[0:64], self.rot_d.ap, reads=[self.rot_d], writes=[rot])
        dec = P.alloc("rdec", 4 * 2 * 512)
        dec4 = dec.ap.rearrange("p (h a q) -> p h a q", h=4, a=2)
        P.dma(dec.ap, self.rdec_d.ap, reads=[self.rdec_d], writes=[dec])
        rg = P.alloc("retg", 4)
        P.dma(rg.ap[0:64], self.retg_d.ap[l], reads=[self.retg_d], writes=[rg])
        for h in range(4):
            self.ret_head(l, h, rot, rot3, dec, dec4, rg)
        P.release(rot, dec, rg)

    def ret_head(self, l, h, rot, rot3, dec, dec4, rg):
        P = self.P
        if True:
            lg = float(np.log(1.0 - 2.0 ** (-5.0 - h)))
            qk = []
            for base, bsw in ((self.ZRQ, self.ZRQS), (self.ZRK, self.ZRKS)):
                u = P.alloc("ru", S)
                us = P.alloc("rus", S)
                P.dma(u.ap[0:64], self.ZT.ap[base + h * 64:base + (h + 1) * 64, :], reads=[(self.ZT, (base + h * 64) // 128)], writes=[u])
                P.dma(us.ap[0:64], self.ZT.ap[bsw + h * 64:bsw + (h + 1) * 64, :], reads=[(self.ZT, (bsw + h * 64) // 128)], writes=[us])
                P.op("dve", lambda e, u=u: e.tensor_tensor(out=u.ap[0:64], in0=u.ap[0:64], in1=rot3[0:64, 0, :], op=ALU.mult), reads=[u, rot], writes=[u])
                P.op("dve", lambda e, us=us: e.tensor_tensor(out=us.ap[0:64], in0=us.ap[0:64], in1=rot3[0:64, 1, :], op=ALU.mult), reads=[us, rot], writes=[us])
                P.op("dve", lambda e, u=u, us=us: e.tensor_tensor(out=u.ap[0:64], in0=u.ap[0:64], in1=us.ap[0:64], op=ALU.add), reads=[u, us], writes=[u])
                P.release(us)
                qk.append(u)
            qT, kT = qk
            vh = P.alloc("rv", 16 * 64)
            vh3 = vh.ap.rearrange("p (t d) -> p t d", t=16)
            P.dma(vh3, self.ZN.ap[:, self.NRV + h * 64:self.NRV + (h + 1) * 64].rearrange("(t p) d -> p t d", p=128), reads=[self.ZN], writes=[vh])
            for Q in range(4):
                pso = P.ps(hold=True)
                nkb = 4 * Q + 4

                def geo(kb, Q=Q):
                    j = kb - 4 * Q
                    c0 = 128 * j if j > 0 else 0
                    return j, c0, 512 - c0

                def qk(kb, Q=Q):
                    j, c0, nq = geo(kb)
                    pss = P.ps()
                    P.op("pe", lambda e, pss=pss, kb=kb, Q=Q, c0=c0, nq=nq: e.matmul(
                        pss.ap[:, 0:nq], kT.ap[0:64, kb * 128:(kb + 1) * 128], qT.ap[0:64, Q * 512 + c0:(Q + 1) * 512], start=True, stop=True),
                        reads=[kT, qT], writes=[pss])
                    return pss

                def post(kb, pss, Q=Q, pso=pso, nkb=nkb):
                    j, c0, nq = geo(kb)
                    pT = P.alloc("rp", 512)
                    if j < 0:
                        sc = float(np.exp(lg * 128.0 * (4 * Q - kb)))
                        P.op("dve", lambda e, pss=pss, pT=pT, sc=sc, h=h: e.scalar_tensor_tensor(
                            out=pT.ap, in0=pss.ap, scalar=sc, in1=dec4[:, h, 0, :], op0=ALU.mult, op1=ALU.mult), reads=[pss, dec], writes=[pT])
                    else:
                        P.op("dve", lambda e, pss=pss, pT=pT, nq=nq, h=h: e.tensor_tensor(
                            out=pT.ap[:, 0:nq], in0=pss.ap[:, 0:nq], in1=dec4[:, h, 1, 0:nq], op=ALU.mult), reads=[pss, dec], writes=[pT])
                    P.op("pe", lambda e, pso=pso, pT=pT, kb=kb, c0=c0, nq=nq, first=(kb == 0), last=(kb == nkb - 1): e.matmul(
                        pso.ap[0:64, c0:512], vh3[:, kb, :], pT.ap[:, 0:nq], start=first, stop=last, skip_group_check=True),
                        reads=[vh, pT], writes=[pso])
                    P.release(pT)

                nxt = qk(0)
                for kb in range(nkb):
                    cur = nxt
                    if kb + 1 < nkb:
                        nxt = qk(kb + 1)
                    post(kb, cur)
                self.ret_epilogue(l, h, Q, pso, rg)
                P.ps_free(pso)
            P.release(qT, kT, vh)

    def ret_epilogue(self, l, h, Q, pso, rg):
        P = self.P
        n = 512
        o = P.alloc("ro", n)
        sq = P.alloc("rsq", n)
        P.op("act", lambda e: e.copy(o.ap[0:64], pso.ap[0:64, :]), reads=[pso], writes=[o])
        P.op("act", lambda e: e.activation(sq.ap[0:64], pso.ap[0:64, :], AF.Square), reads=[pso], writes=[sq])
        p1 = P.ps()
        p2 = P.ps()
        P.op("pe", lambda e: e.matmul(p1.ap[0:64, :], self.ones.ap[0:64, 0:64], o.ap[0:64], start=True, stop=True), reads=[o, self.ones], writes=[p1])
        P.op("pe", lambda e: e.matmul(p2.ap[0:64, :], self.ones.ap[0:64, 0:64], sq.ap[0:64], start=True, stop=True), reads=[sq, self.ones], writes=[p2])
        mean = P.alloc("rmean", n)
        P.op("dve", lambda e: e.tensor_scalar(out=mean.ap[0:64], in0=p1.ap[0:64, :], scalar1=1.0 / 64, scalar2=None, op0=ALU.mult), reads=[p1], writes=[mean])
        P.op("dve", lambda e: e.tensor_tensor(out=o.ap[0:64], in0=o.ap[0:64], in1=mean.ap[0:64], op=ALU.subtract), reads=[o, mean], writes=[o])
        P.op("dve", lambda e: e.tensor_tensor(out=mean.ap[0:64], in0=mean.ap[0:64], in1=mean.ap[0:64], op=ALU.mult), reads=[mean], writes=[mean])
        P.op("dve", lambda e: e.scalar_tensor_tensor(out=sq.ap[0:64], in0=p2.ap[0:64, :], scalar=1.0 / 64, in1=mean.ap[0:64],
                                                     op0=ALU.mult, op1=ALU.subtract), reads=[p2, mean], writes=[sq])
        P.op("act", lambda e: e.activation(sq.ap[0:64], sq.ap[0:64], AF.Sqrt, bias=self.epst.ap[0:64, 1:2], scale=1.0), reads=[sq, self.epst], writes=[sq])
        P.op("dve", lambda e: e.reciprocal(sq.ap[0:64], sq.ap[0:64]), reads=[sq], writes=[sq])
        P.op("dve", lambda e: e.tensor_tensor(out=o.ap[0:64], in0=o.ap[0:64], in1=sq.ap[0:64], op=ALU.mult), reads=[o, sq], writes=[o])
        g = P.alloc("rgate", n)
        r0 = self.ZRG + h * 64
        P.dma(g.ap[0:64], self.ZT.ap[r0:r0 + 64, Q * n:(Q + 1) * n], reads=[(self.ZT, r0 // 128)], writes=[g])
        P.op("act", lambda e: e.activation(g.ap[0:64], g.ap[0:64], AF.Silu), reads=[g], writes=[g])
        P.op("dve", lambda e: e.scalar_tensor_tensor(out=o.ap[0:64], in0=o.ap[0:64], scalar=rg.ap[0:64, h:h + 1], in1=g.ap[0:64],
                                                     op0=ALU.mult, op1=ALU.mult), reads=[o, rg, g], writes=[o])
        P.dma(self.OBR.ap[256 + h * 64:256 + (h + 1) * 64, Q * n:(Q + 1) * n], o.ap[0:64], reads=[o], writes=[(self.OBR, 2 + h // 2)], q="gq")
        P.release(o, sq, mean, g)

    def merge(self, l, hT):
        P = self.P
        TB = 256
        for tb in range(S // TB):
            self.merge_blk(l, hT, tb, TB)

    def merge_blk(self, l, hT, tb, TB):
        P = self.P
        h3 = hT.ap.rearrange("p (c t) -> p c t", c=NCH)
        if True:
            t0 = tb * TB
            obr = P.alloc("obr", NCH * TB)
            obr3 = obr.ap.rearrange("p (c t) -> p c t", c=NCH)
            P.dma(obr3, self.OBR.ap[:, t0:t0 + TB].rearrange("(c p) t -> p c t", p=128), reads=[self.OBR], writes=[obr])
            mrg = P.alloc("mrg", NCH * TB)
            m3 = mrg.ap.rearrange("p (c t) -> p c t", c=NCH)
            for f in range(NCH):
                for m in range(4):
                    wg = P.alloc("wg", NCH * 128)
                    wg3 = wg.ap.rearrange("p (k n) -> p k n", k=NCH)
                    c0 = m * D + f * 128
                    P.dma(wg3, self.wgate_d.ap[l][:, c0:c0 + 128].rearrange("(k p) n -> p k n", p=128), reads=[self.wgate_d], writes=[wg])
                    wb = P.alloc("wb", 2 * 128)
                    wb3 = wb.ap.rearrange("p (k n) -> p k n", k=2)
                    P.dma(wb3, self.wbr_d.ap[l, m][:, f * 128:(f + 1) * 128].rearrange("(k p) n -> p k n", p=128), reads=[self.wbr_d], writes=[wb])
                    ps1 = P.ps()
                    for k in range(NCH):
                        P.op("pe", lambda e, ps1=ps1, k=k, wg3=wg3: e.matmul(ps1.ap[:, 0:TB], wg3[:, k, :], h3[:, k, t0:t0 + TB],
                                                                          start=(k == 0), stop=(k == NCH - 1)), reads=[wg, hT], writes=[ps1])
                    ps2 = P.ps()
                    for k in range(2):
                        P.op("pe", lambda e, ps2=ps2, k=k, m=m, wb3=wb3: e.matmul(ps2.ap[:, 0:TB], wb3[:, k, :], obr3[:, 2 * m + k, :],
                                                                               start=(k == 0), stop=(k == 1)), reads=[wb, obr], writes=[ps2])
                    g = P.alloc("mg", TB)
                    P.op("act", lambda e, ps1=ps1, g=g: e.activation(g.ap, ps1.ap[:, 0:TB], AF.Sigmoid), reads=[ps1], writes=[g])
                    if m == 0:
                        P.op("dve", lambda e, g=g, ps2=ps2, f=f: e.tensor_tensor(out=m3[:, f, :], in0=g.ap, in1=ps2.ap[:, 0:TB], op=ALU.mult),
                             reads=[g, ps2], writes=[(mrg, f)])
                    else:
                        P.op("dve", lambda e, g=g, ps2=ps2: e.tensor_tensor(out=g.ap, in0=g.ap, in1=ps2.ap[:, 0:TB], op=ALU.mult),
                             reads=[g, ps2], writes=[g])
                        P.op("dve", lambda e, g=g, f=f: e.tensor_tensor(out=m3[:, f, :], in0=m3[:, f, :], in1=g.ap, op=ALU.add),
                             reads=[g, (mrg, f)], writes=[(mrg, f)])
                    P.release(g, wg, wb)
            P.release(obr)
            yblk = P.alloc("yblk", NCH * TB)
            y3 = yblk.ap.rearrange("p (c t) -> p c t", c=NCH)
            for f in range(NCH):
                wo = P.alloc("wmo", NCH * 128)
                wo3 = wo.ap.rearrange("p (k n) -> p k n", k=NCH)
                P.dma(wo3, self.wmo_d.ap[l][:, f * 128:(f + 1) * 128].rearrange("(k p) n -> p k n", p=128), reads=[self.wmo_d], writes=[wo])
                ps = P.ps()
                for k in range(NCH):
                    P.op("pe", lambda e, ps=ps, k=k, wo3=wo3: e.matmul(ps.ap[:, 0:TB], wo3[:, k, :], m3[:, k, :], start=(k == 0), stop=(k == NCH - 1)),
                         reads=[wo, mrg], writes=[ps])
                P.op("act", lambda e, ps=ps, f=f: e.copy(y3[:, f, :], ps.ap[:, 0:TB]), reads=[ps], writes=[(yblk, f)])
                P.release(wo)
            P.release(mrg)
            self.post_norm_residual(l, 1, t0, TB, yblk)
            P.release(yblk)


    def merge2(self, l, hT):
        P = self.P
        h3 = hT.ap.bitcast(BF16).rearrange("p (c t) -> p c t", c=NCH)
        obr_ring = [P.alloc("obrm", 2 * S // 2) for _ in range(2)]
        it = 0
        for f in range(NCH):
            mf = P.alloc("mrgf", S)
            for m in range(4):
                self.merge_fm(l, f, m, h3, hT, mf, obr_ring[it % 2])
                it += 1
            P.dma(self.MRG.ap[f * 128:(f + 1) * 128, :], mf.ap, reads=[mf], writes=[(self.MRG, f)], q="gq")
            P.release(mf)
        P.release(*obr_ring)

    def merge_fm(self, l, f, m, h3, hT, mf, obr):
        P = self.P
        obr3 = obr.ap.bitcast(BF16).rearrange("p (k t) -> p k t", k=2)
        P.dma(obr3, self.OBR.ap[m * 256:(m + 1) * 256, :].rearrange("(k p) t -> p k t", p=128),
              reads=[(self.OBR, 2 * m), (self.OBR, 2 * m + 1)], writes=[obr], q="gq")
        wg = P.alloc("wg", NCH * 128 // 2)
        wg3 = wg.ap.bitcast(BF16).rearrange("p (k n) -> p k n", k=NCH)
        c0 = m * D + f * 128
        P.dma(wg3, self.wgate_d.ap[l][:, c0:c0 + 128].rearrange("(k p) n -> p k n", p=128), reads=[self.wgate_d], writes=[wg], q="gq")
        wb = P.alloc("wb", 2 * 128 // 2)
        wb3 = wb.ap.bitcast(BF16).rearrange("p (k n) -> p k n", k=2)
        P.dma(wb3, self.wbr_d.ap[l, m][:, f * 128:(f + 1) * 128].rearrange("(k p) n -> p k n", p=128), reads=[self.wbr_d], writes=[wb], q="gq")
        for tb in range(S // 512):
            ts = slice(tb * 512, (tb + 1) * 512)
            ps1 = P.ps()
            for k in range(NCH):
                P.op("pe", lambda e, ps1=ps1, k=k, ts=ts: e.matmul(ps1.ap, wg3[:, k, :], h3[:, k, ts], start=(k == 0), stop=(k == NCH - 1)),
                     reads=[wg, hT], writes=[ps1])
            ps2 = P.ps()
            for k in range(2):
                P.op("pe", lambda e, ps2=ps2, k=k, ts=ts: e.matmul(ps2.ap, wb3[:, k, :], obr3[:, k, ts], start=(k == 0), stop=(k == 1)),
                     reads=[wb, obr], writes=[ps2])
            g = P.alloc("mg", 512)
            P.op("act", lambda e, ps1=ps1, g=g: e.activation(g.ap, ps1.ap, AF.Sigmoid), reads=[ps1], writes=[g])
            if m == 0:
                P.op("dve", lambda e, g=g, ps2=ps2, ts=ts: e.tensor_tensor(out=mf.ap[:, ts], in0=g.ap, in1=ps2.ap, op=ALU.mult),
                     reads=[g, ps2], writes=[(mf, tb)])
            else:
                P.op("dve", lambda e, g=g, ps2=ps2: e.tensor_tensor(out=g.ap, in0=g.ap, in1=ps2.ap, op=ALU.mult), reads=[g, ps2], writes=[g])
                P.op("dve", lambda e, g=g, ts=ts: e.tensor_tensor(out=mf.ap[:, ts], in0=mf.ap[:, ts], in1=g.ap, op=ALU.add),
                     reads=[g, (mf, tb)], writes=[(mf, tb)])
            P.release(g)
        P.release(wg, wb)

    def mixout(self, l):
        for tb in range(S // 512):
            self.mixout_blk(l, tb, 512)

    def mixout_blk(self, l, tb, TB):
        P = self.P
        t0 = tb * TB
        mrg = P.alloc("mrg", NCH * TB // 2)
        m3 = mrg.ap.bitcast(BF16).rearrange("p (c t) -> p c t", c=NCH)
        P.dma(m3, self.MRG.ap[:, t0:t0 + TB].rearrange("(c p) t -> p c t", p=128), reads=[self.MRG], writes=[mrg], q="gq")
        yblk = P.alloc("yblk", NCH * TB)
        y3 = yblk.ap.rearrange("p (c t) -> p c t", c=NCH)
        self.lin8(self.wmo_d.ap[l], m3, mrg, y3, yblk, TB)
        P.release(mrg)
        self.post_norm_residual(l, 1, t0, TB, yblk)
        P.release(yblk)

    def zero_obr(self, r0, r1):
        P = self.P
        z = P.alloc("zero", S)
        P.op("dve", lambda e: e.memset(z.ap, 0.0), writes=[z])
        for r in range(r0, r1, 128):
            P.dma(self.OBR.ap[r:r + 128, :], z.ap, reads=[z], writes=[(self.OBR, r // 128)], q="gq")
        P.release(z)

    def mixer(self, l):
        P = self.P
        hT = P.alloc("hT", NCH * S // 2)
        h3 = hT.ap.bitcast(BF16).rearrange("p (c t) -> p c t", c=NCH)
        for tb in range(4):
            self.norm_to(l, 0, self.xT3[:, :, tb * 512:(tb + 1) * 512], self.xT, 512, h3[:, :, tb * 512:(tb + 1) * 512], hT)
        self.project(l, hT)
        P.release(hT)
        if "nsa" in self.branches:
            self.nsa_branch(l)
        else:
            self.zero_obr(0, 256)
        if "ret" in self.branches:
            self.retention_branch(l)
        else:
            self.zero_obr(256, 512)
        if "rwkv" in self.branches:
            self.rwkv_branch(l)
        else:
            self.zero_obr(512, 768)
        if "conv" in self.branches:
            self.conv_branch(l)
        else:
            self.zero_obr(768, 1024)
        if "nomerge" not in self.phases:
            hT = P.alloc("hT", NCH * S // 2)
            h3 = hT.ap.bitcast(BF16).rearrange("p (c t) -> p c t", c=NCH)
            for tb in range(4):
                self.norm_to(l, 0, self.xT3[:, :, tb * 512:(tb + 1) * 512], self.xT, 512, h3[:, :, tb * 512:(tb + 1) * 512], hT)
            self.merge2(l, hT)
            P.release(hT)
            self.mixout(l)


    def setup_nsa(self):
        L = self.depth
        self.cmpw_d = self.din("nsa_cmp_w", [L, 2, 32, 64, 64])
        self.peT_d = self.din("nsa_peT", [L, 64, 32])
        self.biasc_d = self.din("nsa_biasc", [128, 4, S])
        self.ntab_d = self.din("nsa_tab", [128, 4 * 512])
        self.e2_d = self.din("nsa_e2", [32, S])
        self.ovl_d = self.din("nsa_ovl", [128, 32])
        self.addt_d = self.din("nsa_addtab", [S, 32])

    def nsa_branch(self, l):
        P = self.P
        W = P.alloc("cmpw", 2 * 32 * 64)
        W3 = W.ap.rearrange("p (a e) -> p a e", a=64)
        P.dma(W3[0:64], self.cmpw_d.ap[l].rearrange("a l d e -> d (a l) e"), reads=[self.cmpw_d], writes=[W])
        peT = P.alloc("peT", 32)
        P.dma(peT.ap[0:64], self.peT_d.ap[l], reads=[self.peT_d], writes=[peT])
        kc = P.alloc("kcT", S)
        vc = P.alloc("vcT", S)
        P.dma(kc.ap[0:64], self.ZT.ap[self.ZKC:self.ZKC + 64, :], reads=[(self.ZT, 2)], writes=[kc])
        P.dma(vc.ap[0:64], self.ZT.ap[self.ZVC:self.ZVC + 64, :], reads=[(self.ZT, 2)], writes=[vc])
        kcmp = P.alloc("kcmpT", 128)
        vaug = P.alloc("vcmp_aug", 97)
        P.op("dve", lambda e: e.memset(kcmp.ap, 0.0), writes=[kcmp])
        P.op("dve", lambda e: e.memset(vaug.ap, 0.0), writes=[vaug])
        P.op("dve", lambda e: e.memset(vaug.ap[0:127, 64:65], 1.0), reads=[vaug], writes=[vaug])
        P.dma(vaug.ap[:, 65:97], self.ovl_d.ap, reads=[vaug, self.ovl_d], writes=[vaug])
        kc3 = kc.ap.rearrange("p (n s) -> p n s", s=16)
        vc3 = vc.ap.rearrange("p (n s) -> p n s", s=16)
        psk = P.ps()
        psb = P.ps()
        for li in range(32):
            rhs = kc3[0:64, li // 16:li // 16 + 127, li % 16]
            P.op("pe", lambda e, li=li, rhs=rhs: e.matmul(psk.ap[0:64, 0:127], W3[0:64, li, :], rhs, start=(li == 0), stop=(li == 31)),
                 reads=[W, kc], writes=[psk])
        for li in range(32):
            P.op("pe", lambda e, li=li: e.matmul(psb.ap[0:64, 0:1], W3[0:64, li, :], peT.ap[0:64, li:li + 1], start=(li == 0), stop=(li == 31)),
                 reads=[W, peT], writes=[psb])
        bk = P.alloc("bk", 1)
        P.op("act", lambda e: e.copy(bk.ap[0:64], psb.ap[0:64, 0:1]), reads=[psb], writes=[bk])
        P.op("dve", lambda e: e.tensor_scalar(out=kcmp.ap[0:64, 0:127], in0=psk.ap[0:64, 0:127], scalar1=bk.ap[0:64, 0:1], scalar2=None, op0=ALU.add),
             reads=[psk, bk, kcmp], writes=[kcmp])
        psv = P.ps()
        psbv = P.ps()
        for li in range(32):
            P.op("pe", lambda e, li=li: e.matmul(psbv.ap[0:1, 0:64], peT.ap[0:64, li:li + 1], W3[0:64, 32 + li, :], start=(li == 0), stop=(li == 31)),
                 reads=[W, peT], writes=[psbv])
        bv = P.alloc("bv", 64)
        P.op("act", lambda e: e.copy(bv.ap[0:1], psbv.ap[0:1, 0:64]), reads=[psbv], writes=[bv])
        for li in range(32):
            lhs = vc3[0:64, li // 16:li // 16 + 127, li % 16]
            P.op("pe", lambda e, li=li, lhs=lhs: e.matmul(psv.ap[0:127, 0:64], lhs, W3[0:64, 32 + li, :], start=(li == 0), stop=False),
                 reads=[W, vc], writes=[psv])
        P.op("pe", lambda e: e.matmul(psv.ap[0:127, 0:64], self.ones.ap[0:1, 0:127], bv.ap[0:1, 0:64], start=False, stop=True),
             reads=[bv, self.ones], writes=[psv])
        P.op("act", lambda e: e.copy(vaug.ap[0:127, 0:64], psv.ap[0:127, 0:64]), reads=[psv, vaug], writes=[vaug])
        P.release(W, peT, kc, vc, bk, bv)
        ks = P.alloc("ksT", S)
        kw = P.alloc("kwT", S)
        P.dma(ks.ap[0:64], self.ZT.ap[self.ZKS:self.ZKS + 64, :], reads=[(self.ZT, 3)], writes=[ks])
        P.dma(kw.ap[0:64], self.ZT.ap[self.ZKW:self.ZKW + 64, :], reads=[(self.ZT, 3)], writes=[kw])
        e2 = P.alloc("e2", S)
        P.dma(e2.ap[0:32], self.e2_d.ap, reads=[self.e2_d], writes=[e2])
        tab = P.alloc("ntab", 4 * 512)
        P.dma(tab.ap, self.ntab_d.ap, reads=[self.ntab_d], writes=[tab])
        vaugs = []
        for c0 in (self.NVS, self.NVW):
            va = P.alloc("vaug", 16 * 65)
            va3 = va.ap.rearrange("p (t d) -> p t d", t=16)
            P.op("dve", lambda e, va=va: e.memset(va.ap, 1.0), writes=[va])
            P.dma(va3[:, :, 0:64], self.ZN.ap[:, c0:c0 + 64].rearrange("(t p) d -> p t d", p=128), reads=[self.ZN, va], writes=[va])
            vaugs.append((va, va3))
        gl = P.alloc("ngl", 16 * 12)
        gl3 = gl.ap.rearrange("p (t g) -> p t g", t=16)
        P.dma(gl3, self.ZN.ap[:, self.NG:self.NG + 12].rearrange("(t p) g -> p t g", p=128), reads=[self.ZN], writes=[gl])
        P.op("act", lambda e: e.activation(gl.ap, gl.ap, AF.Sigmoid), reads=[gl], writes=[gl])
        for qb in range(S // 128):
            self.nsa_qblock(l, qb, kcmp, vaug, ks, kw, e2, tab, vaugs, gl3, gl)
        P.release(kcmp, vaug, ks, kw, e2, tab, vaugs[0][0], vaugs[1][0], gl)

    def nsa_scores(self, ps_s, tabsl, tab, pso, vaug_ap, vaug_tile, first, width):
        P = self.P
        tmp = P.alloc("ntmp", 512)
        src_tiles = [ps_s, tab]
        P.op("dve", lambda e: e.scalar_tensor_tensor(out=tmp.ap, in0=ps_s.ap, scalar=0.125, in1=tabsl, op0=ALU.mult, op1=ALU.add),
             reads=src_tiles, writes=[tmp])
        P.op("act", lambda e: e.activation(tmp.ap, tmp.ap, AF.Exp), reads=[tmp], writes=[tmp])
        for h in range(4):
            P.op("pe", lambda e, h=h: e.matmul(pso.ap[:, h * width:(h + 1) * width], tmp.ap[:, h * 128:(h + 1) * 128], vaug_ap,
                                              start=(first and h == 0), stop=True, skip_group_check=True),
                 reads=[tmp, vaug_tile], writes=[pso])
        P.release(tmp)

    def nsa_qblock(self, l, qb, kcmp, vaug, ks, kw, e2, tab, vaugs, gl3, gl):
        P = self.P
        q0 = qb * 128
        q4 = P.alloc("q4", 512)
        P.dma(q4.ap[0:64].rearrange("p (h t) -> p h t", h=4), self.ZT.ap[0:256, q0:q0 + 128].rearrange("(h d) t -> d h t", d=64),
              reads=[(self.ZT, 0), (self.ZT, 1)], writes=[q4])
        bc = P.alloc("bc", 512)
        P.dma(bc.ap.rearrange("p (h t) -> p h t", h=4), self.biasc_d.ap[:, :, q0:q0 + 128], reads=[self.biasc_d], writes=[bc])
        ps_c = P.ps()
        P.op("pe", lambda e: e.matmul(ps_c.ap, kcmp.ap[0:64, :], q4.ap[0:64, :], start=True, stop=True), reads=[kcmp, q4], writes=[ps_c])
        ps_oc = P.ps(hold=True)
        self.nsa_scores(ps_c, bc.ap, bc, ps_oc, vaug.ap, vaug, True, 97)
        P.release(bc)
        oc3 = ps_oc.ap[:, 0:388].rearrange("p (h w) -> p h w", h=4)
        rdc = P.alloc("rdc", 4)
        P.op("dve", lambda e: e.tensor_scalar(out=rdc.ap, in0=oc3[:, :, 64], scalar1=1e-30, scalar2=None, op0=ALU.max), reads=[ps_oc], writes=[rdc])
        P.op("dve", lambda e: e.reciprocal(rdc.ap, rdc.ap), reads=[rdc], writes=[rdc])
        imp = P.alloc("imp", 32)
        P.dma(imp.ap, self.addt_d.ap[q0:q0 + 128, :], reads=[self.addt_d], writes=[imp])
        for h in range(4):
            P.op("dve", lambda e, h=h: e.scalar_tensor_tensor(out=imp.ap, in0=oc3[:, h, 65:97], scalar=rdc.ap[:, h:h + 1], in1=imp.ap,
                                                              op0=ALU.mult, op1=ALU.add), reads=[ps_oc, rdc, imp], writes=[imp])
        top8 = P.alloc("top8", 8)
        P.op("dve", lambda e: e.max(out=top8.ap, in_=imp.ap), reads=[imp], writes=[top8])
        P.op("dve", lambda e: e.tensor_scalar(out=imp.ap, in0=imp.ap, scalar1=top8.ap[:, 7:8], scalar2=1.0, op0=ALU.is_ge, op1=ALU.subtract),
             reads=[imp, top8], writes=[imp])
        ps_t = P.ps()
        P.op("pe", lambda e: e.transpose(ps_t.ap[0:32, 0:128], imp.ap, self.ident.ap), reads=[imp, self.ident], writes=[ps_t])
        ns4 = P.alloc("ns4", 512)
        P.op("dve", lambda e: e.tensor_copy(ns4.ap[0:32].rearrange("p (h t) -> p h t", h=4),
                                            ps_t.ap[0:32, 0:128].rearrange("p (o t) -> p o t", o=1).broadcast_to([32, 4, 128])),
             reads=[ps_t], writes=[ns4])
        P.release(imp, top8)
        ps_os = P.ps(hold=True)
        ps_ow = P.ps(hold=True)
        kb0 = max(0, qb - 4)

        def qk_sel(kb):
            ps_s = P.ps()
            P.op("pe", lambda e, kb=kb, ps_s=ps_s: e.matmul(ps_s.ap, ks.ap[0:64, kb * 128:(kb + 1) * 128], q4.ap[0:64, :], start=True, stop=False),
                 reads=[ks, q4], writes=[ps_s])
            P.op("pe", lambda e, kb=kb, ps_s=ps_s: e.matmul(ps_s.ap, e2.ap[0:32, kb * 128:(kb + 1) * 128], ns4.ap[0:32, :], start=False, stop=True),
                 reads=[e2, ns4], writes=[ps_s])
            return ps_s

        def post_sel(kb, ps_s):
            ti = min(qb - kb, 2)
            self.nsa_scores(ps_s, tab.ap[:, ti * 512:(ti + 1) * 512], tab, ps_os, vaugs[0][1][:, kb, :], vaugs[0][0], kb == 0, 65)

        def qk_win(kb):
            ps_s = P.ps()
            P.op("pe", lambda e, kb=kb, ps_s=ps_s: e.matmul(ps_s.ap, kw.ap[0:64, kb * 128:(kb + 1) * 128], q4.ap[0:64, :], start=True, stop=True),
                 reads=[kw, q4], writes=[ps_s])
            return ps_s

        def post_win(kb, ps_s):
            ti = (0, 1, 2, 2, 3)[qb - kb]
            self.nsa_scores(ps_s, tab.ap[:, ti * 512:(ti + 1) * 512], tab, ps_ow, vaugs[1][1][:, kb, :], vaugs[1][0], kb == kb0, 65)

        tasks = [(qk_win, post_win, kb) for kb in range(kb0, qb + 1)] + [(qk_sel, post_sel, kb) for kb in range(qb + 1)]
        nxt = tasks[0][0](tasks[0][2])
        for i, (qf, pf, kb) in enumerate(tasks):
            cur = nxt
            if i + 1 < len(tasks):
                nxt = tasks[i + 1][0](tasks[i + 1][2])
            pf(kb, cur)
        P.release(q4, ns4)
        acc = P.alloc("nacc", 256)
        acc3 = acc.ap.rearrange("p (h d) -> p h d", h=4)
        g3 = gl3[:, qb, :].rearrange("p (h b) -> p h b", b=3)
        for b, (pso, w) in enumerate(((ps_oc, 97), (ps_os, 65), (ps_ow, 65))):
            o3 = pso.ap[:, 0:4 * w].rearrange("p (h w) -> p h w", h=4)
            scl = P.alloc("nscl", 4)
            P.op("dve", lambda e, o3=o3, scl=scl: e.tensor_scalar(out=scl.ap, in0=o3[:, :, 64], scalar1=1e-30, scalar2=None, op0=ALU.max),
                 reads=[pso], writes=[scl])
            P.op("dve", lambda e, scl=scl: e.reciprocal(scl.ap, scl.ap), reads=[scl], writes=[scl])
            P.op("dve", lambda e, scl=scl, b=b: e.tensor_tensor(out=scl.ap, in0=scl.ap, in1=g3[:, :, b], op=ALU.mult), reads=[scl, gl], writes=[scl])
            sb = scl.ap.rearrange("p (h o) -> p h o", o=1).broadcast_to([128, 4, 64])
            if b == 0:
                P.op("dve", lambda e, o3=o3, sb=sb: e.tensor_tensor(out=acc3, in0=o3[:, :, 0:64], in1=sb, op=ALU.mult), reads=[pso, scl], writes=[acc])
            else:
                t2 = P.alloc("nt2", 256)
                t23 = t2.ap.rearrange("p (h d) -> p h d", h=4)
                P.op("dve", lambda e, o3=o3, sb=sb, t23=t23: e.tensor_tensor(out=t23, in0=o3[:, :, 0:64], in1=sb, op=ALU.mult), reads=[pso, scl], writes=[t2])
                P.op("dve", lambda e, t2=t2: e.tensor_tensor(out=acc.ap, in0=acc.ap, in1=t2.ap, op=ALU.add), reads=[acc, t2], writes=[acc])
                P.release(t2)
            P.release(scl)
        P.ps_free(ps_oc, ps_os, ps_ow)
        P.release(rdc)
        ps_o = P.ps()
        for c in range(2):
            P.op("pe", lambda e, c=c: e.transpose(ps_o.ap[:, c * 128:(c + 1) * 128], acc.ap[:, c * 128:(c + 1) * 128], self.ident.ap),
                 reads=[acc, self.ident], writes=[ps_o])
        stg = P.alloc("nstg", 256)
        P.op("act", lambda e: e.copy(stg.ap, ps_o.ap[:, 0:256]), reads=[ps_o], writes=[stg])
        P.dma(self.OBR.ap[0:256, q0:q0 + 128].rearrange("(c p) t -> p c t", p=128), stg.ap.rearrange("p (c t) -> p c t", c=2),
              reads=[stg], writes=[(self.OBR, 0), (self.OBR, 1)], q="gq")
        P.release(acc, stg)


    RC = 64

    def setup_rwkv(self):
        L = self.depth
        self.rwpar_d = self.din("rw_par", [L, 64, 43])
        self.rww2_d = self.din("rwkv_w2", [L, 32, 256])
        self.rwa2_d = self.din("rwkv_a2", [L, 32, 256])
        self.rwg2_d = self.din("rwkv_g2", [L, 64, 256])
        self.rwmask_d = self.din("rw_mask5", [64, 320])

    def rw_shift(self, z, n, mu_ap, par):
        P = self.P
        d = P.alloc("rwd", S)
        P.op("dve", lambda e: e.tensor_tensor(out=d.ap[0:n, 1:S], in0=z.ap[0:n, 0:S - 1], in1=z.ap[0:n, 1:S], op=ALU.subtract), reads=[z], writes=[d])
        P.op("dve", lambda e: e.tensor_scalar(out=d.ap[0:n, 0:1], in0=z.ap[0:n, 0:1], scalar1=-1.0, scalar2=None, op0=ALU.mult), reads=[z, d], writes=[d])
        P.op("dve", lambda e: e.scalar_tensor_tensor(out=z.ap[0:n], in0=d.ap[0:n], scalar=mu_ap, in1=z.ap[0:n], op0=ALU.mult, op1=ALU.add),
             reads=[d, z, par], writes=[z])
        P.release(d)

    def rwkv_branch(self, l):
        P = self.P
        par = P.alloc("rwpar", 43)
        P.dma(par.ap[0:64], self.rwpar_d.ap[l], reads=[self.rwpar_d], writes=[par])
        omk = P.alloc("rwomk", 4)
        P.op("dve", lambda e: e.tensor_scalar(out=omk.ap[0:64], in0=par.ap[0:64, 15 + 3 * 4:15 + 4 * 4], scalar1=-1.0, scalar2=1.0, op0=ALU.mult, op1=ALU.add),
             reads=[par], writes=[omk])
        lw = P.alloc("rwlw", 3 * 256)
        P.dma(lw.ap[0:32, 0:256], self.rww2_d.ap[l], reads=[self.rww2_d], writes=[(lw, 0)])
        P.dma(lw.ap[0:32, 256:512], self.rwa2_d.ap[l], reads=[self.rwa2_d], writes=[(lw, 1)])
        P.dma(lw.ap[0:64, 512:768], self.rwg2_d.ap[l], reads=[self.rwg2_d], writes=[(lw, 2)])
        m5 = P.alloc("rwm5", 320)
        P.dma(m5.ap[0:64], self.rwmask_d.ap, reads=[self.rwmask_d], writes=[m5])
        smask = P.alloc("rwsm", S)
        P.op("dve", lambda e: e.memset(smask.ap, 1.0), writes=[smask])
        P.op("dve", lambda e: e.memset(smask.ap.rearrange("p (n c) -> p n c", c=self.RC)[:, :, 0:1], 0.0), reads=[smask], writes=[smask])
        base = self.ZRW + 768
        twl = P.alloc("rwtwl", S)
        tal = P.alloc("rwtal", S)
        tgl = P.alloc("rwtgl", S)
        for t, r0, n, mc, fn in ((twl, base, 32, 12, AF.Tanh), (tal, base + 32, 32, 13, None), (tgl, base + 64, 64, 14, AF.Sigmoid)):
            self.rw_lora_in(t, r0, n, mc, fn, par)
        import os
        for h in range(4 if int(os.environ.get("RWDBG", "9")) >= 9 else 1):
            self.rwkv_head(l, h, par, omk, lw, m5, smask, twl, tal, tgl)
        P.release(par, omk, lw, m5, smask, twl, tal, tgl)

    def rw_lora_in(self, t, r0, n, mc, fn, par):
        P = self.P
        P.dma(t.ap[0:n], self.ZT.ap[r0:r0 + n, :], reads=[(self.ZT, r0 // 128)], writes=[t])
        self.rw_shift(t, n, par.ap[0:n, mc:mc + 1], par)
        if fn is not None:
            P.op("act", lambda e: e.activation(t.ap[0:n], t.ap[0:n], fn), reads=[t], writes=[t])

    def rwkv_head(self, l, h, par, omk, lw, m5, smask, twl, tal, tgl):
        P = self.P
        C = self.RC
        NCK = S // C
        pc = lambda which: par.ap[0:64, 15 + which * 4 + h:15 + which * 4 + h + 1]
        hc = slice(h * 64, (h + 1) * 64)
        r = P.alloc("rw_r", S)
        k = P.alloc("rw_k", S)
        v = P.alloc("rw_v", S)
        for j, t in enumerate((r, k, v)):
            r0 = self.ZRW + j * 256 + h * 64
            P.dma(t.ap[0:64], self.ZT.ap[r0:r0 + 64, :], reads=[(self.ZT, r0 // 128)], writes=[t])
            self.rw_shift(t, 64, par.ap[0:64, j * 4 + h:j * 4 + h + 1], par)
        a = P.alloc("rw_a", S)
        logw = P.alloc("rw_lw", S)
        kkn = P.alloc("rw_kkn", S)
        P.op("dve", lambda e: e.tensor_scalar(out=kkn.ap[0:64], in0=k.ap[0:64], scalar1=pc(2), scalar2=None, op0=ALU.mult), reads=[k, par], writes=[kkn])
        for tb in range(4):
            self.rw_prep_blk(h, tb, par, pc, lw, twl, tal, a, logw, kkn)
        kt = P.alloc("rw_kt", S)
        P.op("dve", lambda e: e.tensor_scalar(out=kt.ap[0:64], in0=a.ap[0:64], scalar1=pc(3), scalar2=omk.ap[0:64, h:h + 1], op0=ALU.mult, op1=ALU.add),
             reads=[a, par, omk], writes=[kt])
        P.op("dve", lambda e: e.tensor_tensor(out=kt.ap[0:64], in0=kt.ap[0:64], in1=k.ap[0:64], op=ALU.mult), reads=[kt, k], writes=[kt])
        P.release(k)
        P.op("dve", lambda e: e.tensor_tensor(out=a.ap[0:64], in0=a.ap[0:64], in1=kkn.ap[0:64], op=ALU.mult), reads=[a, kkn], writes=[a])
        bb = a
        import os
        dbg = int(os.environ.get("RWDBG", "9"))
        bonv = P.alloc("rw_bon", S)
        P.op("dve", lambda e: e.scalar_tensor_tensor(out=bonv.ap[0:64], in0=r.ap[0:64], scalar=pc(4), in1=kt.ap[0:64], op0=ALU.mult, op1=ALU.mult),
             reads=[r, kt, par], writes=[bonv])
        for tb in range(4):
            ps = P.ps()
            P.op("pe", lambda e, ps=ps, tb=tb: e.matmul(ps.ap[0:64, :], self.ones.ap[0:64, 0:64], bonv.ap[0:64, tb * 512:(tb + 1) * 512], start=True, stop=True),
                 reads=[bonv, self.ones], writes=[ps])
            P.op("dve", lambda e, ps=ps, tb=tb: e.tensor_tensor(out=bonv.ap[0:64, tb * 512:(tb + 1) * 512], in0=ps.ap[0:64, :], in1=v.ap[0:64, tb * 512:(tb + 1) * 512], op=ALU.mult),
                 reads=[ps, v, bonv], writes=[bonv])
        if dbg <= 1:
            return
        cum = P.alloc("rw_cum", S)
        P.op("dve", lambda e: e.tensor_tensor_scan(out=cum.ap[0:64], data0=smask.ap[0:64], data1=logw.ap[0:64], initial=0.0, op0=ALU.mult, op1=ALU.add),
             reads=[smask, logw], writes=[cum])
        eg = P.alloc("rw_eg", S)
        P.op("act", lambda e: e.activation(eg.ap[0:64], cum.ap[0:64], AF.Exp), reads=[cum], writes=[eg])
        gC = P.alloc("rw_gC", NCK)
        P.op("dve", lambda e: e.tensor_copy(gC.ap[0:64], eg.ap[0:64].rearrange("p (n c) -> p n c", c=C)[:, :, C - 1]), reads=[eg], writes=[gC])
        P.op("dve", lambda e: e.tensor_tensor(out=r.ap[0:64], in0=r.ap[0:64], in1=eg.ap[0:64], op=ALU.mult), reads=[r, eg], writes=[r])
        P.release(eg)
        RH = r
        P.op("dve", lambda e: e.tensor_tensor(out=logw.ap[0:64], in0=cum.ap[0:64], in1=logw.ap[0:64], op=ALU.subtract), reads=[cum, logw], writes=[logw])
        P.op("act", lambda e: e.activation(logw.ap[0:64], logw.ap[0:64], AF.Exp), reads=[logw], writes=[logw])
        P.op("dve", lambda e: e.tensor_tensor(out=kkn.ap[0:64], in0=kkn.ap[0:64], in1=logw.ap[0:64], op=ALU.mult), reads=[kkn, logw], writes=[kkn])
        P.release(logw)
        KH = kkn
        P.op("act", lambda e: e.activation(cum.ap[0:64], cum.ap[0:64], AF.Exp, scale=-1.0), reads=[cum], writes=[cum])
        P.op("dve", lambda e: e.tensor_tensor(out=kt.ap[0:64], in0=kt.ap[0:64], in1=cum.ap[0:64], op=ALU.mult), reads=[kt, cum], writes=[kt])
        P.op("dve", lambda e: e.tensor_tensor(out=bb.ap[0:64], in0=bb.ap[0:64], in1=cum.ap[0:64], op=ALU.mult), reads=[bb, cum], writes=[bb])
        P.release(cum)
        KG, BG = kt, bb
        if dbg <= 2:
            return
        tms = []
        for src in (v, KG, BG):
            tm = P.alloc("rw_tm", NCK * 64)
            tm3 = tm.ap.rearrange("p (n c) -> p n c", c=64)
            for g8 in range(NCK // 8):
                ps = P.ps()
                for j in range(8):
                    n = g8 * 8 + j
                    P.op("pe", lambda e, ps=ps, j=j, n=n, src=src: e.transpose(ps.ap[0:64, j * 64:(j + 1) * 64], src.ap[0:64, n * C:(n + 1) * C], self.ident.ap[0:64, 0:64]),
                         reads=[src, self.ident], writes=[ps])
                P.op("act", lambda e, ps=ps, g8=g8, tm=tm: e.copy(tm.ap[0:64, g8 * 512:(g8 + 1) * 512], ps.ap[0:64, :]), reads=[ps], writes=[(tm, g8)])
            tms.append((tm, tm3))
        P.release(v)
        (Vt, Vt3), (KGt, KGt3), (BGt, BGt3) = tms
        if dbg <= 3:
            return
        yT = P.alloc("rw_y", S)
        ST = P.alloc("rw_ST", 64)
        P.op("dve", lambda e: e.memset(ST.ap[0:64], 0.0), writes=[ST])
        G = 4
        for g0 in range(0, NCK, G):
            As, TTs = self.rw_group_prep(g0, G, KH, RH, KG, BG, m5)
            for gi in range(G):
                if dbg > 4:
                    self.rw_chunk(g0 + gi, As[gi], TTs[gi], KH, RH, Vt3, Vt, KGt3, KGt, BGt3, BGt, ST, gC, yT)
            P.release(*As)
            P.release(*TTs)
        P.release(RH, KH, KG, BG, Vt, KGt, BGt, ST, gC)
        self.rw_epilogue(l, h, yT, bonv, pc, par, lw, tgl)
        P.release(yT, bonv)

    def rw_prep_blk(self, h, tb, par, pc, lw, twl, tal, a, logw, kkn):
        P = self.P
        ts = slice(tb * 512, (tb + 1) * 512)
        ps = P.ps()
        P.op("pe", lambda e: e.matmul(ps.ap[0:64, :], lw.ap[0:32, h * 64:(h + 1) * 64], twl.ap[0:32, ts], start=True, stop=True), reads=[lw, twl], writes=[ps])
        P.op("act", lambda e: e.activation(logw.ap[0:64, ts], ps.ap[0:64, :], AF.Sigmoid, bias=pc(0), scale=1.0), reads=[ps, par], writes=[logw])
        P.op("dve", lambda e: e.tensor_scalar(out=logw.ap[0:64, ts], in0=logw.ap[0:64, ts], scalar1=-0.6065306597126334, scalar2=None, op0=ALU.mult),
             reads=[logw], writes=[logw])
        ps2 = P.ps()
        P.op("pe", lambda e: e.matmul(ps2.ap[0:64, :], lw.ap[0:32, 256 + h * 64:256 + (h + 1) * 64], tal.ap[0:32, ts], start=True, stop=True), reads=[lw, tal], writes=[ps2])
        P.op("act", lambda e: e.activation(a.ap[0:64, ts], ps2.ap[0:64, :], AF.Sigmoid, bias=pc(1), scale=1.0), reads=[ps2, par], writes=[a])
        sq = P.alloc("rw_sq", 512)
        P.op("act", lambda e: e.activation(sq.ap[0:64], kkn.ap[0:64, ts], AF.Square), reads=[kkn], writes=[sq])
        ps3 = P.ps()
        P.op("pe", lambda e: e.matmul(ps3.ap[0:64, :], self.ones.ap[0:64, 0:64], sq.ap[0:64], start=True, stop=True), reads=[sq, self.ones], writes=[ps3])
        P.op("act", lambda e: e.activation(sq.ap[0:64], ps3.ap[0:64, :], AF.Sqrt), reads=[ps3], writes=[sq])
        P.op("dve", lambda e: e.tensor_scalar(out=sq.ap[0:64], in0=sq.ap[0:64], scalar1=1e-12, scalar2=None, op0=ALU.max), reads=[sq], writes=[sq])
        P.op("dve", lambda e: e.reciprocal(sq.ap[0:64], sq.ap[0:64]), reads=[sq], writes=[sq])
        P.op("dve", lambda e: e.tensor_tensor(out=kkn.ap[0:64, ts], in0=kkn.ap[0:64, ts], in1=sq.ap[0:64], op=ALU.mult), reads=[kkn, sq], writes=[kkn])
        P.release(sq)

    def rw_group_prep(self, g0, G, KH, RH, KG, BG, m5):
        P = self.P
        C = self.RC
        As, Ms, Ps = [], [], []
        for gi in range(G):
            cs = slice((g0 + gi) * C, (g0 + gi + 1) * C)
            ps = P.ps()
            for j, (lh, rh) in enumerate(((KG, KH), (KG, RH), (BG, KH), (BG, RH), (KH, BG))):
                P.op("pe", lambda e, ps=ps, j=j, lh=lh, rh=rh, cs=cs: e.matmul(ps.ap[0:64, j * 64:(j + 1) * 64], lh.ap[0:64, cs], rh.ap[0:64, cs], start=True, stop=True),
                     reads=[lh, rh], writes=[ps])
            A = P.alloc("rw_A", 320)
            P.op("dve", lambda e, ps=ps, A=A: e.tensor_tensor(out=A.ap[0:64], in0=ps.ap[0:64, 0:320], in1=m5.ap[0:64], op=ALU.mult), reads=[ps, m5], writes=[A])
            As.append(A)
            Pm = P.alloc("rw_P", 64)
            P.op("dve", lambda e, A=A, Pm=Pm: e.tensor_tensor(out=Pm.ap[0:64], in0=self.ident.ap[0:64, 0:64], in1=A.ap[0:64, 128:192], op=ALU.subtract),
                 reads=[A, self.ident], writes=[Pm])
            Ps.append(Pm)
            Ms.append((A.ap[0:64, 128:192], A.ap[0:64, 256:320], A))
        import os
        nsteps = int(os.environ.get("RWSTEPS", "6"))
        for step in range(6):
            if step >= nsteps:
                for gi in range(G):
                    if Ms[gi][2] is not As[gi]:
                        P.release(Ms[gi][2])
                break
            pss = []
            for gi in range(G):
                M, MT, Mt = Ms[gi]
                ps = P.ps()
                if step < 5:
                    P.op("pe", lambda e, ps=ps, M=M, MT=MT: e.matmul(ps.ap[0:64, 0:64], MT, M, start=True, stop=True), reads=[Mt], writes=[ps])
                    P.op("pe", lambda e, ps=ps, M=M, MT=MT: e.matmul(ps.ap[0:64, 64:128], M, MT, start=True, stop=True), reads=[Mt], writes=[ps])
                if step > 0:
                    P.op("pe", lambda e, ps=ps, MT=MT, Pm=Ps[gi]: e.matmul(ps.ap[0:64, 128:192], MT, Pm.ap[0:64], start=True, stop=True), reads=[Mt, Ps[gi]], writes=[ps])
                pss.append(ps)
            for gi in range(G):
                ps = pss[gi]
                if step > 0:
                    P.op("dve", lambda e, ps=ps, Pm=Ps[gi]: e.tensor_tensor(out=Pm.ap[0:64], in0=Pm.ap[0:64], in1=ps.ap[0:64, 128:192], op=ALU.add),
                         reads=[ps, Ps[gi]], writes=[Ps[gi]])
                if step < 5:
                    Mn = P.alloc("rw_M", 128)
                    P.op("act", lambda e, ps=ps, Mn=Mn: e.copy(Mn.ap[0:64], ps.ap[0:64, 0:128]), reads=[ps], writes=[Mn])
                    old = Ms[gi][2]
                    Ms[gi] = (Mn.ap[0:64, 0:64], Mn.ap[0:64, 64:128], Mn)
                    if old is not As[gi]:
                        P.release(old)
                elif Ms[gi][2] is not As[gi]:
                    P.release(Ms[gi][2])
        return As, Ps

    def rw_chunk(self, n, A, TT, KH, RH, Vt3, Vt, KGt3, KGt, BGt3, BGt, ST, gC, yT):
        P = self.P
        C = self.RC
        cs = slice(n * C, (n + 1) * C)
        psx = P.ps()
        P.op("pe", lambda e: e.matmul(psx.ap[0:64, 0:64], KH.ap[0:64, cs], ST.ap[0:64], start=True, stop=False), reads=[KH, ST], writes=[psx])
        P.op("pe", lambda e: e.matmul(psx.ap[0:64, 0:64], A.ap[0:64, 0:64], Vt3[0:64, n, :], start=False, stop=True), reads=[A, Vt], writes=[psx])
        nx = P.alloc("rw_nx", 64)
        P.op("act", lambda e: e.mul(nx.ap[0:64], psx.ap[0:64, 0:64], -1.0), reads=[psx], writes=[nx])
        psu = P.ps()
        P.op("pe", lambda e: e.matmul(psu.ap[0:64, 0:64], TT.ap[0:64], nx.ap[0:64], start=True, stop=True), reads=[TT, nx], writes=[psu])
        U = P.alloc("rw_U", 64)
        P.op("act", lambda e: e.copy(U.ap[0:64], psu.ap[0:64, 0:64]), reads=[psu], writes=[U])
        psy = P.ps()
        P.op("pe", lambda e: e.matmul(psy.ap[0:64, 0:64], ST.ap[0:64], RH.ap[0:64, cs], start=True, stop=False), reads=[ST, RH], writes=[psy])
        P.op("pe", lambda e: e.matmul(psy.ap[0:64, 0:64], Vt3[0:64, n, :], A.ap[0:64, 64:128], start=False, stop=False), reads=[Vt, A], writes=[psy])
        P.op("pe", lambda e: e.matmul(psy.ap[0:64, 0:64], U.ap[0:64], A.ap[0:64, 192:256], start=False, stop=True), reads=[U, A], writes=[psy])
        P.op("act", lambda e: e.copy(yT.ap[0:64, cs], psy.ap[0:64, 0:64]), reads=[psy], writes=[(yT, n)])
        pss = P.ps()
        P.op("pe", lambda e: e.matmul(pss.ap[0:64, 0:64], KGt3[0:64, n, :], Vt3[0:64, n, :], start=True, stop=False), reads=[KGt, Vt], writes=[pss])
        P.op("pe", lambda e: e.matmul(pss.ap[0:64, 0:64], BGt3[0:64, n, :], U.ap[0:64], start=False, stop=True), reads=[BGt, U], writes=[pss])
        P.op("dve", lambda e: e.tensor_tensor(out=ST.ap[0:64], in0=ST.ap[0:64], in1=pss.ap[0:64, 0:64], op=ALU.add), reads=[ST, pss], writes=[ST])
        P.op("dve", lambda e: e.tensor_scalar(out=ST.ap[0:64], in0=ST.ap[0:64], scalar1=gC.ap[0:64, n:n + 1], scalar2=None, op0=ALU.mult),
             reads=[ST, gC], writes=[ST])
        P.release(nx, U)

    def rw_epilogue(self, l, h, yT, bonv, pc, par, lw, tgl):
        P = self.P
        n = 512
        for Q in range(S // n):
            self.rw_epi_blk(h, Q, n, yT, bonv, pc, par, lw, tgl)

    def rw_epi_blk(self, h, Q, n, yT, bonv, pc, par, lw, tgl):
        P = self.P
        ts = slice(Q * n, (Q + 1) * n)
        o = P.alloc("ro", n)
        sq = P.alloc("rsq", n)
        P.op("act", lambda e: e.activation(sq.ap[0:64], yT.ap[0:64, ts], AF.Square), reads=[yT], writes=[sq])
        p1 = P.ps()
        p2 = P.ps()
        P.op("pe", lambda e: e.matmul(p1.ap[0:64, :], self.ones.ap[0:64, 0:64], yT.ap[0:64, ts], start=True, stop=True), reads=[yT, self.ones], writes=[p1])
        P.op("pe", lambda e: e.matmul(p2.ap[0:64, :], self.ones.ap[0:64, 0:64], sq.ap[0:64], start=True, stop=True), reads=[sq, self.ones], writes=[p2])
        mean = P.alloc("rmean", n)
        P.op("dve", lambda e: e.tensor_scalar(out=mean.ap[0:64], in0=p1.ap[0:64, :], scalar1=1.0 / 64, scalar2=None, op0=ALU.mult), reads=[p1], writes=[mean])
        P.op("dve", lambda e: e.tensor_tensor(out=o.ap[0:64], in0=yT.ap[0:64, ts], in1=mean.ap[0:64], op=ALU.subtract), reads=[yT, mean], writes=[o])
        P.op("dve", lambda e: e.tensor_tensor(out=mean.ap[0:64], in0=mean.ap[0:64], in1=mean.ap[0:64], op=ALU.mult), reads=[mean], writes=[mean])
        P.op("dve", lambda e: e.scalar_tensor_tensor(out=sq.ap[0:64], in0=p2.ap[0:64, :], scalar=1.0 / 64, in1=mean.ap[0:64],
                                                     op0=ALU.mult, op1=ALU.subtract), reads=[p2, mean], writes=[sq])
        P.op("act", lambda e: e.activation(sq.ap[0:64], sq.ap[0:64], AF.Sqrt, bias=self.epst.ap[0:64, 2:3], scale=1.0), reads=[sq, self.epst], writes=[sq])
        P.op("dve", lambda e: e.reciprocal(sq.ap[0:64], sq.ap[0:64]), reads=[sq], writes=[sq])
        P.op("dve", lambda e: e.tensor_tensor(out=o.ap[0:64], in0=o.ap[0:64], in1=sq.ap[0:64], op=ALU.mult), reads=[o, sq], writes=[o])
        P.op("dve", lambda e: e.tensor_scalar(out=o.ap[0:64], in0=o.ap[0:64], scalar1=pc(5), scalar2=pc(6), op0=ALU.mult, op1=ALU.add), reads=[o, par], writes=[o])
        P.op("dve", lambda e: e.tensor_tensor(out=o.ap[0:64], in0=o.ap[0:64], in1=bonv.ap[0:64, ts], op=ALU.add), reads=[o, bonv], writes=[o])
        pg = P.ps()
        P.op("pe", lambda e: e.matmul(pg.ap[0:64, :], lw.ap[0:64, 512 + h * 64:512 + (h + 1) * 64], tgl.ap[0:64, ts], start=True, stop=True), reads=[lw, tgl], writes=[pg])
        P.op("dve", lambda e: e.tensor_tensor(out=o.ap[0:64], in0=o.ap[0:64], in1=pg.ap[0:64, :], op=ALU.mult), reads=[o, pg], writes=[o])
        P.dma(self.OBR.ap[512 + h * 64:512 + (h + 1) * 64, ts], o.ap[0:64], reads=[o], writes=[(self.OBR, 4 + h // 2)], q="gq")
        P.release(o, sq, mean)

    def setup_xa(self):
        P = self.P
        L = self.depth
        self.mem_d = self.din("mem", [MEM, D])
        self.wq_d = self.din("xa_wq", [L, D, D])
        self.wkv_d = self.din("xa_wkv", [L, D, 2 * D])
        self.wo_d = self.din("xa_wo", [L, D, D])
        self.memT = P.alloc("memT", NCH * MEM)
        memT3 = self.memT.ap.rearrange("p (c t) -> p c t", c=NCH)
        for tt in range(MEM // 128):
            self.xa_load_mem(tt, memT3)

    def xa_load_mem(self, tt, memT3):
        P = self.P
        xin = P.alloc("xin", D)
        P.dma(xin.ap, self.mem_d.ap[tt * 128:(tt + 1) * 128, :], reads=[self.mem_d], writes=[xin])
        for half in range(2):
            ps = P.ps()
            for j in range(4):
                c = half * 4 + j
                P.op("pe", lambda e, ps=ps, j=j, c=c: e.transpose(ps.ap[:, j * 128:(j + 1) * 128], xin.ap[:, c * 128:(c + 1) * 128], self.ident.ap),
                     reads=[xin, self.ident], writes=[ps])
            dst = memT3[:, half * 4:half * 4 + 4, tt * 128:(tt + 1) * 128]
            P.op("act", lambda e, ps=ps, dst=dst: e.copy(dst, ps.ap.rearrange("p (c t) -> p c t", c=4)), reads=[ps], writes=[self.memT])
        P.release(xin)

    def xattn(self, l):
        P = self.P
        mnT = P.alloc("mnT", NCH * MEM)
        mn3 = mnT.ap.rearrange("p (c t) -> p c t", c=NCH)
        self.norm_to(l, 6, self.memT.ap.rearrange("p (c t) -> p c t", c=NCH), self.memT, MEM, mn3, mnT)
        kT = P.alloc("xkT", NCH * MEM)
        kT3 = kT.ap.rearrange("p (c t) -> p c t", c=NCH)
        vN = P.alloc("xv", 2 * D)
        vN3 = vN.ap.rearrange("p (m n) -> p m n", m=2)
        for f in range(NCH):
            self.xa_k(l, f, mn3, mnT, kT3, kT)
        for half in range(2):
            self.xa_v(l, half, mn3, mnT, vN3, vN)
        P.release(mnT)
        TB = 512
        for tb in range(S // TB):
            self.xa_blk(l, tb, TB, kT3, kT, vN3, vN)
        P.release(kT, vN)

    def xa_k(self, l, f, mn3, mnT, kT3, kT):
        P = self.P
        w = P.alloc("xw", NCH * 128)
        w3 = w.ap.rearrange("p (k n) -> p k n", k=NCH)
        P.dma(w3, self.wkv_d.ap[l][:, f * 128:(f + 1) * 128].rearrange("(k p) n -> p k n", p=128), reads=[self.wkv_d], writes=[w])
        ps = P.ps()
        for k in range(NCH):
            P.op("pe", lambda e, k=k: e.matmul(ps.ap[:, 0:MEM], w3[:, k, :], mn3[:, k, :], start=(k == 0), stop=(k == NCH - 1)),
                 reads=[w, mnT], writes=[ps])
        P.op("act", lambda e: e.copy(kT3[:, f, :], ps.ap[:, 0:MEM]), reads=[ps], writes=[(kT, f)])
        P.release(w)

    def xa_v(self, l, half, mn3, mnT, vN3, vN):
        P = self.P
        w = P.alloc("xwv", NCH * 512)
        w3 = w.ap.rearrange("p (k n) -> p k n", k=NCH)
        c0 = D + half * 512
        P.dma(w3, self.wkv_d.ap[l][:, c0:c0 + 512].rearrange("(k p) n -> p k n", p=128), reads=[self.wkv_d], writes=[w])
        for mc in range(2):
            ps = P.ps()
            for k in range(NCH):
                P.op("pe", lambda e, k=k, ps=ps, mc=mc: e.matmul(ps.ap, mn3[:, k, mc * 128:(mc + 1) * 128], w3[:, k, :], start=(k == 0), stop=(k == NCH - 1)),
                     reads=[w, mnT], writes=[ps])
            P.op("act", lambda e, ps=ps, mc=mc: e.copy(vN3[:, mc, half * 512:(half + 1) * 512], ps.ap), reads=[ps], writes=[(vN, (mc, half))])
        P.release(w)

    def lin8(self, w_ap, src3, src_tile, dst3, dst_tile, TB):
        P = self.P
        for f in range(NCH):
            self.lin8_f(w_ap, f, src3, src_tile, dst3, dst_tile, TB)

    def lin8_f(self, w_ap, f, src3, src_tile, dst3, dst_tile, TB):
        P = self.P
        w = P.alloc("xw", NCH * 128 // 2)
        w3 = w.ap.bitcast(BF16).rearrange("p (k n) -> p k n", k=NCH)
        P.dma(w3, w_ap[:, f * 128:(f + 1) * 128].rearrange("(k p) n -> p k n", p=128), reads=[], writes=[w], q="gq")
        ps = P.ps()
        for k in range(NCH):
            P.op("pe", lambda e, k=k: e.matmul(ps.ap[:, 0:TB], w3[:, k, :], src3[:, k, :], start=(k == 0), stop=(k == NCH - 1)),
                 reads=[w, src_tile], writes=[ps])
        P.op("act", lambda e: e.copy(dst3[:, f, :], ps.ap[:, 0:TB]), reads=[ps], writes=[(dst_tile, f)])
        P.release(w)

    def xa_blk(self, l, tb, TB, kT3, kT, vN3, vN):
        P = self.P
        t0 = tb * TB
        hblk = P.alloc("hblk", NCH * TB // 2)
        h3 = hblk.ap.bitcast(BF16).rearrange("p (c t) -> p c t", c=NCH)
        self.norm_to(l, 2, self.xT3[:, :, t0:t0 + TB], self.xT, TB, h3, hblk)
        qT = P.alloc("xq", NCH * TB)
        q3 = qT.ap.rearrange("p (c t) -> p c t", c=NCH)
        self.lin8(self.wq_d.ap[l], h3, hblk, q3, qT, TB)
        P.release(hblk)
        at = P.alloc("xat", NCH * TB // 2)
        at3 = at.ap.bitcast(BF16).rearrange("p (c t) -> p c t", c=NCH)
        for h in range(4):
            self.xa_head(h, TB, kT3, kT, vN3, vN, q3, qT, at3, at)
        P.release(qT)
        yblk = P.alloc("yblk", NCH * TB)
        y3 = yblk.ap.rearrange("p (c t) -> p c t", c=NCH)
        self.lin8(self.wo_d.ap[l], at3, at, y3, yblk, TB)
        P.release(at)
        self.post_norm_residual(l, 3, t0, TB, yblk)
        P.release(yblk)

    def xa_head(self, h, TB, kT3, kT, vN3, vN, q3, qT, at3, at):
        P = self.P
        pT = P.alloc("xp", 2 * TB)
        p3 = pT.ap.rearrange("p (m t) -> p m t", m=2)
        for mc in range(2):
            ps = P.ps()
            for c in range(2):
                P.op("pe", lambda e, ps=ps, c=c, mc=mc: e.matmul(ps.ap[:, 0:TB], kT3[:, 2 * h + c, mc * 128:(mc + 1) * 128], q3[:, 2 * h + c, :],
                                                             start=(c == 0), stop=(c == 1)), reads=[kT, qT], writes=[ps])
            P.op("act", lambda e, ps=ps, mc=mc: e.activation(p3[:, mc, :], ps.ap[:, 0:TB], AF.Exp, scale=1.0 / 16.0), reads=[ps], writes=[(pT, mc)])
        psd = P.ps()
        for mc in range(2):
            P.op("pe", lambda e, mc=mc: e.matmul(psd.ap[:, 0:TB], self.ones.ap, p3[:, mc, :], start=(mc == 0), stop=(mc == 1)),
                 reads=[pT, self.ones], writes=[psd])
        rden = P.alloc("xrd", TB)
        P.op("dve", lambda e: e.reciprocal(rden.ap, psd.ap[:, 0:TB]), reads=[psd], writes=[rden])
        for dc in range(2):
            pso = P.ps()
            for mc in range(2):
                P.op("pe", lambda e, pso=pso, mc=mc, dc=dc: e.matmul(pso.ap[:, 0:TB], vN3[:, mc, h * 256 + dc * 128:h * 256 + (dc + 1) * 128], p3[:, mc, :],
                                                                 start=(mc == 0), stop=(mc == 1)), reads=[vN, pT], writes=[pso])
            P.op("dve", lambda e, pso=pso, dc=dc: e.tensor_tensor(out=at3[:, 2 * h + dc, :], in0=pso.ap[:, 0:TB], in1=rden.ap, op=ALU.mult),
                 reads=[pso, rden], writes=[(at, 2 * h + dc)])
        P.release(pT, rden)

    def build(self):
        self.setup()
        self.setup_eps()
        if "mix" in self.phases:
            self.setup_mixer()
            if "nsa" in self.branches:
                self.setup_nsa()
            if "rwkv" in self.branches:
                self.setup_rwkv()
        self.load_x()
        if "xa" in self.phases:
            self.setup_xa()
        for l in range(self.depth):
            if "mix" in self.phases:
                self.mixer(l)
            if "xa" in self.phases:
                self.xattn(l)
            if "mlp" in self.phases:
                self.mlp(l)
        fin = self.store_x()
        self.P.finalize(fin)
        return self.nc


def host_inputs(inputs, depth=DEPTH):
    f = np.float32
    g = np.stack([np.asarray(inputs[k], f)[:depth] for k in
                  ("ln_mix_pre", "ln_mix_post", "ln_xa_pre", "ln_xa_post", "ln_mlp_pre", "ln_mlp_post", "ln_mem")], axis=1)
    gains = np.ascontiguousarray(g.reshape(depth, 7, NCH, 128).transpose(3, 0, 1, 2).reshape(128, depth * 7 * NCH))
    common = {
        "ident": np.eye(128, dtype=f),
        "gains": gains,
        "mlp_w1": np.ascontiguousarray(np.asarray(inputs["mlp_w1"], f)[:depth]),
        "mlp_w2": np.ascontiguousarray(np.asarray(inputs["mlp_w2"], f)[:depth]),
    }
    w_in = np.asarray(inputs["w_in"], f)[:depth]
    q = w_in[:, :, 0:256]
    kv = w_in[:, :, 256:640]
    gl = w_in[:, :, 640:652]
    ret = w_in[:, :, 652:1676]
    rw = w_in[:, :, 1676:2572]
    cv = w_in[:, :, 2572:3340]

    def swp(w):
        w4 = w.reshape(w.shape[0], w.shape[1], 4, 2, 32)
        return w4[:, :, :, ::-1, :].reshape(w.shape)
    rq, rk, rv, rgt = ret[..., 0:256], ret[..., 256:512], ret[..., 512:768], ret[..., 768:1024]
    common["w_T"] = np.ascontiguousarray(np.concatenate(
        [q, kv[..., 0:64], kv[..., 64:128], kv[..., 128:192], kv[..., 256:320], rq, swp(rq), rk, swp(rk), rgt, rw, cv], axis=-1))
    common["w_N"] = np.ascontiguousarray(np.concatenate([kv[..., 192:256], kv[..., 320:384], gl, rv], axis=-1))
    common["w_gate"] = np.ascontiguousarray(w_in[:, :, 3340:7436])
    common["w_branch"] = np.ascontiguousarray(np.asarray(inputs["w_branch"], f)[:depth])
    common["w_mix_out"] = np.ascontiguousarray(np.asarray(inputs["w_mix_out"], f)[:depth])
    cw = np.asarray(inputs["conv_w"], f)[:depth]
    common["conv_wT"] = np.ascontiguousarray(cw.reshape(depth, 3, 2, 128).transpose(0, 3, 2, 1).reshape(depth, 128, 6))
    common["ret_gT"] = np.ascontiguousarray(np.asarray(inputs["ret_norm_g"], f)[:depth].reshape(depth, 4, 64).transpose(0, 2, 1))
    half = 32
    inv_freq = (10000.0 ** (-np.arange(half, dtype=np.float32) / half)).astype(f)
    ang = np.arange(S, dtype=f)[:, None] * inv_freq[None, :]
    cosT = np.concatenate([np.cos(ang), np.cos(ang)], axis=1).T
    sinT = np.concatenate([-np.sin(ang), np.sin(ang)], axis=1).T
    common["rot_tab"] = np.ascontiguousarray(np.stack([cosT, sinT], axis=1).astype(f))
    kk = np.arange(128)[:, None]
    qq = np.arange(512)[None, :]
    dec = np.zeros((128, 4, 2, 512), np.float64)
    for h in range(4):
        lg = np.log(1.0 - 2.0 ** (-5.0 - h))
        full = np.exp(lg * (qq - kk)) * 0.125
        dec[:, h, 0] = full
        dec[:, h, 1] = np.where(qq >= kk, full, 0.0)
    common["ret_dec"] = np.ascontiguousarray(dec.reshape(128, -1).astype(f))
    import math
    common["nsa_cmp_w"] = np.ascontiguousarray(np.asarray(inputs["nsa_cmp_w"], f)[:depth])
    common["nsa_peT"] = np.ascontiguousarray(np.asarray(inputs["nsa_cmp_pe"], f)[:depth].transpose(0, 2, 1))
    rb = np.asarray(inputs["rel_bias"], f)

    def bucket(dist):
        n = np.maximum(dist, 0)
        nf = np.maximum(n, 1).astype(np.float32)
        large = 16 + (np.log(nf / np.float32(16)) / np.float32(math.log(128 / 16)) * np.float32(16)).astype(np.int32)
        large = np.minimum(large, 31)
        return np.where(n < 16, n, large)
    NEGB = np.float32(-30000.0)
    tpos = np.arange(S)
    nidx = np.arange(128)
    d_c = tpos[None, :] - (16 * nidx[:, None] + 31)
    bc = rb[bucket(d_c)]
    bc = np.where(((d_c >= 0) & (nidx[:, None] < 127))[:, :, None], bc, NEGB).transpose(0, 2, 1)
    common["nsa_biasc"] = np.ascontiguousarray(bc.astype(f))
    kk = np.arange(128)[:, None]
    qq = np.arange(128)[None, :]
    tabs = np.zeros((128, 4, 4, 128), f)
    d0 = qq - kk
    tabs[:, 0] = np.where((d0 >= 0)[:, None, :], rb[bucket(d0)].transpose(0, 2, 1), NEGB)
    tabs[:, 1] = rb[bucket(d0 + 128)].transpose(0, 2, 1)
    tabs[:, 2] = rb[31][None, :, None]
    tabs[:, 3] = np.where((d0 < 0)[:, None, :], rb[31][None, :, None], NEGB)
    common["nsa_tab"] = np.ascontiguousarray(tabs.reshape(128, -1))
    keys = np.arange(S)
    common["nsa_e2"] = np.ascontiguousarray(((keys[None, :] // 64) == np.arange(32)[:, None]).astype(f) * f(240000.0))
    cs = np.arange(127) * 16
    ss = np.arange(32) * 64
    ov = np.clip(np.minimum(cs[:, None] + 32, ss[None, :] + 64) - np.maximum(cs[:, None], ss[None, :]), 0, None).astype(f) / f(32)
    common["nsa_ovl"] = np.ascontiguousarray(np.concatenate([ov, np.zeros((1, 32), f)], axis=0))
    cur = tpos // 64
    blk = np.arange(32)
    forced = (blk[None, :] == 0) | (blk[None, :] == cur[:, None]) | (blk[None, :] == cur[:, None] - 1)
    addt = np.where(blk[None, :] <= cur[:, None], np.where(forced, f(1e4), f(0.0)), f(-1e30)).astype(f)
    common["nsa_addtab"] = np.ascontiguousarray(addt)
    mu = np.asarray(inputs["rwkv_mu"], f)[:depth]
    par = np.zeros((depth, 64, 43), f)
    par[:, :, 0:12] = mu[:, 0:768].reshape(depth, 3, 4, 64).transpose(0, 3, 1, 2).reshape(depth, 64, 12)
    par[:, 0:32, 12] = mu[:, 768:800]
    par[:, 0:32, 13] = mu[:, 800:832]
    par[:, 0:64, 14] = mu[:, 832:896]
    for wi, nm in enumerate(("rwkv_w0", "rwkv_a0", "rwkv_k_k", "rwkv_k_a", "rwkv_r_k", "rwkv_ln_g", "rwkv_ln_b")):
        par[:, :, 15 + wi * 4:15 + (wi + 1) * 4] = np.asarray(inputs[nm], f)[:depth].reshape(depth, 4, 64).transpose(0, 2, 1)
    common["rw_par"] = par
    for nm in ("rwkv_w2", "rwkv_a2", "rwkv_g2"):
        common[nm] = np.ascontiguousarray(np.asarray(inputs[nm], f)[:depth])
    ii = np.arange(64)[:, None]
    tt2 = np.arange(64)[None, :]
    mu_ = (ii < tt2).astype(f)
    mui = (ii <= tt2).astype(f)
    ml = (ii > tt2).astype(f)
    common["rw_mask5"] = np.ascontiguousarray(np.concatenate([mu_, mui, mu_, mui, ml], axis=1))
    for k in ("xa_wq", "xa_wkv", "xa_wo"):
        common[k] = np.ascontiguousarray(np.asarray(inputs[k], f)[:depth])
    maps = []
    for b in range(8):
        m = dict(common)
        m["mem"] = np.ascontiguousarray(np.asarray(inputs["mem"], f)[b])
        m["x"] = np.ascontiguousarray(np.asarray(inputs["x"], f)[b])
        maps.append(m)
    return maps


def kernel(**inputs):
    bld = Builder()
    nc = bld.build()
    maps = host_inputs(inputs)
    maps = [{k: v for k, v in m.items() if k in bld.inp} for m in maps]
    res = run_bass_kernel_spmd(nc, maps, core_ids=list(range(8)))
    return np.stack([np.asarray(r["out"], np.float32) for r in res.results], axis=0)
```
